# Optimizing a Trainium2 kernel written in Bass

```python
import math
import jax, jax.numpy as jnp
from jax import lax
import numpy as np

D_MODEL = 1024
BATCH = 4
SEQ = 4096
DEPTH = 1
DEC_BATCH = 128
DEC_SEQ = 8
PAST_LEN = 2048
PAGE_SIZE = 128

A_HEADS = 8
A_KV_HEADS = 4
A_DH = 64
A_GROUP = A_HEADS // A_KV_HEADS
IDX_HEADS = 4
IDX_DH = 64
TOPK_MAX = 256
Q_BLOCK = 128
G_HEADS = 4
G_DK = 128
G_DV = 128
CONV_W = 4
CONV_CH = 2 * G_HEADS * G_DK + G_HEADS * G_DV
G_CHUNK = 64
MEM_TOKENS = 256
M_HEADS = 4
M_DH = 128
D_FF = -(-8 * D_MODEL // (3 * 256)) * 256
EPS = 1e-6

SPLIT_SIZES = (A_HEADS * A_DH, A_KV_HEADS * A_DH, A_KV_HEADS * A_DH,
               IDX_HEADS * IDX_DH, IDX_DH, IDX_HEADS,
               CONV_CH, G_HEADS * G_DV, G_HEADS, G_HEADS,
               M_HEADS * M_DH,
               3 * D_MODEL)
D_IN = sum(SPLIT_SIZES)

kernel_name = 'hybrid_dsa_gdn_memory_step'


def rms_norm(x, gain):
    xf = x.astype(jnp.float32)
    y = xf * lax.rsqrt(jnp.mean(xf * xf, axis=-1, keepdims=True) + EPS)
    return (y * gain.astype(jnp.float32)).astype(x.dtype)


def l2_norm(x):
    xf = x.astype(jnp.float32)
    return xf * lax.rsqrt(jnp.sum(xf * xf, axis=-1, keepdims=True) + EPS)


def split_projection(h):
    offs = np.cumsum(SPLIT_SIZES)[:-1].tolist()
    return jnp.split(h, offs, axis=-1)


def project_inputs(x, norm_mix, w_in, a_q_norm, a_k_norm, m_q_norm):
    B, T, _ = x.shape
    h = rms_norm(x, norm_mix) @ w_in
    aq, ak, av, iq, ik, iw, gqkv, gz, gb, ga, mq, gates = split_projection(h)
    aq = rms_norm(aq.reshape(B, T, A_HEADS, A_DH), a_q_norm)
    ak = rms_norm(ak.reshape(B, T, A_KV_HEADS, A_DH), a_k_norm)
    av = av.reshape(B, T, A_KV_HEADS, A_DH)
    iq = iq.reshape(B, T, IDX_HEADS, IDX_DH)
    iw = iw * IDX_HEADS ** -0.5
    mq = rms_norm(mq.reshape(B, T, M_HEADS, M_DH), m_q_norm)
    return aq, ak, av, iq, ik, iw, gqkv, gz, gb, ga, mq, gates


def indexer_topk(qi, wi, ki, q_pos, k_top):
    L = ki.shape[1]
    dots = jnp.einsum('bthd,bsd->bths', qi, ki).astype(jnp.float32) * IDX_DH ** -0.5
    score = jnp.einsum('bths,bth->bts', jax.nn.relu(dots), wi.astype(jnp.float32))
    admissible = jnp.arange(L)[None, :] <= q_pos[:, None]
    score = jnp.where(admissible[None], score, -jnp.inf)
    _, idx = lax.top_k(score, k_top)
    valid = idx <= q_pos[None, :, None]
    return idx, valid


def sparse_attend(q, k_sel, v_sel, valid):
    B, T = q.shape[:2]
    qg = q.reshape(B, T, A_KV_HEADS, A_GROUP, A_DH)
    s = jnp.einsum('btgrd,btkgd->btgrk', qg, k_sel).astype(jnp.float32) * A_DH ** -0.5
    s = jnp.where(valid[:, :, None, None, :], s, -jnp.inf)
    p = jax.nn.softmax(s, axis=-1).astype(v_sel.dtype)
    o = jnp.einsum('btgrk,btkgd->btgrd', p, v_sel)
    return o.reshape(B, T, A_HEADS * A_DH)


def prompt_sparse_attention(q, k, v, qi, wi, ki):
    B, S = q.shape[:2]
    k_top = min(TOPK_MAX, S // 4)
    nb = S // Q_BLOCK

    def blocks(a):
        return jnp.moveaxis(a.reshape((B, nb, Q_BLOCK) + a.shape[2:]), 1, 0)

    take = jax.vmap(lambda rows, idx: rows[idx])

    def one_block(args):
        q_b, qi_b, wi_b, start = args
        q_pos = start + jnp.arange(Q_BLOCK)
        idx, valid = indexer_topk(qi_b, wi_b, ki, q_pos, k_top)
        return sparse_attend(q_b, take(k, idx), take(v, idx), valid)

    out = lax.map(one_block, (blocks(q), blocks(qi), blocks(wi), jnp.arange(nb) * Q_BLOCK))
    return jnp.moveaxis(out, 0, 1).reshape(B, S, A_HEADS * A_DH)


def sample_sparse_attention(q, k_new, v_new, qi, wi, ki_new, pool_k, pool_v, pool_ki, page_table):
    DB, T = q.shape[:2]
    past = page_table.shape[1] * PAGE_SIZE
    k_top = min(TOPK_MAX, (past + T) // 4)
    ki_past = pool_ki[page_table].reshape(DB, past, IDX_DH)
    ki_all = jnp.concatenate([ki_past, ki_new.astype(ki_past.dtype)], axis=1)
    q_pos = past + jnp.arange(T)
    idx, valid = indexer_topk(qi, wi, ki_all, q_pos, k_top)
    b = jnp.arange(DB)[:, None, None]
    in_past = (idx < past)[..., None, None]
    pidx = jnp.minimum(idx, past - 1)
    phys = page_table[b, pidx // PAGE_SIZE]
    off = pidx % PAGE_SIZE
    nidx = jnp.clip(idx - past, 0, T - 1)
    k_sel = jnp.where(in_past, pool_k[phys, off], k_new[b, nidx].astype(pool_k.dtype))
    v_sel = jnp.where(in_past, pool_v[phys, off], v_new[b, nidx].astype(pool_v.dtype))
    return sparse_attend(q, k_sel, v_sel, valid)


def short_conv(u, buf, w):
    T = u.shape[1]
    ucat = jnp.concatenate([buf.astype(u.dtype), u], axis=1)
    out = sum(ucat[:, j:j + T] * w[j] for j in range(CONV_W))
    return jax.nn.silu(out), ucat[:, -(CONV_W - 1):]


def gated_delta_chunked(q, k, v, beta, g, s0):
    B, T, H, DK = q.shape
    DV = v.shape[-1]
    C = min(G_CHUNK, T)
    n = -(-T // C)
    pad = n * C - T

    def prep(a):
        a = jnp.pad(a, [(0, 0), (0, pad)] + [(0, 0)] * (a.ndim - 2))
        return jnp.moveaxis(a.reshape((B, n, C) + a.shape[2:]), 3, 2)

    q, k, v, beta, g = (prep(a) for a in (q, k, v, beta, g))
    q = q * DK ** -0.5
    gc = jnp.cumsum(g, axis=-1)
    i = jnp.arange(C)
    causal = i[:, None] >= i[None, :]
    strict = i[:, None] > i[None, :]
    gamma = jnp.exp(jnp.where(causal, gc[..., :, None] - gc[..., None, :], -jnp.inf))
    kb = k * beta[..., None]
    m = jnp.where(strict, jnp.einsum('bnhid,bnhjd->bnhij', kb, k) * gamma, 0.0)
    a = m + jnp.eye(C, dtype=m.dtype)
    rhs = jnp.concatenate([v * beta[..., None], kb * jnp.exp(gc)[..., None]], axis=-1)
    sol = lax.linalg.triangular_solve(a, rhs, left_side=True, lower=True, unit_diagonal=True)
    u, w = sol[..., :DV], sol[..., DV:]
    aqk = jnp.einsum('bnhid,bnhjd->bnhij', q, k) * gamma
    dq = q * jnp.exp(gc)[..., None]
    g_last = gc[..., -1]
    k_tail = k * jnp.exp(g_last[..., None] - gc)[..., None]

    def step(S, xs):
        u_c, w_c, aqk_c, dq_c, kt_c, gl_c = xs
        v_new = u_c - jnp.einsum('bhck,bhkv->bhcv', w_c, S)
        o = jnp.einsum('bhck,bhkv->bhcv', dq_c, S) + jnp.einsum('bhij,bhjv->bhiv', aqk_c, v_new)
        S = S * jnp.exp(gl_c)[..., None, None] + jnp.einsum('bhck,bhcv->bhkv', kt_c, v_new)
        return S, o

    xs = tuple(jnp.moveaxis(t, 1, 0) for t in (u, w, aqk, dq, k_tail, g_last))
    S, o = lax.scan(step, s0, xs)
    o = jnp.transpose(o, (1, 0, 3, 2, 4)).reshape(B, n * C, H, DV)[:, :T]
    return o, S


def gdn_branch(gqkv, gz, gb, ga, conv_buf, s0, g_conv, g_a_log, g_dt_bias, g_o_norm):
    B, T, _ = gqkv.shape
    u, new_buf = short_conv(gqkv, conv_buf, g_conv)
    q, k, v = jnp.split(u, [G_HEADS * G_DK, 2 * G_HEADS * G_DK], axis=-1)
    q = l2_norm(q.reshape(B, T, G_HEADS, G_DK))
    k = l2_norm(k.reshape(B, T, G_HEADS, G_DK))
    v = v.reshape(B, T, G_HEADS, G_DV).astype(jnp.float32)
    beta = jax.nn.sigmoid(gb.astype(jnp.float32))
    g = -jnp.exp(g_a_log.astype(jnp.float32)) * jax.nn.softplus(ga.astype(jnp.float32) + g_dt_bias.astype(jnp.float32))
    o, s_new = gated_delta_chunked(q, k, v, beta, g, s0.astype(jnp.float32))
    o = rms_norm(o, g_o_norm) * jax.nn.silu(gz.reshape(B, T, G_HEADS, G_DV).astype(jnp.float32))
    return o.reshape(B, T, G_HEADS * G_DV).astype(gqkv.dtype), new_buf, s_new.astype(gqkv.dtype)


def memory_kv(mem, mem_norm, w_mem_kv, m_k_norm):
    B, M, _ = mem.shape
    mk, mv = jnp.split(rms_norm(mem, mem_norm) @ w_mem_kv, 2, axis=-1)
    mk = rms_norm(mk.reshape(B, M, M_HEADS, M_DH), m_k_norm)
    return mk, mv.reshape(B, M, M_HEADS, M_DH)


def memory_attend(q, mk, mv):
    B, T = q.shape[:2]
    s = jnp.einsum('bthd,bmhd->bhtm', q, mk).astype(jnp.float32) * M_DH ** -0.5
    p = jax.nn.softmax(s, axis=-1).astype(mv.dtype)
    return jnp.einsum('bhtm,bmhd->bthd', p, mv).reshape(B, T, M_HEADS * M_DH)


def merge_and_ffn(x, a_out, g_out, m_out, gates, w_a_out, w_g_out, w_m_out, w_o, norm_ffn, w_ffn_in, w_ffn_out):
    ga_, gg_, gm_ = jnp.split(jax.nn.sigmoid(gates), 3, axis=-1)
    h = ga_ * (a_out @ w_a_out) + gg_ * (g_out @ w_g_out) + gm_ * (m_out @ w_m_out)
    x = x + h @ w_o
    gate, up = jnp.split(rms_norm(x, norm_ffn) @ w_ffn_in, 2, axis=-1)
    return x + (jax.nn.silu(gate) * up) @ w_ffn_out


def prompt_layer(x, mem, lw):
    (norm_mix, w_in, a_q_norm, a_k_norm, g_conv, g_a_log, g_dt_bias, g_o_norm, mem_norm, w_mem_kv,
     m_q_norm, m_k_norm, w_a_out, w_g_out, w_m_out, w_o, norm_ffn, w_ffn_in, w_ffn_out) = lw
    B = x.shape[0]
    aq, ak, av, iq, ik, iw, gqkv, gz, gb, ga, mq, gates = project_inputs(x, norm_mix, w_in, a_q_norm, a_k_norm, m_q_norm)
    a_out = prompt_sparse_attention(aq, ak, av, iq, iw, ik)
    buf0 = jnp.zeros((B, CONV_W - 1, CONV_CH), x.dtype)
    s0 = jnp.zeros((B, G_HEADS, G_DK, G_DV), jnp.float32)
    g_out, conv_buf, s_gdn = gdn_branch(gqkv, gz, gb, ga, buf0, s0, g_conv, g_a_log, g_dt_bias, g_o_norm)
    mk, mv = memory_kv(mem, mem_norm, w_mem_kv, m_k_norm)
    m_out = memory_attend(mq, mk, mv)
    y = merge_and_ffn(x, a_out, g_out, m_out, gates, w_a_out, w_g_out, w_m_out, w_o, norm_ffn, w_ffn_in, w_ffn_out)
    return y, (ak, av, ik, s_gdn, conv_buf, mk, mv)


def sample_layer(x, c_k, c_v, c_ik, page_table, s_gdn, s_conv, c_mk, c_mv, lw):
    (norm_mix, w_in, a_q_norm, a_k_norm, g_conv, g_a_log, g_dt_bias, g_o_norm, mem_norm, w_mem_kv,
     m_q_norm, m_k_norm, w_a_out, w_g_out, w_m_out, w_o, norm_ffn, w_ffn_in, w_ffn_out) = lw
    aq, ak, av, iq, ik, iw, gqkv, gz, gb, ga, mq, gates = project_inputs(x, norm_mix, w_in, a_q_norm, a_k_norm, m_q_norm)
    a_out = sample_sparse_attention(aq, ak, av, iq, iw, ik, c_k, c_v, c_ik, page_table)
    g_out, conv_buf, s_new = gdn_branch(gqkv, gz, gb, ga, s_conv, s_gdn, g_conv, g_a_log, g_dt_bias, g_o_norm)
    m_out = memory_attend(mq, c_mk, c_mv)
    y = merge_and_ffn(x, a_out, g_out, m_out, gates, w_a_out, w_g_out, w_m_out, w_o, norm_ffn, w_ffn_in, w_ffn_out)
    return y, (ak, av, ik, s_new, conv_buf)


def setup_inputs(seed: int = 0) -> dict:
    key = jax.random.key(seed)
    keys = jax.random.split(key, 48)
    counter = [0]

    def nxt():
        counter[0] += 1
        return keys[counter[0] - 1]

    def nrm(shape, scale=1.0):
        return jax.random.normal(nxt(), shape, jnp.float32) * scale

    def gain(shape):
        return 1.0 + 0.02 * jax.random.normal(nxt(), shape, jnp.float32)

    n_pages = PAST_LEN // PAGE_SIZE
    n_used = DEC_BATCH * n_pages
    n_phys = n_used + max(1, n_used // 4)
    page_table = jax.random.permutation(nxt(), n_phys)[:n_used].reshape(DEC_BATCH, n_pages).astype(jnp.int32)
    a_log = jnp.log(jax.random.uniform(nxt(), (DEPTH, G_HEADS), jnp.float32, 1.0, 16.0))
    dt = jnp.exp(jax.random.uniform(nxt(), (DEPTH, G_HEADS), jnp.float32, math.log(1e-3), math.log(1e-1)))
    dt_bias = dt + jnp.log(-jnp.expm1(-dt))
    d_br = A_HEADS * A_DH
    return {
        'x_prompt': nrm((BATCH, SEQ, D_MODEL)),
        'x_sample': nrm((DEC_BATCH, DEC_SEQ, D_MODEL)),
        'mem_prompt': nrm((BATCH, MEM_TOKENS, D_MODEL)),
        'cache_k': nrm((DEPTH, n_phys, PAGE_SIZE, A_KV_HEADS, A_DH)),
        'cache_v': nrm((DEPTH, n_phys, PAGE_SIZE, A_KV_HEADS, A_DH)),
        'cache_idx_k': nrm((DEPTH, n_phys, PAGE_SIZE, IDX_DH)),
        'page_table': page_table,
        'state_gdn': nrm((DEPTH, DEC_BATCH, G_HEADS, G_DK, G_DV), 0.1),
        'state_conv': nrm((DEPTH, DEC_BATCH, CONV_W - 1, CONV_CH)),
        'cache_mem_k': nrm((DEPTH, DEC_BATCH, MEM_TOKENS, M_HEADS, M_DH)),
        'cache_mem_v': nrm((DEPTH, DEC_BATCH, MEM_TOKENS, M_HEADS, M_DH)),
        'norm_mix': gain((DEPTH, D_MODEL)),
        'w_in': nrm((DEPTH, D_MODEL, D_IN), D_MODEL ** -0.5),
        'a_q_norm': gain((DEPTH, A_DH)),
        'a_k_norm': gain((DEPTH, A_DH)),
        'g_conv': nrm((DEPTH, CONV_W, CONV_CH), CONV_W ** -0.5),
        'g_a_log': a_log,
        'g_dt_bias': dt_bias,
        'g_o_norm': gain((DEPTH, G_DV)),
        'mem_norm': gain((DEPTH, D_MODEL)),
        'w_mem_kv': nrm((DEPTH, D_MODEL, 2 * M_HEADS * M_DH), D_MODEL ** -0.5),
        'm_q_norm': gain((DEPTH, M_DH)),
        'm_k_norm': gain((DEPTH, M_DH)),
        'w_a_out': nrm((DEPTH, d_br, D_MODEL), d_br ** -0.5),
        'w_g_out': nrm((DEPTH, G_HEADS * G_DV, D_MODEL), (G_HEADS * G_DV) ** -0.5),
        'w_m_out': nrm((DEPTH, M_HEADS * M_DH, D_MODEL), (M_HEADS * M_DH) ** -0.5),
        'w_o': nrm((DEPTH, D_MODEL, D_MODEL), D_MODEL ** -0.5),
        'norm_ffn': gain((DEPTH, D_MODEL)),
        'w_ffn_in': nrm((DEPTH, D_MODEL, 2 * D_FF), D_MODEL ** -0.5),
        'w_ffn_out': nrm((DEPTH, D_FF, D_MODEL), D_FF ** -0.5),
    }


def reference(x_prompt, x_sample, mem_prompt, cache_k, cache_v, cache_idx_k, page_table, state_gdn, state_conv,
              cache_mem_k, cache_mem_v, norm_mix, w_in, a_q_norm, a_k_norm, g_conv, g_a_log, g_dt_bias, g_o_norm,
              mem_norm, w_mem_kv, m_q_norm, m_k_norm, w_a_out, w_g_out, w_m_out, w_o, norm_ffn, w_ffn_in, w_ffn_out):
    yp, ys = x_prompt, x_sample
    p_states, s_states = [], []
    for l in range(DEPTH):
        lw = (norm_mix[l], w_in[l], a_q_norm[l], a_k_norm[l], g_conv[l], g_a_log[l], g_dt_bias[l], g_o_norm[l],
              mem_norm[l], w_mem_kv[l], m_q_norm[l], m_k_norm[l], w_a_out[l], w_g_out[l], w_m_out[l], w_o[l],
              norm_ffn[l], w_ffn_in[l], w_ffn_out[l])
        yp, ps = prompt_layer(yp, mem_prompt, lw)
        ys, ss = sample_layer(ys, cache_k[l], cache_v[l], cache_idx_k[l], page_table, state_gdn[l], state_conv[l],
                              cache_mem_k[l], cache_mem_v[l], lw)
        p_states.append(ps)
        s_states.append(ss)
    p_k, p_v, p_idx_k, p_gdn, p_conv, p_mem_k, p_mem_v = [jnp.stack(t) for t in zip(*p_states)]
    s_k, s_v, s_idx_k, s_gdn, s_conv = [jnp.stack(t) for t in zip(*s_states)]
    return (yp, ys, p_k, p_v, p_idx_k, p_gdn, p_conv, p_mem_k, p_mem_v, s_k, s_v, s_idx_k, s_gdn, s_conv)
```

```python
import contextlib
import numpy as np
import concourse.bass as bass
import concourse.mybir as mybir
from concourse.bass_utils import run_bass_kernel_spmd

F32 = mybir.dt.float32
BF16 = mybir.dt.bfloat16
I32 = mybir.dt.int32
AF = mybir.ActivationFunctionType
ALU = mybir.AluOpType
AX = mybir.AxisListType

D = 1024
KC = 8
SEQ = 4096
NT_ALL = 32
NT_OWN = 16
D_IN = 6988
D_FF = 2816
EPS = 1e-6
O_AQ, O_AK, O_AV, O_IQ, O_IK, O_IW = 0, 512, 768, 1024, 1280, 1344
O_GQKV, O_GZ, O_GB, O_GA, O_MQ, O_GATES = 1348, 2884, 3396, 3400, 3404, 3916


class Buf:
    __slots__ = ("name", "w", "r")

    def __init__(self, name=""):
        self.name = name
        self.w = None
        self.r = []


class K:
    def __init__(self, nc, n_dma_sems=40):
        self.nc = nc
        self.eng = {"pe": nc.tensor, "act": nc.scalar, "dve": nc.vector,
                    "pool": nc.gpsimd, "sp": nc.sync}
        self.sem, self.cnt, self.seen = {}, {}, {}
        for e in self.eng:
            self.sem[e] = nc.alloc_semaphore("prog_" + e)
            self.cnt[e] = 0
            self.seen[e] = {}
        self.dsems = [nc.alloc_semaphore("dma%d" % i) for i in range(n_dma_sems)]
        self.dcnt = [0] * n_dma_sems
        self.dnext = 0
        self.nops = 0
        self.nwaits = 0

    def _wait(self, e, tok):
        if tok is None:
            return
        sem, val, src = tok
        if src == e and e == "pe":
            return
        key = sem.num
        if self.seen[e].get(key, 0) >= val:
            return
        self.eng[e].wait_ge(sem, val)
        self.seen[e][key] = val
        self.nwaits += 1

    def _deps(self, e, reads, writes):
        for b in reads:
            self._wait(e, b.w)
        for b in writes:
            self._wait(e, b.w)
            for t in b.r:
                self._wait(e, t)

    def _commit(self, tok, reads, writes):
        for b in reads:
            b.r.append(tok)
            if len(b.r) > 64:
                b.r = b.r[-64:] if False else b.r
        for b in writes:
            b.w = tok
            b.r = []

    def op(self, e, fn, reads=(), writes=()):
        self._deps(e, reads, writes)
        ins = fn(self.eng[e])
        self.cnt[e] += 1
        ins.then_inc(self.sem[e], 1)
        tok = (self.sem[e], self.cnt[e], e)
        self._commit(tok, reads, writes)
        self.nops += 1
        return tok

    def dma(self, q, out, in_, reads=(), writes=(), indirect=None, **kw):
        self._deps(q, reads, writes)
        i = self.dnext
        self.dnext = (self.dnext + 1) % len(self.dsems)
        sem = self.dsems[i]
        if self.dcnt[i] > 0:
            self._wait(q, (sem, self.dcnt[i], "dma"))
        if indirect is not None:
            ins = self.eng[q].indirect_dma_start(out=out, out_offset=None, in_=in_,
                                                 in_offset=indirect, **kw)
        else:
            ins = self.eng[q].dma_start(out=out, in_=in_, **kw)
        self.dcnt[i] += 16
        ins.then_inc(sem, 16)
        tok = (sem, self.dcnt[i], "dma")
        self._commit(tok, reads, writes)
        self.nops += 1
        return tok

    def barrier(self):
        for e in self.eng:
            for e2 in self.eng:
                if e2 != e and self.cnt[e2] > 0:
                    self._wait(e, (self.sem[e2], self.cnt[e2], e2))
            for i, s_ in enumerate(self.dsems):
                if self.dcnt[i] > 0:
                    self._wait(e, (s_, self.dcnt[i], "dma"))

    def finish(self, bufs, e="sp"):
        for b in bufs:
            self._wait(e, b.w)
            for t in b.r:
                self._wait(e, t)


class T:
    __slots__ = ("t", "b")

    def __init__(self, t, name=""):
        self.t = t
        self.b = Buf(name)

    def __getitem__(self, key):
        return self.t[key]


def own_tiles(half):
    res = []
    for g in range(8):
        res += [4 * g, 4 * g + 3] if half == 0 else [4 * g + 1, 4 * g + 2]
    return res


def build(cfg):
    nc = bass.Bass("TRN2", target_bir_lowering=False)
    k = K(nc)
    dt_in = {}

    def din(name, shape, dt=F32):
        dt_in[name] = nc.dram_tensor(name, list(shape), dt, kind="ExternalInput").ap()
        return dt_in[name]

    def dout(name, shape, dt=F32):
        return nc.dram_tensor(name, list(shape), dt, kind="ExternalOutput").ap()

    x_all = din("x_all", [SEQ, D])
    x_own = din("x_own", [NT_OWN * 128, D])
    x_smp = din("x_smp", [128, D])
    mem = din("mem", [256, D])
    w_in = din("w_in", [D, D_IN])
    w_mem_kv = din("w_mem_kv", [D, 1024])
    w_a_out = din("w_a_out", [512, D])
    w_g_out = din("w_g_out", [512, D])
    w_m_out = din("w_m_out", [512, D])
    w_o = din("w_o", [D, D])
    w_ffn_in = din("w_ffn_in", [D, 2 * D_FF])
    w_ffn_out = din("w_ffn_out", [D_FF, D])
    ident_d = din("ident", [128, 128])
    gains_d = din("gains", [128, 24])
    hv_d = din("headvecs", [1, 576])

    y_own = dout("y_own", [NT_OWN * 128, D])
    y_smp = dout("y_smp", [128, D])
    o_pk = dout("o_pk", [SEQ, 256])
    o_pv = dout("o_pv", [SEQ, 256])
    o_pik = dout("o_pik", [SEQ, 64])
    o_pconv = dout("o_pconv", [3, 1536])
    o_pmk = dout("o_pmk", [256, 512])
    o_pmv = dout("o_pmv", [256, 512])
    o_sk = dout("o_sk", [128, 256])
    o_sv = dout("o_sv", [128, 256])
    o_sik = dout("o_sik", [128, 64])
    o_sconv = dout("o_sconv", [16, 3, 1536])
    DBG = cfg.get("debug", False)
    skind = "ExternalOutput" if DBG else "Internal"
    cmk_d = din("cache_mem_k", [16, 256, 512])
    cmv_d = din("cache_mem_v", [16, 256, 512])
    cmask_d = din("cmask", [128, 2048])
    selw_d = din("selw", [128, 64])
    ckv_d = din("cache_kv", [2560 * 128, 512])
    cik_d = din("cache_idx_k", [2560 * 128, 64])
    pt_d = din("page_table", [1, 256], I32)
    pen_d = din("pen", [17, 128, 256])
    dconst_d = din("dconst", [128, 64])
    gconst_d = din("gconst", [128, 10 * 128 + 16 + 2048])
    gconvT_d = din("gconvT", [128, 48])
    sgdn_d = din("state_gdn", [16, 4, 128, 128])
    sconv_d = din("state_conv", [48, 1536])
    o_pgdn = dout("o_pgdn", [4, 128, 128])
    o_sgdn = dout("o_sgdn", [16, 4, 128, 128])
    if DBG:
        dbg_tm = dout("dbg_tm", [128, 1536])
        dbg_sm = dout("dbg_sm", [128, 64])
        dbg_o = dout("dbg_o", [128, 512])
        dbg_mask = dout("dbg_mask", [128, 512])
        dbg_bis = dout("dbg_bis", [128, 52])
        dbg_sc = dout("dbg_sc", [128, 512])
        dbg_uw = dout("dbg_uw", [128, 256])
        dbg_vn = dout("dbg_vn", [128, 128])
        dbg_aq = dout("dbg_aq", [128, 128])
        dbg_tt = dout("dbg_tt", [128, 128])
        dbg_wtm = dout("dbg_wtm", [128, 2048])
        dbg_p1 = dout("dbg_p1", [128, 128])
    a_sc = nc.dram_tensor("a_sc", [17 * 128, 512], F32, kind=skind).ap()
    m_sc = nc.dram_tensor("m_sc", [17 * 128, 512], F32, kind=skind).ap()
    g_sc = nc.dram_tensor("g_sc", [33 * 128, 512], F32, kind=skind).ap()
    x2_sc = nc.dram_tensor("x2_sc", [17 * 128, D], F32, kind=skind).ap()
    a_sc_b = [Buf() for _ in range(17)]
    m_sc_b = [Buf() for _ in range(17)]
    g_sc_b = [Buf() for _ in range(33)]
    x2_sc_b = [Buf() for _ in range(17)]
    outs_done = []

    with contextlib.ExitStack() as glob:
        def sb(st, name, shape, dt=F32):
            return T(st.enter_context(nc.sbuf_tensor("sb_" + name, list(shape), dt)), name)

        psum = [T(glob.enter_context(nc.psum_tensor("ps%d" % i, [128, 512], F32)), "ps%d" % i)
                for i in range(8)]
        ps_i = [0]
        ps_hold = set()

        def ps(hold=False):
            while True:
                idx = ps_i[0] % 8
                ps_i[0] += 1
                if idx not in ps_hold:
                    break
            if hold:
                ps_hold.add(idx)
            return psum[idx]

        def ps_release(p):
            ps_hold.discard(psum.index(p))

        ident = sb(glob, "ident", [128, 128])
        gains = sb(glob, "gains", [128, 24])
        hv = sb(glob, "hv", [128, 576])
        k.dma("sp", ident[:], ident_d[:, :], writes=[ident.b])
        k.dma("sp", gains[:], gains_d[:, :], writes=[gains.b])
        k.dma("sp", hv[:], hv_d[0:1, :].partition_broadcast(128), writes=[hv.b])

        cmask = sb(glob, "cmask", [128, 16, 128], BF16)
        selw = sb(glob, "selw", [128, 64])
        k.dma("pool", cmask[:], cmask_d[:, :].rearrange("p (b t) -> p b t", b=16), writes=[cmask.b])
        k.dma("sp", selw[:], selw_d[:, :], writes=[selw.b])
        MKT = sb(glob, "MKT", [128, 4, 256], BF16)
        MVa = sb(glob, "MVa", [128, 2, 4, 129], BF16)
        k.op("pool", lambda e: e.memset(MVa[:], 1.0), writes=[MVa.b])
        zb = sb(glob, "zb", [128, 512], BF16)
        k.op("pool", lambda e: e.memset(zb[:], 0.0), writes=[zb.b])

        def ps_zero(p):
            k.op("pe", lambda e: e.matmul(p[:, :], lhsT=zb[:, 0:128], rhs=zb[:, :], start=True, stop=False),
                 reads=[zb.b], writes=[p.b])


        def kside_store(kk_ap, vv_ap, ii_ap, rd, KT_ap, IKT_ap, VA_ap, wr):
            if kk_ap is not None:
                p = ps()
                for g in range(4):
                    k.op("pe", lambda e, g=g: e.transpose(p[0:64, g * 128:(g + 1) * 128],
                                                          kk_ap[:, g * 64:(g + 1) * 64], ident[:]),
                         reads=rd + [ident.b], writes=[p.b])
                k.op("act", lambda e: e.activation(out=KT_ap, in_=p[0:64, :].rearrange("p (g s) -> p g s", g=4),
                                                   func=AF.Copy), reads=[p.b], writes=wr)
            if ii_ap is not None:
                p2 = ps()
                k.op("pe", lambda e: e.transpose(p2[0:64, 0:128], ii_ap, ident[:]),
                     reads=rd + [ident.b], writes=[p2.b])
                k.op("act", lambda e: e.activation(out=IKT_ap, in_=p2[0:64, 0:128], func=AF.Copy),
                     reads=[p2.b], writes=wr)
            if vv_ap is not None:
                k.op("pool", lambda e: e.tensor_copy(VA_ap, vv_ap.rearrange("p (g d) -> p g d", g=4)),
                     reads=rd, writes=wr)

        def pipe_ahead(gens, depth):
            n = len(gens)

            def toB(g):
                for tok in g:
                    if tok == "B":
                        return
            for j_ in range(min(depth, n)):
                toB(gens[j_])
            for j_ in range(n):
                for _ in gens[j_]:
                    pass
                if j_ + depth < n:
                    toB(gens[j_ + depth])

        def load_xT(st_tiles, src_ap, gain_col, q="sp", rd=()):
            x32, xn, xT, stat = st_tiles
            k.dma(q, x32[:], src_ap, reads=list(rd), writes=[x32.b])
            k.op("act", lambda e: e.activation(out=xn[:], in_=x32[:], func=AF.Square,
                                               accum_out=stat[:, 0:1]),
                 reads=[x32.b], writes=[xn.b, stat.b])
            k.op("dve", lambda e: e.tensor_scalar(stat[:, 1:2], stat[:, 0:1], 1.0 / D, EPS,
                                                  op0=ALU.mult, op1=ALU.add),
                 reads=[stat.b], writes=[stat.b])
            k.op("act", lambda e: e.activation(out=stat[:, 3:4], in_=stat[:, 1:2], func=AF.Sqrt),
                 reads=[stat.b], writes=[stat.b])
            k.op("dve", lambda e: e.reciprocal(stat[:, 2:3], stat[:, 3:4]),
                 reads=[stat.b], writes=[stat.b])
            k.op("act", lambda e: e.activation(out=xn[:], in_=x32[:], func=AF.Copy,
                                               scale=stat[:, 2:3]),
                 reads=[x32.b, stat.b], writes=[xn.b])
            for half in range(2):
                p = ps()
                for j in range(4):
                    kc = half * 4 + j
                    k.op("pe", lambda e, kc=kc, j=j: e.transpose(
                        p[:, j * 128:(j + 1) * 128], xn[:, kc * 128:(kc + 1) * 128], ident[:]),
                        reads=[xn.b, ident.b], writes=[p.b])
                g = gains[:, gain_col + half * 4: gain_col + half * 4 + 4]
                k.op("dve", lambda e, half=half, g=g, p=p: e.tensor_tensor(
                    xT[:, half * 4:(half + 1) * 4, :],
                    p[:, :].rearrange("p (a b) -> p a b", a=4),
                    g.unsqueeze(2).to_broadcast([128, 4, 128]), op=ALU.mult),
                    reads=[p.b, gains.b], writes=[xT.b])
            return x32, xT

        def linear(xT, W, c0, ncol, p, kcs=KC):
            for kc in range(kcs):
                k.op("pe", lambda e, kc=kc: e.matmul(
                    p[:, 0:ncol], lhsT=xT[:, kc, :], rhs=W[:, kc, c0:c0 + ncol],
                    start=(kc == 0), stop=(kc == kcs - 1)),
                    reads=[xT.b, W.b], writes=[p.b])

        def load_w(W, src, c0, ncol, dst0=0, rows=D):
            nkc = rows // 128
            for kc in range(nkc):
                k.dma("pool", W[:, kc, dst0:dst0 + ncol], src[kc * 128:(kc + 1) * 128, c0:c0 + ncol],
                      writes=[W.b])

        def head_rmsnorm(st, src_ps, nh, dh, gain_ap, out32, tmp, stat, name):
            k.op("act", lambda e: e.activation(out=tmp[:, 0:nh * dh], in_=src_ps, func=AF.Square),
                 reads=[st], writes=[tmp.b])
            k.op("dve", lambda e: e.tensor_reduce(
                stat[:, 0:nh], tmp[:, 0:nh * dh].rearrange("p (h d) -> p h d", h=nh),
                axis=AX.X, op=ALU.add), reads=[tmp.b], writes=[stat.b])
            k.op("dve", lambda e: e.tensor_scalar(stat[:, 8:8 + nh], stat[:, 0:nh], 1.0 / dh, EPS,
                                                  op0=ALU.mult, op1=ALU.add),
                 reads=[stat.b], writes=[stat.b])
            k.op("act", lambda e: e.activation(out=stat[:, 0:nh], in_=stat[:, 8:8 + nh], func=AF.Sqrt),
                 reads=[stat.b], writes=[stat.b])
            k.op("dve", lambda e: e.reciprocal(stat[:, 16:16 + nh], stat[:, 0:nh]),
                 reads=[stat.b], writes=[stat.b])
            k.op("dve", lambda e: e.tensor_tensor(
                out32[:, 0:nh * dh].rearrange("p (h d) -> p h d", h=nh),
                src_ps.rearrange("p (h d) -> p h d", h=nh),
                stat[:, 16:16 + nh].unsqueeze(2).to_broadcast([128, nh, dh]), op=ALU.mult),
                reads=[st, stat.b], writes=[out32.b])
            k.op("dve", lambda e: e.tensor_tensor(
                out32[:, 0:nh * dh].rearrange("p (h d) -> p h d", h=nh),
                out32[:, 0:nh * dh].rearrange("p (h d) -> p h d", h=nh),
                gain_ap.unsqueeze(1).to_broadcast([128, nh, dh]), op=ALU.mult),
                reads=[out32.b, hv.b], writes=[out32.b])

        PH = cfg.get('phases', 'BCGEFD')
        with contextlib.ExitStack() as st:
          if 'B' in PH:
              Wm = sb(st, "Wm", [128, KC, 1024], BF16)
              load_w(Wm, w_mem_kv, 0, 1024)
              tiles = (sb(st, "mx32", [128, D]), sb(st, "mxn", [128, D]),
                       sb(st, "mxT", [128, KC, 128], BF16), sb(st, "mstat", [128, 4]))
              tmp = sb(st, "mtmp", [128, 512])
              hstat = sb(st, "mhstat", [128, 24])
              mk32 = [sb(st, "mk32_%d" % i, [128, 512]) for i in range(2)]
              mv32 = [sb(st, "mv32_%d" % i, [128, 512]) for i in range(2)]
              for mt in range(2):
                  _, xT = load_xT(tiles, mem[mt * 128:(mt + 1) * 128, :], 8)
                  pk = ps()
                  linear(xT, Wm, 0, 512, pk)
                  pv = ps()
                  linear(xT, Wm, 512, 512, pv)
                  head_rmsnorm(pk.b, pk[:, :], 4, 128, hv[:, 256:384], mk32[mt], tmp, hstat, "mk")
                  k.op("act", lambda e, mt=mt, pv=pv: e.activation(out=mv32[mt][:], in_=pv[:, :], func=AF.Copy),
                       reads=[pv.b], writes=[mv32[mt].b])
                  p = ps()
                  for h in range(4):
                      k.op("pe", lambda e, h=h, p=p, mt=mt: e.transpose(
                          p[:, h * 128:(h + 1) * 128], mk32[mt][:, h * 128:(h + 1) * 128], ident[:]),
                          reads=[mk32[mt].b, ident.b], writes=[p.b])
                  k.op("act", lambda e, p=p, mt=mt: e.activation(
                      out=MKT[:, :, mt * 128:(mt + 1) * 128], in_=p[:, :].rearrange("p (h m) -> p h m", h=4),
                      func=AF.Copy), reads=[p.b], writes=[MKT.b])
                  k.op("dve", lambda e, mt=mt: e.tensor_copy(
                      MVa[:, mt, :, 0:128], mv32[mt][:, :].rearrange("p (h d) -> p h d", h=4)),
                      reads=[mv32[mt].b], writes=[MVa.b])
                  k.dma("sp", o_pmk[mt * 128:(mt + 1) * 128, :], mk32[mt][:], reads=[mk32[mt].b])
                  k.dma("sp", o_pmv[mt * 128:(mt + 1) * 128, :], mv32[mt][:], reads=[mv32[mt].b])
                  outs_done += [mk32[mt].b, mv32[mt].b]

        k.barrier()
        with contextlib.ExitStack() as st:
          if 'G' in PH:
              Wg2 = sb(st, "Wg2", [128, KC, 2056], BF16)
              load_w(Wg2, w_in, O_GQKV, 2056, 0)
              gcs = sb(st, "gcs", [128, 10 * 128 + 16 + 2048])
              k.dma("sp", gcs[:], gconst_d[:, :], writes=[gcs.b])
              cv = lambda i: gcs[:, i * 128:(i + 1) * 128]
              LTRI, BONES, SLm, SUIm = [cv(0), cv(1)], [cv(2), cv(3)], [cv(4), cv(5)], [cv(6), cv(7)]
              USTR, ONES = cv(8), cv(9)
              G8 = gcs[:, 1280:1296]
              CM32 = gcs[:, 1296:1296 + 2048].rearrange("p (b t) -> p b t", b=16)
              gcv = sb(st, "gcv", [128, 48])
              k.dma("sp", gcv[:], gconvT_d[:, :], writes=[gcv.b])
              Dg = sb(st, "Dg", [128, 12, 4, 128], BF16)
              for cc in range(12):
                  for jj in range(4):
                      k.op("dve", lambda e, cc=cc, jj=jj: e.tensor_scalar(
                          Dg[:, cc, jj, :], ident[:], gcv[:, cc * 4 + jj:cc * 4 + jj + 1], None, op0=ALU.mult),
                          reads=[ident.b, gcv.b], writes=[Dg.b])
              negA = sb(st, "negA", [128, 4])
              k.op("act", lambda e: e.activation(out=negA[:], in_=hv[:, 516:520], func=AF.Exp),
                   reads=[hv.b], writes=[negA.b])
              k.op("dve", lambda e: e.tensor_scalar(negA[:], negA[:], -1.0, None, op0=ALU.mult),
                   reads=[negA.b], writes=[negA.b])
              Sst = [sb(st, "Sst%d" % h, [128, 128]) for h in range(4)]
              for h in range(4):
                  k.op("pool", lambda e, h=h: e.memset(Sst[h][:], 0.0), writes=[Sst[h].b])
              tsets = [(sb(st, "gx32_%d" % i, [128, D]), sb(st, "gxn_%d" % i, [128, D]),
                        sb(st, "gxT_%d" % i, [128, KC, 128], BF16), sb(st, "gstat_%d" % i, [128, 4]))
                       for i in range(1)] * 2
              Up = sb(st, "Up", [128, 12, 131], BF16)
              k.op("pool", lambda e: e.memset(Up[:], 0.0), writes=[Up.b])
              UC = sb(st, "UC", [128, 12, 128])
              TM = sb(st, "TM", [128, 1536])
              sq = sb(st, "gsq", [128, 1024])
              sgz2 = [sb(st, "sgz%d" % q, [128, 512]) for q in range(1)]
              sm2 = [sb(st, "gsm%d" % q, [128, 64]) for q in range(1)]
              scal = sb(st, "gscal", [128, 6, 4])
              KQ = sb(st, "KQ", [128, 12, 128])
              KQT2 = [sb(st, "KQT%d" % q, [128, 12, 128]) for q in range(1)]
              KBG2 = [sb(st, "KBG%d" % q, [128, 4, 128]) for q in range(1)]
              KTL2 = [sb(st, "KTL%d" % q, [128, 4, 128]) for q in range(1)]
              VB2 = [sb(st, "VB%d" % q, [128, 4, 128]) for q in range(1)]
              O32 = sb(st, "O32", [128, 512])
              go32 = sb(st, "go32", [128, 512])
              gtmp = sb(st, "ggtmp", [128, 512])
              ghstat = sb(st, "ghstat", [128, 24])
              hb = [dict(GU=sb(st, "GU%d" % i, [128, 128]), G2=sb(st, "G2%d" % i, [128, 256]),
                         t1=sb(st, "t1%d" % i, [128, 128]), aq=sb(st, "aqkT%d" % i, [128, 128]),
                         P=[sb(st, "P%d_%d" % (i, q), [128, 256]) for q in range(2)],
                         TT=[sb(st, "TT%d_%d" % (i, q), [128, 128]) for q in range(2)],
                         UW=sb(st, "UW%d" % i, [128, 256]), vn=sb(st, "vn%d" % i, [128, 128]))
                    for i in range(4)]
              DK5 = 128.0 ** -0.5

              def gdn_tile(j, sm_st=None):
                  smp = (j == NT_ALL)
                  m = 1 if smp else 0
                  bf_ = j % 2
                  sgz, KQT, KBG, KTL, VB, sm = sgz2[bf_], KQT2[bf_], KBG2[bf_], KTL2[bf_], VB2[bf_], sm2[bf_]
                  src = x_smp[:, :] if smp else x_all[j * 128:(j + 1) * 128, :]
                  x32, xT = load_xT(tsets[j % 2], src, 0)
                  yield
                  U = sm_st["Us"] if smp else Up
                  for cc0 in (0, 4, 8):
                      p = ps()
                      for c4 in range(4):
                          cc = cc0 + c4
                          for kc in range(KC):
                              k.op("pe", lambda e, kc=kc, cc=cc, c4=c4, p=p: e.matmul(
                                  p[:, c4 * 128:(c4 + 1) * 128], lhsT=Wg2[:, kc, cc * 128:(cc + 1) * 128],
                                  rhs=xT[:, kc, :], start=(kc == 0), stop=(kc == KC - 1)),
                                  reads=[Wg2.b, xT.b], writes=[p.b])
                      if smp:
                          for c4 in range(4):
                              k.op("act", lambda e, p=p, cc0=cc0, c4=c4: e.activation(
                                  out=U[:, cc0 + c4, :, 3:11],
                                  in_=p[:, c4 * 128:(c4 + 1) * 128].rearrange("p (b t) -> p b t", b=16),
                                  func=AF.Copy), reads=[p.b], writes=[U.b])
                      else:
                          k.op("act", lambda e, p=p, cc0=cc0: e.activation(
                              out=U[:, cc0:cc0 + 4, 3:131], in_=p[:, :].rearrange("p (a t) -> p a t", a=4),
                              func=AF.Copy), reads=[p.b], writes=[U.b])
                      yield
                  for cc0 in (0, 4, 8):
                      p = ps()
                      for c4 in range(4):
                          cc = cc0 + c4
                          for jj in range(4):
                              rhs = U[:, cc, :, jj:jj + 8] if smp else U[:, cc, jj:jj + 128]
                              k.op("pe", lambda e, jj=jj, cc=cc, c4=c4, p=p, rhs=rhs: e.matmul(
                                  p[:, c4 * 128:(c4 + 1) * 128], lhsT=Dg[:, cc, jj, :], rhs=rhs,
                                  start=(jj == 0), stop=(jj == 3)), reads=[Dg.b, U.b], writes=[p.b])
                      k.op("act", lambda e, p=p, cc0=cc0: e.activation(
                          out=UC[:, cc0:cc0 + 4, :], in_=p[:, :].rearrange("p (a t) -> p a t", a=4),
                          func=AF.Silu), reads=[p.b], writes=[UC.b])
                      yield
                  if not smp:
                      k.op("dve", lambda e: e.tensor_copy(U[:, :, 0:3], U[:, :, 128:131]),
                           reads=[U.b], writes=[U.b])
                  for cc0 in (0, 4, 8):
                      p = ps()
                      for c4 in range(4):
                          k.op("pe", lambda e, cc0=cc0, c4=c4, p=p: e.transpose(
                              p[:, c4 * 128:(c4 + 1) * 128], UC[:, cc0 + c4, :], ident[:]),
                              reads=[UC.b, ident.b], writes=[p.b])
                      k.op("act", lambda e, p=p, cc0=cc0: e.activation(
                          out=TM[:, cc0 * 128:(cc0 + 4) * 128], in_=p[:, :], func=AF.Copy),
                          reads=[p.b], writes=[TM.b])
                      yield
                  pgz = ps()
                  linear(xT, Wg2, 1536, 512, pgz)
                  pgb = ps()
                  linear(xT, Wg2, 2048, 8, pgb)
                  k.op("act", lambda e: e.activation(out=sgz[:], in_=pgz[:, :], func=AF.Silu),
                       reads=[pgz.b], writes=[sgz.b])
                  k.op("act", lambda e: e.activation(out=sm[:, 0:4], in_=pgb[:, 0:4], func=AF.Sigmoid),
                       reads=[pgb.b], writes=[sm.b])
                  k.op("dve", lambda e: e.tensor_tensor(sm[:, 4:8], pgb[:, 4:8], hv[:, 512:516], op=ALU.add),
                       reads=[pgb.b, hv.b], writes=[sm.b])
                  k.op("act", lambda e: e.activation(out=sm[:, 8:12], in_=sm[:, 4:8], func=AF.Exp),
                       reads=[sm.b], writes=[sm.b])
                  k.op("act", lambda e: e.activation(out=sm[:, 12:16], in_=sm[:, 8:12], func=AF.Ln, bias=1.0),
                       reads=[sm.b], writes=[sm.b])
                  k.op("dve", lambda e: e.tensor_tensor(sm[:, 16:20], sm[:, 12:16], negA[:], op=ALU.mult),
                       reads=[sm.b, negA.b], writes=[sm.b])
                  gt = sm[:, 16:20]
                  yield
                  k.op("act", lambda e: e.activation(out=sq[:], in_=TM[:, 0:1024], func=AF.Square),
                       reads=[TM.b], writes=[sq.b])
                  k.op("dve", lambda e: e.tensor_reduce(
                      sm[:, 20:28], sq[:, :].rearrange("p (h d) -> p h d", h=8), axis=AX.X, op=ALU.add),
                      reads=[sq.b], writes=[sm.b])
                  k.op("dve", lambda e: e.tensor_scalar(sm[:, 20:28], sm[:, 20:28], EPS, None, op0=ALU.add),
                       reads=[sm.b], writes=[sm.b])
                  k.op("act", lambda e: e.activation(out=sm[:, 28:36], in_=sm[:, 20:28], func=AF.Sqrt),
                       reads=[sm.b], writes=[sm.b])
                  k.op("dve", lambda e: e.reciprocal(sm[:, 36:44], sm[:, 28:36]), reads=[sm.b], writes=[sm.b])
                  rq, rk = sm[:, 36:40], sm[:, 40:44]
                  pc = ps()
                  k.op("pe", lambda e: e.matmul(pc[:, 0:4], lhsT=LTRI[m], rhs=gt, start=True, stop=True),
                       reads=[gcs.b, sm.b], writes=[pc.b])
                  k.op("pe", lambda e: e.matmul(pc[:, 4:8], lhsT=BONES[m], rhs=gt, start=True, stop=True),
                       reads=[gcs.b, sm.b], writes=[pc.b])
                  k.op("dve", lambda e: e.tensor_copy(sm[:, 44:52], pc[:, 0:8]), reads=[pc.b], writes=[sm.b])
                  k.op("act", lambda e: e.activation(out=sm[:, 52:56], in_=sm[:, 44:48], func=AF.Exp),
                       reads=[sm.b], writes=[sm.b])
                  k.op("dve", lambda e: e.tensor_tensor(sm[:, 56:60], sm[:, 48:52], sm[:, 44:48], op=ALU.subtract),
                       reads=[sm.b], writes=[sm.b])
                  k.op("act", lambda e: e.activation(out=sm[:, 56:60], in_=sm[:, 56:60], func=AF.Exp),
                       reads=[sm.b], writes=[sm.b])
                  k.op("act", lambda e: e.activation(out=sm[:, 60:64], in_=sm[:, 48:52], func=AF.Exp),
                       reads=[sm.b], writes=[sm.b])
                  egc, etl, dec, beta = sm[:, 52:56], sm[:, 56:60], sm[:, 60:64], sm[:, 0:4]
                  k.op("dve", lambda e: e.tensor_copy(scal[:, 0, :], rq), reads=[sm.b], writes=[scal.b])
                  k.op("dve", lambda e: e.scalar_tensor_tensor(out=scal[:, 1, :], in0=rq, scalar=DK5, in1=egc,
                                                               op0=ALU.mult, op1=ALU.mult),
                       reads=[sm.b], writes=[scal.b])
                  k.op("dve", lambda e: e.tensor_copy(scal[:, 2, :], rk), reads=[sm.b], writes=[scal.b])
                  k.op("dve", lambda e: e.tensor_tensor(scal[:, 5, :], rk, beta, op=ALU.mult),
                       reads=[sm.b], writes=[scal.b])
                  k.op("dve", lambda e: e.tensor_tensor(scal[:, 3, :], scal[:, 5, :], egc, op=ALU.mult),
                       reads=[sm.b, scal.b], writes=[scal.b])
                  k.op("dve", lambda e: e.tensor_tensor(scal[:, 4, :], rk, etl, op=ALU.mult),
                       reads=[sm.b], writes=[scal.b])
                  yield
                  TMq = TM[:, 0:512].rearrange("p (h d) -> p h d", h=4)
                  TMk = TM[:, 512:1024].rearrange("p (h d) -> p h d", h=4)
                  TMv = TM[:, 1024:1536].rearrange("p (h d) -> p h d", h=4)
                  bc = lambda a: a.unsqueeze(2).to_broadcast([128, 4, 128])
                  for dst, src_, sc_ in ((KQ[:, 0:4, :], TMq, scal[:, 0, :]), (KQ[:, 4:8, :], TMk, scal[:, 2, :]),
                                         (KQ[:, 8:12, :], TMq, scal[:, 1, :]), (KBG[:], TMk, scal[:, 3, :]),
                                         (KTL[:], TMk, scal[:, 4, :]), (VB[:], TMv, beta)):
                      wb_ = KQ.b if dst.tensor.name == KQ[:].tensor.name else (
                          KBG.b if dst.tensor.name == KBG[:].tensor.name else (
                              KTL.b if dst.tensor.name == KTL[:].tensor.name else VB.b))
                      k.op("dve", lambda e, dst=dst, src_=src_, sc_=sc_: e.tensor_tensor(
                          dst, src_, bc(sc_), op=ALU.mult), reads=[TM.b, scal.b, sm.b], writes=[wb_])
                  for cc0 in (0, 4, 8):
                      p = ps()
                      for c4 in range(4):
                          k.op("pe", lambda e, cc0=cc0, c4=c4, p=p: e.transpose(
                              p[:, c4 * 128:(c4 + 1) * 128], KQ[:, cc0 + c4, :], ident[:]),
                              reads=[KQ.b, ident.b], writes=[p.b])
                      k.op("act", lambda e, p=p, cc0=cc0: e.activation(
                          out=KQT[:, cc0:cc0 + 4, :], in_=p[:, :].rearrange("p (a t) -> p a t", a=4),
                          func=AF.Copy), reads=[p.b], writes=[KQT.b])
                      yield
                  if smp:
                      rhs3 = sm_st["rhs3"]
                      k.op("dve", lambda e: e.tensor_tensor(
                          rhs3[:], gt.unsqueeze(1).to_broadcast([128, 16, 4]),
                          G8.unsqueeze(2).to_broadcast([128, 16, 4]), op=ALU.mult),
                          reads=[sm.b, gcs.b], writes=[rhs3.b])
                      pdb = ps()
                      k.op("pe", lambda e: e.matmul(pdb[:, 0:64], lhsT=ONES,
                                                    rhs=rhs3[:].rearrange("p b h -> p (b h)"), start=True, stop=True),
                           reads=[gcs.b, rhs3.b], writes=[pdb.b])
                      decB = sm_st["decB"]
                      k.op("act", lambda e: e.activation(out=decB[:], in_=pdb[:, 0:64], func=AF.Exp),
                           reads=[pdb.b], writes=[decB.b])
                  def head_gen(h):
                      H = hb[h]
                      GU, G2, t1, aq, UW, vn = H["GU"], H["G2"], H["t1"], H["aq"], H["UW"], H["vn"]
                      KnT, QnT, DQT = KQT[:, 4 + h, :], KQT[:, h, :], KQT[:, 8 + h, :]
                      k.op("dve", lambda e: e.tensor_scalar(GU[:], USTR, gt[:, h:h + 1], None, op0=ALU.mult),
                           reads=[gcs.b, sm.b], writes=[GU.b])
                      pD = ps()
                      k.op("pe", lambda e: e.matmul(pD[:, 0:128], lhsT=LTRI[m], rhs=GU[:], start=True, stop=True),
                           reads=[gcs.b, GU.b], writes=[pD.b])
                      k.op("pe", lambda e: e.matmul(pD[:, 128:256], lhsT=GU[:], rhs=LTRI[m], start=True, stop=True),
                           reads=[gcs.b, GU.b], writes=[pD.b])
                      k.op("pe", lambda e: e.matmul(pD[:, 256:384], lhsT=KnT, rhs=KnT, start=True, stop=True),
                           reads=[KQT.b], writes=[pD.b])
                      k.op("pe", lambda e: e.matmul(pD[:, 384:512], lhsT=KnT, rhs=QnT, start=True, stop=True),
                           reads=[KQT.b], writes=[pD.b])
                      yield
                      k.op("act", lambda e: e.activation(out=G2[:], in_=pD[:, 0:256], func=AF.Exp),
                           reads=[pD.b], writes=[G2.b])
                      yield
                      k.op("dve", lambda e: e.tensor_tensor(t1[:], pD[:, 256:384], G2[:, 0:128], op=ALU.mult),
                           reads=[pD.b, G2.b], writes=[t1.b])
                      P0 = H["P"][0]
                      k.op("dve", lambda e: e.tensor_scalar(t1[:], t1[:], beta[:, h:h + 1], -1.0,
                                                            op0=ALU.mult, op1=ALU.mult),
                           reads=[t1.b, sm.b], writes=[t1.b])
                      k.op("dve", lambda e: e.tensor_tensor(P0[:, 0:128], t1[:], SLm[m], op=ALU.mult),
                           reads=[t1.b, gcs.b], writes=[P0.b])
                      k.op("dve", lambda e: e.tensor_tensor(t1[:], pD[:, 384:512], G2[:, 128:256], op=ALU.mult),
                           reads=[pD.b, G2.b], writes=[t1.b])
                      k.op("dve", lambda e: e.tensor_tensor(aq[:], t1[:], SUIm[m], op=ALU.mult),
                           reads=[t1.b, gcs.b], writes=[aq.b])
                      yield
                      pN = ps()
                      k.op("pe", lambda e: e.transpose(pN[:, 0:128], P0[:, 0:128], ident[:]),
                           reads=[P0.b, ident.b], writes=[pN.b])
                      k.op("act", lambda e: e.activation(out=P0[:, 128:256], in_=pN[:, 0:128], func=AF.Copy),
                           reads=[pN.b], writes=[P0.b])
                      TTc = H["TT"][0]
                      k.op("dve", lambda e: e.tensor_tensor(TTc[:], P0[:, 128:256], ident[:], op=ALU.add),
                           reads=[P0.b, ident.b], writes=[TTc.b])
                      yield
                      Pc = P0
                      for n in range(1, 7):
                          Pn = H["P"][n % 2]
                          TTn = H["TT"][n % 2]
                          pP = ps()
                          k.op("pe", lambda e, Pc=Pc, pP=pP: e.matmul(pP[:, 0:128], lhsT=Pc[:, 128:256], rhs=Pc[:, 0:128],
                                                                      start=True, stop=True),
                               reads=[Pc.b], writes=[pP.b])
                          if n < 6:
                              k.op("pe", lambda e, Pc=Pc, pP=pP: e.matmul(pP[:, 128:256], lhsT=Pc[:, 0:128],
                                                                          rhs=Pc[:, 128:256], start=True, stop=True),
                                   reads=[Pc.b], writes=[pP.b])
                          k.op("act", lambda e, Pn=Pn, pP=pP: e.activation(out=Pn[:], in_=pP[:, 0:256], func=AF.Copy),
                               reads=[pP.b], writes=[Pn.b])
                          yield
                          pT = ps()
                          k.op("pe", lambda e, Pn=Pn, pT=pT, TTc=TTc: e.matmul(pT[:, 0:128], lhsT=Pn[:, 0:128], rhs=TTc[:],
                                                                               start=True, stop=True),
                               reads=[Pn.b, TTc.b], writes=[pT.b])
                          k.op("dve", lambda e, TTn=TTn, TTc=TTc, pT=pT: e.tensor_tensor(
                              TTn[:], TTc[:], pT[:, 0:128], op=ALU.add), reads=[TTc.b, pT.b], writes=[TTn.b])
                          yield
                          Pc, TTc = Pn, TTn
                      pU = ps()
                      k.op("pe", lambda e, TTc=TTc: e.matmul(pU[:, 0:128], lhsT=TTc[:], rhs=VB[:, h, :], start=True, stop=True),
                           reads=[TTc.b, VB.b], writes=[pU.b])
                      k.op("pe", lambda e, TTc=TTc: e.matmul(pU[:, 128:256], lhsT=KBG[:, h, :], rhs=TTc[:], start=True, stop=True),
                           reads=[TTc.b, KBG.b], writes=[pU.b])
                      k.op("act", lambda e: e.activation(out=UW[:], in_=pU[:, 0:256], func=AF.Copy),
                           reads=[pU.b], writes=[UW.b])
                      yield
                      if not smp:
                          S_ = Sst[h]
                          p1 = ps()
                          k.op("pe", lambda e: e.matmul(p1[:, 0:128], lhsT=UW[:, 128:256], rhs=S_[:], start=True, stop=True),
                               reads=[UW.b, S_.b], writes=[p1.b])
                          k.op("dve", lambda e: e.tensor_tensor(vn[:], UW[:, 0:128], p1[:, 0:128], op=ALU.subtract),
                               reads=[UW.b, p1.b], writes=[vn.b])
                          yield
                          p2 = ps()
                          k.op("pe", lambda e: e.matmul(p2[:, 0:128], lhsT=DQT, rhs=S_[:], start=True, stop=False),
                               reads=[KQT.b, S_.b], writes=[p2.b])
                          k.op("pe", lambda e: e.matmul(p2[:, 0:128], lhsT=aq[:], rhs=vn[:], start=False, stop=True),
                               reads=[aq.b, vn.b], writes=[p2.b])
                          k.op("act", lambda e: e.activation(out=O32[:, h * 128:(h + 1) * 128], in_=p2[:, 0:128], func=AF.Copy),
                               reads=[p2.b], writes=[O32.b])
                          p3 = ps()
                          k.op("pe", lambda e: e.matmul(p3[:, 0:128], lhsT=KTL[:, h, :], rhs=vn[:], start=True, stop=True),
                               reads=[KTL.b, vn.b], writes=[p3.b])
                          k.op("dve", lambda e: e.scalar_tensor_tensor(out=S_[:], in0=S_[:], scalar=dec[:, h:h + 1],
                                                                       in1=p3[:, 0:128], op0=ALU.mult, op1=ALU.add),
                               reads=[S_.b, sm.b, p3.b], writes=[S_.b])
                      else:
                          Ssm, WTm, DQm, vm, So = (sm_st["Ssm"], sm_st["WTm"], sm_st["DQm"], sm_st["vm"], sm_st["So"])
                          decB = sm_st["decB"]
                          k.op("dve", lambda e: e.tensor_tensor(
                              WTm[:], UW[:, 128:256].unsqueeze(1).to_broadcast([128, 16, 128]), CM32, op=ALU.mult),
                              reads=[UW.b, gcs.b], writes=[WTm.b])
                          p1 = ps(hold=True)
                          for b in range(16):
                              k.op("pe", lambda e, b=b: e.matmul(p1[:, 0:128], lhsT=WTm[:, b, :], rhs=Ssm[:, b * 4 + h, :],
                                                                 start=(b == 0), stop=(b == 15)),
                                   reads=[WTm.b, Ssm.b], writes=[p1.b])
                          k.op("dve", lambda e: e.tensor_tensor(vn[:], UW[:, 0:128], p1[:, 0:128], op=ALU.subtract),
                               reads=[UW.b, p1.b], writes=[vn.b])
                          ps_release(p1)
                          k.op("dve", lambda e: e.tensor_tensor(
                              DQm[:], DQT.unsqueeze(1).to_broadcast([128, 16, 128]), CM32, op=ALU.mult),
                              reads=[KQT.b, gcs.b], writes=[DQm.b])
                          p2 = ps(hold=True)
                          for b in range(16):
                              k.op("pe", lambda e, b=b: e.matmul(p2[:, 0:128], lhsT=DQm[:, b, :], rhs=Ssm[:, b * 4 + h, :],
                                                                 start=(b == 0), stop=False),
                                   reads=[DQm.b, Ssm.b], writes=[p2.b])
                          k.op("pe", lambda e: e.matmul(p2[:, 0:128], lhsT=aq[:], rhs=vn[:], start=False, stop=True),
                               reads=[aq.b, vn.b], writes=[p2.b])
                          k.op("act", lambda e: e.activation(out=O32[:, h * 128:(h + 1) * 128], in_=p2[:, 0:128], func=AF.Copy),
                               reads=[p2.b], writes=[O32.b])
                          ps_release(p2)
                          k.op("dve", lambda e: e.tensor_tensor(
                              vm[:], vn[:].unsqueeze(1).to_broadcast([128, 16, 128]),
                              G8.unsqueeze(2).to_broadcast([128, 16, 128]), op=ALU.mult),
                              reads=[vn.b, gcs.b], writes=[vm.b])
                          for b in range(16):
                              p3 = ps()
                              k.op("pe", lambda e, b=b, p3=p3: e.matmul(p3[:, 0:128], lhsT=KTL[:, h, :], rhs=vm[:, b, :],
                                                                        start=True, stop=True),
                                   reads=[KTL.b, vm.b], writes=[p3.b])
                              so = So[b % 2]
                              k.op("dve", lambda e, b=b, p3=p3, so=so: e.scalar_tensor_tensor(
                                  out=so[:], in0=Ssm[:, b * 4 + h, :], scalar=decB[:, b * 4 + h:b * 4 + h + 1],
                                  in1=p3[:, 0:128], op0=ALU.mult, op1=ALU.add),
                                  reads=[Ssm.b, decB.b, p3.b], writes=[so.b])
                              k.dma("sp", o_sgdn[b, h], so[:], reads=[so.b])
                  if smp and DBG:
                      k.dma("sp", dbg_tm[:, :], TM[:], reads=[TM.b])
                      k.dma("sp", dbg_sm[:, :], sm[:], reads=[sm.b])
                      k.dma("sp", dbg_o[:, :], O32[:], reads=[O32.b])
                      H = hb[1]
                      k.dma("sp", dbg_uw[:, :], H["UW"][:], reads=[H["UW"].b])
                      k.dma("sp", dbg_vn[:, :], H["vn"][:], reads=[H["vn"].b])
                      k.dma("sp", dbg_aq[:, :], H["aq"][:], reads=[H["aq"].b])
                      k.dma("sp", dbg_tt[:, :], H["TT"][0][:], reads=[H["TT"][0].b])
                  yield "B"
                  if smp:
                      for h in range(4):
                          for _ in head_gen(h):
                              pass
                  else:
                      gens = [head_gen(h) for h in range(4)]
                      alive = [True] * 4
                      while any(alive):
                          for gi, g_ in enumerate(gens):
                              if alive[gi]:
                                  try:
                                      next(g_)
                                  except StopIteration:
                                      alive[gi] = False
                          yield
                  head_rmsnorm(O32.b, O32[:, :], 4, 128, hv[:, 384:512], go32, gtmp, ghstat, "go")
                  k.op("dve", lambda e: e.tensor_tensor(go32[:], go32[:], sgz[:], op=ALU.mult),
                       reads=[go32.b, sgz.b], writes=[go32.b])
                  k.dma("sp", g_sc[j * 128:(j + 1) * 128, :], go32[:], reads=[go32.b], writes=[g_sc_b[j]])

              with contextlib.ExitStack() as st2:
                  sm_st = dict(Us=sb(st2, "Us", [128, 12, 16, 11], BF16), Ssm=sb(st2, "Ssm", [128, 64, 128]),
                               WTm=sb(st2, "WTm", [128, 16, 128]), rhs3=sb(st2, "rhs3", [128, 16, 4]),
                               decB=sb(st2, "decB", [128, 64]),
                               So=[sb(st2, "So%d" % i, [128, 128]) for i in range(2)])
                  sm_st["DQm"] = sm_st["WTm"]
                  sm_st["vm"] = sm_st["WTm"]
                  Ssm = sm_st["Ssm"]
                  for b in range(16):
                      k.dma("sp", Ssm[:, b * 4:(b + 1) * 4, :], sgdn_d[b].rearrange("h k v -> k h v"), writes=[Ssm.b])
                  cb = sb(st2, "cb", [48, 1536])
                  k.dma("sp", cb[:], sconv_d[:, :], writes=[cb.b])
                  Us = sm_st["Us"]
                  for cc0 in (0, 4, 8):
                      p = ps()
                      for c4 in range(4):
                          cc = cc0 + c4
                          k.op("pe", lambda e, cc=cc, c4=c4, p=p: e.transpose(
                              p[:, c4 * 48:(c4 + 1) * 48], cb[:, cc * 128:(cc + 1) * 128], ident[0:48, 0:48]),
                              reads=[cb.b, ident.b], writes=[p.b])
                      for c4 in range(4):
                          k.op("act", lambda e, cc0=cc0, c4=c4, p=p: e.activation(
                              out=Us[:, cc0 + c4, :, 0:3],
                              in_=p[:, c4 * 48:(c4 + 1) * 48].rearrange("p (b t) -> p b t", b=16),
                              func=AF.Copy), reads=[p.b], writes=[Us.b])
                  for _ in gdn_tile(NT_ALL, sm_st):
                      pass
                  outs_done += [s_.b for s_ in sm_st["So"]]
                  k.barrier()
              sgz2.append(sb(st, "sgz1", [128, 512]))
              KQT2.append(sb(st, "KQT1", [128, 12, 128]))
              KBG2.append(sb(st, "KBG1", [128, 4, 128]))
              KTL2.append(sb(st, "KTL1", [128, 4, 128]))
              VB2.append(sb(st, "VB1", [128, 4, 128]))
              sm2.append(sb(st, "gsm1", [128, 64]))
              gensG = [gdn_tile(j) for j in range(NT_ALL)]
              for tok in gensG[0]:
                  if tok == "B":
                      break
              for j in range(NT_ALL):
                  cur = gensG[j]
                  nxt = gensG[j + 1] if j + 1 < NT_ALL else None
                  cur_alive, nxt_inA = True, nxt is not None
                  while cur_alive or nxt_inA:
                      if cur_alive:
                          try:
                              next(cur)
                          except StopIteration:
                              cur_alive = False
                      if nxt_inA:
                          if next(nxt) == "B":
                              nxt_inA = False
              for h in range(4):
                  k.dma("sp", o_pgdn[h], Sst[h][:], reads=[Sst[h].b])
              outs_done += [s_.b for s_ in Sst]

        k.barrier()
        kst = contextlib.ExitStack()
        KT = sb(kst, "KT", [64, 4, SEQ + 256], BF16)
        VA = sb(kst, "VA", [128, NT_ALL + 2, 4, 65], BF16)
        IKT = sb(kst, "IKT", [64, SEQ + 256], BF16)
        KTn = sb(kst, "KTn", [64, 4, 128], BF16)
        IKTn = sb(kst, "IKTn", [64, 128], BF16)
        VAn = sb(kst, "VAn", [128, 4, 65], BF16)
        k.op("pool", lambda e: e.memset(VA[:], 1.0), writes=[VA.b])
        k.op("pool", lambda e: e.memset(VAn[:], 1.0), writes=[VAn.b])
        k.barrier()
        with contextlib.ExitStack() as st:
          if 'C' in PH:
              NCOL = 576 + 1536
              Wk = sb(st, "Wk", [128, KC, NCOL], BF16)
              load_w(Wk, w_in, O_AK, 512, 0)
              load_w(Wk, w_in, O_IK, 64, 512)
              load_w(Wk, w_in, O_GQKV, 1536, 576)
              tsets = [(sb(st, "cx32_%d" % i, [128, D]), sb(st, "cxn_%d" % i, [128, D]),
                        sb(st, "cxT_%d" % i, [128, KC, 128], BF16), sb(st, "cstat_%d" % i, [128, 4]))
                       for i in range(3)]
              tmp = sb(st, "ctmp", [128, 512])
              hstat = sb(st, "chstat", [128, 24])
              ko = [sb(st, "ko%d" % i, [128, 256]) for i in range(2)]
              vo = [sb(st, "vo%d" % i, [128, 256]) for i in range(2)]
              io = [sb(st, "io%d" % i, [128, 64]) for i in range(2)]
              gq = sb(st, "gq", [128, 1536])
              def c_gen(j):
                  smp = (j == NT_ALL)
                  src = x_smp[:, :] if smp else x_all[j * 128:(j + 1) * 128, :]
                  _, xT = load_xT(tsets[j % 3], src, 0)
                  yield "B"
                  pkv = ps()
                  linear(xT, Wk, 0, 512, pkv)
                  pik = ps()
                  linear(xT, Wk, 512, 64, pik)
                  kk, vv, ii = ko[j % 2], vo[j % 2], io[j % 2]
                  head_rmsnorm(pkv.b, pkv[:, 0:256], 4, 64, hv[:, 64:128], kk, tmp, hstat, "ak")
                  k.op("act", lambda e, vv=vv, pkv=pkv: e.activation(out=vv[:], in_=pkv[:, 256:512], func=AF.Copy),
                       reads=[pkv.b], writes=[vv.b])
                  k.op("act", lambda e, ii=ii, pik=pik: e.activation(out=ii[:], in_=pik[:, 0:64], func=AF.Copy),
                       reads=[pik.b], writes=[ii.b])
                  if smp:
                      kside_store(kk[:], vv[:], ii[:], [kk.b, vv.b, ii.b], KTn[:], IKTn[:], VAn[:, :, 0:64],
                                  [KTn.b, IKTn.b, VAn.b])
                  else:
                      kside_store(kk[:], vv[:], ii[:], [kk.b, vv.b, ii.b], KT[:, :, j * 128:(j + 1) * 128],
                                  IKT[:, j * 128:(j + 1) * 128], VA[:, j, :, 0:64], [KT.b, IKT.b, VA.b])
                  if smp:
                      k.dma("sp", o_sk[:, :], kk[:], reads=[kk.b])
                      k.dma("sp", o_sv[:, :], vv[:], reads=[vv.b])
                      k.dma("sp", o_sik[:, :], ii[:], reads=[ii.b])
                  else:
                      k.dma("sp", o_pk[j * 128:(j + 1) * 128, :], kk[:], reads=[kk.b])
                      k.dma("sp", o_pv[j * 128:(j + 1) * 128, :], vv[:], reads=[vv.b])
                      k.dma("sp", o_pik[j * 128:(j + 1) * 128, :], ii[:], reads=[ii.b])
                  if j >= NT_ALL - 1:
                      for c in range(3):
                          pg = ps()
                          linear(xT, Wk, 576 + c * 512, 512, pg)
                          k.op("act", lambda e, c=c, pg=pg: e.activation(
                              out=gq[:, c * 512:(c + 1) * 512], in_=pg[:, :], func=AF.Copy),
                              reads=[pg.b], writes=[gq.b])
                      if smp:
                          for t3 in range(3):
                              k.dma("sp", o_sconv[:, t3, :], gq[5 + t3::8, :], reads=[gq.b])
                      else:
                          k.dma("sp", o_pconv[:, :], gq[125:128, :], reads=[gq.b])
              pipe_ahead([c_gen(j) for j in range(NT_ALL + 1)], 2)
              outs_done += [b.b for b in ko + vo + io] + [gq.b]

        k.barrier()
        with contextlib.ExitStack() as st:
          if 'E' in PH:
              NQ = 512 + 256 + 4 + 512
              Wq = sb(st, "Wq", [128, KC, NQ], BF16)
              load_w(Wq, w_in, O_AQ, 512, 0)
              load_w(Wq, w_in, O_IQ, 256, 512)
              load_w(Wq, w_in, O_IW, 4, 768)
              load_w(Wq, w_in, O_MQ, 512, 772)
              tsets = [(sb(st, "ex32_%d" % i, [128, D]), sb(st, "exn_%d" % i, [128, D]),
                        sb(st, "exT_%d" % i, [128, KC, 128], BF16), sb(st, "estat_%d" % i, [128, 4]))
                       for i in range(1)] * 2
              tmp = sb(st, "etmp", [128, 512])
              hstat = sb(st, "ehstat", [128, 24])
              mq32 = sb(st, "mq32", [128, 512])
              MQT2 = [sb(st, "MQT%d" % q, [128, 4, 128], BF16) for q in range(2)]
              PT = sb(st, "PT", [128, 2, 4, 128], BF16)
              tE = sb(st, "tE", [128, 4, 128], BF16)
              rec = sb(st, "rec", [128, 8])
              mo32 = [sb(st, "mo32_%d" % i, [128, 512]) for i in range(2)]
              ao32 = [sb(st, "ao32_%d" % i, [128, 512]) for i in range(2)]
              mkb = [sb(st, "mkb%d" % i, [128, 2, 512]) for i in range(2)]
              MKTb = [sb(st, "MKTb%d" % i, [128, 4, 256], BF16) for i in range(2)]
              MVb = [sb(st, "MVb%d" % i, [128, 2, 4, 129], BF16) for i in range(2)]
              for t_ in MVb:
                  k.op("pool", lambda e, t_=t_: e.memset(t_[:], 1.0), writes=[t_.b])
              SC_M = 128.0 ** -0.5

              def mem_scores(MKT_, MQT_, mb, dstPT, cm=None):
                  pS = ps()
                  for h in range(4):
                      k.op("pe", lambda e, h=h: e.matmul(
                          pS[:, h * 128:(h + 1) * 128], lhsT=MKT_[:, h, mb * 128:(mb + 1) * 128],
                          rhs=MQT_[:, h, :], start=True, stop=True),
                          reads=[MKT_.b, MQT_.b], writes=[pS.b])
                  if cm is None:
                      k.op("act", lambda e: e.activation(
                          out=dstPT[:, mb, :, :], in_=pS[:, :].rearrange("p (h t) -> p h t", h=4),
                          func=AF.Exp, scale=SC_M), reads=[pS.b], writes=[dstPT.b])
                  else:
                      k.op("act", lambda e: e.activation(
                          out=tE[:], in_=pS[:, :].rearrange("p (h t) -> p h t", h=4),
                          func=AF.Exp, scale=SC_M), reads=[pS.b], writes=[tE.b])
                      k.op("dve", lambda e: e.tensor_tensor(
                          dstPT[:, mb, :, :], tE[:], cm.unsqueeze(1).to_broadcast([128, 4, 128]), op=ALU.mult),
                          reads=[tE.b, cmask.b], writes=[dstPT.b])

              def mem_pv(pO, PT_, MV_, first, last):
                  for h in range(4):
                      for mb in range(2):
                          k.op("pe", lambda e, h=h, mb=mb: e.matmul(
                              pO[h // 2][:, (h % 2) * 129:(h % 2) * 129 + 129], lhsT=PT_[:, mb, h, :],
                              rhs=MV_[:, mb, h, :], start=False, stop=(last and mb == 1)),
                              reads=[PT_.b, MV_.b], writes=[pO[h // 2].b])

              NIT = 18
              dcs = sb(st, "dcs", [128, 64])
              k.dma("sp", dcs[:], dconst_d[:, :], writes=[dcs.b])
              score = sb(st, "score", [128, SEQ])
              junk = sb(st, "junk", [128, SEQ], BF16)
              maskT2 = [sb(st, "maskT%d" % q, [128, NT_ALL, 128], BF16) for q in range(2)]
              Eb = [sb(st, "Eb%d" % i, [128, 8, 128], BF16) for i in range(2)]
              Pm = [sb(st, "Pm%d" % i, [128, 8, 128], BF16) for i in range(2)]
              QT2 = [sb(st, "QT%d" % q, [64, 8, 128], BF16) for q in range(2)]
              IQT = sb(st, "IQT", [64, 4, 128], BF16)
              aq32 = sb(st, "aq32", [128, 512])
              iq32 = sb(st, "iq32", [128, 260])
              wst = sb(st, "wst", [128, 16])
              bis = sb(st, "bis", [128, 8 + 2 * NIT])
              rtmp = sb(st, "rtmp", [128, 512])
              pent = sb(st, "pent", [128, 256])
              den8 = sb(st, "den8", [128, 8])
              SC_A = 64.0 ** -0.5
              cmask32e = dcs[:, 40:56]

              def dsa_q(xT, QTb):
                  pq = ps()
                  linear(xT, Wq, 0, 512, pq)
                  head_rmsnorm(pq.b, pq[:, :], 8, 64, hv[:, 0:64], aq32, tmp, hstat, "aq")
                  for half in range(2):
                      p = ps()
                      for hh in range(4):
                          k.op("pe", lambda e, hh=hh, half=half, p=p: e.transpose(
                              p[0:64, hh * 128:(hh + 1) * 128],
                              aq32[:, (half * 4 + hh) * 64:(half * 4 + hh + 1) * 64], ident[:]),
                              reads=[aq32.b, ident.b], writes=[p.b])
                      k.op("act", lambda e, half=half, p=p: e.activation(
                          out=QTb[:, half * 4:(half + 1) * 4, :], in_=p[0:64, :].rearrange("p (h t) -> p h t", h=4),
                          func=AF.Copy), reads=[p.b], writes=[QTb.b])
                  yield
                  pi = ps()
                  linear(xT, Wq, 512, 260, pi)
                  k.op("act", lambda e: e.activation(out=iq32[:], in_=pi[:, 0:260], func=AF.Copy),
                       reads=[pi.b], writes=[iq32.b])
                  p = ps()
                  for hh in range(4):
                      k.op("pe", lambda e, hh=hh, p=p: e.transpose(
                          p[0:64, hh * 128:(hh + 1) * 128], iq32[:, hh * 64:(hh + 1) * 64], ident[:]),
                          reads=[iq32.b, ident.b], writes=[p.b])
                  k.op("act", lambda e, p=p: e.activation(
                      out=IQT[:], in_=p[0:64, :].rearrange("p (h t) -> p h t", h=4), func=AF.Copy),
                      reads=[p.b], writes=[IQT.b])
                  yield
                  k.op("act", lambda e: e.activation(out=wst[:, 0:4], in_=iq32[:, 256:260], func=AF.Abs),
                       reads=[iq32.b], writes=[wst.b])
                  k.op("dve", lambda e: e.tensor_scalar(wst[:, 4:8], iq32[:, 256:260], 0.0, 2.0,
                                                        op0=ALU.is_gt, op1=ALU.mult),
                       reads=[iq32.b], writes=[wst.b])
                  k.op("dve", lambda e: e.tensor_scalar(wst[:, 4:8], wst[:, 4:8], -1.0, None, op0=ALU.add),
                       reads=[wst.b], writes=[wst.b])
              def dsa_index(L):
                  for c0 in range(0, L, 512):
                      n = min(512, L - c0)
                      for hh in range(4):
                          pS = ps()
                          k.op("pe", lambda e, hh=hh, pS=pS, c0=c0, n=n: e.matmul(
                              pS[:, 0:n], lhsT=IQT[:, hh, :], rhs=IKT[:, c0:c0 + n], start=True, stop=True),
                              reads=[IQT.b, IKT.b], writes=[pS.b])
                          k.op("act", lambda e, hh=hh, pS=pS, n=n: e.activation(
                              out=rtmp[:, 0:n], in_=pS[:, 0:n], func=AF.Relu, scale=wst[:, hh:hh + 1]),
                              reads=[pS.b, wst.b], writes=[rtmp.b])
                          if hh == 0:
                              k.op("dve", lambda e, c0=c0, n=n: e.tensor_scalar(
                                  score[:, c0:c0 + n], rtmp[:, 0:n], wst[:, 4:5], None, op0=ALU.mult),
                                  reads=[rtmp.b, wst.b], writes=[score.b])
                          else:
                              k.op("dve", lambda e, hh=hh, c0=c0, n=n: e.scalar_tensor_tensor(
                                  out=score[:, c0:c0 + n], in0=rtmp[:, 0:n], scalar=wst[:, 4 + hh:5 + hh],
                                  in1=score[:, c0:c0 + n], op0=ALU.mult, op1=ALU.add),
                                  reads=[rtmp.b, wst.b, score.b], writes=[score.b])
                          yield
              def dsa_select(i, nk, L, sel, mTb):
                  k.op("dve", lambda e: e.tensor_reduce(bis[:, 4:5], score[:, 0:L], axis=AX.X, op=ALU.max),
                       reads=[score.b], writes=[bis.b])
                  k.op("dve", lambda e: e.tensor_reduce(bis[:, 5:6], score[:, 0:L], axis=AX.X, op=ALU.min),
                       reads=[score.b], writes=[bis.b])
                  k.dma("sp", pent[:], pen_d[i], writes=[pent.b])
                  k.op("dve", lambda e: e.tensor_tensor(score[:, L - 256:L], score[:, L - 256:L], pent[:], op=ALU.add),
                       reads=[score.b, pent.b], writes=[score.b])
                  if DBG and i == 1 and sel is None:
                      k.dma("sp", dbg_sc[:, :], score[:, 0:512], reads=[score.b])
                  k.op("dve", lambda e: e.tensor_tensor(bis[:, 0:1], bis[:, 4:5], bis[:, 5:6], op=ALU.add),
                       reads=[bis.b], writes=[bis.b])
                  k.op("dve", lambda e: e.tensor_scalar(bis[:, 0:1], bis[:, 0:1], 0.5, None, op0=ALU.mult),
                       reads=[bis.b], writes=[bis.b])
                  k.op("dve", lambda e: e.tensor_tensor(bis[:, 3:4], bis[:, 4:5], bis[:, 5:6], op=ALU.subtract),
                       reads=[bis.b], writes=[bis.b])
                  k.op("dve", lambda e: e.tensor_scalar(bis[:, 3:4], bis[:, 3:4], 2.0, None, op0=ALU.add),
                       reads=[bis.b], writes=[bis.b])
                  k.op("dve", lambda e: e.tensor_scalar(bis[:, 8:8 + NIT], dcs[:, 0:NIT], bis[:, 3:4], None, op0=ALU.mult),
                       reads=[bis.b, dcs.b], writes=[bis.b])
                  k.op("dve", lambda e: e.tensor_scalar(bis[:, 8 + NIT:8 + 2 * NIT], bis[:, 8:8 + NIT], -0.5, None,
                                                        op0=ALU.mult), reads=[bis.b], writes=[bis.b])
                  for n_ in range(NIT):
                      k.op("dve", lambda e: e.tensor_scalar(junk[:, 0:L], score[:, 0:L], bis[:, 0:1], None,
                                                            op0=ALU.is_ge, op1=ALU.add, accum_out=bis[:, 1:2]),
                           reads=[score.b, bis.b], writes=[junk.b, bis.b])
                      k.op("dve", lambda e, n_=n_: e.tensor_scalar(bis[:, 2:3], bis[:, 1:2], dcs[:, 32:33],
                                                                  bis[:, 8 + n_:9 + n_], op0=ALU.is_ge, op1=ALU.mult),
                           reads=[bis.b, dcs.b], writes=[bis.b])
                      k.op("dve", lambda e, n_=n_: e.scalar_tensor_tensor(
                          out=bis[:, 0:1], in0=bis[:, 2:3], scalar=bis[:, 8 + NIT + n_:9 + NIT + n_], in1=bis[:, 0:1],
                          op0=ALU.add, op1=ALU.add), reads=[bis.b], writes=[bis.b])
                      yield
                  k.op("dve", lambda e: e.tensor_tensor(bis[:, 0:1], bis[:, 0:1], bis[:, 8 + NIT - 1:8 + NIT],
                                                        op=ALU.subtract), reads=[bis.b], writes=[bis.b])
                  k.op("dve", lambda e: e.tensor_scalar(score[:, 0:L], score[:, 0:L], bis[:, 0:1], None, op0=ALU.is_ge),
                       reads=[score.b, bis.b], writes=[score.b])
                  if DBG and i == 1 and sel is None:
                      k.dma("sp", dbg_mask[:, :], score[:, 0:512], reads=[score.b])
                      k.dma("sp", dbg_bis[:, :], bis[:], reads=[bis.b])
                  for kb0 in range(0, nk, 4):
                      nb_ = min(4, nk - kb0)
                      p = ps()
                      for j_ in range(nb_):
                          k.op("pe", lambda e, j_=j_, kb0=kb0, p=p: e.transpose(
                              p[:, j_ * 128:(j_ + 1) * 128], score[:, (kb0 + j_) * 128:(kb0 + j_ + 1) * 128], ident[:]),
                              reads=[score.b, ident.b], writes=[p.b])
                      k.op("act", lambda e, kb0=kb0, nb_=nb_, p=p: e.activation(
                          out=mTb[:, kb0:kb0 + nb_, :], in_=p[:, 0:nb_ * 128].rearrange("p (a t) -> p a t", a=nb_),
                          func=AF.Copy), reads=[p.b], writes=[mTb.b])
                      yield
              def dsa_attend(kblocks, ao, sel, accumulate, QTb, mTb):
                  pO = [ps(hold=True), ps(hold=True)]
                  ps_zero(pO[0]); ps_zero(pO[1])
                  nkb = len(kblocks)
                  for ki, (ktT, kc0, vaT, vblk, mi, kbufs) in enumerate(kblocks):
                      E_, P_ = Eb[ki % 2], Pm[ki % 2]
                      for g2 in range(2):
                          pS = ps()
                          for gg in range(2):
                              g = g2 * 2 + gg
                              k.op("pe", lambda e, g=g, gg=gg, pS=pS: e.matmul(
                                  pS[:, gg * 256:(gg + 1) * 256], lhsT=ktT[:, g, kc0:kc0 + 128],
                                  rhs=QTb[:, 2 * g:2 * g + 2, :], start=True, stop=True),
                                  reads=kbufs + [QTb.b], writes=[pS.b])
                          k.op("act", lambda e, g2=g2, pS=pS, E_=E_: e.activation(
                              out=E_[:, g2 * 4:(g2 + 1) * 4, :], in_=pS[:, :].rearrange("p (h t) -> p h t", h=4),
                              func=AF.Exp, scale=SC_A), reads=[pS.b], writes=[E_.b])
                      k.op("dve", lambda e, mi=mi, E_=E_, P_=P_: e.tensor_tensor(
                          P_[:], E_[:], mTb[:, mi, :].unsqueeze(1).to_broadcast([128, 8, 128]), op=ALU.mult),
                          reads=[E_.b, mTb.b], writes=[P_.b])
                      for hh in range(8):
                          rhs_ = vaT[:, hh // 2, :] if vblk is None else vaT[:, vblk, hh // 2, :]
                          k.op("pe", lambda e, hh=hh, P_=P_, rhs_=rhs_: e.matmul(
                              pO[hh // 4][:, (hh % 4) * 65:(hh % 4) * 65 + 65], lhsT=P_[:, hh, :],
                              rhs=rhs_, start=False, stop=(ki == nkb - 1)),
                              reads=[P_.b] + kbufs, writes=[pO[hh // 4].b])
                      yield
                  for j_ in range(2):
                      pv3 = pO[j_][:, 0:260].rearrange("p (h c) -> p h c", h=4)
                      k.op("dve", lambda e, j_=j_, pv3=pv3: e.reciprocal(
                          den8[:, 4 * j_:4 * j_ + 4].unsqueeze(2), pv3[:, :, 64:65]),
                          reads=[pO[j_].b], writes=[den8.b])
                      if sel is not None:
                          k.op("dve", lambda e, j_=j_: e.tensor_scalar(
                              den8[:, 4 * j_:4 * j_ + 4], den8[:, 4 * j_:4 * j_ + 4], sel, None, op0=ALU.mult),
                              reads=[den8.b, cmask.b, selw.b], writes=[den8.b])
                      dst = ao[:, 256 * j_:256 * j_ + 256].rearrange("p (h d) -> p h d", h=4)
                      bc_ = den8[:, 4 * j_:4 * j_ + 4].unsqueeze(2).to_broadcast([128, 4, 64])
                      if not accumulate:
                          k.op("dve", lambda e, pv3=pv3, dst=dst, bc_=bc_: e.tensor_tensor(
                              dst, pv3[:, :, 0:64], bc_, op=ALU.mult), reads=[pO[j_].b, den8.b], writes=[ao.b])
                      else:
                          r3 = rtmp[:, 0:256].rearrange("p (h d) -> p h d", h=4)
                          k.op("dve", lambda e, pv3=pv3, r3=r3, bc_=bc_: e.tensor_tensor(
                              r3, pv3[:, :, 0:64], bc_, op=ALU.mult), reads=[pO[j_].b, den8.b], writes=[rtmp.b])
                          k.op("dve", lambda e, dst=dst, r3=r3: e.tensor_tensor(dst, dst, r3, op=ALU.add),
                               reads=[rtmp.b, ao.b], writes=[ao.b])
                  ps_release(pO[0]); ps_release(pO[1])

              def run(g):
                  for _ in g:
                      pass

              def rr(*gens):
                  alive = [g for g in gens if g is not None]
                  while alive:
                      for g in list(alive):
                          try:
                              next(g)
                          except StopIteration:
                              alive.remove(g)

              def mq_proj(xT, MQTb):
                  pmq = ps()
                  linear(xT, Wq, 772, 512, pmq)
                  head_rmsnorm(pmq.b, pmq[:, :], 4, 128, hv[:, 128:256], mq32, tmp, hstat, "mq")
                  p = ps()
                  for h in range(4):
                      k.op("pe", lambda e, h=h, p=p: e.transpose(
                          p[:, h * 128:(h + 1) * 128], mq32[:, h * 128:(h + 1) * 128], ident[:]),
                          reads=[mq32.b, ident.b], writes=[p.b])
                  k.op("act", lambda e, p=p: e.activation(
                      out=MQTb[:], in_=p[:, :].rearrange("p (h t) -> p h t", h=4), func=AF.Copy),
                      reads=[p.b], writes=[MQTb.b])

              def mem_attn(i, smp, MQT):
                  pO = [ps(hold=True), ps(hold=True)]
                  ps_zero(pO[0]); ps_zero(pO[1])
                  if not smp:
                      for mb in range(2):
                          mem_scores(MKT, MQT, mb, PT)
                          yield
                      mem_pv(pO, PT, MVa, True, True)
                      yield
                  else:
                      for b in range(16):
                          kb_, Kt_, Vb_ = mkb[b % 2], MKTb[b % 2], MVb[b % 2]
                          k.dma("sp", kb_[:], cmk_d[b].rearrange("(mb p) c -> p mb c", p=128), writes=[kb_.b])
                          for mb in range(2):
                              k.dma("pool", Vb_[:, mb, :, 0:128],
                                    cmv_d[b, mb * 128:(mb + 1) * 128, :].rearrange("p (h d) -> p h d", h=4),
                                    writes=[Vb_.b])
                          for mb in range(2):
                              p = ps()
                              for h in range(4):
                                  k.op("pe", lambda e, h=h, p=p, mb=mb: e.transpose(
                                      p[:, h * 128:(h + 1) * 128], kb_[:, mb, h * 128:(h + 1) * 128], ident[:]),
                                      reads=[kb_.b, ident.b], writes=[p.b])
                              k.op("act", lambda e, p=p, mb=mb: e.activation(
                                  out=Kt_[:, :, mb * 128:(mb + 1) * 128],
                                  in_=p[:, :].rearrange("p (h m) -> p h m", h=4), func=AF.Copy),
                                  reads=[p.b], writes=[Kt_.b])
                          for mb in range(2):
                              mem_scores(Kt_, MQT, mb, PT, cm=cmask[:, b, :])
                          mem_pv(pO, PT, Vb_, b == 0, b == 15)
                          yield
                  mo = mo32[i % 2]
                  for j in range(2):
                      pv3 = pO[j][:, 0:258].rearrange("p (h c) -> p h c", h=2)
                      k.op("dve", lambda e, j=j, pv3=pv3: e.reciprocal(
                          rec[:, 2 * j:2 * j + 2].unsqueeze(2), pv3[:, :, 128:129]),
                          reads=[pO[j].b], writes=[rec.b])
                      k.op("dve", lambda e, j=j, pv3=pv3, mo=mo: e.tensor_tensor(
                          mo[:, 256 * j:256 * j + 256].rearrange("p (h d) -> p h d", h=2), pv3[:, :, 0:128],
                          rec[:, 2 * j:2 * j + 2].unsqueeze(2).to_broadcast([128, 2, 128]), op=ALU.mult),
                          reads=[pO[j].b, rec.b], writes=[mo.b])
                  ps_release(pO[0]); ps_release(pO[1])
                  k.dma("sp", m_sc[i * 128:(i + 1) * 128, :], mo[:], reads=[mo.b], writes=[m_sc_b[i]])

              NDSA = cfg.get("dsa_tiles", 17)

              def tile_gen(i):
                  bf = i % 2
                  QTb, mTb, MQTb = QT2[bf], maskT2[bf], MQT2[bf]
                  x32, xT = load_xT(tsets[0], x_own[i * 128:(i + 1) * 128, :], 0)
                  yield
                  mq_proj(xT, MQTb)
                  yield
                  nk_i = 4 * (i // 2) + (2 if i % 2 == 0 else 4)
                  if i < NDSA:
                      yield from dsa_q(xT, QTb)
                      yield from dsa_index(nk_i * 128)
                      yield from dsa_select(i, nk_i, nk_i * 128, None, mTb)
                  yield "B"
                  ao = ao32[bf]
                  if i < NDSA:
                      kbl = [(KT, kb * 128, VA, kb, kb, [KT.b, VA.b]) for kb in range(nk_i)]
                      yield from dsa_attend(kbl, ao, None, False, QTb, mTb)
                  else:
                      k.op("pool", lambda e: e.memset(ao[:], 0.0), writes=[ao.b])
                  k.dma("sp", a_sc[i * 128:(i + 1) * 128, :], ao[:], reads=[ao.b], writes=[a_sc_b[i]])
                  yield from mem_attn(i, False, MQTb)

              gens = [tile_gen(i) for i in range(NT_OWN)]

              def to_B(g):
                  for tok in g:
                      if tok == "B":
                          return

              to_B(gens[0])
              for i in range(NT_OWN):
                  cur = gens[i]
                  nxt = gens[i + 1] if i + 1 < NT_OWN else None
                  cur_alive, nxt_inA = True, nxt is not None
                  while cur_alive or nxt_inA:
                      if cur_alive:
                          try:
                              next(cur)
                          except StopIteration:
                              cur_alive = False
                      if nxt_inA:
                          if next(nxt) == "B":
                              nxt_inA = False

              i = NT_OWN
              x32, xT = load_xT(tsets[0], x_smp[:, :], 0)
              QTb, mTb, MQTb = QT2[0], maskT2[0], MQT2[0]
              mq_proj(xT, MQTb)
              run(mem_attn(i, True, MQTb))
              ao = ao32[0]
              if NDSA < 17:
                  k.op("pool", lambda e: e.memset(ao[:], 0.0), writes=[ao.b])
              else:
                  ptb = sb(st, "ptb", [128, 256], I32)
                  ptf = sb(st, "ptf", [128, 256])
                  pti = sb(st, "pti", [128, 256], I32)
                  k.dma("sp", ptb[:], pt_d[0:1, :].partition_broadcast(128), writes=[ptb.b])
                  k.op("dve", lambda e: e.tensor_copy(ptf[:], ptb[:]), reads=[ptb.b], writes=[ptf.b])
                  k.op("dve", lambda e: e.tensor_scalar(ptf[:], ptf[:], 128.0, dcs[:, 33:34],
                                                        op0=ALU.mult, op1=ALU.add),
                       reads=[ptf.b, dcs.b], writes=[ptf.b])
                  k.op("dve", lambda e: e.tensor_copy(pti[:], ptf[:]), reads=[ptf.b], writes=[pti.b])
                  kvpg = [sb(st, "kvpg%d" % q, [128, 512]) for q in range(2)]
                  ipg = [sb(st, "ipg%d" % q, [128, 64]) for q in range(2)]
                  IQTm = [sb(st, "IQTm%d" % q, [64, 4, 128], BF16) for q in range(2)]
                  KTh, VAh, IKTh = [Buf(), Buf()], [Buf(), Buf()], [Buf(), Buf()]
                  run(dsa_q(xT, QTb))
                  k.op("pool", lambda e: e.memset(score[:, 0:2048], 0.0), writes=[score.b])

                  def idx_gather(b):
                      hf = b % 2
                      base = 17 * hf
                      for pg in range(16):
                          q_ = pg % 2
                          col = b * 16 + pg
                          off = bass.IndirectOffsetOnAxis(ap=pti[:, col:col + 1], axis=0)
                          k.dma("pool", ipg[q_][:], cik_d[:, :], reads=[pti.b], writes=[ipg[q_].b], indirect=off)
                          kside_store(None, None, ipg[q_][:], [ipg[q_].b], None,
                                      IKT[:, (base + pg) * 128:(base + pg + 1) * 128], None, [IKTh[hf]])
                          yield

                  def idx_score(b):
                      hf = b % 2
                      base = 17 * hf
                      Im = IQTm[hf]
                      k.op("dve", lambda e: e.tensor_tensor(
                          Im[:], IQT[:], cmask[0:64, b, :].unsqueeze(1).to_broadcast([64, 4, 128]), op=ALU.mult),
                          reads=[IQT.b, cmask.b], writes=[Im.b])
                      for c in range(4):
                          for hh in range(4):
                              pS = ps()
                              c0 = base * 128 + c * 512
                              k.op("pe", lambda e, hh=hh, pS=pS, c0=c0: e.matmul(
                                  pS[:, :], lhsT=Im[:, hh, :], rhs=IKT[:, c0:c0 + 512], start=True, stop=True),
                                  reads=[Im.b, IKTh[hf]], writes=[pS.b])
                              k.op("act", lambda e, hh=hh, pS=pS: e.activation(
                                  out=rtmp[:], in_=pS[:, :], func=AF.Relu, scale=wst[:, hh:hh + 1]),
                                  reads=[pS.b, wst.b], writes=[rtmp.b])
                              k.op("dve", lambda e, hh=hh, c=c: e.scalar_tensor_tensor(
                                  out=score[:, c * 512:(c + 1) * 512], in0=rtmp[:], scalar=wst[:, 4 + hh:5 + hh],
                                  in1=score[:, c * 512:(c + 1) * 512], op0=ALU.mult, op1=ALU.add),
                                  reads=[rtmp.b, wst.b, score.b], writes=[score.b])
                              yield

                  run(idx_gather(0))
                  for b in range(16):
                      rr(idx_score(b), idx_gather(b + 1) if b + 1 < 16 else None)
                  for hh in range(4):
                      pS = ps()
                      k.op("pe", lambda e, hh=hh, pS=pS: e.matmul(
                          pS[:, 0:128], lhsT=IQT[:, hh, :], rhs=IKTn[:, :], start=True, stop=True),
                          reads=[IQT.b, IKTn.b], writes=[pS.b])
                      k.op("act", lambda e, hh=hh, pS=pS: e.activation(
                          out=rtmp[:, 0:128], in_=pS[:, 0:128], func=AF.Relu, scale=wst[:, hh:hh + 1]),
                          reads=[pS.b, wst.b], writes=[rtmp.b])
                      if hh == 0:
                          k.op("dve", lambda e: e.tensor_scalar(
                              score[:, 2048:2176], rtmp[:, 0:128], wst[:, 4:5], None, op0=ALU.mult),
                              reads=[rtmp.b, wst.b], writes=[score.b])
                      else:
                          k.op("dve", lambda e, hh=hh: e.scalar_tensor_tensor(
                              out=score[:, 2048:2176], in0=rtmp[:, 0:128], scalar=wst[:, 4 + hh:5 + hh],
                              in1=score[:, 2048:2176], op0=ALU.mult, op1=ALU.add),
                              reads=[rtmp.b, wst.b, score.b], writes=[score.b])
                  run(dsa_select(16, 17, 17 * 128, None, mTb))

                  def kv_gather(b):
                      hf = b % 2
                      base = 17 * hf
                      for pg in range(16):
                          q_ = pg % 2
                          col = b * 16 + pg
                          off = bass.IndirectOffsetOnAxis(ap=pti[:, col:col + 1], axis=0)
                          k.dma("pool", kvpg[q_][:], ckv_d[:, :], reads=[pti.b], writes=[kvpg[q_].b], indirect=off)
                          kside_store(kvpg[q_][:, 0:256], None, None, [kvpg[q_].b],
                                      KT[:, :, (base + pg) * 128:(base + pg + 1) * 128], None, None, [KTh[hf]])
                          k.op("dve", lambda e, q_=q_, base=base, pg=pg: e.tensor_copy(
                              VA[:, base + pg, :, 0:64], kvpg[q_][:, 256:512].rearrange("p (g d) -> p g d", g=4)),
                              reads=[kvpg[q_].b], writes=[VAh[hf]])
                          yield

                  def seq_attend(b):
                      hf = b % 2
                      base = 17 * hf
                      kbl = [(KT, (base + pg) * 128, VA, base + pg, pg, [KTh[hf], VAh[hf]]) for pg in range(16)]
                      kbl.append((KTn, 0, VAn, None, 16, [KTn.b, VAn.b]))
                      yield from dsa_attend(kbl, ao, cmask32e[:, b:b + 1], b > 0, QTb, mTb)

                  run(kv_gather(0))
                  for b in range(16):
                      rr(seq_attend(b), kv_gather(b + 1) if b + 1 < 16 else None)
              k.dma("sp", a_sc[i * 128:(i + 1) * 128, :], ao[:], reads=[ao.b], writes=[a_sc_b[i]])

        k.barrier()
        kst.close()
        k.barrier()
        with contextlib.ExitStack() as st:
          if 'F' in PH:
              Wg = sb(st, "Wg", [128, KC, 3072], BF16)
              Wbr = sb(st, "Wbr", [128, 12, D], BF16)
              Wo = sb(st, "Wo", [128, KC, D], BF16)
              load_w(Wg, w_in, O_GATES, 3072)
              for bi, wsrc in enumerate((w_a_out, w_g_out, w_m_out)):
                  for kc in range(4):
                      k.dma("pool", Wbr[:, bi * 4 + kc, :], wsrc[kc * 128:(kc + 1) * 128, :], writes=[Wbr.b])
              load_w(Wo, w_o, 0, D)
              tsets = [(sb(st, "fx32_%d" % i, [128, D]), sb(st, "fxn_%d" % i, [128, D]),
                        sb(st, "fxT_%d" % i, [128, KC, 128], BF16), sb(st, "fstat_%d" % i, [128, 4]))
                       for i in range(3)]
              br32 = [sb(st, "br32_%d" % i, [128, 3, 512]) for i in range(2)]
              cand = [sb(st, "cand%d" % i, [128, 4, 512]) for i in range(2)]
              brT = sb(st, "brT", [128, 12, 128], BF16)
              sig = sb(st, "sig", [128, 512])
              term = sb(st, "term", [128, 512])
              h32 = sb(st, "h32", [128, D])
              hT = sb(st, "hT", [128, KC, 128], BF16)
              x2o = [sb(st, "x2o%d" % i, [128, D]) for i in range(2)]
              def f_gen(i):
                  smp = (i == NT_OWN)
                  src = x_smp[:, :] if smp else x_own[i * 128:(i + 1) * 128, :]
                  x32, xT = load_xT(tsets[i % 3], src, 0)
                  yield "B"
                  br = br32[i % 2]
                  k.dma("sp", br[:, 0, :], a_sc[i * 128:(i + 1) * 128, :], reads=[a_sc_b[i]], writes=[br.b])
                  k.dma("sp", br[:, 2, :], m_sc[i * 128:(i + 1) * 128, :], reads=[m_sc_b[i]], writes=[br.b])
                  if smp:
                      k.dma("sp", br[:, 1, :], g_sc[32 * 128:33 * 128, :], reads=[g_sc_b[32]], writes=[br.b])
                  else:
                      grp = i // 2
                      cd = cand[i % 2]
                      if i % 2 == 0:
                          cdl = cand[(i // 2) % 2]
                          k.dma("sp", cdl[:], g_sc[grp * 512:(grp + 1) * 512, :].rearrange("(c p) n -> p c n", p=128),
                                reads=g_sc_b[4 * grp:4 * grp + 4], writes=[cdl.b])
                      cdl = cand[(i // 2) % 2]
                      for c in range(4):
                          sc_ = selw[:, i * 4 + c:i * 4 + c + 1]
                          if c == 0:
                              k.op("dve", lambda e, sc_=sc_, br=br, cdl=cdl: e.tensor_scalar(
                                  br[:, 1, :], cdl[:, 0, :], sc_, None, op0=ALU.mult),
                                  reads=[cdl.b, selw.b], writes=[br.b])
                          else:
                              k.op("dve", lambda e, sc_=sc_, br=br, cdl=cdl, c=c: e.scalar_tensor_tensor(
                                  out=br[:, 1, :], in0=cdl[:, c, :], scalar=sc_, in1=br[:, 1, :],
                                  op0=ALU.mult, op1=ALU.add), reads=[cdl.b, selw.b, br.b], writes=[br.b])
                  for bi in range(3):
                      p = ps()
                      for j in range(4):
                          k.op("pe", lambda e, bi=bi, j=j, p=p, br=br: e.transpose(
                              p[:, j * 128:(j + 1) * 128], br[:, bi, j * 128:(j + 1) * 128], ident[:]),
                              reads=[br.b, ident.b], writes=[p.b])
                      k.op("act", lambda e, bi=bi, p=p: e.activation(
                          out=brT[:, bi * 4:(bi + 1) * 4, :], in_=p[:, :].rearrange("p (a b) -> p a b", a=4),
                          func=AF.Copy), reads=[p.b], writes=[brT.b])
                  for c in range(2):
                      for bi in range(3):
                          pg = ps()
                          linear(xT, Wg, bi * 1024 + c * 512, 512, pg)
                          pb = ps()
                          for kc in range(4):
                              k.op("pe", lambda e, kc=kc, bi=bi, c=c, pb=pb: e.matmul(
                                  pb[:, :], lhsT=brT[:, bi * 4 + kc, :], rhs=Wbr[:, bi * 4 + kc, c * 512:(c + 1) * 512],
                                  start=(kc == 0), stop=(kc == 3)), reads=[brT.b, Wbr.b], writes=[pb.b])
                          k.op("act", lambda e, pg=pg: e.activation(out=sig[:], in_=pg[:, :], func=AF.Sigmoid),
                               reads=[pg.b], writes=[sig.b])
                          if bi == 0:
                              k.op("dve", lambda e, pb=pb, c=c: e.tensor_tensor(
                                  h32[:, c * 512:(c + 1) * 512], sig[:], pb[:, :], op=ALU.mult),
                                  reads=[sig.b, pb.b], writes=[h32.b])
                          else:
                              k.op("dve", lambda e, pb=pb: e.tensor_tensor(term[:], sig[:], pb[:, :], op=ALU.mult),
                                   reads=[sig.b, pb.b], writes=[term.b])
                              k.op("dve", lambda e, c=c: e.tensor_tensor(
                                  h32[:, c * 512:(c + 1) * 512], h32[:, c * 512:(c + 1) * 512], term[:], op=ALU.add),
                                  reads=[term.b, h32.b], writes=[h32.b])
                  for half in range(2):
                      p = ps()
                      for j in range(4):
                          kc = half * 4 + j
                          k.op("pe", lambda e, kc=kc, j=j, p=p: e.transpose(
                              p[:, j * 128:(j + 1) * 128], h32[:, kc * 128:(kc + 1) * 128], ident[:]),
                              reads=[h32.b, ident.b], writes=[p.b])
                      k.op("act", lambda e, half=half, p=p: e.activation(
                          out=hT[:, half * 4:(half + 1) * 4, :], in_=p[:, :].rearrange("p (a b) -> p a b", a=4),
                          func=AF.Copy), reads=[p.b], writes=[hT.b])
                  xo_ = x2o[i % 2]
                  for c in range(2):
                      p = ps()
                      linear(hT, Wo, c * 512, 512, p)
                      k.op("dve", lambda e, c=c, p=p, xo_=xo_, x32=x32: e.tensor_tensor(
                          xo_[:, c * 512:(c + 1) * 512], p[:, :], x32[:, c * 512:(c + 1) * 512], op=ALU.add),
                          reads=[p.b, x32.b], writes=[xo_.b])
                  k.dma("sp", x2_sc[i * 128:(i + 1) * 128, :], xo_[:], reads=[xo_.b], writes=[x2_sc_b[i]])
              pipe_ahead([f_gen(i) for i in range(NT_OWN + 1)], 2)

        k.barrier()
        with contextlib.ExitStack() as st:
          if 'D' in PH:
              Wf1 = sb(st, "Wf1", [128, KC, 2 * D_FF], BF16)
              Wf2 = sb(st, "Wf2", [128, D_FF // 128, D], BF16)
              load_w(Wf1, w_ffn_in, 0, 2 * D_FF)
              load_w(Wf2, w_ffn_out, 0, D, rows=D_FF)
              tsets = [(sb(st, "dx32_%d" % i, [128, D]), sb(st, "dxn_%d" % i, [128, D]),
                        sb(st, "dxT_%d" % i, [128, KC, 128], BF16), sb(st, "dstat_%d" % i, [128, 4]))
                       for i in range(2)]
              act2 = [sb(st, "ffact%d" % q, [128, D_FF]) for q in range(2)]
              sg2 = [sb(st, "ffsg%d" % q, [128, 512]) for q in range(1)] * 2
              actT2 = [sb(st, "ffactT%d" % q, [128, D_FF // 128, 128], BF16) for q in range(2)]
              yo = [sb(st, "yo%d" % i, [128, D]) for i in range(2)]
              nb = D_FF // 128

              def ffn_gen(i):
                  smp = (i == NT_OWN)
                  bf = i % 2
                  act, actT = act2[bf], actT2[bf]
                  if 'F' in PH:
                      src = x2_sc[i * 128:(i + 1) * 128, :]
                      x32, xT = load_xT(tsets[bf], src, 16, rd=[x2_sc_b[i]])
                  else:
                      src = x_smp[:, :] if smp else x_own[i * 128:(i + 1) * 128, :]
                      x32, xT = load_xT(tsets[bf], src, 16)
                  yield
                  c0 = 0
                  ci = 0
                  while c0 < D_FF:
                      n = min(512, D_FF - c0)
                      sg = sg2[ci % 2]
                      pg = ps()
                      linear(xT, Wf1, c0, n, pg)
                      pu = ps()
                      linear(xT, Wf1, D_FF + c0, n, pu)
                      k.op("act", lambda e, pg=pg, n=n, sg=sg: e.activation(out=sg[:, 0:n], in_=pg[:, 0:n], func=AF.Silu),
                           reads=[pg.b], writes=[sg.b])
                      k.op("dve", lambda e, pu=pu, n=n, c0=c0, sg=sg: e.tensor_tensor(
                          act[:, c0:c0 + n], sg[:, 0:n], pu[:, 0:n], op=ALU.mult),
                          reads=[sg.b, pu.b], writes=[act.b])
                      c0 += n
                      ci += 1
                      yield
                  yield "B"
                  for b0 in range(0, nb, 4):
                      nbb = min(4, nb - b0)
                      p = ps()
                      for j in range(nbb):
                          k.op("pe", lambda e, j=j, b0=b0, p=p: e.transpose(
                              p[:, j * 128:(j + 1) * 128], act[:, (b0 + j) * 128:(b0 + j + 1) * 128], ident[:]),
                              reads=[act.b, ident.b], writes=[p.b])
                      k.op("act", lambda e, p=p, b0=b0, nbb=nbb: e.activation(
                          out=actT[:, b0:b0 + nbb, :], in_=p[:, 0:nbb * 128].rearrange("p (a b) -> p a b", a=nbb),
                          func=AF.Copy), reads=[p.b], writes=[actT.b])
                      yield
                  yy = yo[bf]
                  for h in range(2):
                      p = ps()
                      for kc in range(nb):
                          k.op("pe", lambda e, kc=kc, h=h, p=p: e.matmul(
                              p[:, :], lhsT=actT[:, kc, :], rhs=Wf2[:, kc, h * 512:(h + 1) * 512],
                              start=(kc == 0), stop=(kc == nb - 1)),
                              reads=[actT.b, Wf2.b], writes=[p.b])
                          if kc % 6 == 5:
                              yield
                      k.op("dve", lambda e, h=h, p=p, yy=yy, x32=x32: e.tensor_tensor(
                          yy[:, h * 512:(h + 1) * 512], p[:, :], x32[:, h * 512:(h + 1) * 512], op=ALU.add),
                          reads=[p.b, x32.b], writes=[yy.b])
                      yield
                  dst = y_smp[:, :] if smp else y_own[i * 128:(i + 1) * 128, :]
                  k.dma("sp", dst, yy[:], reads=[yy.b])

              gensD = [ffn_gen(i) for i in range(NT_OWN + 1)]
              for tok in gensD[0]:
                  if tok == "B":
                      break
              for i in range(NT_OWN + 1):
                  cur = gensD[i]
                  nxt = gensD[i + 1] if i + 1 < NT_OWN + 1 else None
                  cur_alive, nxt_inA = True, nxt is not None
                  while cur_alive or nxt_inA:
                      if cur_alive:
                          try:
                              next(cur)
                          except StopIteration:
                              cur_alive = False
                      if nxt_inA:
                          if next(nxt) == "B":
                              nxt_inA = False
              outs_done += [b.b for b in yo]

        k.barrier()
        k.finish(outs_done, "sp")
    print("ops", k.nops, "waits", k.nwaits)
    return nc


_NC_CACHE = {}


def make_in_maps(I):
    f = lambda a: np.ascontiguousarray(np.asarray(a), dtype=np.float32)
    x_prompt, x_sample, mem_prompt = f(I["x_prompt"]), f(I["x_sample"]), f(I["mem_prompt"])
    gains = np.concatenate([f(I["norm_mix"])[0].reshape(8, 128).T, f(I["mem_norm"])[0].reshape(8, 128).T,
                            f(I["norm_ffn"])[0].reshape(8, 128).T], axis=1)
    hv = np.concatenate([f(I["a_q_norm"])[0], f(I["a_k_norm"])[0], f(I["m_q_norm"])[0], f(I["m_k_norm"])[0],
                         f(I["g_o_norm"])[0], f(I["g_dt_bias"])[0], f(I["g_a_log"])[0],
                         np.zeros(56, np.float32)])[None, :]
    r = np.arange(128)
    same = (r[:, None] // 8) == (r[None, :] // 8)
    one = np.ones((128, 128), bool)
    mats = []
    for blk in (one, same):
        pass
    ltri = [(r[:, None] <= r[None, :]) & blk for blk in (one, same)]
    bones = [blk for blk in (one, same)]
    slm = [(r[:, None] > r[None, :]) & blk for blk in (one, same)]
    sui = [((r[None, :] >= r[:, None]) & blk) * (128.0 ** -0.5) for blk in (one, same)]
    ustr = (r[:, None] > r[None, :])
    g8 = (r[:, None] // 8) == np.arange(16)[None, :]
    cm32 = np.broadcast_to((np.arange(128)[None, :] // 8 == np.arange(16)[:, None]).reshape(1, 2048), (128, 2048))
    gconst = np.concatenate([np.asarray(a, np.float32) for a in
                             (ltri[0], ltri[1], bones[0], bones[1], slm[0], slm[1], sui[0], sui[1], ustr,
                              np.ones((128, 128)), g8, cm32)], axis=1)
    gconvT = f(I["g_conv"])[0].reshape(4, 12, 128).transpose(2, 1, 0).reshape(128, 48)
    dconst = np.zeros((128, 64), np.float32)
    dconst[:, 0:32] = (2.0 ** -(np.arange(32) + 1.0))[None, :]
    dconst[:, 32] = 256.0
    dconst[:, 33] = np.arange(128)
    dconst[:, 40:56] = g8
    ckv = np.concatenate([f(I["cache_k"])[0].reshape(2560 * 128, 256),
                          f(I["cache_v"])[0].reshape(2560 * 128, 256)], axis=1)
    cik = f(I["cache_idx_k"])[0].reshape(2560 * 128, 64)
    ptab = np.asarray(I["page_table"]).astype(np.int32)
    shared = {
        "w_in": f(I["w_in"])[0], "w_mem_kv": f(I["w_mem_kv"])[0], "w_a_out": f(I["w_a_out"])[0],
        "w_g_out": f(I["w_g_out"])[0], "w_m_out": f(I["w_m_out"])[0], "w_o": f(I["w_o"])[0],
        "w_ffn_in": f(I["w_ffn_in"])[0], "w_ffn_out": f(I["w_ffn_out"])[0],
        "ident": np.eye(128, dtype=np.float32),
        "dconst": dconst, "cache_kv": ckv, "cache_idx_k": cik,
        "gconst": np.ascontiguousarray(gconst, dtype=np.float32), "gconvT": np.ascontiguousarray(gconvT),
        "cmask": np.ascontiguousarray(np.broadcast_to(
            (np.arange(128)[None, :] // 8 == np.arange(16)[:, None]).astype(np.float32).reshape(1, 2048), (128, 2048))), "gains": np.ascontiguousarray(gains), "headvecs": hv,
    }
    in_maps = []
    for c in range(8):
        b, half = c // 2, c % 2
        ot = own_tiles(half)
        xo = np.concatenate([x_prompt[b, t * 128:(t + 1) * 128] for t in ot], axis=0)
        m = dict(shared)
        m["x_all"] = x_prompt[b]
        m["x_own"] = np.ascontiguousarray(xo)
        m["x_smp"] = np.ascontiguousarray(x_sample[16 * c:16 * c + 16].reshape(128, D))
        m["mem"] = mem_prompt[b]
        m["cache_mem_k"] = np.ascontiguousarray(f(I["cache_mem_k"])[0, 16 * c:16 * c + 16].reshape(16, 256, 512))
        m["cache_mem_v"] = np.ascontiguousarray(f(I["cache_mem_v"])[0, 16 * c:16 * c + 16].reshape(16, 256, 512))
        m["state_gdn"] = np.ascontiguousarray(f(I["state_gdn"])[0, 16 * c:16 * c + 16])
        m["state_conv"] = np.ascontiguousarray(f(I["state_conv"])[0, 16 * c:16 * c + 16].reshape(48, 1536))
        m["page_table"] = np.ascontiguousarray(ptab[16 * c:16 * c + 16].reshape(1, 256))
        pen = np.zeros((17, 128, 256), np.float32)
        tt = np.arange(128)
        for i_, t_ in enumerate(ot):
            nk_ = 4 * (i_ // 2) + (2 if i_ % 2 == 0 else 4)
            spos = (nk_ - 2) * 128 + np.arange(256)
            pen[i_] = np.where(spos[None, :] <= (t_ * 128 + tt)[:, None], 0.0, -1e30)
        newok = ((tt[:, None] // 8) == (tt[None, :] // 8)) & ((tt[None, :] % 8) <= (tt[:, None] % 8))
        pen[16, :, 128:] = np.where(newok, 0.0, -1e30)
        m["pen"] = pen
        sw = np.zeros((16, 4), np.float32)
        for i_, t_ in enumerate(ot):
            sw[i_, t_ % 4] = 1.0
        m["selw"] = np.ascontiguousarray(np.broadcast_to(sw.reshape(1, 64), (128, 64)))
        in_maps.append(m)
    return in_maps


def kernel(**I):
    if "nc" not in _NC_CACHE:
        _NC_CACHE["nc"] = build({})
    nc = _NC_CACHE["nc"]
    in_maps = make_in_maps(I)
    res = run_bass_kernel_spmd(nc, in_maps, core_ids=list(range(8)))
    R = res.results
    yp = np.zeros((4, SEQ, D), np.float32)
    for c in range(8):
        b, half = c // 2, c % 2
        for i, t in enumerate(own_tiles(half)):
            yp[b, t * 128:(t + 1) * 128] = R[c]["y_own"][i * 128:(i + 1) * 128]
    ys = np.concatenate([R[c]["y_smp"].reshape(16, 8, D) for c in range(8)], axis=0)
    p_k = np.stack([R[2 * b]["o_pk"].reshape(SEQ, 4, 64) for b in range(4)])[None]
    p_v = np.stack([R[2 * b]["o_pv"].reshape(SEQ, 4, 64) for b in range(4)])[None]
    p_ik = np.stack([R[2 * b]["o_pik"] for b in range(4)])[None]
    p_gdn = np.stack([R[2 * b]["o_pgdn"] for b in range(4)])[None]
    p_conv = np.stack([R[2 * b]["o_pconv"] for b in range(4)])[None]
    p_mk = np.stack([R[2 * b]["o_pmk"].reshape(256, 4, 128) for b in range(4)])[None]
    p_mv = np.stack([R[2 * b]["o_pmv"].reshape(256, 4, 128) for b in range(4)])[None]
    s_k = np.concatenate([R[c]["o_sk"].reshape(16, 8, 4, 64) for c in range(8)], axis=0)[None]
    s_v = np.concatenate([R[c]["o_sv"].reshape(16, 8, 4, 64) for c in range(8)], axis=0)[None]
    s_ik = np.concatenate([R[c]["o_sik"].reshape(16, 8, 64) for c in range(8)], axis=0)[None]
    s_gdn = np.concatenate([R[c]["o_sgdn"] for c in range(8)], axis=0)[None]
    s_conv = np.concatenate([R[c]["o_sconv"] for c in range(8)], axis=0)[None]
    return (yp, ys, p_k, p_v, p_ik, p_gdn, p_conv, p_mk, p_mv, s_k, s_v, s_ik, s_gdn, s_conv)
```

```python
import contextlib
import numpy as np
import concourse.bass as bass
import concourse.mybir as mybir
from concourse.bass_utils import run_bass_kernel_spmd

F32 = mybir.dt.float32
BF16 = mybir.dt.bfloat16
I32 = mybir.dt.int32
AF = mybir.ActivationFunctionType
ALU = mybir.AluOpType
AX = mybir.AxisListType

D = 1024
KC = 8
SEQ = 4096
NT_ALL = 32
NT_OWN = 16
D_IN = 6988
D_FF = 2816
EPS = 1e-6
O_AQ, O_AK, O_AV, O_IQ, O_IK, O_IW = 0, 512, 768, 1024, 1280, 1344
O_GQKV, O_GZ, O_GB, O_GA, O_MQ, O_GATES = 1348, 2884, 3396, 3400, 3404, 3916


class Buf:
    __slots__ = ("name", "w", "r")

    def __init__(self, name=""):
        self.name = name
        self.w = None
        self.r = []


class K:
    def __init__(self, nc, n_dma_sems=40):
        self.nc = nc
        self.eng = {"pe": nc.tensor, "act": nc.scalar, "dve": nc.vector,
                    "pool": nc.gpsimd, "sp": nc.sync}
        self.sem, self.cnt, self.seen = {}, {}, {}
        for e in self.eng:
            self.sem[e] = nc.alloc_semaphore("prog_" + e)
            self.cnt[e] = 0
            self.seen[e] = {}
        self.dsems = [nc.alloc_semaphore("dma%d" % i) for i in range(n_dma_sems)]
        self.dcnt = [0] * n_dma_sems
        self.dnext = 0
        self.nops = 0
        self.nwaits = 0

    def _wait(self, e, tok):
        if tok is None:
            return
        sem, val, src = tok
        if src == e and e == "pe":
            return
        key = sem.num
        if self.seen[e].get(key, 0) >= val:
            return
        self.eng[e].wait_ge(sem, val)
        self.seen[e][key] = val
        self.nwaits += 1

    def _deps(self, e, reads, writes):
        for b in reads:
            self._wait(e, b.w)
        for b in writes:
            self._wait(e, b.w)
            for t in b.r:
                self._wait(e, t)

    def _commit(self, tok, reads, writes):
        for b in reads:
            b.r.append(tok)
            if len(b.r) > 64:
                b.r = b.r[-64:] if False else b.r
        for b in writes:
            b.w = tok
            b.r = []

    def op(self, e, fn, reads=(), writes=()):
        self._deps(e, reads, writes)
        ins = fn(self.eng[e])
        self.cnt[e] += 1
        ins.then_inc(self.sem[e], 1)
        tok = (self.sem[e], self.cnt[e], e)
        self._commit(tok, reads, writes)
        self.nops += 1
        return tok

    def dma(self, q, out, in_, reads=(), writes=(), indirect=None, **kw):
        self._deps(q, reads, writes)
        i = self.dnext
        self.dnext = (self.dnext + 1) % len(self.dsems)
        sem = self.dsems[i]
        if self.dcnt[i] > 0:
            self._wait(q, (sem, self.dcnt[i], "dma"))
        if indirect is not None:
            ins = self.eng[q].indirect_dma_start(out=out, out_offset=None, in_=in_,
                                                 in_offset=indirect, **kw)
        else:
            ins = self.eng[q].dma_start(out=out, in_=in_, **kw)
        self.dcnt[i] += 16
        ins.then_inc(sem, 16)
        tok = (sem, self.dcnt[i], "dma")
        self._commit(tok, reads, writes)
        self.nops += 1
        return tok

    def barrier(self):
        for e in self.eng:
            for e2 in self.eng:
                if e2 != e and self.cnt[e2] > 0:
                    self._wait(e, (self.sem[e2], self.cnt[e2], e2))
            for i, s_ in enumerate(self.dsems):
                if self.dcnt[i] > 0:
                    self._wait(e, (s_, self.dcnt[i], "dma"))

    def finish(self, bufs, e="sp"):
        for b in bufs:
            self._wait(e, b.w)
            for t in b.r:
                self._wait(e, t)


class T:
    __slots__ = ("t", "b")

    def __init__(self, t, name=""):
        self.t = t
        self.b = Buf(name)

    def __getitem__(self, key):
        return self.t[key]


def own_tiles(half):
    res = []
    for g in range(8):
        res += [4 * g, 4 * g + 3] if half == 0 else [4 * g + 1, 4 * g + 2]
    return res


def build(cfg):
    nc = bass.Bass("TRN2", target_bir_lowering=False)
    k = K(nc)
    dt_in = {}

    def din(name, shape, dt=F32):
        dt_in[name] = nc.dram_tensor(name, list(shape), dt, kind="ExternalInput").ap()
        return dt_in[name]

    def dout(name, shape, dt=F32):
        return nc.dram_tensor(name, list(shape), dt, kind="ExternalOutput").ap()

    x_all = din("x_all", [SEQ, D])
    x_own = din("x_own", [NT_OWN * 128, D])
    x_smp = din("x_smp", [128, D])
    mem = din("mem", [256, D])
    w_in = din("w_in", [D, D_IN])
    w_mem_kv = din("w_mem_kv", [D, 1024])
    w_a_out = din("w_a_out", [512, D])
    w_g_out = din("w_g_out", [512, D])
    w_m_out = din("w_m_out", [512, D])
    w_o = din("w_o", [D, D])
    w_ffn_in = din("w_ffn_in", [D, 2 * D_FF])
    w_ffn_out = din("w_ffn_out", [D_FF, D])
    ident_d = din("ident", [128, 128])
    gains_d = din("gains", [128, 24])
    hv_d = din("headvecs", [1, 576])

    y_own = dout("y_own", [NT_OWN * 128, D])
    y_smp = dout("y_smp", [128, D])
    o_pk = dout("o_pk", [SEQ, 256])
    o_pv = dout("o_pv", [SEQ, 256])
    o_pik = dout("o_pik", [SEQ, 64])
    o_pconv = dout("o_pconv", [3, 1536])
    o_pmk = dout("o_pmk", [256, 512])
    o_pmv = dout("o_pmv", [256, 512])
    o_sk = dout("o_sk", [128, 256])
    o_sv = dout("o_sv", [128, 256])
    o_sik = dout("o_sik", [128, 64])
    o_sconv = dout("o_sconv", [16, 3, 1536])
    DBG = cfg.get("debug", False)
    skind = "ExternalOutput" if DBG else "Internal"
    cmk_d = din("cache_mem_k", [16, 256, 512])
    cmv_d = din("cache_mem_v", [16, 256, 512])
    cmask_d = din("cmask", [128, 2048])
    selw_d = din("selw", [128, 64])
    ckv_d = din("cache_kv", [2560 * 128, 512])
    cik_d = din("cache_idx_k", [2560 * 128, 64])
    pt_d = din("page_table", [1, 256], I32)
    pen_d = din("pen", [17, 128, 256])
    dconst_d = din("dconst", [128, 64])
    gconst_d = din("gconst", [128, 10 * 128 + 16 + 2048])
    gconvT_d = din("gconvT", [128, 48])
    sgdn_d = din("state_gdn", [16, 4, 128, 128])
    sconv_d = din("state_conv", [48, 1536])
    o_pgdn = dout("o_pgdn", [4, 128, 128])
    o_sgdn = dout("o_sgdn", [16, 4, 128, 128])
    if DBG:
        dbg_tm = dout("dbg_tm", [128, 1536])
        dbg_sm = dout("dbg_sm", [128, 64])
        dbg_o = dout("dbg_o", [128, 512])
        dbg_mask = dout("dbg_mask", [128, 512])
        dbg_bis = dout("dbg_bis", [128, 44])
        dbg_sc = dout("dbg_sc", [128, 512])
        dbg_uw = dout("dbg_uw", [128, 256])
        dbg_vn = dout("dbg_vn", [128, 128])
        dbg_aq = dout("dbg_aq", [128, 128])
        dbg_tt = dout("dbg_tt", [128, 128])
        dbg_wtm = dout("dbg_wtm", [128, 2048])
        dbg_p1 = dout("dbg_p1", [128, 128])
    a_sc = nc.dram_tensor("a_sc", [17 * 128, 512], F32, kind=skind).ap()
    m_sc = nc.dram_tensor("m_sc", [17 * 128, 512], F32, kind=skind).ap()
    g_sc = nc.dram_tensor("g_sc", [33 * 128, 512], F32, kind=skind).ap()
    x2_sc = nc.dram_tensor("x2_sc", [17 * 128, D], F32, kind=skind).ap()
    a_sc_b = [Buf() for _ in range(17)]
    m_sc_b = [Buf() for _ in range(17)]
    g_sc_b = [Buf() for _ in range(33)]
    x2_sc_b = [Buf() for _ in range(17)]
    outs_done = []

    with contextlib.ExitStack() as glob:
        def sb(st, name, shape, dt=F32):
            return T(st.enter_context(nc.sbuf_tensor("sb_" + name, list(shape), dt)), name)

        psum = [T(glob.enter_context(nc.psum_tensor("ps%d" % i, [128, 512], F32)), "ps%d" % i)
                for i in range(8)]
        ps_i = [0]
        ps_hold = set()

        def ps(hold=False):
            while True:
                idx = ps_i[0] % 8
                ps_i[0] += 1
                if idx not in ps_hold:
                    break
            if hold:
                ps_hold.add(idx)
            return psum[idx]

        def ps_release(p):
            ps_hold.discard(psum.index(p))

        ident = sb(glob, "ident", [128, 128])
        gains = sb(glob, "gains", [128, 24])
        hv = sb(glob, "hv", [128, 576])
        k.dma("sp", ident[:], ident_d[:, :], writes=[ident.b])
        k.dma("sp", gains[:], gains_d[:, :], writes=[gains.b])
        k.dma("sp", hv[:], hv_d[0:1, :].partition_broadcast(128), writes=[hv.b])

        cmask = sb(glob, "cmask", [128, 16, 128], BF16)
        selw = sb(glob, "selw", [128, 64])
        k.dma("pool", cmask[:], cmask_d[:, :].rearrange("p (b t) -> p b t", b=16), writes=[cmask.b])
        k.dma("sp", selw[:], selw_d[:, :], writes=[selw.b])
        MKT = sb(glob, "MKT", [128, 4, 256], BF16)
        MVa = sb(glob, "MVa", [128, 2, 4, 129], BF16)
        k.op("pool", lambda e: e.memset(MVa[:], 1.0), writes=[MVa.b])
        zb = sb(glob, "zb", [128, 512], BF16)
        k.op("pool", lambda e: e.memset(zb[:], 0.0), writes=[zb.b])

        def ps_zero(p):
            k.op("pe", lambda e: e.matmul(p[:, :], lhsT=zb[:, 0:128], rhs=zb[:, :], start=True, stop=False),
                 reads=[zb.b], writes=[p.b])


        def kside_store(kk_ap, vv_ap, ii_ap, rd, KT_ap, IKT_ap, VA_ap, wr):
            if kk_ap is not None:
                p = ps()
                for g in range(4):
                    k.op("pe", lambda e, g=g: e.transpose(p[0:64, g * 128:(g + 1) * 128],
                                                          kk_ap[:, g * 64:(g + 1) * 64], ident[:]),
                         reads=rd + [ident.b], writes=[p.b])
                k.op("act", lambda e: e.activation(out=KT_ap, in_=p[0:64, :].rearrange("p (g s) -> p g s", g=4),
                                                   func=AF.Copy), reads=[p.b], writes=wr)
            if ii_ap is not None:
                p2 = ps()
                k.op("pe", lambda e: e.transpose(p2[0:64, 0:128], ii_ap, ident[:]),
                     reads=rd + [ident.b], writes=[p2.b])
                k.op("act", lambda e: e.activation(out=IKT_ap, in_=p2[0:64, 0:128], func=AF.Copy),
                     reads=[p2.b], writes=wr)
            if vv_ap is not None:
                k.op("pool", lambda e: e.tensor_copy(VA_ap, vv_ap.rearrange("p (g d) -> p g d", g=4)),
                     reads=rd, writes=wr)

        def pipe_ahead(gens, depth):
            n = len(gens)

            def toB(g):
                for tok in g:
                    if tok == "B":
                        return
            for j_ in range(min(depth, n)):
                toB(gens[j_])
            for j_ in range(n):
                for _ in gens[j_]:
                    pass
                if j_ + depth < n:
                    toB(gens[j_ + depth])

        def load_xT(st_tiles, src_ap, gain_col, q="sp", rd=()):
            x32, xn, xT, stat = st_tiles
            k.dma(q, x32[:], src_ap, reads=list(rd), writes=[x32.b])
            k.op("act", lambda e: e.activation(out=xn[:], in_=x32[:], func=AF.Square,
                                               accum_out=stat[:, 0:1]),
                 reads=[x32.b], writes=[xn.b, stat.b])
            k.op("dve", lambda e: e.tensor_scalar(stat[:, 1:2], stat[:, 0:1], 1.0 / D, EPS,
                                                  op0=ALU.mult, op1=ALU.add),
                 reads=[stat.b], writes=[stat.b])
            k.op("act", lambda e: e.activation(out=stat[:, 3:4], in_=stat[:, 1:2], func=AF.Sqrt),
                 reads=[stat.b], writes=[stat.b])
            k.op("dve", lambda e: e.reciprocal(stat[:, 2:3], stat[:, 3:4]),
                 reads=[stat.b], writes=[stat.b])
            k.op("act", lambda e: e.activation(out=xn[:], in_=x32[:], func=AF.Copy,
                                               scale=stat[:, 2:3]),
                 reads=[x32.b, stat.b], writes=[xn.b])
            for half in range(2):
                p = ps()
                for j in range(4):
                    kc = half * 4 + j
                    k.op("pe", lambda e, kc=kc, j=j: e.transpose(
                        p[:, j * 128:(j + 1) * 128], xn[:, kc * 128:(kc + 1) * 128], ident[:]),
                        reads=[xn.b, ident.b], writes=[p.b])
                g = gains[:, gain_col + half * 4: gain_col + half * 4 + 4]
                k.op("dve", lambda e, half=half, g=g, p=p: e.tensor_tensor(
                    xT[:, half * 4:(half + 1) * 4, :],
                    p[:, :].rearrange("p (a b) -> p a b", a=4),
                    g.unsqueeze(2).to_broadcast([128, 4, 128]), op=ALU.mult),
                    reads=[p.b, gains.b], writes=[xT.b])
            return x32, xT

        def linear(xT, W, c0, ncol, p, kcs=KC):
            for kc in range(kcs):
                k.op("pe", lambda e, kc=kc: e.matmul(
                    p[:, 0:ncol], lhsT=xT[:, kc, :], rhs=W[:, kc, c0:c0 + ncol],
                    start=(kc == 0), stop=(kc == kcs - 1)),
                    reads=[xT.b, W.b], writes=[p.b])

        def load_w(W, src, c0, ncol, dst0=0, rows=D):
            nkc = rows // 128
            for kc in range(nkc):
                k.dma("pool", W[:, kc, dst0:dst0 + ncol], src[kc * 128:(kc + 1) * 128, c0:c0 + ncol],
                      writes=[W.b])

        def head_rmsnorm(st, src_ps, nh, dh, gain_ap, out32, tmp, stat, name):
            k.op("act", lambda e: e.activation(out=tmp[:, 0:nh * dh], in_=src_ps, func=AF.Square),
                 reads=[st], writes=[tmp.b])
            k.op("dve", lambda e: e.tensor_reduce(
                stat[:, 0:nh], tmp[:, 0:nh * dh].rearrange("p (h d) -> p h d", h=nh),
                axis=AX.X, op=ALU.add), reads=[tmp.b], writes=[stat.b])
            k.op("dve", lambda e: e.tensor_scalar(stat[:, 8:8 + nh], stat[:, 0:nh], 1.0 / dh, EPS,
                                                  op0=ALU.mult, op1=ALU.add),
                 reads=[stat.b], writes=[stat.b])
            k.op("act", lambda e: e.activation(out=stat[:, 0:nh], in_=stat[:, 8:8 + nh], func=AF.Sqrt),
                 reads=[stat.b], writes=[stat.b])
            k.op("dve", lambda e: e.reciprocal(stat[:, 16:16 + nh], stat[:, 0:nh]),
                 reads=[stat.b], writes=[stat.b])
            k.op("dve", lambda e: e.tensor_tensor(
                out32[:, 0:nh * dh].rearrange("p (h d) -> p h d", h=nh),
                src_ps.rearrange("p (h d) -> p h d", h=nh),
                stat[:, 16:16 + nh].unsqueeze(2).to_broadcast([128, nh, dh]), op=ALU.mult),
                reads=[st, stat.b], writes=[out32.b])
            k.op("dve", lambda e: e.tensor_tensor(
                out32[:, 0:nh * dh].rearrange("p (h d) -> p h d", h=nh),
                out32[:, 0:nh * dh].rearrange("p (h d) -> p h d", h=nh),
                gain_ap.unsqueeze(1).to_broadcast([128, nh, dh]), op=ALU.mult),
                reads=[out32.b, hv.b], writes=[out32.b])

        PH = cfg.get('phases', 'BCGEFD')
        with contextlib.ExitStack() as st:
          if 'B' in PH:
              Wm = sb(st, "Wm", [128, KC, 1024], BF16)
              load_w(Wm, w_mem_kv, 0, 1024)
              tiles = (sb(st, "mx32", [128, D]), sb(st, "mxn", [128, D]),
                       sb(st, "mxT", [128, KC, 128], BF16), sb(st, "mstat", [128, 4]))
              tmp = sb(st, "mtmp", [128, 512])
              hstat = sb(st, "mhstat", [128, 24])
              mk32 = [sb(st, "mk32_%d" % i, [128, 512]) for i in range(2)]
              mv32 = [sb(st, "mv32_%d" % i, [128, 512]) for i in range(2)]
              for mt in range(2):
                  _, xT = load_xT(tiles, mem[mt * 128:(mt + 1) * 128, :], 8)
                  pk = ps()
                  linear(xT, Wm, 0, 512, pk)
                  pv = ps()
                  linear(xT, Wm, 512, 512, pv)
                  head_rmsnorm(pk.b, pk[:, :], 4, 128, hv[:, 256:384], mk32[mt], tmp, hstat, "mk")
                  k.op("act", lambda e, mt=mt, pv=pv: e.activation(out=mv32[mt][:], in_=pv[:, :], func=AF.Copy),
                       reads=[pv.b], writes=[mv32[mt].b])
                  p = ps()
                  for h in range(4):
                      k.op("pe", lambda e, h=h, p=p, mt=mt: e.transpose(
                          p[:, h * 128:(h + 1) * 128], mk32[mt][:, h * 128:(h + 1) * 128], ident[:]),
                          reads=[mk32[mt].b, ident.b], writes=[p.b])
                  k.op("act", lambda e, p=p, mt=mt: e.activation(
                      out=MKT[:, :, mt * 128:(mt + 1) * 128], in_=p[:, :].rearrange("p (h m) -> p h m", h=4),
                      func=AF.Copy), reads=[p.b], writes=[MKT.b])
                  k.op("dve", lambda e, mt=mt: e.tensor_copy(
                      MVa[:, mt, :, 0:128], mv32[mt][:, :].rearrange("p (h d) -> p h d", h=4)),
                      reads=[mv32[mt].b], writes=[MVa.b])
                  k.dma("sp", o_pmk[mt * 128:(mt + 1) * 128, :], mk32[mt][:], reads=[mk32[mt].b])
                  k.dma("sp", o_pmv[mt * 128:(mt + 1) * 128, :], mv32[mt][:], reads=[mv32[mt].b])
                  outs_done += [mk32[mt].b, mv32[mt].b]

        k.barrier()
        with contextlib.ExitStack() as st:
          if 'G' in PH:
              Wg2 = sb(st, "Wg2", [128, KC, 2056], BF16)
              load_w(Wg2, w_in, O_GQKV, 2056, 0)
              gcs = sb(st, "gcs", [128, 10 * 128 + 16 + 2048])
              k.dma("sp", gcs[:], gconst_d[:, :], writes=[gcs.b])
              cv = lambda i: gcs[:, i * 128:(i + 1) * 128]
              LTRI, BONES, SLm, SUIm = [cv(0), cv(1)], [cv(2), cv(3)], [cv(4), cv(5)], [cv(6), cv(7)]
              USTR, ONES = cv(8), cv(9)
              G8 = gcs[:, 1280:1296]
              CM32 = gcs[:, 1296:1296 + 2048].rearrange("p (b t) -> p b t", b=16)
              gcv = sb(st, "gcv", [128, 48])
              k.dma("sp", gcv[:], gconvT_d[:, :], writes=[gcv.b])
              Dg = sb(st, "Dg", [128, 12, 4, 128], BF16)
              for cc in range(12):
                  for jj in range(4):
                      k.op("dve", lambda e, cc=cc, jj=jj: e.tensor_scalar(
                          Dg[:, cc, jj, :], ident[:], gcv[:, cc * 4 + jj:cc * 4 + jj + 1], None, op0=ALU.mult),
                          reads=[ident.b, gcv.b], writes=[Dg.b])
              negA = sb(st, "negA", [128, 4])
              k.op("act", lambda e: e.activation(out=negA[:], in_=hv[:, 516:520], func=AF.Exp),
                   reads=[hv.b], writes=[negA.b])
              k.op("dve", lambda e: e.tensor_scalar(negA[:], negA[:], -1.0, None, op0=ALU.mult),
                   reads=[negA.b], writes=[negA.b])
              Sst = [sb(st, "Sst%d" % h, [128, 128]) for h in range(4)]
              for h in range(4):
                  k.op("pool", lambda e, h=h: e.memset(Sst[h][:], 0.0), writes=[Sst[h].b])
              tsets = [(sb(st, "gx32_%d" % i, [128, D]), sb(st, "gxn_%d" % i, [128, D]),
                        sb(st, "gxT_%d" % i, [128, KC, 128], BF16), sb(st, "gstat_%d" % i, [128, 4]))
                       for i in range(1)] * 2
              Up = sb(st, "Up", [128, 12, 131], BF16)
              k.op("pool", lambda e: e.memset(Up[:], 0.0), writes=[Up.b])
              UC = sb(st, "UC", [128, 12, 128])
              TM = sb(st, "TM", [128, 1536])
              sq = sb(st, "gsq", [128, 1024])
              sgz2 = [sb(st, "sgz%d" % q, [128, 512]) for q in range(1)]
              sm2 = [sb(st, "gsm%d" % q, [128, 64]) for q in range(1)]
              scal = sb(st, "gscal", [128, 6, 4])
              KQ = sb(st, "KQ", [128, 12, 128])
              KQT2 = [sb(st, "KQT%d" % q, [128, 12, 128]) for q in range(1)]
              KBG2 = [sb(st, "KBG%d" % q, [128, 4, 128]) for q in range(1)]
              KTL2 = [sb(st, "KTL%d" % q, [128, 4, 128]) for q in range(1)]
              VB2 = [sb(st, "VB%d" % q, [128, 4, 128]) for q in range(1)]
              O32 = sb(st, "O32", [128, 512])
              go32 = sb(st, "go32", [128, 512])
              gtmp = sb(st, "ggtmp", [128, 512])
              ghstat = sb(st, "ghstat", [128, 24])
              hb = [dict(GU=sb(st, "GU%d" % i, [128, 128]), G2=sb(st, "G2%d" % i, [128, 256]),
                         t1=sb(st, "t1%d" % i, [128, 128]), aq=sb(st, "aqkT%d" % i, [128, 128]),
                         P=[sb(st, "P%d_%d" % (i, q), [128, 256]) for q in range(2)],
                         TT=[sb(st, "TT%d_%d" % (i, q), [128, 128]) for q in range(2)],
                         UW=sb(st, "UW%d" % i, [128, 256]), vn=sb(st, "vn%d" % i, [128, 128]))
                    for i in range(4)]
              DK5 = 128.0 ** -0.5

              def gdn_tile(j, sm_st=None):
                  smp = (j == NT_ALL)
                  m = 1 if smp else 0
                  bf_ = j % 2
                  sgz, KQT, KBG, KTL, VB, sm = sgz2[bf_], KQT2[bf_], KBG2[bf_], KTL2[bf_], VB2[bf_], sm2[bf_]
                  src = x_smp[:, :] if smp else x_all[j * 128:(j + 1) * 128, :]
                  x32, xT = load_xT(tsets[j % 2], src, 0)
                  yield
                  U = sm_st["Us"] if smp else Up
                  for cc0 in (0, 4, 8):
                      p = ps()
                      for c4 in range(4):
                          cc = cc0 + c4
                          for kc in range(KC):
                              k.op("pe", lambda e, kc=kc, cc=cc, c4=c4, p=p: e.matmul(
                                  p[:, c4 * 128:(c4 + 1) * 128], lhsT=Wg2[:, kc, cc * 128:(cc + 1) * 128],
                                  rhs=xT[:, kc, :], start=(kc == 0), stop=(kc == KC - 1)),
                                  reads=[Wg2.b, xT.b], writes=[p.b])
                      if smp:
                          for c4 in range(4):
                              k.op("act", lambda e, p=p, cc0=cc0, c4=c4: e.activation(
                                  out=U[:, cc0 + c4, :, 3:11],
                                  in_=p[:, c4 * 128:(c4 + 1) * 128].rearrange("p (b t) -> p b t", b=16),
                                  func=AF.Copy), reads=[p.b], writes=[U.b])
                      else:
                          k.op("act", lambda e, p=p, cc0=cc0: e.activation(
                              out=U[:, cc0:cc0 + 4, 3:131], in_=p[:, :].rearrange("p (a t) -> p a t", a=4),
                              func=AF.Copy), reads=[p.b], writes=[U.b])
                      yield
                  for cc0 in (0, 4, 8):
                      p = ps()
                      for c4 in range(4):
                          cc = cc0 + c4
                          for jj in range(4):
                              rhs = U[:, cc, :, jj:jj + 8] if smp else U[:, cc, jj:jj + 128]
                              k.op("pe", lambda e, jj=jj, cc=cc, c4=c4, p=p, rhs=rhs: e.matmul(
                                  p[:, c4 * 128:(c4 + 1) * 128], lhsT=Dg[:, cc, jj, :], rhs=rhs,
                                  start=(jj == 0), stop=(jj == 3)), reads=[Dg.b, U.b], writes=[p.b])
                      k.op("act", lambda e, p=p, cc0=cc0: e.activation(
                          out=UC[:, cc0:cc0 + 4, :], in_=p[:, :].rearrange("p (a t) -> p a t", a=4),
                          func=AF.Silu), reads=[p.b], writes=[UC.b])
                      yield
                  if not smp:
                      k.op("dve", lambda e: e.tensor_copy(U[:, :, 0:3], U[:, :, 128:131]),
                           reads=[U.b], writes=[U.b])
                  for cc0 in (0, 4, 8):
                      p = ps()
                      for c4 in range(4):
                          k.op("pe", lambda e, cc0=cc0, c4=c4, p=p: e.transpose(
                              p[:, c4 * 128:(c4 + 1) * 128], UC[:, cc0 + c4, :], ident[:]),
                              reads=[UC.b, ident.b], writes=[p.b])
                      k.op("act", lambda e, p=p, cc0=cc0: e.activation(
                          out=TM[:, cc0 * 128:(cc0 + 4) * 128], in_=p[:, :], func=AF.Copy),
                          reads=[p.b], writes=[TM.b])
                      yield
                  pgz = ps()
                  linear(xT, Wg2, 1536, 512, pgz)
                  pgb = ps()
                  linear(xT, Wg2, 2048, 8, pgb)
                  k.op("act", lambda e: e.activation(out=sgz[:], in_=pgz[:, :], func=AF.Silu),
                       reads=[pgz.b], writes=[sgz.b])
                  k.op("act", lambda e: e.activation(out=sm[:, 0:4], in_=pgb[:, 0:4], func=AF.Sigmoid),
                       reads=[pgb.b], writes=[sm.b])
                  k.op("dve", lambda e: e.tensor_tensor(sm[:, 4:8], pgb[:, 4:8], hv[:, 512:516], op=ALU.add),
                       reads=[pgb.b, hv.b], writes=[sm.b])
                  k.op("act", lambda e: e.activation(out=sm[:, 8:12], in_=sm[:, 4:8], func=AF.Exp),
                       reads=[sm.b], writes=[sm.b])
                  k.op("act", lambda e: e.activation(out=sm[:, 12:16], in_=sm[:, 8:12], func=AF.Ln, bias=1.0),
                       reads=[sm.b], writes=[sm.b])
                  k.op("dve", lambda e: e.tensor_tensor(sm[:, 16:20], sm[:, 12:16], negA[:], op=ALU.mult),
                       reads=[sm.b, negA.b], writes=[sm.b])
                  gt = sm[:, 16:20]
                  yield
                  k.op("act", lambda e: e.activation(out=sq[:], in_=TM[:, 0:1024], func=AF.Square),
                       reads=[TM.b], writes=[sq.b])
                  k.op("dve", lambda e: e.tensor_reduce(
                      sm[:, 20:28], sq[:, :].rearrange("p (h d) -> p h d", h=8), axis=AX.X, op=ALU.add),
                      reads=[sq.b], writes=[sm.b])
                  k.op("dve", lambda e: e.tensor_scalar(sm[:, 20:28], sm[:, 20:28], EPS, None, op0=ALU.add),
                       reads=[sm.b], writes=[sm.b])
                  k.op("act", lambda e: e.activation(out=sm[:, 28:36], in_=sm[:, 20:28], func=AF.Sqrt),
                       reads=[sm.b], writes=[sm.b])
                  k.op("dve", lambda e: e.reciprocal(sm[:, 36:44], sm[:, 28:36]), reads=[sm.b], writes=[sm.b])
                  rq, rk = sm[:, 36:40], sm[:, 40:44]
                  pc = ps()
                  k.op("pe", lambda e: e.matmul(pc[:, 0:4], lhsT=LTRI[m], rhs=gt, start=True, stop=True),
                       reads=[gcs.b, sm.b], writes=[pc.b])
                  k.op("pe", lambda e: e.matmul(pc[:, 4:8], lhsT=BONES[m], rhs=gt, start=True, stop=True),
                       reads=[gcs.b, sm.b], writes=[pc.b])
                  k.op("dve", lambda e: e.tensor_copy(sm[:, 44:52], pc[:, 0:8]), reads=[pc.b], writes=[sm.b])
                  k.op("act", lambda e: e.activation(out=sm[:, 52:56], in_=sm[:, 44:48], func=AF.Exp),
                       reads=[sm.b], writes=[sm.b])
                  k.op("dve", lambda e: e.tensor_tensor(sm[:, 56:60], sm[:, 48:52], sm[:, 44:48], op=ALU.subtract),
                       reads=[sm.b], writes=[sm.b])
                  k.op("act", lambda e: e.activation(out=sm[:, 56:60], in_=sm[:, 56:60], func=AF.Exp),
                       reads=[sm.b], writes=[sm.b])
                  k.op("act", lambda e: e.activation(out=sm[:, 60:64], in_=sm[:, 48:52], func=AF.Exp),
                       reads=[sm.b], writes=[sm.b])
                  egc, etl, dec, beta = sm[:, 52:56], sm[:, 56:60], sm[:, 60:64], sm[:, 0:4]
                  k.op("dve", lambda e: e.tensor_copy(scal[:, 0, :], rq), reads=[sm.b], writes=[scal.b])
                  k.op("dve", lambda e: e.scalar_tensor_tensor(out=scal[:, 1, :], in0=rq, scalar=DK5, in1=egc,
                                                               op0=ALU.mult, op1=ALU.mult),
                       reads=[sm.b], writes=[scal.b])
                  k.op("dve", lambda e: e.tensor_copy(scal[:, 2, :], rk), reads=[sm.b], writes=[scal.b])
                  k.op("dve", lambda e: e.tensor_tensor(scal[:, 5, :], rk, beta, op=ALU.mult),
                       reads=[sm.b], writes=[scal.b])
                  k.op("dve", lambda e: e.tensor_tensor(scal[:, 3, :], scal[:, 5, :], egc, op=ALU.mult),
                       reads=[sm.b, scal.b], writes=[scal.b])
                  k.op("dve", lambda e: e.tensor_tensor(scal[:, 4, :], rk, etl, op=ALU.mult),
                       reads=[sm.b], writes=[scal.b])
                  yield
                  TMq = TM[:, 0:512].rearrange("p (h d) -> p h d", h=4)
                  TMk = TM[:, 512:1024].rearrange("p (h d) -> p h d", h=4)
                  TMv = TM[:, 1024:1536].rearrange("p (h d) -> p h d", h=4)
                  bc = lambda a: a.unsqueeze(2).to_broadcast([128, 4, 128])
                  for dst, src_, sc_ in ((KQ[:, 0:4, :], TMq, scal[:, 0, :]), (KQ[:, 4:8, :], TMk, scal[:, 2, :]),
                                         (KQ[:, 8:12, :], TMq, scal[:, 1, :]), (KBG[:], TMk, scal[:, 3, :]),
                                         (KTL[:], TMk, scal[:, 4, :]), (VB[:], TMv, beta)):
                      wb_ = KQ.b if dst.tensor.name == KQ[:].tensor.name else (
                          KBG.b if dst.tensor.name == KBG[:].tensor.name else (
                              KTL.b if dst.tensor.name == KTL[:].tensor.name else VB.b))
                      k.op("dve", lambda e, dst=dst, src_=src_, sc_=sc_: e.tensor_tensor(
                          dst, src_, bc(sc_), op=ALU.mult), reads=[TM.b, scal.b, sm.b], writes=[wb_])
                  for cc0 in (0, 4, 8):
                      p = ps()
                      for c4 in range(4):
                          k.op("pe", lambda e, cc0=cc0, c4=c4, p=p: e.transpose(
                              p[:, c4 * 128:(c4 + 1) * 128], KQ[:, cc0 + c4, :], ident[:]),
                              reads=[KQ.b, ident.b], writes=[p.b])
                      k.op("act", lambda e, p=p, cc0=cc0: e.activation(
                          out=KQT[:, cc0:cc0 + 4, :], in_=p[:, :].rearrange("p (a t) -> p a t", a=4),
                          func=AF.Copy), reads=[p.b], writes=[KQT.b])
                      yield
                  if smp:
                      rhs3 = sm_st["rhs3"]
                      k.op("dve", lambda e: e.tensor_tensor(
                          rhs3[:], gt.unsqueeze(1).to_broadcast([128, 16, 4]),
                          G8.unsqueeze(2).to_broadcast([128, 16, 4]), op=ALU.mult),
                          reads=[sm.b, gcs.b], writes=[rhs3.b])
                      pdb = ps()
                      k.op("pe", lambda e: e.matmul(pdb[:, 0:64], lhsT=ONES,
                                                    rhs=rhs3[:].rearrange("p b h -> p (b h)"), start=True, stop=True),
                           reads=[gcs.b, rhs3.b], writes=[pdb.b])
                      decB = sm_st["decB"]
                      k.op("act", lambda e: e.activation(out=decB[:], in_=pdb[:, 0:64], func=AF.Exp),
                           reads=[pdb.b], writes=[decB.b])
                  def head_gen(h):
                      H = hb[h]
                      GU, G2, t1, aq, UW, vn = H["GU"], H["G2"], H["t1"], H["aq"], H["UW"], H["vn"]
                      KnT, QnT, DQT = KQT[:, 4 + h, :], KQT[:, h, :], KQT[:, 8 + h, :]
                      k.op("dve", lambda e: e.tensor_scalar(GU[:], USTR, gt[:, h:h + 1], None, op0=ALU.mult),
                           reads=[gcs.b, sm.b], writes=[GU.b])
                      pD = ps()
                      k.op("pe", lambda e: e.matmul(pD[:, 0:128], lhsT=LTRI[m], rhs=GU[:], start=True, stop=True),
                           reads=[gcs.b, GU.b], writes=[pD.b])
                      k.op("pe", lambda e: e.matmul(pD[:, 128:256], lhsT=GU[:], rhs=LTRI[m], start=True, stop=True),
                           reads=[gcs.b, GU.b], writes=[pD.b])
                      k.op("pe", lambda e: e.matmul(pD[:, 256:384], lhsT=KnT, rhs=KnT, start=True, stop=True),
                           reads=[KQT.b], writes=[pD.b])
                      k.op("pe", lambda e: e.matmul(pD[:, 384:512], lhsT=KnT, rhs=QnT, start=True, stop=True),
                           reads=[KQT.b], writes=[pD.b])
                      yield
                      k.op("act", lambda e: e.activation(out=G2[:], in_=pD[:, 0:256], func=AF.Exp),
                           reads=[pD.b], writes=[G2.b])
                      yield
                      k.op("dve", lambda e: e.tensor_tensor(t1[:], pD[:, 256:384], G2[:, 0:128], op=ALU.mult),
                           reads=[pD.b, G2.b], writes=[t1.b])
                      P0 = H["P"][0]
                      k.op("dve", lambda e: e.tensor_scalar(t1[:], t1[:], beta[:, h:h + 1], -1.0,
                                                            op0=ALU.mult, op1=ALU.mult),
                           reads=[t1.b, sm.b], writes=[t1.b])
                      k.op("dve", lambda e: e.tensor_tensor(P0[:, 0:128], t1[:], SLm[m], op=ALU.mult),
                           reads=[t1.b, gcs.b], writes=[P0.b])
                      k.op("dve", lambda e: e.tensor_tensor(t1[:], pD[:, 384:512], G2[:, 128:256], op=ALU.mult),
                           reads=[pD.b, G2.b], writes=[t1.b])
                      k.op("dve", lambda e: e.tensor_tensor(aq[:], t1[:], SUIm[m], op=ALU.mult),
                           reads=[t1.b, gcs.b], writes=[aq.b])
                      yield
                      pN = ps()
                      k.op("pe", lambda e: e.transpose(pN[:, 0:128], P0[:, 0:128], ident[:]),
                           reads=[P0.b, ident.b], writes=[pN.b])
                      k.op("act", lambda e: e.activation(out=P0[:, 128:256], in_=pN[:, 0:128], func=AF.Copy),
                           reads=[pN.b], writes=[P0.b])
                      TTc = H["TT"][0]
                      k.op("dve", lambda e: e.tensor_tensor(TTc[:], P0[:, 128:256], ident[:], op=ALU.add),
                           reads=[P0.b, ident.b], writes=[TTc.b])
                      yield
                      Pc = P0
                      for n in range(1, 7):
                          Pn = H["P"][n % 2]
                          TTn = H["TT"][n % 2]
                          pP = ps()
                          k.op("pe", lambda e, Pc=Pc, pP=pP: e.matmul(pP[:, 0:128], lhsT=Pc[:, 128:256], rhs=Pc[:, 0:128],
                                                                      start=True, stop=True),
                               reads=[Pc.b], writes=[pP.b])
                          if n < 6:
                              k.op("pe", lambda e, Pc=Pc, pP=pP: e.matmul(pP[:, 128:256], lhsT=Pc[:, 0:128],
                                                                          rhs=Pc[:, 128:256], start=True, stop=True),
                                   reads=[Pc.b], writes=[pP.b])
                          k.op("act", lambda e, Pn=Pn, pP=pP: e.activation(out=Pn[:], in_=pP[:, 0:256], func=AF.Copy),
                               reads=[pP.b], writes=[Pn.b])
                          yield
                          pT = ps()
                          k.op("pe", lambda e, Pn=Pn, pT=pT, TTc=TTc: e.matmul(pT[:, 0:128], lhsT=Pn[:, 0:128], rhs=TTc[:],
                                                                               start=True, stop=True),
                               reads=[Pn.b, TTc.b], writes=[pT.b])
                          k.op("dve", lambda e, TTn=TTn, TTc=TTc, pT=pT: e.tensor_tensor(
                              TTn[:], TTc[:], pT[:, 0:128], op=ALU.add), reads=[TTc.b, pT.b], writes=[TTn.b])
                          yield
                          Pc, TTc = Pn, TTn
                      pU = ps()
                      k.op("pe", lambda e, TTc=TTc: e.matmul(pU[:, 0:128], lhsT=TTc[:], rhs=VB[:, h, :], start=True, stop=True),
                           reads=[TTc.b, VB.b], writes=[pU.b])
                      k.op("pe", lambda e, TTc=TTc: e.matmul(pU[:, 128:256], lhsT=KBG[:, h, :], rhs=TTc[:], start=True, stop=True),
                           reads=[TTc.b, KBG.b], writes=[pU.b])
                      k.op("act", lambda e: e.activation(out=UW[:], in_=pU[:, 0:256], func=AF.Copy),
                           reads=[pU.b], writes=[UW.b])
                      yield
                      if not smp:
                          S_ = Sst[h]
                          p1 = ps()
                          k.op("pe", lambda e: e.matmul(p1[:, 0:128], lhsT=UW[:, 128:256], rhs=S_[:], start=True, stop=True),
                               reads=[UW.b, S_.b], writes=[p1.b])
                          k.op("dve", lambda e: e.tensor_tensor(vn[:], UW[:, 0:128], p1[:, 0:128], op=ALU.subtract),
                               reads=[UW.b, p1.b], writes=[vn.b])
                          yield
                          p2 = ps()
                          k.op("pe", lambda e: e.matmul(p2[:, 0:128], lhsT=DQT, rhs=S_[:], start=True, stop=False),
                               reads=[KQT.b, S_.b], writes=[p2.b])
                          k.op("pe", lambda e: e.matmul(p2[:, 0:128], lhsT=aq[:], rhs=vn[:], start=False, stop=True),
                               reads=[aq.b, vn.b], writes=[p2.b])
                          k.op("act", lambda e: e.activation(out=O32[:, h * 128:(h + 1) * 128], in_=p2[:, 0:128], func=AF.Copy),
                               reads=[p2.b], writes=[O32.b])
                          p3 = ps()
                          k.op("pe", lambda e: e.matmul(p3[:, 0:128], lhsT=KTL[:, h, :], rhs=vn[:], start=True, stop=True),
                               reads=[KTL.b, vn.b], writes=[p3.b])
                          k.op("dve", lambda e: e.scalar_tensor_tensor(out=S_[:], in0=S_[:], scalar=dec[:, h:h + 1],
                                                                       in1=p3[:, 0:128], op0=ALU.mult, op1=ALU.add),
                               reads=[S_.b, sm.b, p3.b], writes=[S_.b])
                      else:
                          Ssm, WTm, DQm, vm, So = (sm_st["Ssm"], sm_st["WTm"], sm_st["DQm"], sm_st["vm"], sm_st["So"])
                          decB = sm_st["decB"]
                          k.op("dve", lambda e: e.tensor_tensor(
                              WTm[:], UW[:, 128:256].unsqueeze(1).to_broadcast([128, 16, 128]), CM32, op=ALU.mult),
                              reads=[UW.b, gcs.b], writes=[WTm.b])
                          p1 = ps(hold=True)
                          for b in range(16):
                              k.op("pe", lambda e, b=b: e.matmul(p1[:, 0:128], lhsT=WTm[:, b, :], rhs=Ssm[:, b * 4 + h, :],
                                                                 start=(b == 0), stop=(b == 15)),
                                   reads=[WTm.b, Ssm.b], writes=[p1.b])
                          k.op("dve", lambda e: e.tensor_tensor(vn[:], UW[:, 0:128], p1[:, 0:128], op=ALU.subtract),
                               reads=[UW.b, p1.b], writes=[vn.b])
                          ps_release(p1)
                          k.op("dve", lambda e: e.tensor_tensor(
                              DQm[:], DQT.unsqueeze(1).to_broadcast([128, 16, 128]), CM32, op=ALU.mult),
                              reads=[KQT.b, gcs.b], writes=[DQm.b])
                          p2 = ps(hold=True)
                          for b in range(16):
                              k.op("pe", lambda e, b=b: e.matmul(p2[:, 0:128], lhsT=DQm[:, b, :], rhs=Ssm[:, b * 4 + h, :],
                                                                 start=(b == 0), stop=False),
                                   reads=[DQm.b, Ssm.b], writes=[p2.b])
                          k.op("pe", lambda e: e.matmul(p2[:, 0:128], lhsT=aq[:], rhs=vn[:], start=False, stop=True),
                               reads=[aq.b, vn.b], writes=[p2.b])
                          k.op("act", lambda e: e.activation(out=O32[:, h * 128:(h + 1) * 128], in_=p2[:, 0:128], func=AF.Copy),
                               reads=[p2.b], writes=[O32.b])
                          ps_release(p2)
                          k.op("dve", lambda e: e.tensor_tensor(
                              vm[:], vn[:].unsqueeze(1).to_broadcast([128, 16, 128]),
                              G8.unsqueeze(2).to_broadcast([128, 16, 128]), op=ALU.mult),
                              reads=[vn.b, gcs.b], writes=[vm.b])
                          for b in range(16):
                              p3 = ps()
                              k.op("pe", lambda e, b=b, p3=p3: e.matmul(p3[:, 0:128], lhsT=KTL[:, h, :], rhs=vm[:, b, :],
                                                                        start=True, stop=True),
                                   reads=[KTL.b, vm.b], writes=[p3.b])
                              so = So[b % 2]
                              k.op("dve", lambda e, b=b, p3=p3, so=so: e.scalar_tensor_tensor(
                                  out=so[:], in0=Ssm[:, b * 4 + h, :], scalar=decB[:, b * 4 + h:b * 4 + h + 1],
                                  in1=p3[:, 0:128], op0=ALU.mult, op1=ALU.add),
                                  reads=[Ssm.b, decB.b, p3.b], writes=[so.b])
                              k.dma("sp", o_sgdn[b, h], so[:], reads=[so.b])
                  if smp and DBG:
                      k.dma("sp", dbg_tm[:, :], TM[:], reads=[TM.b])
                      k.dma("sp", dbg_sm[:, :], sm[:], reads=[sm.b])
                      k.dma("sp", dbg_o[:, :], O32[:], reads=[O32.b])
                      H = hb[1]
                      k.dma("sp", dbg_uw[:, :], H["UW"][:], reads=[H["UW"].b])
                      k.dma("sp", dbg_vn[:, :], H["vn"][:], reads=[H["vn"].b])
                      k.dma("sp", dbg_aq[:, :], H["aq"][:], reads=[H["aq"].b])
                      k.dma("sp", dbg_tt[:, :], H["TT"][0][:], reads=[H["TT"][0].b])
                  yield "B"
                  if smp:
                      for h in range(4):
                          for _ in head_gen(h):
                              pass
                  else:
                      gens = [head_gen(h) for h in range(4)]
                      alive = [True] * 4
                      while any(alive):
                          for gi, g_ in enumerate(gens):
                              if alive[gi]:
                                  try:
                                      next(g_)
                                  except StopIteration:
                                      alive[gi] = False
                          yield
                  head_rmsnorm(O32.b, O32[:, :], 4, 128, hv[:, 384:512], go32, gtmp, ghstat, "go")
                  k.op("dve", lambda e: e.tensor_tensor(go32[:], go32[:], sgz[:], op=ALU.mult),
                       reads=[go32.b, sgz.b], writes=[go32.b])
                  k.dma("sp", g_sc[j * 128:(j + 1) * 128, :], go32[:], reads=[go32.b], writes=[g_sc_b[j]])

              with contextlib.ExitStack() as st2:
                  sm_st = dict(Us=sb(st2, "Us", [128, 12, 16, 11], BF16), Ssm=sb(st2, "Ssm", [128, 64, 128]),
                               WTm=sb(st2, "WTm", [128, 16, 128]), rhs3=sb(st2, "rhs3", [128, 16, 4]),
                               decB=sb(st2, "decB", [128, 64]),
                               So=[sb(st2, "So%d" % i, [128, 128]) for i in range(2)])
                  sm_st["DQm"] = sm_st["WTm"]
                  sm_st["vm"] = sm_st["WTm"]
                  Ssm = sm_st["Ssm"]
                  for b in range(16):
                      k.dma("sp", Ssm[:, b * 4:(b + 1) * 4, :], sgdn_d[b].rearrange("h k v -> k h v"), writes=[Ssm.b])
                  cb = sb(st2, "cb", [48, 1536])
                  k.dma("sp", cb[:], sconv_d[:, :], writes=[cb.b])
                  Us = sm_st["Us"]
                  for cc0 in (0, 4, 8):
                      p = ps()
                      for c4 in range(4):
                          cc = cc0 + c4
                          k.op("pe", lambda e, cc=cc, c4=c4, p=p: e.transpose(
                              p[:, c4 * 48:(c4 + 1) * 48], cb[:, cc * 128:(cc + 1) * 128], ident[0:48, 0:48]),
                              reads=[cb.b, ident.b], writes=[p.b])
                      for c4 in range(4):
                          k.op("act", lambda e, cc0=cc0, c4=c4, p=p: e.activation(
                              out=Us[:, cc0 + c4, :, 0:3],
                              in_=p[:, c4 * 48:(c4 + 1) * 48].rearrange("p (b t) -> p b t", b=16),
                              func=AF.Copy), reads=[p.b], writes=[Us.b])
                  for _ in gdn_tile(NT_ALL, sm_st):
                      pass
                  outs_done += [s_.b for s_ in sm_st["So"]]
                  k.barrier()
              sgz2.append(sb(st, "sgz1", [128, 512]))
              KQT2.append(sb(st, "KQT1", [128, 12, 128]))
              KBG2.append(sb(st, "KBG1", [128, 4, 128]))
              KTL2.append(sb(st, "KTL1", [128, 4, 128]))
              VB2.append(sb(st, "VB1", [128, 4, 128]))
              sm2.append(sb(st, "gsm1", [128, 64]))
              gensG = [gdn_tile(j) for j in range(NT_ALL)]
              for tok in gensG[0]:
                  if tok == "B":
                      break
              for j in range(NT_ALL):
                  cur = gensG[j]
                  nxt = gensG[j + 1] if j + 1 < NT_ALL else None
                  cur_alive, nxt_inA = True, nxt is not None
                  while cur_alive or nxt_inA:
                      if cur_alive:
                          try:
                              next(cur)
                          except StopIteration:
                              cur_alive = False
                      if nxt_inA:
                          if next(nxt) == "B":
                              nxt_inA = False
              for h in range(4):
                  k.dma("sp", o_pgdn[h], Sst[h][:], reads=[Sst[h].b])
              outs_done += [s_.b for s_ in Sst]

        k.barrier()
        kst = contextlib.ExitStack()
        KT = sb(kst, "KT", [64, 4, SEQ + 256], BF16)
        VA = sb(kst, "VA", [128, NT_ALL + 2, 4, 65], BF16)
        IKT = sb(kst, "IKT", [64, SEQ + 256], BF16)
        KTn = sb(kst, "KTn", [64, 4, 128], BF16)
        IKTn = sb(kst, "IKTn", [64, 128], BF16)
        VAn = sb(kst, "VAn", [128, 4, 65], BF16)
        k.op("pool", lambda e: e.memset(VA[:], 1.0), writes=[VA.b])
        k.op("pool", lambda e: e.memset(VAn[:], 1.0), writes=[VAn.b])
        k.barrier()
        with contextlib.ExitStack() as st:
          if 'C' in PH:
              NCOL = 576 + 1536
              Wk = sb(st, "Wk", [128, KC, NCOL], BF16)
              load_w(Wk, w_in, O_AK, 512, 0)
              load_w(Wk, w_in, O_IK, 64, 512)
              load_w(Wk, w_in, O_GQKV, 1536, 576)
              tsets = [(sb(st, "cx32_%d" % i, [128, D]), sb(st, "cxn_%d" % i, [128, D]),
                        sb(st, "cxT_%d" % i, [128, KC, 128], BF16), sb(st, "cstat_%d" % i, [128, 4]))
                       for i in range(3)]
              tmp = sb(st, "ctmp", [128, 512])
              hstat = sb(st, "chstat", [128, 24])
              ko = [sb(st, "ko%d" % i, [128, 256]) for i in range(2)]
              vo = [sb(st, "vo%d" % i, [128, 256]) for i in range(2)]
              io = [sb(st, "io%d" % i, [128, 64]) for i in range(2)]
              gq = sb(st, "gq", [128, 1536])
              def c_gen(j):
                  smp = (j == NT_ALL)
                  src = x_smp[:, :] if smp else x_all[j * 128:(j + 1) * 128, :]
                  _, xT = load_xT(tsets[j % 3], src, 0)
                  yield "B"
                  pkv = ps()
                  linear(xT, Wk, 0, 512, pkv)
                  pik = ps()
                  linear(xT, Wk, 512, 64, pik)
                  kk, vv, ii = ko[j % 2], vo[j % 2], io[j % 2]
                  head_rmsnorm(pkv.b, pkv[:, 0:256], 4, 64, hv[:, 64:128], kk, tmp, hstat, "ak")
                  k.op("act", lambda e, vv=vv, pkv=pkv: e.activation(out=vv[:], in_=pkv[:, 256:512], func=AF.Copy),
                       reads=[pkv.b], writes=[vv.b])
                  k.op("act", lambda e, ii=ii, pik=pik: e.activation(out=ii[:], in_=pik[:, 0:64], func=AF.Copy),
                       reads=[pik.b], writes=[ii.b])
                  if smp:
                      kside_store(kk[:], vv[:], ii[:], [kk.b, vv.b, ii.b], KTn[:], IKTn[:], VAn[:, :, 0:64],
                                  [KTn.b, IKTn.b, VAn.b])
                  else:
                      kside_store(kk[:], vv[:], ii[:], [kk.b, vv.b, ii.b], KT[:, :, j * 128:(j + 1) * 128],
                                  IKT[:, j * 128:(j + 1) * 128], VA[:, j, :, 0:64], [KT.b, IKT.b, VA.b])
                  if smp:
                      k.dma("sp", o_sk[:, :], kk[:], reads=[kk.b])
                      k.dma("sp", o_sv[:, :], vv[:], reads=[vv.b])
                      k.dma("sp", o_sik[:, :], ii[:], reads=[ii.b])
                  else:
                      k.dma("sp", o_pk[j * 128:(j + 1) * 128, :], kk[:], reads=[kk.b])
                      k.dma("sp", o_pv[j * 128:(j + 1) * 128, :], vv[:], reads=[vv.b])
                      k.dma("sp", o_pik[j * 128:(j + 1) * 128, :], ii[:], reads=[ii.b])
                  if j >= NT_ALL - 1:
                      for c in range(3):
                          pg = ps()
                          linear(xT, Wk, 576 + c * 512, 512, pg)
                          k.op("act", lambda e, c=c, pg=pg: e.activation(
                              out=gq[:, c * 512:(c + 1) * 512], in_=pg[:, :], func=AF.Copy),
                              reads=[pg.b], writes=[gq.b])
                      if smp:
                          for t3 in range(3):
                              k.dma("sp", o_sconv[:, t3, :], gq[5 + t3::8, :], reads=[gq.b])
                      else:
                          k.dma("sp", o_pconv[:, :], gq[125:128, :], reads=[gq.b])
              pipe_ahead([c_gen(j) for j in range(NT_ALL + 1)], 2)
              outs_done += [b.b for b in ko + vo + io] + [gq.b]

        k.barrier()
        with contextlib.ExitStack() as st:
          if 'E' in PH:
              NQ = 512 + 256 + 4 + 512
              Wq = sb(st, "Wq", [128, KC, NQ], BF16)
              load_w(Wq, w_in, O_AQ, 512, 0)
              load_w(Wq, w_in, O_IQ, 256, 512)
              load_w(Wq, w_in, O_IW, 4, 768)
              load_w(Wq, w_in, O_MQ, 512, 772)
              tsets = [(sb(st, "ex32_%d" % i, [128, D]), sb(st, "exn_%d" % i, [128, D]),
                        sb(st, "exT_%d" % i, [128, KC, 128], BF16), sb(st, "estat_%d" % i, [128, 4]))
                       for i in range(1)] * 2
              tmp = sb(st, "etmp", [128, 512])
              hstat = sb(st, "ehstat", [128, 24])
              mq32 = sb(st, "mq32", [128, 512])
              MQT2 = [sb(st, "MQT%d" % q, [128, 4, 128], BF16) for q in range(2)]
              PT = sb(st, "PT", [128, 2, 4, 128], BF16)
              tE = sb(st, "tE", [128, 4, 128], BF16)
              rec = sb(st, "rec", [128, 8])
              mo32 = [sb(st, "mo32_%d" % i, [128, 512]) for i in range(2)]
              ao32 = [sb(st, "ao32_%d" % i, [128, 512]) for i in range(2)]
              mkb = [sb(st, "mkb%d" % i, [128, 2, 512]) for i in range(1)] * 2
              MKTb = [sb(st, "MKTb%d" % i, [128, 4, 256], BF16) for i in range(2)]
              MVb = [sb(st, "MVb%d" % i, [128, 2, 4, 129], BF16) for i in range(2)]
              for t_ in MVb:
                  k.op("pool", lambda e, t_=t_: e.memset(t_[:], 1.0), writes=[t_.b])
              SC_M = 128.0 ** -0.5

              def mem_scores(MKT_, MQT_, mb, dstPT, cm=None):
                  pS = ps()
                  for h in range(4):
                      k.op("pe", lambda e, h=h: e.matmul(
                          pS[:, h * 128:(h + 1) * 128], lhsT=MKT_[:, h, mb * 128:(mb + 1) * 128],
                          rhs=MQT_[:, h, :], start=True, stop=True),
                          reads=[MKT_.b, MQT_.b], writes=[pS.b])
                  if cm is None:
                      k.op("act", lambda e: e.activation(
                          out=dstPT[:, mb, :, :], in_=pS[:, :].rearrange("p (h t) -> p h t", h=4),
                          func=AF.Exp, scale=SC_M), reads=[pS.b], writes=[dstPT.b])
                  else:
                      k.op("act", lambda e: e.activation(
                          out=tE[:], in_=pS[:, :].rearrange("p (h t) -> p h t", h=4),
                          func=AF.Exp, scale=SC_M), reads=[pS.b], writes=[tE.b])
                      k.op("dve", lambda e: e.tensor_tensor(
                          dstPT[:, mb, :, :], tE[:], cm.unsqueeze(1).to_broadcast([128, 4, 128]), op=ALU.mult),
                          reads=[tE.b, cmask.b], writes=[dstPT.b])

              def mem_pv(pO, PT_, MV_, first, last):
                  for h in range(4):
                      for mb in range(2):
                          k.op("pe", lambda e, h=h, mb=mb: e.matmul(
                              pO[h // 2][:, (h % 2) * 129:(h % 2) * 129 + 129], lhsT=PT_[:, mb, h, :],
                              rhs=MV_[:, mb, h, :], start=False, stop=(last and mb == 1)),
                              reads=[PT_.b, MV_.b], writes=[pO[h // 2].b])

              NIT = 18
              dcs = sb(st, "dcs", [128, 64])
              k.dma("sp", dcs[:], dconst_d[:, :], writes=[dcs.b])
              score = sb(st, "score", [128, SEQ])
              junk = sb(st, "junk", [128, SEQ], BF16)
              maskT2 = [sb(st, "maskT%d" % q, [128, NT_ALL, 128], BF16) for q in range(2)]
              Eb = [sb(st, "Eb%d" % i, [128, 8, 128], BF16) for i in range(2)]
              Pm = [sb(st, "Pm%d" % i, [128, 8, 128], BF16) for i in range(2)]
              QT2 = [sb(st, "QT%d" % q, [64, 8, 128], BF16) for q in range(2)]
              IQT = sb(st, "IQT", [64, 4, 128], BF16)
              aq32 = sb(st, "aq32", [128, 512])
              iq32 = sb(st, "iq32", [128, 260])
              wst = sb(st, "wst", [128, 16])
              bis = sb(st, "bis", [128, 8 + 2 * NIT])
              rtmp = sb(st, "rtmp", [128, 512])
              rtmpB = sb(st, "rtmpB", [128, 512])
              pent = sb(st, "pent", [128, 256])
              den8 = sb(st, "den8", [128, 8])
              SC_A = 64.0 ** -0.5
              cmask32e = dcs[:, 40:56]

              def dsa_q(xT, QTb):
                  pq = ps()
                  linear(xT, Wq, 0, 512, pq)
                  head_rmsnorm(pq.b, pq[:, :], 8, 64, hv[:, 0:64], aq32, tmp, hstat, "aq")
                  for half in range(2):
                      p = ps()
                      for hh in range(4):
                          k.op("pe", lambda e, hh=hh, half=half, p=p: e.transpose(
                              p[0:64, hh * 128:(hh + 1) * 128],
                              aq32[:, (half * 4 + hh) * 64:(half * 4 + hh + 1) * 64], ident[:]),
                              reads=[aq32.b, ident.b], writes=[p.b])
                      k.op("act", lambda e, half=half, p=p: e.activation(
                          out=QTb[:, half * 4:(half + 1) * 4, :], in_=p[0:64, :].rearrange("p (h t) -> p h t", h=4),
                          func=AF.Copy), reads=[p.b], writes=[QTb.b])
                  yield
                  pi = ps()
                  linear(xT, Wq, 512, 260, pi)
                  k.op("act", lambda e: e.activation(out=iq32[:], in_=pi[:, 0:260], func=AF.Copy),
                       reads=[pi.b], writes=[iq32.b])
                  p = ps()
                  for hh in range(4):
                      k.op("pe", lambda e, hh=hh, p=p: e.transpose(
                          p[0:64, hh * 128:(hh + 1) * 128], iq32[:, hh * 64:(hh + 1) * 64], ident[:]),
                          reads=[iq32.b, ident.b], writes=[p.b])
                  k.op("act", lambda e, p=p: e.activation(
                      out=IQT[:], in_=p[0:64, :].rearrange("p (h t) -> p h t", h=4), func=AF.Copy),
                      reads=[p.b], writes=[IQT.b])
                  yield
                  k.op("act", lambda e: e.activation(out=wst[:, 0:4], in_=iq32[:, 256:260], func=AF.Abs),
                       reads=[iq32.b], writes=[wst.b])
                  k.op("dve", lambda e: e.tensor_scalar(wst[:, 4:8], iq32[:, 256:260], 0.0, 2.0,
                                                        op0=ALU.is_gt, op1=ALU.mult),
                       reads=[iq32.b], writes=[wst.b])
                  k.op("dve", lambda e: e.tensor_scalar(wst[:, 4:8], wst[:, 4:8], -1.0, None, op0=ALU.add),
                       reads=[wst.b], writes=[wst.b])
              def dsa_index(L):
                  for c0 in range(0, L, 512):
                      n = min(512, L - c0)
                      for hh in range(4):
                          pS = ps()
                          k.op("pe", lambda e, hh=hh, pS=pS, c0=c0, n=n: e.matmul(
                              pS[:, 0:n], lhsT=IQT[:, hh, :], rhs=IKT[:, c0:c0 + n], start=True, stop=True),
                              reads=[IQT.b, IKT.b], writes=[pS.b])
                          rt_ = rtmp if hh % 2 == 0 else rtmpB
                          k.op("act", lambda e, hh=hh, pS=pS, n=n, rt_=rt_: e.activation(
                              out=rt_[:, 0:n], in_=pS[:, 0:n], func=AF.Relu, scale=wst[:, hh:hh + 1]),
                              reads=[pS.b, wst.b], writes=[rt_.b])
                          if hh == 0:
                              k.op("dve", lambda e, c0=c0, n=n, rt_=rt_: e.tensor_scalar(
                                  score[:, c0:c0 + n], rt_[:, 0:n], wst[:, 4:5], None, op0=ALU.mult),
                                  reads=[rt_.b, wst.b], writes=[score.b])
                          else:
                              k.op("dve", lambda e, hh=hh, c0=c0, n=n, rt_=rt_: e.scalar_tensor_tensor(
                                  out=score[:, c0:c0 + n], in0=rt_[:, 0:n], scalar=wst[:, 4 + hh:5 + hh],
                                  in1=score[:, c0:c0 + n], op0=ALU.mult, op1=ALU.add),
                                  reads=[rt_.b, wst.b, score.b], writes=[score.b])
                          yield
              def dsa_select(i, nk, L, sel, mTb):
                  k.op("dve", lambda e: e.tensor_reduce(bis[:, 4:5], score[:, 0:L], axis=AX.X, op=ALU.max),
                       reads=[score.b], writes=[bis.b])
                  k.op("dve", lambda e: e.tensor_reduce(bis[:, 5:6], score[:, 0:L], axis=AX.X, op=ALU.min),
                       reads=[score.b], writes=[bis.b])
                  k.dma("sp", pent[:], pen_d[i], writes=[pent.b])
                  k.op("dve", lambda e: e.tensor_tensor(score[:, L - 256:L], score[:, L - 256:L], pent[:], op=ALU.add),
                       reads=[score.b, pent.b], writes=[score.b])
                  if DBG and i == 1 and sel is None:
                      k.dma("sp", dbg_sc[:, :], score[:, 0:512], reads=[score.b])
                  k.op("dve", lambda e: e.tensor_tensor(bis[:, 0:1], bis[:, 4:5], bis[:, 5:6], op=ALU.add),
                       reads=[bis.b], writes=[bis.b])
                  k.op("dve", lambda e: e.tensor_scalar(bis[:, 0:1], bis[:, 0:1], 0.5, None, op0=ALU.mult),
                       reads=[bis.b], writes=[bis.b])
                  k.op("dve", lambda e: e.tensor_tensor(bis[:, 3:4], bis[:, 4:5], bis[:, 5:6], op=ALU.subtract),
                       reads=[bis.b], writes=[bis.b])
                  k.op("dve", lambda e: e.tensor_scalar(bis[:, 3:4], bis[:, 3:4], 2.0, None, op0=ALU.add),
                       reads=[bis.b], writes=[bis.b])
                  k.op("dve", lambda e: e.tensor_scalar(bis[:, 8:8 + NIT], dcs[:, 0:NIT], bis[:, 3:4], None, op0=ALU.mult),
                       reads=[bis.b, dcs.b], writes=[bis.b])
                  k.op("dve", lambda e: e.tensor_scalar(bis[:, 8 + NIT:8 + 2 * NIT], bis[:, 8:8 + NIT], -0.5, None,
                                                        op0=ALU.mult), reads=[bis.b], writes=[bis.b])
                  for n_ in range(NIT):
                      k.op("dve", lambda e: e.tensor_scalar(junk[:, 0:L], score[:, 0:L], bis[:, 0:1], None,
                                                            op0=ALU.is_ge, op1=ALU.add, accum_out=bis[:, 1:2]),
                           reads=[score.b, bis.b], writes=[junk.b, bis.b])
                      k.op("dve", lambda e, n_=n_: e.tensor_scalar(bis[:, 2:3], bis[:, 1:2], dcs[:, 32:33],
                                                                  bis[:, 8 + n_:9 + n_], op0=ALU.is_ge, op1=ALU.mult),
                           reads=[bis.b, dcs.b], writes=[bis.b])
                      k.op("dve", lambda e, n_=n_: e.scalar_tensor_tensor(
                          out=bis[:, 0:1], in0=bis[:, 2:3], scalar=bis[:, 8 + NIT + n_:9 + NIT + n_], in1=bis[:, 0:1],
                          op0=ALU.add, op1=ALU.add), reads=[bis.b], writes=[bis.b])
                      yield
                  k.op("dve", lambda e: e.tensor_tensor(bis[:, 0:1], bis[:, 0:1], bis[:, 8 + NIT - 1:8 + NIT],
                                                        op=ALU.subtract), reads=[bis.b], writes=[bis.b])
                  k.op("dve", lambda e: e.tensor_scalar(score[:, 0:L], score[:, 0:L], bis[:, 0:1], None, op0=ALU.is_ge),
                       reads=[score.b, bis.b], writes=[score.b])
                  if DBG and i == 1 and sel is None:
                      k.dma("sp", dbg_mask[:, :], score[:, 0:512], reads=[score.b])
                      k.dma("sp", dbg_bis[:, :], bis[:], reads=[bis.b])
                  for kb0 in range(0, nk, 4):
                      nb_ = min(4, nk - kb0)
                      p = ps()
                      for j_ in range(nb_):
                          k.op("pe", lambda e, j_=j_, kb0=kb0, p=p: e.transpose(
                              p[:, j_ * 128:(j_ + 1) * 128], score[:, (kb0 + j_) * 128:(kb0 + j_ + 1) * 128], ident[:]),
                              reads=[score.b, ident.b], writes=[p.b])
                      k.op("act", lambda e, kb0=kb0, nb_=nb_, p=p: e.activation(
                          out=mTb[:, kb0:kb0 + nb_, :], in_=p[:, 0:nb_ * 128].rearrange("p (a t) -> p a t", a=nb_),
                          func=AF.Copy), reads=[p.b], writes=[mTb.b])
                      yield
              def dsa_attend(kblocks, ao, sel, accumulate, QTb, mTb):
                  pO = [ps(hold=True), ps(hold=True)]
                  ps_zero(pO[0]); ps_zero(pO[1])
                  nkb = len(kblocks)
                  def st_exp(ki):
                      ktT, kc0, vaT, vblk, mi, kbufs = kblocks[ki]
                      E_ = Eb[ki % 2]
                      for g2 in range(2):
                          pS = ps()
                          for gg in range(2):
                              g = g2 * 2 + gg
                              k.op("pe", lambda e, g=g, gg=gg, pS=pS: e.matmul(
                                  pS[:, gg * 256:(gg + 1) * 256], lhsT=ktT[:, g, kc0:kc0 + 128],
                                  rhs=QTb[:, 2 * g:2 * g + 2, :], start=True, stop=True),
                                  reads=kbufs + [QTb.b], writes=[pS.b])
                          k.op("act", lambda e, g2=g2, pS=pS, E_=E_: e.activation(
                              out=E_[:, g2 * 4:(g2 + 1) * 4, :], in_=pS[:, :].rearrange("p (h t) -> p h t", h=4),
                              func=AF.Exp, scale=SC_A), reads=[pS.b], writes=[E_.b])

                  st_exp(0)
                  for ki, (ktT, kc0, vaT, vblk, mi, kbufs) in enumerate(kblocks):
                      E_, P_ = Eb[ki % 2], Pm[ki % 2]
                      if ki + 1 < nkb:
                          st_exp(ki + 1)
                      k.op("dve", lambda e, mi=mi, E_=E_, P_=P_: e.tensor_tensor(
                          P_[:], E_[:], mTb[:, mi, :].unsqueeze(1).to_broadcast([128, 8, 128]), op=ALU.mult),
                          reads=[E_.b, mTb.b], writes=[P_.b])
                      for hh in range(8):
                          rhs_ = vaT[:, hh // 2, :] if vblk is None else vaT[:, vblk, hh // 2, :]
                          k.op("pe", lambda e, hh=hh, P_=P_, rhs_=rhs_: e.matmul(
                              pO[hh // 4][:, (hh % 4) * 65:(hh % 4) * 65 + 65], lhsT=P_[:, hh, :],
                              rhs=rhs_, start=False, stop=(ki == nkb - 1)),
                              reads=[P_.b] + kbufs, writes=[pO[hh // 4].b])
                      yield
                  for j_ in range(2):
                      pv3 = pO[j_][:, 0:260].rearrange("p (h c) -> p h c", h=4)
                      k.op("dve", lambda e, j_=j_, pv3=pv3: e.reciprocal(
                          den8[:, 4 * j_:4 * j_ + 4].unsqueeze(2), pv3[:, :, 64:65]),
                          reads=[pO[j_].b], writes=[den8.b])
                      if sel is not None:
                          k.op("dve", lambda e, j_=j_: e.tensor_scalar(
                              den8[:, 4 * j_:4 * j_ + 4], den8[:, 4 * j_:4 * j_ + 4], sel, None, op0=ALU.mult),
                              reads=[den8.b, cmask.b, selw.b], writes=[den8.b])
                      dst = ao[:, 256 * j_:256 * j_ + 256].rearrange("p (h d) -> p h d", h=4)
                      bc_ = den8[:, 4 * j_:4 * j_ + 4].unsqueeze(2).to_broadcast([128, 4, 64])
                      if not accumulate:
                          k.op("dve", lambda e, pv3=pv3, dst=dst, bc_=bc_: e.tensor_tensor(
                              dst, pv3[:, :, 0:64], bc_, op=ALU.mult), reads=[pO[j_].b, den8.b], writes=[ao.b])
                      else:
                          r3 = rtmp[:, 0:256].rearrange("p (h d) -> p h d", h=4)
                          k.op("dve", lambda e, pv3=pv3, r3=r3, bc_=bc_: e.tensor_tensor(
                              r3, pv3[:, :, 0:64], bc_, op=ALU.mult), reads=[pO[j_].b, den8.b], writes=[rtmp.b])
                          k.op("dve", lambda e, dst=dst, r3=r3: e.tensor_tensor(dst, dst, r3, op=ALU.add),
                               reads=[rtmp.b, ao.b], writes=[ao.b])
                  ps_release(pO[0]); ps_release(pO[1])

              def run(g):
                  for _ in g:
                      pass

              def rr(*gens):
                  alive = [g for g in gens if g is not None]
                  while alive:
                      for g in list(alive):
                          try:
                              next(g)
                          except StopIteration:
                              alive.remove(g)

              def mq_proj(xT, MQTb):
                  pmq = ps()
                  linear(xT, Wq, 772, 512, pmq)
                  head_rmsnorm(pmq.b, pmq[:, :], 4, 128, hv[:, 128:256], mq32, tmp, hstat, "mq")
                  p = ps()
                  for h in range(4):
                      k.op("pe", lambda e, h=h, p=p: e.transpose(
                          p[:, h * 128:(h + 1) * 128], mq32[:, h * 128:(h + 1) * 128], ident[:]),
                          reads=[mq32.b, ident.b], writes=[p.b])
                  k.op("act", lambda e, p=p: e.activation(
                      out=MQTb[:], in_=p[:, :].rearrange("p (h t) -> p h t", h=4), func=AF.Copy),
                      reads=[p.b], writes=[MQTb.b])

              def mem_attn(i, smp, MQT):
                  pO = [ps(hold=True), ps(hold=True)]
                  ps_zero(pO[0]); ps_zero(pO[1])
                  if not smp:
                      for mb in range(2):
                          mem_scores(MKT, MQT, mb, PT)
                          yield
                      mem_pv(pO, PT, MVa, True, True)
                      yield
                  else:
                      for b in range(16):
                          kb_, Kt_, Vb_ = mkb[b % 2], MKTb[b % 2], MVb[b % 2]
                          k.dma("sp", kb_[:], cmk_d[b].rearrange("(mb p) c -> p mb c", p=128), writes=[kb_.b])
                          for mb in range(2):
                              k.dma("pool", Vb_[:, mb, :, 0:128],
                                    cmv_d[b, mb * 128:(mb + 1) * 128, :].rearrange("p (h d) -> p h d", h=4),
                                    writes=[Vb_.b])
                          for mb in range(2):
                              p = ps()
                              for h in range(4):
                                  k.op("pe", lambda e, h=h, p=p, mb=mb: e.transpose(
                                      p[:, h * 128:(h + 1) * 128], kb_[:, mb, h * 128:(h + 1) * 128], ident[:]),
                                      reads=[kb_.b, ident.b], writes=[p.b])
                              k.op("act", lambda e, p=p, mb=mb: e.activation(
                                  out=Kt_[:, :, mb * 128:(mb + 1) * 128],
                                  in_=p[:, :].rearrange("p (h m) -> p h m", h=4), func=AF.Copy),
                                  reads=[p.b], writes=[Kt_.b])
                          for mb in range(2):
                              mem_scores(Kt_, MQT, mb, PT, cm=cmask[:, b, :])
                          mem_pv(pO, PT, Vb_, b == 0, b == 15)
                          yield
                  mo = mo32[i % 2]
                  for j in range(2):
                      pv3 = pO[j][:, 0:258].rearrange("p (h c) -> p h c", h=2)
                      k.op("dve", lambda e, j=j, pv3=pv3: e.reciprocal(
                          rec[:, 2 * j:2 * j + 2].unsqueeze(2), pv3[:, :, 128:129]),
                          reads=[pO[j].b], writes=[rec.b])
                      k.op("dve", lambda e, j=j, pv3=pv3, mo=mo: e.tensor_tensor(
                          mo[:, 256 * j:256 * j + 256].rearrange("p (h d) -> p h d", h=2), pv3[:, :, 0:128],
                          rec[:, 2 * j:2 * j + 2].unsqueeze(2).to_broadcast([128, 2, 128]), op=ALU.mult),
                          reads=[pO[j].b, rec.b], writes=[mo.b])
                  ps_release(pO[0]); ps_release(pO[1])
                  k.dma("sp", m_sc[i * 128:(i + 1) * 128, :], mo[:], reads=[mo.b], writes=[m_sc_b[i]])

              NDSA = cfg.get("dsa_tiles", 17)

              def tile_gen(i):
                  bf = i % 2
                  QTb, mTb, MQTb = QT2[bf], maskT2[bf], MQT2[bf]
                  x32, xT = load_xT(tsets[0], x_own[i * 128:(i + 1) * 128, :], 0)
                  yield
                  mq_proj(xT, MQTb)
                  yield
                  nk_i = 4 * (i // 2) + (2 if i % 2 == 0 else 4)
                  if i < NDSA:
                      yield from dsa_q(xT, QTb)
                      yield from dsa_index(nk_i * 128)
                      yield from dsa_select(i, nk_i, nk_i * 128, None, mTb)
                  yield "B"
                  ao = ao32[bf]
                  if i < NDSA:
                      kbl = [(KT, kb * 128, VA, kb, kb, [KT.b, VA.b]) for kb in range(nk_i)]
                      yield from dsa_attend(kbl, ao, None, False, QTb, mTb)
                  else:
                      k.op("pool", lambda e: e.memset(ao[:], 0.0), writes=[ao.b])
                  k.dma("sp", a_sc[i * 128:(i + 1) * 128, :], ao[:], reads=[ao.b], writes=[a_sc_b[i]])
                  yield from mem_attn(i, False, MQTb)

              gens = [tile_gen(i) for i in range(NT_OWN)]

              def to_B(g):
                  for tok in g:
                      if tok == "B":
                          return

              to_B(gens[0])
              for i in range(NT_OWN):
                  cur = gens[i]
                  nxt = gens[i + 1] if i + 1 < NT_OWN else None
                  cur_alive, nxt_inA = True, nxt is not None
                  while cur_alive or nxt_inA:
                      if cur_alive:
                          try:
                              next(cur)
                          except StopIteration:
                              cur_alive = False
                      if nxt_inA:
                          if next(nxt) == "B":
                              nxt_inA = False

              i = NT_OWN
              x32, xT = load_xT(tsets[0], x_smp[:, :], 0)
              QTb, mTb, MQTb = QT2[0], maskT2[0], MQT2[0]
              mq_proj(xT, MQTb)
              run(mem_attn(i, True, MQTb))
              ao = ao32[0]
              if NDSA < 17:
                  k.op("pool", lambda e: e.memset(ao[:], 0.0), writes=[ao.b])
              else:
                  ptb = sb(st, "ptb", [128, 256], I32)
                  ptf = sb(st, "ptf", [128, 256])
                  pti = sb(st, "pti", [128, 256], I32)
                  k.dma("sp", ptb[:], pt_d[0:1, :].partition_broadcast(128), writes=[ptb.b])
                  k.op("dve", lambda e: e.tensor_copy(ptf[:], ptb[:]), reads=[ptb.b], writes=[ptf.b])
                  k.op("dve", lambda e: e.tensor_scalar(ptf[:], ptf[:], 128.0, dcs[:, 33:34],
                                                        op0=ALU.mult, op1=ALU.add),
                       reads=[ptf.b, dcs.b], writes=[ptf.b])
                  k.op("dve", lambda e: e.tensor_copy(pti[:], ptf[:]), reads=[ptf.b], writes=[pti.b])
                  kvpg = [sb(st, "kvpg%d" % q, [128, 512]) for q in range(2)]
                  ipg = [sb(st, "ipg%d" % q, [128, 64]) for q in range(2)]
                  IQTm = [sb(st, "IQTm%d" % q, [64, 4, 128], BF16) for q in range(2)]
                  KTh, VAh, IKTh = [Buf(), Buf()], [Buf(), Buf()], [Buf(), Buf()]
                  run(dsa_q(xT, QTb))
                  k.op("pool", lambda e: e.memset(score[:, 0:2048], 0.0), writes=[score.b])

                  def idx_gather(b):
                      hf = b % 2
                      base = 17 * hf
                      for pg in range(16):
                          q_ = pg % 2
                          col = b * 16 + pg
                          off = bass.IndirectOffsetOnAxis(ap=pti[:, col:col + 1], axis=0)
                          k.dma("pool", ipg[q_][:], cik_d[:, :], reads=[pti.b], writes=[ipg[q_].b], indirect=off)
                          kside_store(None, None, ipg[q_][:], [ipg[q_].b], None,
                                      IKT[:, (base + pg) * 128:(base + pg + 1) * 128], None, [IKTh[hf]])
                          yield

                  def idx_score(b):
                      hf = b % 2
                      base = 17 * hf
                      Im = IQTm[hf]
                      k.op("dve", lambda e: e.tensor_tensor(
                          Im[:], IQT[:], cmask[0:64, b, :].unsqueeze(1).to_broadcast([64, 4, 128]), op=ALU.mult),
                          reads=[IQT.b, cmask.b], writes=[Im.b])
                      for c in range(4):
                          for hh in range(4):
                              pS = ps()
                              c0 = base * 128 + c * 512
                              k.op("pe", lambda e, hh=hh, pS=pS, c0=c0: e.matmul(
                                  pS[:, :], lhsT=Im[:, hh, :], rhs=IKT[:, c0:c0 + 512], start=True, stop=True),
                                  reads=[Im.b, IKTh[hf]], writes=[pS.b])
                              k.op("act", lambda e, hh=hh, pS=pS: e.activation(
                                  out=rtmp[:], in_=pS[:, :], func=AF.Relu, scale=wst[:, hh:hh + 1]),
                                  reads=[pS.b, wst.b], writes=[rtmp.b])
                              k.op("dve", lambda e, hh=hh, c=c: e.scalar_tensor_tensor(
                                  out=score[:, c * 512:(c + 1) * 512], in0=rtmp[:], scalar=wst[:, 4 + hh:5 + hh],
                                  in1=score[:, c * 512:(c + 1) * 512], op0=ALU.mult, op1=ALU.add),
                                  reads=[rtmp.b, wst.b, score.b], writes=[score.b])
                              yield

                  run(idx_gather(0))
                  for b in range(16):
                      rr(idx_score(b), idx_gather(b + 1) if b + 1 < 16 else None)
                  for hh in range(4):
                      pS = ps()
                      k.op("pe", lambda e, hh=hh, pS=pS: e.matmul(
                          pS[:, 0:128], lhsT=IQT[:, hh, :], rhs=IKTn[:, :], start=True, stop=True),
                          reads=[IQT.b, IKTn.b], writes=[pS.b])
                      k.op("act", lambda e, hh=hh, pS=pS: e.activation(
                          out=rtmp[:, 0:128], in_=pS[:, 0:128], func=AF.Relu, scale=wst[:, hh:hh + 1]),
                          reads=[pS.b, wst.b], writes=[rtmp.b])
                      if hh == 0:
                          k.op("dve", lambda e: e.tensor_scalar(
                              score[:, 2048:2176], rtmp[:, 0:128], wst[:, 4:5], None, op0=ALU.mult),
                              reads=[rtmp.b, wst.b], writes=[score.b])
                      else:
                          k.op("dve", lambda e, hh=hh: e.scalar_tensor_tensor(
                              out=score[:, 2048:2176], in0=rtmp[:, 0:128], scalar=wst[:, 4 + hh:5 + hh],
                              in1=score[:, 2048:2176], op0=ALU.mult, op1=ALU.add),
                              reads=[rtmp.b, wst.b, score.b], writes=[score.b])
                  run(dsa_select(16, 17, 17 * 128, None, mTb))

                  def kv_gather(b):
                      hf = b % 2
                      base = 17 * hf
                      for pg in range(16):
                          q_ = pg % 2
                          col = b * 16 + pg
                          off = bass.IndirectOffsetOnAxis(ap=pti[:, col:col + 1], axis=0)
                          k.dma("pool", kvpg[q_][:], ckv_d[:, :], reads=[pti.b], writes=[kvpg[q_].b], indirect=off)
                          kside_store(kvpg[q_][:, 0:256], None, None, [kvpg[q_].b],
                                      KT[:, :, (base + pg) * 128:(base + pg + 1) * 128], None, None, [KTh[hf]])
                          k.op("dve", lambda e, q_=q_, base=base, pg=pg: e.tensor_copy(
                              VA[:, base + pg, :, 0:64], kvpg[q_][:, 256:512].rearrange("p (g d) -> p g d", g=4)),
                              reads=[kvpg[q_].b], writes=[VAh[hf]])
                          yield

                  def seq_attend(b):
                      hf = b % 2
                      base = 17 * hf
                      kbl = [(KT, (base + pg) * 128, VA, base + pg, pg, [KTh[hf], VAh[hf]]) for pg in range(16)]
                      kbl.append((KTn, 0, VAn, None, 16, [KTn.b, VAn.b]))
                      yield from dsa_attend(kbl, ao, cmask32e[:, b:b + 1], b > 0, QTb, mTb)

                  run(kv_gather(0))
                  for b in range(16):
                      rr(seq_attend(b), kv_gather(b + 1) if b + 1 < 16 else None)
              k.dma("sp", a_sc[i * 128:(i + 1) * 128, :], ao[:], reads=[ao.b], writes=[a_sc_b[i]])

        k.barrier()
        kst.close()
        k.barrier()
        with contextlib.ExitStack() as st:
          if 'F' in PH:
              Wg = sb(st, "Wg", [128, KC, 3072], BF16)
              Wbr = sb(st, "Wbr", [128, 12, D], BF16)
              Wo = sb(st, "Wo", [128, KC, D], BF16)
              load_w(Wg, w_in, O_GATES, 3072)
              for bi, wsrc in enumerate((w_a_out, w_g_out, w_m_out)):
                  for kc in range(4):
                      k.dma("pool", Wbr[:, bi * 4 + kc, :], wsrc[kc * 128:(kc + 1) * 128, :], writes=[Wbr.b])
              load_w(Wo, w_o, 0, D)
              tsets = [(sb(st, "fx32_%d" % i, [128, D]), sb(st, "fxn_%d" % i, [128, D]),
                        sb(st, "fxT_%d" % i, [128, KC, 128], BF16), sb(st, "fstat_%d" % i, [128, 4]))
                       for i in range(3)]
              br32 = [sb(st, "br32_%d" % i, [128, 3, 512]) for i in range(2)]
              cand = [sb(st, "cand%d" % i, [128, 4, 512]) for i in range(2)]
              brT = sb(st, "brT", [128, 12, 128], BF16)
              sig = sb(st, "sig", [128, 512])
              term = sb(st, "term", [128, 512])
              h32 = sb(st, "h32", [128, D])
              hT = sb(st, "hT", [128, KC, 128], BF16)
              x2o = [sb(st, "x2o%d" % i, [128, D]) for i in range(2)]
              def f_gen(i):
                  smp = (i == NT_OWN)
                  src = x_smp[:, :] if smp else x_own[i * 128:(i + 1) * 128, :]
                  x32, xT = load_xT(tsets[i % 3], src, 0)
                  yield "B"
                  br = br32[i % 2]
                  k.dma("sp", br[:, 0, :], a_sc[i * 128:(i + 1) * 128, :], reads=[a_sc_b[i]], writes=[br.b])
                  k.dma("sp", br[:, 2, :], m_sc[i * 128:(i + 1) * 128, :], reads=[m_sc_b[i]], writes=[br.b])
                  if smp:
                      k.dma("sp", br[:, 1, :], g_sc[32 * 128:33 * 128, :], reads=[g_sc_b[32]], writes=[br.b])
                  else:
                      grp = i // 2
                      cd = cand[i % 2]
                      if i % 2 == 0:
                          cdl = cand[(i // 2) % 2]
                          k.dma("sp", cdl[:], g_sc[grp * 512:(grp + 1) * 512, :].rearrange("(c p) n -> p c n", p=128),
                                reads=g_sc_b[4 * grp:4 * grp + 4], writes=[cdl.b])
                      cdl = cand[(i // 2) % 2]
                      for c in range(4):
                          sc_ = selw[:, i * 4 + c:i * 4 + c + 1]
                          if c == 0:
                              k.op("dve", lambda e, sc_=sc_, br=br, cdl=cdl: e.tensor_scalar(
                                  br[:, 1, :], cdl[:, 0, :], sc_, None, op0=ALU.mult),
                                  reads=[cdl.b, selw.b], writes=[br.b])
                          else:
                              k.op("dve", lambda e, sc_=sc_, br=br, cdl=cdl, c=c: e.scalar_tensor_tensor(
                                  out=br[:, 1, :], in0=cdl[:, c, :], scalar=sc_, in1=br[:, 1, :],
                                  op0=ALU.mult, op1=ALU.add), reads=[cdl.b, selw.b, br.b], writes=[br.b])
                  for bi in range(3):
                      p = ps()
                      for j in range(4):
                          k.op("pe", lambda e, bi=bi, j=j, p=p, br=br: e.transpose(
                              p[:, j * 128:(j + 1) * 128], br[:, bi, j * 128:(j + 1) * 128], ident[:]),
                              reads=[br.b, ident.b], writes=[p.b])
                      k.op("act", lambda e, bi=bi, p=p: e.activation(
                          out=brT[:, bi * 4:(bi + 1) * 4, :], in_=p[:, :].rearrange("p (a b) -> p a b", a=4),
                          func=AF.Copy), reads=[p.b], writes=[brT.b])
                  for c in range(2):
                      for bi in range(3):
                          pg = ps()
                          linear(xT, Wg, bi * 1024 + c * 512, 512, pg)
                          pb = ps()
                          for kc in range(4):
                              k.op("pe", lambda e, kc=kc, bi=bi, c=c, pb=pb: e.matmul(
                                  pb[:, :], lhsT=brT[:, bi * 4 + kc, :], rhs=Wbr[:, bi * 4 + kc, c * 512:(c + 1) * 512],
                                  start=(kc == 0), stop=(kc == 3)), reads=[brT.b, Wbr.b], writes=[pb.b])
                          k.op("act", lambda e, pg=pg: e.activation(out=sig[:], in_=pg[:, :], func=AF.Sigmoid),
                               reads=[pg.b], writes=[sig.b])
                          if bi == 0:
                              k.op("dve", lambda e, pb=pb, c=c: e.tensor_tensor(
                                  h32[:, c * 512:(c + 1) * 512], sig[:], pb[:, :], op=ALU.mult),
                                  reads=[sig.b, pb.b], writes=[h32.b])
                          else:
                              k.op("dve", lambda e, pb=pb: e.tensor_tensor(term[:], sig[:], pb[:, :], op=ALU.mult),
                                   reads=[sig.b, pb.b], writes=[term.b])
                              k.op("dve", lambda e, c=c: e.tensor_tensor(
                                  h32[:, c * 512:(c + 1) * 512], h32[:, c * 512:(c + 1) * 512], term[:], op=ALU.add),
                                  reads=[term.b, h32.b], writes=[h32.b])
                  for half in range(2):
                      p = ps()
                      for j in range(4):
                          kc = half * 4 + j
                          k.op("pe", lambda e, kc=kc, j=j, p=p: e.transpose(
                              p[:, j * 128:(j + 1) * 128], h32[:, kc * 128:(kc + 1) * 128], ident[:]),
                              reads=[h32.b, ident.b], writes=[p.b])
                      k.op("act", lambda e, half=half, p=p: e.activation(
                          out=hT[:, half * 4:(half + 1) * 4, :], in_=p[:, :].rearrange("p (a b) -> p a b", a=4),
                          func=AF.Copy), reads=[p.b], writes=[hT.b])
                  xo_ = x2o[i % 2]
                  for c in range(2):
                      p = ps()
                      linear(hT, Wo, c * 512, 512, p)
                      k.op("dve", lambda e, c=c, p=p, xo_=xo_, x32=x32: e.tensor_tensor(
                          xo_[:, c * 512:(c + 1) * 512], p[:, :], x32[:, c * 512:(c + 1) * 512], op=ALU.add),
                          reads=[p.b, x32.b], writes=[xo_.b])
                  k.dma("sp", x2_sc[i * 128:(i + 1) * 128, :], xo_[:], reads=[xo_.b], writes=[x2_sc_b[i]])
              pipe_ahead([f_gen(i) for i in range(NT_OWN + 1)], 2)

        k.barrier()
        with contextlib.ExitStack() as st:
          if 'D' in PH:
              Wf1 = sb(st, "Wf1", [128, KC, 2 * D_FF], BF16)
              Wf2 = sb(st, "Wf2", [128, D_FF // 128, D], BF16)
              load_w(Wf1, w_ffn_in, 0, 2 * D_FF)
              load_w(Wf2, w_ffn_out, 0, D, rows=D_FF)
              tsets = [(sb(st, "dx32_%d" % i, [128, D]), sb(st, "dxn_%d" % i, [128, D]),
                        sb(st, "dxT_%d" % i, [128, KC, 128], BF16), sb(st, "dstat_%d" % i, [128, 4]))
                       for i in range(2)]
              act2 = [sb(st, "ffact%d" % q, [128, D_FF]) for q in range(2)]
              sg2 = [sb(st, "ffsg%d" % q, [128, 512]) for q in range(1)] * 2
              actT2 = [sb(st, "ffactT%d" % q, [128, D_FF // 128, 128], BF16) for q in range(2)]
              yo = [sb(st, "yo%d" % i, [128, D]) for i in range(2)]
              nb = D_FF // 128

              def ffn_gen(i):
                  smp = (i == NT_OWN)
                  bf = i % 2
                  act, actT = act2[bf], actT2[bf]
                  if 'F' in PH:
                      src = x2_sc[i * 128:(i + 1) * 128, :]
                      x32, xT = load_xT(tsets[bf], src, 16, rd=[x2_sc_b[i]])
                  else:
                      src = x_smp[:, :] if smp else x_own[i * 128:(i + 1) * 128, :]
                      x32, xT = load_xT(tsets[bf], src, 16)
                  yield
                  c0 = 0
                  ci = 0
                  while c0 < D_FF:
                      n = min(512, D_FF - c0)
                      sg = sg2[ci % 2]
                      pg = ps()
                      linear(xT, Wf1, c0, n, pg)
                      pu = ps()
                      linear(xT, Wf1, D_FF + c0, n, pu)
                      k.op("act", lambda e, pg=pg, n=n, sg=sg: e.activation(out=sg[:, 0:n], in_=pg[:, 0:n], func=AF.Silu),
                           reads=[pg.b], writes=[sg.b])
                      k.op("dve", lambda e, pu=pu, n=n, c0=c0, sg=sg: e.tensor_tensor(
                          act[:, c0:c0 + n], sg[:, 0:n], pu[:, 0:n], op=ALU.mult),
                          reads=[sg.b, pu.b], writes=[act.b])
                      c0 += n
                      ci += 1
                      yield
                  yield "B"
                  for b0 in range(0, nb, 4):
                      nbb = min(4, nb - b0)
                      p = ps()
                      for j in range(nbb):
                          k.op("pe", lambda e, j=j, b0=b0, p=p: e.transpose(
                              p[:, j * 128:(j + 1) * 128], act[:, (b0 + j) * 128:(b0 + j + 1) * 128], ident[:]),
                              reads=[act.b, ident.b], writes=[p.b])
                      k.op("act", lambda e, p=p, b0=b0, nbb=nbb: e.activation(
                          out=actT[:, b0:b0 + nbb, :], in_=p[:, 0:nbb * 128].rearrange("p (a b) -> p a b", a=nbb),
                          func=AF.Copy), reads=[p.b], writes=[actT.b])
                      yield
                  yy = yo[bf]
                  for h in range(2):
                      p = ps()
                      for kc in range(nb):
                          k.op("pe", lambda e, kc=kc, h=h, p=p: e.matmul(
                              p[:, :], lhsT=actT[:, kc, :], rhs=Wf2[:, kc, h * 512:(h + 1) * 512],
                              start=(kc == 0), stop=(kc == nb - 1)),
                              reads=[actT.b, Wf2.b], writes=[p.b])
                          if kc % 6 == 5:
                              yield
                      k.op("dve", lambda e, h=h, p=p, yy=yy, x32=x32: e.tensor_tensor(
                          yy[:, h * 512:(h + 1) * 512], p[:, :], x32[:, h * 512:(h + 1) * 512], op=ALU.add),
                          reads=[p.b, x32.b], writes=[yy.b])
                      yield
                  dst = y_smp[:, :] if smp else y_own[i * 128:(i + 1) * 128, :]
                  k.dma("sp", dst, yy[:], reads=[yy.b])

              gensD = [ffn_gen(i) for i in range(NT_OWN + 1)]
              for tok in gensD[0]:
                  if tok == "B":
                      break
              for i in range(NT_OWN + 1):
                  cur = gensD[i]
                  nxt = gensD[i + 1] if i + 1 < NT_OWN + 1 else None
                  cur_alive, nxt_inA = True, nxt is not None
                  while cur_alive or nxt_inA:
                      if cur_alive:
                          try:
                              next(cur)
                          except StopIteration:
                              cur_alive = False
                      if nxt_inA:
                          if next(nxt) == "B":
                              nxt_inA = False
              outs_done += [b.b for b in yo]

        k.barrier()
        k.finish(outs_done, "sp")
    print("ops", k.nops, "waits", k.nwaits)
    return nc


_NC_CACHE = {}


def make_in_maps(I):
    f = lambda a: np.ascontiguousarray(np.asarray(a), dtype=np.float32)
    x_prompt, x_sample, mem_prompt = f(I["x_prompt"]), f(I["x_sample"]), f(I["mem_prompt"])
    gains = np.concatenate([f(I["norm_mix"])[0].reshape(8, 128).T, f(I["mem_norm"])[0].reshape(8, 128).T,
                            f(I["norm_ffn"])[0].reshape(8, 128).T], axis=1)
    hv = np.concatenate([f(I["a_q_norm"])[0], f(I["a_k_norm"])[0], f(I["m_q_norm"])[0], f(I["m_k_norm"])[0],
                         f(I["g_o_norm"])[0], f(I["g_dt_bias"])[0], f(I["g_a_log"])[0],
                         np.zeros(56, np.float32)])[None, :]
    r = np.arange(128)
    same = (r[:, None] // 8) == (r[None, :] // 8)
    one = np.ones((128, 128), bool)
    mats = []
    for blk in (one, same):
        pass
    ltri = [(r[:, None] <= r[None, :]) & blk for blk in (one, same)]
    bones = [blk for blk in (one, same)]
    slm = [(r[:, None] > r[None, :]) & blk for blk in (one, same)]
    sui = [((r[None, :] >= r[:, None]) & blk) * (128.0 ** -0.5) for blk in (one, same)]
    ustr = (r[:, None] > r[None, :])
    g8 = (r[:, None] // 8) == np.arange(16)[None, :]
    cm32 = np.broadcast_to((np.arange(128)[None, :] // 8 == np.arange(16)[:, None]).reshape(1, 2048), (128, 2048))
    gconst = np.concatenate([np.asarray(a, np.float32) for a in
                             (ltri[0], ltri[1], bones[0], bones[1], slm[0], slm[1], sui[0], sui[1], ustr,
                              np.ones((128, 128)), g8, cm32)], axis=1)
    gconvT = f(I["g_conv"])[0].reshape(4, 12, 128).transpose(2, 1, 0).reshape(128, 48)
    dconst = np.zeros((128, 64), np.float32)
    dconst[:, 0:32] = (2.0 ** -(np.arange(32) + 1.0))[None, :]
    dconst[:, 32] = 256.0
    dconst[:, 33] = np.arange(128)
    dconst[:, 40:56] = g8
    ckv = np.concatenate([f(I["cache_k"])[0].reshape(2560 * 128, 256),
                          f(I["cache_v"])[0].reshape(2560 * 128, 256)], axis=1)
    cik = f(I["cache_idx_k"])[0].reshape(2560 * 128, 64)
    ptab = np.asarray(I["page_table"]).astype(np.int32)
    shared = {
        "w_in": f(I["w_in"])[0], "w_mem_kv": f(I["w_mem_kv"])[0], "w_a_out": f(I["w_a_out"])[0],
        "w_g_out": f(I["w_g_out"])[0], "w_m_out": f(I["w_m_out"])[0], "w_o": f(I["w_o"])[0],
        "w_ffn_in": f(I["w_ffn_in"])[0], "w_ffn_out": f(I["w_ffn_out"])[0],
        "ident": np.eye(128, dtype=np.float32),
        "dconst": dconst, "cache_kv": ckv, "cache_idx_k": cik,
        "gconst": np.ascontiguousarray(gconst, dtype=np.float32), "gconvT": np.ascontiguousarray(gconvT),
        "cmask": np.ascontiguousarray(np.broadcast_to(
            (np.arange(128)[None, :] // 8 == np.arange(16)[:, None]).astype(np.float32).reshape(1, 2048), (128, 2048))), "gains": np.ascontiguousarray(gains), "headvecs": hv,
    }
    in_maps = []
    for c in range(8):
        b, half = c // 2, c % 2
        ot = own_tiles(half)
        xo = np.concatenate([x_prompt[b, t * 128:(t + 1) * 128] for t in ot], axis=0)
        m = dict(shared)
        m["x_all"] = x_prompt[b]
        m["x_own"] = np.ascontiguousarray(xo)
        m["x_smp"] = np.ascontiguousarray(x_sample[16 * c:16 * c + 16].reshape(128, D))
        m["mem"] = mem_prompt[b]
        m["cache_mem_k"] = np.ascontiguousarray(f(I["cache_mem_k"])[0, 16 * c:16 * c + 16].reshape(16, 256, 512))
        m["cache_mem_v"] = np.ascontiguousarray(f(I["cache_mem_v"])[0, 16 * c:16 * c + 16].reshape(16, 256, 512))
        m["state_gdn"] = np.ascontiguousarray(f(I["state_gdn"])[0, 16 * c:16 * c + 16])
        m["state_conv"] = np.ascontiguousarray(f(I["state_conv"])[0, 16 * c:16 * c + 16].reshape(48, 1536))
        m["page_table"] = np.ascontiguousarray(ptab[16 * c:16 * c + 16].reshape(1, 256))
        pen = np.zeros((17, 128, 256), np.float32)
        tt = np.arange(128)
        for i_, t_ in enumerate(ot):
            nk_ = 4 * (i_ // 2) + (2 if i_ % 2 == 0 else 4)
            spos = (nk_ - 2) * 128 + np.arange(256)
            pen[i_] = np.where(spos[None, :] <= (t_ * 128 + tt)[:, None], 0.0, -1e30)
        newok = ((tt[:, None] // 8) == (tt[None, :] // 8)) & ((tt[None, :] % 8) <= (tt[:, None] % 8))
        pen[16, :, 128:] = np.where(newok, 0.0, -1e30)
        m["pen"] = pen
        sw = np.zeros((16, 4), np.float32)
        for i_, t_ in enumerate(ot):
            sw[i_, t_ % 4] = 1.0
        m["selw"] = np.ascontiguousarray(np.broadcast_to(sw.reshape(1, 64), (128, 64)))
        in_maps.append(m)
    return in_maps


def kernel(**I):
    if "nc" not in _NC_CACHE:
        _NC_CACHE["nc"] = build({})
    nc = _NC_CACHE["nc"]
    in_maps = make_in_maps(I)
    res = run_bass_kernel_spmd(nc, in_maps, core_ids=list(range(8)))
    R = res.results
    yp = np.zeros((4, SEQ, D), np.float32)
    for c in range(8):
        b, half = c // 2, c % 2
        for i, t in enumerate(own_tiles(half)):
            yp[b, t * 128:(t + 1) * 128] = R[c]["y_own"][i * 128:(i + 1) * 128]
    ys = np.concatenate([R[c]["y_smp"].reshape(16, 8, D) for c in range(8)], axis=0)
    p_k = np.stack([R[2 * b]["o_pk"].reshape(SEQ, 4, 64) for b in range(4)])[None]
    p_v = np.stack([R[2 * b]["o_pv"].reshape(SEQ, 4, 64) for b in range(4)])[None]
    p_ik = np.stack([R[2 * b]["o_pik"] for b in range(4)])[None]
    p_gdn = np.stack([R[2 * b]["o_pgdn"] for b in range(4)])[None]
    p_conv = np.stack([R[2 * b]["o_pconv"] for b in range(4)])[None]
    p_mk = np.stack([R[2 * b]["o_pmk"].reshape(256, 4, 128) for b in range(4)])[None]
    p_mv = np.stack([R[2 * b]["o_pmv"].reshape(256, 4, 128) for b in range(4)])[None]
    s_k = np.concatenate([R[c]["o_sk"].reshape(16, 8, 4, 64) for c in range(8)], axis=0)[None]
    s_v = np.concatenate([R[c]["o_sv"].reshape(16, 8, 4, 64) for c in range(8)], axis=0)[None]
    s_ik = np.concatenate([R[c]["o_sik"].reshape(16, 8, 64) for c in range(8)], axis=0)[None]
    s_gdn = np.concatenate([R[c]["o_sgdn"] for c in range(8)], axis=0)[None]
    s_conv = np.concatenate([R[c]["o_sconv"] for c in range(8)], axis=0)[None]
    return (yp, ys, p_k, p_v, p_ik, p_gdn, p_conv, p_mk, p_mv, s_k, s_v, s_ik, s_gdn, s_conv)
```

```python
import contextlib
import numpy as np
import concourse.bass as bass
import concourse.mybir as mybir
from concourse.bass_utils import run_bass_kernel_spmd

F32 = mybir.dt.float32
BF16 = mybir.dt.bfloat16
I32 = mybir.dt.int32
AF = mybir.ActivationFunctionType
ALU = mybir.AluOpType
AX = mybir.AxisListType

D = 1024
KC = 8
SEQ = 4096
NT_ALL = 32
NT_OWN = 16
D_IN = 6988
D_FF = 2816
EPS = 1e-6
O_AQ, O_AK, O_AV, O_IQ, O_IK, O_IW = 0, 512, 768, 1024, 1280, 1344
O_GQKV, O_GZ, O_GB, O_GA, O_MQ, O_GATES = 1348, 2884, 3396, 3400, 3404, 3916


class Buf:
    __slots__ = ("name", "w", "r")

    def __init__(self, name=""):
        self.name = name
        self.w = None
        self.r = []


class K:
    def __init__(self, nc, n_dma_sems=40):
        self.nc = nc
        self.eng = {"pe": nc.tensor, "act": nc.scalar, "dve": nc.vector,
                    "pool": nc.gpsimd, "sp": nc.sync}
        self.sem, self.cnt, self.seen = {}, {}, {}
        for e in self.eng:
            self.sem[e] = nc.alloc_semaphore("prog_" + e)
            self.cnt[e] = 0
            self.seen[e] = {}
        self.dsems = [nc.alloc_semaphore("dma%d" % i) for i in range(n_dma_sems)]
        self.dcnt = [0] * n_dma_sems
        self.dnext = 0
        self.nops = 0
        self.nwaits = 0

    def _wait(self, e, tok):
        if tok is None:
            return
        sem, val, src = tok
        if src == e and e == "pe":
            return
        key = sem.num
        if self.seen[e].get(key, 0) >= val:
            return
        self.eng[e].wait_ge(sem, val)
        self.seen[e][key] = val
        self.nwaits += 1

    def _deps(self, e, reads, writes):
        for b in reads:
            self._wait(e, b.w)
        for b in writes:
            self._wait(e, b.w)
            for t in b.r:
                self._wait(e, t)

    def _commit(self, tok, reads, writes):
        for b in reads:
            b.r.append(tok)
            if len(b.r) > 64:
                b.r = b.r[-64:] if False else b.r
        for b in writes:
            b.w = tok
            b.r = []

    def op(self, e, fn, reads=(), writes=()):
        self._deps(e, reads, writes)
        ins = fn(self.eng[e])
        self.cnt[e] += 1
        ins.then_inc(self.sem[e], 1)
        tok = (self.sem[e], self.cnt[e], e)
        self._commit(tok, reads, writes)
        self.nops += 1
        return tok

    def dma(self, q, out, in_, reads=(), writes=(), indirect=None, **kw):
        self._deps(q, reads, writes)
        i = self.dnext
        self.dnext = (self.dnext + 1) % len(self.dsems)
        sem = self.dsems[i]
        if self.dcnt[i] > 0:
            self._wait(q, (sem, self.dcnt[i], "dma"))
        if indirect is not None:
            ins = self.eng[q].indirect_dma_start(out=out, out_offset=None, in_=in_,
                                                 in_offset=indirect, **kw)
        else:
            ins = self.eng[q].dma_start(out=out, in_=in_, **kw)
        self.dcnt[i] += 16
        ins.then_inc(sem, 16)
        tok = (sem, self.dcnt[i], "dma")
        self._commit(tok, reads, writes)
        self.nops += 1
        return tok

    def barrier(self):
        for e in self.eng:
            for e2 in self.eng:
                if e2 != e and self.cnt[e2] > 0:
                    self._wait(e, (self.sem[e2], self.cnt[e2], e2))
            for i, s_ in enumerate(self.dsems):
                if self.dcnt[i] > 0:
                    self._wait(e, (s_, self.dcnt[i], "dma"))

    def finish(self, bufs, e="sp"):
        for b in bufs:
            self._wait(e, b.w)
            for t in b.r:
                self._wait(e, t)


class T:
    __slots__ = ("t", "b")

    def __init__(self, t, name=""):
        self.t = t
        self.b = Buf(name)

    def __getitem__(self, key):
        return self.t[key]


def own_tiles(half):
    res = []
    for g in range(8):
        res += [4 * g, 4 * g + 3] if half == 0 else [4 * g + 1, 4 * g + 2]
    return res


def build(cfg):
    nc = bass.Bass("TRN2", target_bir_lowering=False)
    k = K(nc)
    dt_in = {}

    def din(name, shape, dt=F32):
        dt_in[name] = nc.dram_tensor(name, list(shape), dt, kind="ExternalInput").ap()
        return dt_in[name]

    def dout(name, shape, dt=F32):
        return nc.dram_tensor(name, list(shape), dt, kind="ExternalOutput").ap()

    x_all = din("x_all", [SEQ, D])
    x_own = din("x_own", [NT_OWN * 128, D])
    x_smp = din("x_smp", [128, D])
    mem = din("mem", [256, D])
    w_in = din("w_in", [D, D_IN])
    w_mem_kv = din("w_mem_kv", [D, 1024])
    w_a_out = din("w_a_out", [512, D])
    w_g_out = din("w_g_out", [512, D])
    w_m_out = din("w_m_out", [512, D])
    w_o = din("w_o", [D, D])
    w_ffn_in = din("w_ffn_in", [D, 2 * D_FF])
    w_ffn_out = din("w_ffn_out", [D_FF, D])
    ident_d = din("ident", [128, 128])
    gains_d = din("gains", [128, 24])
    hv_d = din("headvecs", [1, 576])

    y_own = dout("y_own", [NT_OWN * 128, D])
    y_smp = dout("y_smp", [128, D])
    o_pk = dout("o_pk", [SEQ, 256])
    o_pv = dout("o_pv", [SEQ, 256])
    o_pik = dout("o_pik", [SEQ, 64])
    o_pconv = dout("o_pconv", [3, 1536])
    o_pmk = dout("o_pmk", [256, 512])
    o_pmv = dout("o_pmv", [256, 512])
    o_sk = dout("o_sk", [128, 256])
    o_sv = dout("o_sv", [128, 256])
    o_sik = dout("o_sik", [128, 64])
    o_sconv = dout("o_sconv", [16, 3, 1536])
    DBG = cfg.get("debug", False)
    skind = "ExternalOutput" if DBG else "Internal"
    cmk_d = din("cache_mem_k", [16, 256, 512])
    cmv_d = din("cache_mem_v", [16, 256, 512])
    cmask_d = din("cmask", [128, 2048])
    selw_d = din("selw", [128, 64])
    ckv_d = din("cache_kv", [2560 * 128, 512])
    cik_d = din("cache_idx_k", [2560 * 128, 64])
    pt_d = din("page_table", [1, 256], I32)
    pen_d = din("pen", [17, 128, 256])
    dconst_d = din("dconst", [128, 64])
    gconst_d = din("gconst", [128, 10 * 128 + 16 + 2048])
    gconvT_d = din("gconvT", [128, 48])
    sgdn_d = din("state_gdn", [16, 4, 128, 128])
    sconv_d = din("state_conv", [48, 1536])
    o_pgdn = dout("o_pgdn", [4, 128, 128])
    o_sgdn = dout("o_sgdn", [16, 4, 128, 128])
    if DBG:
        dbg_tm = dout("dbg_tm", [128, 1536])
        dbg_sm = dout("dbg_sm", [128, 64])
        dbg_o = dout("dbg_o", [128, 512])
        dbg_mask = dout("dbg_mask", [128, 512])
        dbg_bis = dout("dbg_bis", [128, 44])
        dbg_sc = dout("dbg_sc", [128, 512])
        dbg_uw = dout("dbg_uw", [128, 256])
        dbg_vn = dout("dbg_vn", [128, 128])
        dbg_aq = dout("dbg_aq", [128, 128])
        dbg_tt = dout("dbg_tt", [128, 128])
        dbg_wtm = dout("dbg_wtm", [128, 2048])
        dbg_p1 = dout("dbg_p1", [128, 128])
    a_sc = nc.dram_tensor("a_sc", [17 * 128, 512], F32, kind=skind).ap()
    m_sc = nc.dram_tensor("m_sc", [17 * 128, 512], F32, kind=skind).ap()
    g_sc = nc.dram_tensor("g_sc", [33 * 128, 512], F32, kind=skind).ap()
    x2_sc = nc.dram_tensor("x2_sc", [17 * 128, D], F32, kind=skind).ap()
    a_sc_b = [Buf() for _ in range(17)]
    m_sc_b = [Buf() for _ in range(17)]
    g_sc_b = [Buf() for _ in range(33)]
    x2_sc_b = [Buf() for _ in range(17)]
    outs_done = []

    with contextlib.ExitStack() as glob:
        def sb(st, name, shape, dt=F32):
            return T(st.enter_context(nc.sbuf_tensor("sb_" + name, list(shape), dt)), name)

        psum = [T(glob.enter_context(nc.psum_tensor("ps%d" % i, [128, 512], F32)), "ps%d" % i)
                for i in range(8)]
        ps_i = [0]
        ps_hold = set()

        def ps(hold=False):
            while True:
                idx = ps_i[0] % 8
                ps_i[0] += 1
                if idx not in ps_hold:
                    break
            if hold:
                ps_hold.add(idx)
            return psum[idx]

        def ps_release(p):
            ps_hold.discard(psum.index(p))

        ident = sb(glob, "ident", [128, 128])
        gains = sb(glob, "gains", [128, 24])
        hv = sb(glob, "hv", [128, 576])
        k.dma("sp", ident[:], ident_d[:, :], writes=[ident.b])
        k.dma("sp", gains[:], gains_d[:, :], writes=[gains.b])
        k.dma("sp", hv[:], hv_d[0:1, :].partition_broadcast(128), writes=[hv.b])

        cmask = sb(glob, "cmask", [128, 16, 128], BF16)
        selw = sb(glob, "selw", [128, 64])
        k.dma("pool", cmask[:], cmask_d[:, :].rearrange("p (b t) -> p b t", b=16), writes=[cmask.b])
        k.dma("sp", selw[:], selw_d[:, :], writes=[selw.b])
        MKT = sb(glob, "MKT", [128, 4, 256], BF16)
        MVa = sb(glob, "MVa", [128, 2, 4, 129], BF16)
        k.op("pool", lambda e: e.memset(MVa[:], 1.0), writes=[MVa.b])
        zb = sb(glob, "zb", [128, 512], BF16)
        k.op("pool", lambda e: e.memset(zb[:], 0.0), writes=[zb.b])

        def ps_zero(p):
            k.op("pe", lambda e: e.matmul(p[:, :], lhsT=zb[:, 0:128], rhs=zb[:, :], start=True, stop=False),
                 reads=[zb.b], writes=[p.b])


        def kside_store(kk_ap, vv_ap, ii_ap, rd, KT_ap, IKT_ap, VA_ap, wr):
            if kk_ap is not None:
                p = ps()
                for g in range(4):
                    k.op("pe", lambda e, g=g: e.transpose(p[0:64, g * 128:(g + 1) * 128],
                                                          kk_ap[:, g * 64:(g + 1) * 64], ident[:]),
                         reads=rd + [ident.b], writes=[p.b])
                k.op("act", lambda e: e.activation(out=KT_ap, in_=p[0:64, :].rearrange("p (g s) -> p g s", g=4),
                                                   func=AF.Copy), reads=[p.b], writes=wr)
            if ii_ap is not None:
                p2 = ps()
                k.op("pe", lambda e: e.transpose(p2[0:64, 0:128], ii_ap, ident[:]),
                     reads=rd + [ident.b], writes=[p2.b])
                k.op("act", lambda e: e.activation(out=IKT_ap, in_=p2[0:64, 0:128], func=AF.Copy),
                     reads=[p2.b], writes=wr)
            if vv_ap is not None:
                k.op("pool", lambda e: e.tensor_copy(VA_ap, vv_ap.rearrange("p (g d) -> p g d", g=4)),
                     reads=rd, writes=wr)

        def pipe_ahead(gens, depth):
            n = len(gens)

            def toB(g):
                for tok in g:
                    if tok == "B":
                        return
            for j_ in range(min(depth, n)):
                toB(gens[j_])
            for j_ in range(n):
                for _ in gens[j_]:
                    pass
                if j_ + depth < n:
                    toB(gens[j_ + depth])

        def load_xT(st_tiles, src_ap, gain_col, q="sp", rd=()):
            x32, xn, xT, stat = st_tiles
            k.dma(q, x32[:], src_ap, reads=list(rd), writes=[x32.b])
            k.op("act", lambda e: e.activation(out=xn[:], in_=x32[:], func=AF.Square,
                                               accum_out=stat[:, 0:1]),
                 reads=[x32.b], writes=[xn.b, stat.b])
            k.op("dve", lambda e: e.tensor_scalar(stat[:, 1:2], stat[:, 0:1], 1.0 / D, EPS,
                                                  op0=ALU.mult, op1=ALU.add),
                 reads=[stat.b], writes=[stat.b])
            k.op("act", lambda e: e.activation(out=stat[:, 3:4], in_=stat[:, 1:2], func=AF.Sqrt),
                 reads=[stat.b], writes=[stat.b])
            k.op("dve", lambda e: e.reciprocal(stat[:, 2:3], stat[:, 3:4]),
                 reads=[stat.b], writes=[stat.b])
            k.op("act", lambda e: e.activation(out=xn[:], in_=x32[:], func=AF.Copy,
                                               scale=stat[:, 2:3]),
                 reads=[x32.b, stat.b], writes=[xn.b])
            for half in range(2):
                p = ps()
                for j in range(4):
                    kc = half * 4 + j
                    k.op("pe", lambda e, kc=kc, j=j: e.transpose(
                        p[:, j * 128:(j + 1) * 128], xn[:, kc * 128:(kc + 1) * 128], ident[:]),
                        reads=[xn.b, ident.b], writes=[p.b])
                g = gains[:, gain_col + half * 4: gain_col + half * 4 + 4]
                k.op("dve", lambda e, half=half, g=g, p=p: e.tensor_tensor(
                    xT[:, half * 4:(half + 1) * 4, :],
                    p[:, :].rearrange("p (a b) -> p a b", a=4),
                    g.unsqueeze(2).to_broadcast([128, 4, 128]), op=ALU.mult),
                    reads=[p.b, gains.b], writes=[xT.b])
            return x32, xT

        def linear(xT, W, c0, ncol, p, kcs=KC):
            for kc in range(kcs):
                k.op("pe", lambda e, kc=kc: e.matmul(
                    p[:, 0:ncol], lhsT=xT[:, kc, :], rhs=W[:, kc, c0:c0 + ncol],
                    start=(kc == 0), stop=(kc == kcs - 1)),
                    reads=[xT.b, W.b], writes=[p.b])

        def load_w(W, src, c0, ncol, dst0=0, rows=D):
            nkc = rows // 128
            for kc in range(nkc):
                k.dma("pool", W[:, kc, dst0:dst0 + ncol], src[kc * 128:(kc + 1) * 128, c0:c0 + ncol],
                      writes=[W.b])

        def head_rmsnorm(st, src_ps, nh, dh, gain_ap, out32, tmp, stat, name):
            k.op("act", lambda e: e.activation(out=tmp[:, 0:nh * dh], in_=src_ps, func=AF.Square),
                 reads=[st], writes=[tmp.b])
            k.op("dve", lambda e: e.tensor_reduce(
                stat[:, 0:nh], tmp[:, 0:nh * dh].rearrange("p (h d) -> p h d", h=nh),
                axis=AX.X, op=ALU.add), reads=[tmp.b], writes=[stat.b])
            k.op("dve", lambda e: e.tensor_scalar(stat[:, 8:8 + nh], stat[:, 0:nh], 1.0 / dh, EPS,
                                                  op0=ALU.mult, op1=ALU.add),
                 reads=[stat.b], writes=[stat.b])
            k.op("act", lambda e: e.activation(out=stat[:, 0:nh], in_=stat[:, 8:8 + nh], func=AF.Sqrt),
                 reads=[stat.b], writes=[stat.b])
            k.op("dve", lambda e: e.reciprocal(stat[:, 16:16 + nh], stat[:, 0:nh]),
                 reads=[stat.b], writes=[stat.b])
            k.op("dve", lambda e: e.tensor_tensor(
                out32[:, 0:nh * dh].rearrange("p (h d) -> p h d", h=nh),
                src_ps.rearrange("p (h d) -> p h d", h=nh),
                stat[:, 16:16 + nh].unsqueeze(2).to_broadcast([128, nh, dh]), op=ALU.mult),
                reads=[st, stat.b], writes=[out32.b])
            k.op("dve", lambda e: e.tensor_tensor(
                out32[:, 0:nh * dh].rearrange("p (h d) -> p h d", h=nh),
                out32[:, 0:nh * dh].rearrange("p (h d) -> p h d", h=nh),
                gain_ap.unsqueeze(1).to_broadcast([128, nh, dh]), op=ALU.mult),
                reads=[out32.b, hv.b], writes=[out32.b])

        PH = cfg.get('phases', 'BCGEFD')
        with contextlib.ExitStack() as st:
          if 'B' in PH:
              Wm = sb(st, "Wm", [128, KC, 1024], BF16)
              load_w(Wm, w_mem_kv, 0, 1024)
              tiles = (sb(st, "mx32", [128, D]), sb(st, "mxn", [128, D]),
                       sb(st, "mxT", [128, KC, 128], BF16), sb(st, "mstat", [128, 4]))
              tmp = sb(st, "mtmp", [128, 512])
              hstat = sb(st, "mhstat", [128, 24])
              mk32 = [sb(st, "mk32_%d" % i, [128, 512]) for i in range(2)]
              mv32 = [sb(st, "mv32_%d" % i, [128, 512]) for i in range(2)]
              for mt in range(2):
                  _, xT = load_xT(tiles, mem[mt * 128:(mt + 1) * 128, :], 8)
                  pk = ps()
                  linear(xT, Wm, 0, 512, pk)
                  pv = ps()
                  linear(xT, Wm, 512, 512, pv)
                  head_rmsnorm(pk.b, pk[:, :], 4, 128, hv[:, 256:384], mk32[mt], tmp, hstat, "mk")
                  k.op("act", lambda e, mt=mt, pv=pv: e.activation(out=mv32[mt][:], in_=pv[:, :], func=AF.Copy),
                       reads=[pv.b], writes=[mv32[mt].b])
                  p = ps()
                  for h in range(4):
                      k.op("pe", lambda e, h=h, p=p, mt=mt: e.transpose(
                          p[:, h * 128:(h + 1) * 128], mk32[mt][:, h * 128:(h + 1) * 128], ident[:]),
                          reads=[mk32[mt].b, ident.b], writes=[p.b])
                  k.op("act", lambda e, p=p, mt=mt: e.activation(
                      out=MKT[:, :, mt * 128:(mt + 1) * 128], in_=p[:, :].rearrange("p (h m) -> p h m", h=4),
                      func=AF.Copy), reads=[p.b], writes=[MKT.b])
                  k.op("dve", lambda e, mt=mt: e.tensor_copy(
                      MVa[:, mt, :, 0:128], mv32[mt][:, :].rearrange("p (h d) -> p h d", h=4)),
                      reads=[mv32[mt].b], writes=[MVa.b])
                  k.dma("sp", o_pmk[mt * 128:(mt + 1) * 128, :], mk32[mt][:], reads=[mk32[mt].b])
                  k.dma("sp", o_pmv[mt * 128:(mt + 1) * 128, :], mv32[mt][:], reads=[mv32[mt].b])
                  outs_done += [mk32[mt].b, mv32[mt].b]

        k.barrier()
        with contextlib.ExitStack() as st:
          if 'G' in PH:
              Wg2 = sb(st, "Wg2", [128, KC, 2056], BF16)
              load_w(Wg2, w_in, O_GQKV, 2056, 0)
              gcs = sb(st, "gcs", [128, 10 * 128 + 16 + 2048])
              k.dma("sp", gcs[:], gconst_d[:, :], writes=[gcs.b])
              cv = lambda i: gcs[:, i * 128:(i + 1) * 128]
              LTRI, BONES, SLm, SUIm = [cv(0), cv(1)], [cv(2), cv(3)], [cv(4), cv(5)], [cv(6), cv(7)]
              USTR, ONES = cv(8), cv(9)
              G8 = gcs[:, 1280:1296]
              CM32 = gcs[:, 1296:1296 + 2048].rearrange("p (b t) -> p b t", b=16)
              gcv = sb(st, "gcv", [128, 48])
              k.dma("sp", gcv[:], gconvT_d[:, :], writes=[gcv.b])
              Dg = sb(st, "Dg", [128, 12, 4, 128], BF16)
              for cc in range(12):
                  for jj in range(4):
                      k.op("dve", lambda e, cc=cc, jj=jj: e.tensor_scalar(
                          Dg[:, cc, jj, :], ident[:], gcv[:, cc * 4 + jj:cc * 4 + jj + 1], None, op0=ALU.mult),
                          reads=[ident.b, gcv.b], writes=[Dg.b])
              negA = sb(st, "negA", [128, 4])
              k.op("act", lambda e: e.activation(out=negA[:], in_=hv[:, 516:520], func=AF.Exp),
                   reads=[hv.b], writes=[negA.b])
              k.op("dve", lambda e: e.tensor_scalar(negA[:], negA[:], -1.0, None, op0=ALU.mult),
                   reads=[negA.b], writes=[negA.b])
              Sst = [sb(st, "Sst%d" % h, [128, 128]) for h in range(4)]
              for h in range(4):
                  k.op("pool", lambda e, h=h: e.memset(Sst[h][:], 0.0), writes=[Sst[h].b])
              tsets = [(sb(st, "gx32_%d" % i, [128, D]), sb(st, "gxn_%d" % i, [128, D]),
                        sb(st, "gxT_%d" % i, [128, KC, 128], BF16), sb(st, "gstat_%d" % i, [128, 4]))
                       for i in range(1)] * 2
              Up = sb(st, "Up", [128, 12, 131], BF16)
              k.op("pool", lambda e: e.memset(Up[:], 0.0), writes=[Up.b])
              UC = sb(st, "UC", [128, 12, 128])
              TM = sb(st, "TM", [128, 1536])
              sq = sb(st, "gsq", [128, 1024])
              sgz2 = [sb(st, "sgz%d" % q, [128, 512]) for q in range(1)]
              sm2 = [sb(st, "gsm%d" % q, [128, 64]) for q in range(1)]
              scal = sb(st, "gscal", [128, 6, 4])
              KQ = sb(st, "KQ", [128, 12, 128])
              KQT2 = [sb(st, "KQT%d" % q, [128, 12, 128]) for q in range(1)]
              KBG2 = [sb(st, "KBG%d" % q, [128, 4, 128]) for q in range(1)]
              KTL2 = [sb(st, "KTL%d" % q, [128, 4, 128]) for q in range(1)]
              VB2 = [sb(st, "VB%d" % q, [128, 4, 128]) for q in range(1)]
              O32 = sb(st, "O32", [128, 512])
              go32 = sb(st, "go32", [128, 512])
              gtmp = sb(st, "ggtmp", [128, 512])
              ghstat = sb(st, "ghstat", [128, 24])
              hb = [dict(GU=sb(st, "GU%d" % i, [128, 128]), G2=sb(st, "G2%d" % i, [128, 256]),
                         t1=sb(st, "t1%d" % i, [128, 128]), aq=sb(st, "aqkT%d" % i, [128, 128]),
                         P=[sb(st, "P%d_%d" % (i, q), [128, 256]) for q in range(2)],
                         TT=[sb(st, "TT%d_%d" % (i, q), [128, 128]) for q in range(2)],
                         UW=sb(st, "UW%d" % i, [128, 256]), vn=sb(st, "vn%d" % i, [128, 128]))
                    for i in range(4)]
              DK5 = 128.0 ** -0.5

              def gdn_tile(j, sm_st=None):
                  smp = (j == NT_ALL)
                  m = 1 if smp else 0
                  bf_ = j % 2
                  sgz, KQT, KBG, KTL, VB, sm = sgz2[bf_], KQT2[bf_], KBG2[bf_], KTL2[bf_], VB2[bf_], sm2[bf_]
                  src = x_smp[:, :] if smp else x_all[j * 128:(j + 1) * 128, :]
                  x32, xT = load_xT(tsets[j % 2], src, 0)
                  yield
                  U = sm_st["Us"] if smp else Up
                  for cc0 in (0, 4, 8):
                      p = ps()
                      for c4 in range(4):
                          cc = cc0 + c4
                          for kc in range(KC):
                              k.op("pe", lambda e, kc=kc, cc=cc, c4=c4, p=p: e.matmul(
                                  p[:, c4 * 128:(c4 + 1) * 128], lhsT=Wg2[:, kc, cc * 128:(cc + 1) * 128],
                                  rhs=xT[:, kc, :], start=(kc == 0), stop=(kc == KC - 1)),
                                  reads=[Wg2.b, xT.b], writes=[p.b])
                      if smp:
                          for c4 in range(4):
                              k.op("act", lambda e, p=p, cc0=cc0, c4=c4: e.activation(
                                  out=U[:, cc0 + c4, :, 3:11],
                                  in_=p[:, c4 * 128:(c4 + 1) * 128].rearrange("p (b t) -> p b t", b=16),
                                  func=AF.Copy), reads=[p.b], writes=[U.b])
                      else:
                          k.op("act", lambda e, p=p, cc0=cc0: e.activation(
                              out=U[:, cc0:cc0 + 4, 3:131], in_=p[:, :].rearrange("p (a t) -> p a t", a=4),
                              func=AF.Copy), reads=[p.b], writes=[U.b])
                      yield
                  for cc0 in (0, 4, 8):
                      p = ps()
                      for c4 in range(4):
                          cc = cc0 + c4
                          for jj in range(4):
                              rhs = U[:, cc, :, jj:jj + 8] if smp else U[:, cc, jj:jj + 128]
                              k.op("pe", lambda e, jj=jj, cc=cc, c4=c4, p=p, rhs=rhs: e.matmul(
                                  p[:, c4 * 128:(c4 + 1) * 128], lhsT=Dg[:, cc, jj, :], rhs=rhs,
                                  start=(jj == 0), stop=(jj == 3)), reads=[Dg.b, U.b], writes=[p.b])
                      k.op("act", lambda e, p=p, cc0=cc0: e.activation(
                          out=UC[:, cc0:cc0 + 4, :], in_=p[:, :].rearrange("p (a t) -> p a t", a=4),
                          func=AF.Silu), reads=[p.b], writes=[UC.b])
                      yield
                  if not smp:
                      k.op("dve", lambda e: e.tensor_copy(U[:, :, 0:3], U[:, :, 128:131]),
                           reads=[U.b], writes=[U.b])
                  for cc0 in (0, 4, 8):
                      p = ps()
                      for c4 in range(4):
                          k.op("pe", lambda e, cc0=cc0, c4=c4, p=p: e.transpose(
                              p[:, c4 * 128:(c4 + 1) * 128], UC[:, cc0 + c4, :], ident[:]),
                              reads=[UC.b, ident.b], writes=[p.b])
                      k.op("act", lambda e, p=p, cc0=cc0: e.activation(
                          out=TM[:, cc0 * 128:(cc0 + 4) * 128], in_=p[:, :], func=AF.Copy),
                          reads=[p.b], writes=[TM.b])
                      yield
                  pgz = ps()
                  linear(xT, Wg2, 1536, 512, pgz)
                  pgb = ps()
                  linear(xT, Wg2, 2048, 8, pgb)
                  k.op("act", lambda e: e.activation(out=sgz[:], in_=pgz[:, :], func=AF.Silu),
                       reads=[pgz.b], writes=[sgz.b])
                  k.op("act", lambda e: e.activation(out=sm[:, 0:4], in_=pgb[:, 0:4], func=AF.Sigmoid),
                       reads=[pgb.b], writes=[sm.b])
                  k.op("dve", lambda e: e.tensor_tensor(sm[:, 4:8], pgb[:, 4:8], hv[:, 512:516], op=ALU.add),
                       reads=[pgb.b, hv.b], writes=[sm.b])
                  k.op("act", lambda e: e.activation(out=sm[:, 8:12], in_=sm[:, 4:8], func=AF.Exp),
                       reads=[sm.b], writes=[sm.b])
                  k.op("act", lambda e: e.activation(out=sm[:, 12:16], in_=sm[:, 8:12], func=AF.Ln, bias=1.0),
                       reads=[sm.b], writes=[sm.b])
                  k.op("dve", lambda e: e.tensor_tensor(sm[:, 16:20], sm[:, 12:16], negA[:], op=ALU.mult),
                       reads=[sm.b, negA.b], writes=[sm.b])
                  gt = sm[:, 16:20]
                  yield
                  k.op("act", lambda e: e.activation(out=sq[:], in_=TM[:, 0:1024], func=AF.Square),
                       reads=[TM.b], writes=[sq.b])
                  k.op("dve", lambda e: e.tensor_reduce(
                      sm[:, 20:28], sq[:, :].rearrange("p (h d) -> p h d", h=8), axis=AX.X, op=ALU.add),
                      reads=[sq.b], writes=[sm.b])
                  k.op("dve", lambda e: e.tensor_scalar(sm[:, 20:28], sm[:, 20:28], EPS, None, op0=ALU.add),
                       reads=[sm.b], writes=[sm.b])
                  k.op("act", lambda e: e.activation(out=sm[:, 28:36], in_=sm[:, 20:28], func=AF.Sqrt),
                       reads=[sm.b], writes=[sm.b])
                  k.op("dve", lambda e: e.reciprocal(sm[:, 36:44], sm[:, 28:36]), reads=[sm.b], writes=[sm.b])
                  rq, rk = sm[:, 36:40], sm[:, 40:44]
                  pc = ps()
                  k.op("pe", lambda e: e.matmul(pc[:, 0:4], lhsT=LTRI[m], rhs=gt, start=True, stop=True),
                       reads=[gcs.b, sm.b], writes=[pc.b])
                  k.op("pe", lambda e: e.matmul(pc[:, 4:8], lhsT=BONES[m], rhs=gt, start=True, stop=True),
                       reads=[gcs.b, sm.b], writes=[pc.b])
                  k.op("dve", lambda e: e.tensor_copy(sm[:, 44:52], pc[:, 0:8]), reads=[pc.b], writes=[sm.b])
                  k.op("act", lambda e: e.activation(out=sm[:, 52:56], in_=sm[:, 44:48], func=AF.Exp),
                       reads=[sm.b], writes=[sm.b])
                  k.op("dve", lambda e: e.tensor_tensor(sm[:, 56:60], sm[:, 48:52], sm[:, 44:48], op=ALU.subtract),
                       reads=[sm.b], writes=[sm.b])
                  k.op("act", lambda e: e.activation(out=sm[:, 56:60], in_=sm[:, 56:60], func=AF.Exp),
                       reads=[sm.b], writes=[sm.b])
                  k.op("act", lambda e: e.activation(out=sm[:, 60:64], in_=sm[:, 48:52], func=AF.Exp),
                       reads=[sm.b], writes=[sm.b])
                  egc, etl, dec, beta = sm[:, 52:56], sm[:, 56:60], sm[:, 60:64], sm[:, 0:4]
                  k.op("dve", lambda e: e.tensor_copy(scal[:, 0, :], rq), reads=[sm.b], writes=[scal.b])
                  k.op("dve", lambda e: e.scalar_tensor_tensor(out=scal[:, 1, :], in0=rq, scalar=DK5, in1=egc,
                                                               op0=ALU.mult, op1=ALU.mult),
                       reads=[sm.b], writes=[scal.b])
                  k.op("dve", lambda e: e.tensor_copy(scal[:, 2, :], rk), reads=[sm.b], writes=[scal.b])
                  k.op("dve", lambda e: e.tensor_tensor(scal[:, 5, :], rk, beta, op=ALU.mult),
                       reads=[sm.b], writes=[scal.b])
                  k.op("dve", lambda e: e.tensor_tensor(scal[:, 3, :], scal[:, 5, :], egc, op=ALU.mult),
                       reads=[sm.b, scal.b], writes=[scal.b])
                  k.op("dve", lambda e: e.tensor_tensor(scal[:, 4, :], rk, etl, op=ALU.mult),
                       reads=[sm.b], writes=[scal.b])
                  yield
                  TMq = TM[:, 0:512].rearrange("p (h d) -> p h d", h=4)
                  TMk = TM[:, 512:1024].rearrange("p (h d) -> p h d", h=4)
                  TMv = TM[:, 1024:1536].rearrange("p (h d) -> p h d", h=4)
                  bc = lambda a: a.unsqueeze(2).to_broadcast([128, 4, 128])
                  for dst, src_, sc_ in ((KQ[:, 0:4, :], TMq, scal[:, 0, :]), (KQ[:, 4:8, :], TMk, scal[:, 2, :]),
                                         (KQ[:, 8:12, :], TMq, scal[:, 1, :]), (KBG[:], TMk, scal[:, 3, :]),
                                         (KTL[:], TMk, scal[:, 4, :]), (VB[:], TMv, beta)):
                      wb_ = KQ.b if dst.tensor.name == KQ[:].tensor.name else (
                          KBG.b if dst.tensor.name == KBG[:].tensor.name else (
                              KTL.b if dst.tensor.name == KTL[:].tensor.name else VB.b))
                      k.op("dve", lambda e, dst=dst, src_=src_, sc_=sc_: e.tensor_tensor(
                          dst, src_, bc(sc_), op=ALU.mult), reads=[TM.b, scal.b, sm.b], writes=[wb_])
                  for cc0 in (0, 4, 8):
                      p = ps()
                      for c4 in range(4):
                          k.op("pe", lambda e, cc0=cc0, c4=c4, p=p: e.transpose(
                              p[:, c4 * 128:(c4 + 1) * 128], KQ[:, cc0 + c4, :], ident[:]),
                              reads=[KQ.b, ident.b], writes=[p.b])
                      k.op("act", lambda e, p=p, cc0=cc0: e.activation(
                          out=KQT[:, cc0:cc0 + 4, :], in_=p[:, :].rearrange("p (a t) -> p a t", a=4),
                          func=AF.Copy), reads=[p.b], writes=[KQT.b])
                      yield
                  if smp:
                      rhs3 = sm_st["rhs3"]
                      k.op("dve", lambda e: e.tensor_tensor(
                          rhs3[:], gt.unsqueeze(1).to_broadcast([128, 16, 4]),
                          G8.unsqueeze(2).to_broadcast([128, 16, 4]), op=ALU.mult),
                          reads=[sm.b, gcs.b], writes=[rhs3.b])
                      pdb = ps()
                      k.op("pe", lambda e: e.matmul(pdb[:, 0:64], lhsT=ONES,
                                                    rhs=rhs3[:].rearrange("p b h -> p (b h)"), start=True, stop=True),
                           reads=[gcs.b, rhs3.b], writes=[pdb.b])
                      decB = sm_st["decB"]
                      k.op("act", lambda e: e.activation(out=decB[:], in_=pdb[:, 0:64], func=AF.Exp),
                           reads=[pdb.b], writes=[decB.b])
                  def head_gen(h):
                      H = hb[h]
                      GU, G2, t1, aq, UW, vn = H["GU"], H["G2"], H["t1"], H["aq"], H["UW"], H["vn"]
                      KnT, QnT, DQT = KQT[:, 4 + h, :], KQT[:, h, :], KQT[:, 8 + h, :]
                      k.op("dve", lambda e: e.tensor_scalar(GU[:], USTR, gt[:, h:h + 1], None, op0=ALU.mult),
                           reads=[gcs.b, sm.b], writes=[GU.b])
                      pD = ps()
                      k.op("pe", lambda e: e.matmul(pD[:, 0:128], lhsT=LTRI[m], rhs=GU[:], start=True, stop=True),
                           reads=[gcs.b, GU.b], writes=[pD.b])
                      k.op("pe", lambda e: e.matmul(pD[:, 128:256], lhsT=GU[:], rhs=LTRI[m], start=True, stop=True),
                           reads=[gcs.b, GU.b], writes=[pD.b])
                      k.op("pe", lambda e: e.matmul(pD[:, 256:384], lhsT=KnT, rhs=KnT, start=True, stop=True),
                           reads=[KQT.b], writes=[pD.b])
                      k.op("pe", lambda e: e.matmul(pD[:, 384:512], lhsT=KnT, rhs=QnT, start=True, stop=True),
                           reads=[KQT.b], writes=[pD.b])
                      yield
                      k.op("act", lambda e: e.activation(out=G2[:], in_=pD[:, 0:256], func=AF.Exp),
                           reads=[pD.b], writes=[G2.b])
                      yield
                      k.op("dve", lambda e: e.tensor_tensor(t1[:], pD[:, 256:384], G2[:, 0:128], op=ALU.mult),
                           reads=[pD.b, G2.b], writes=[t1.b])
                      P0 = H["P"][0]
                      k.op("dve", lambda e: e.tensor_scalar(t1[:], t1[:], beta[:, h:h + 1], -1.0,
                                                            op0=ALU.mult, op1=ALU.mult),
                           reads=[t1.b, sm.b], writes=[t1.b])
                      k.op("dve", lambda e: e.tensor_tensor(P0[:, 0:128], t1[:], SLm[m], op=ALU.mult),
                           reads=[t1.b, gcs.b], writes=[P0.b])
                      k.op("dve", lambda e: e.tensor_tensor(t1[:], pD[:, 384:512], G2[:, 128:256], op=ALU.mult),
                           reads=[pD.b, G2.b], writes=[t1.b])
                      k.op("dve", lambda e: e.tensor_tensor(aq[:], t1[:], SUIm[m], op=ALU.mult),
                           reads=[t1.b, gcs.b], writes=[aq.b])
                      yield
                      pN = ps()
                      k.op("pe", lambda e: e.transpose(pN[:, 0:128], P0[:, 0:128], ident[:]),
                           reads=[P0.b, ident.b], writes=[pN.b])
                      k.op("act", lambda e: e.activation(out=P0[:, 128:256], in_=pN[:, 0:128], func=AF.Copy),
                           reads=[pN.b], writes=[P0.b])
                      TTc = H["TT"][0]
                      k.op("dve", lambda e: e.tensor_tensor(TTc[:], P0[:, 128:256], ident[:], op=ALU.add),
                           reads=[P0.b, ident.b], writes=[TTc.b])
                      yield
                      Pc = P0
                      for n in range(1, 7):
                          Pn = H["P"][n % 2]
                          TTn = H["TT"][n % 2]
                          pP = ps()
                          k.op("pe", lambda e, Pc=Pc, pP=pP: e.matmul(pP[:, 0:128], lhsT=Pc[:, 128:256], rhs=Pc[:, 0:128],
                                                                      start=True, stop=True),
                               reads=[Pc.b], writes=[pP.b])
                          if n < 6:
                              k.op("pe", lambda e, Pc=Pc, pP=pP: e.matmul(pP[:, 128:256], lhsT=Pc[:, 0:128],
                                                                          rhs=Pc[:, 128:256], start=True, stop=True),
                                   reads=[Pc.b], writes=[pP.b])
                          k.op("act", lambda e, Pn=Pn, pP=pP: e.activation(out=Pn[:], in_=pP[:, 0:256], func=AF.Copy),
                               reads=[pP.b], writes=[Pn.b])
                          yield
                          pT = ps()
                          k.op("pe", lambda e, Pn=Pn, pT=pT, TTc=TTc: e.matmul(pT[:, 0:128], lhsT=Pn[:, 0:128], rhs=TTc[:],
                                                                               start=True, stop=True),
                               reads=[Pn.b, TTc.b], writes=[pT.b])
                          k.op("dve", lambda e, TTn=TTn, TTc=TTc, pT=pT: e.tensor_tensor(
                              TTn[:], TTc[:], pT[:, 0:128], op=ALU.add), reads=[TTc.b, pT.b], writes=[TTn.b])
                          yield
                          Pc, TTc = Pn, TTn
                      pU = ps()
                      k.op("pe", lambda e, TTc=TTc: e.matmul(pU[:, 0:128], lhsT=TTc[:], rhs=VB[:, h, :], start=True, stop=True),
                           reads=[TTc.b, VB.b], writes=[pU.b])
                      k.op("pe", lambda e, TTc=TTc: e.matmul(pU[:, 128:256], lhsT=KBG[:, h, :], rhs=TTc[:], start=True, stop=True),
                           reads=[TTc.b, KBG.b], writes=[pU.b])
                      k.op("act", lambda e: e.activation(out=UW[:], in_=pU[:, 0:256], func=AF.Copy),
                           reads=[pU.b], writes=[UW.b])
                      yield
                      if not smp:
                          S_ = Sst[h]
                          p1 = ps()
                          k.op("pe", lambda e: e.matmul(p1[:, 0:128], lhsT=UW[:, 128:256], rhs=S_[:], start=True, stop=True),
                               reads=[UW.b, S_.b], writes=[p1.b])
                          k.op("dve", lambda e: e.tensor_tensor(vn[:], UW[:, 0:128], p1[:, 0:128], op=ALU.subtract),
                               reads=[UW.b, p1.b], writes=[vn.b])
                          yield
                          p2 = ps()
                          k.op("pe", lambda e: e.matmul(p2[:, 0:128], lhsT=DQT, rhs=S_[:], start=True, stop=False),
                               reads=[KQT.b, S_.b], writes=[p2.b])
                          k.op("pe", lambda e: e.matmul(p2[:, 0:128], lhsT=aq[:], rhs=vn[:], start=False, stop=True),
                               reads=[aq.b, vn.b], writes=[p2.b])
                          k.op("act", lambda e: e.activation(out=O32[:, h * 128:(h + 1) * 128], in_=p2[:, 0:128], func=AF.Copy),
                               reads=[p2.b], writes=[O32.b])
                          p3 = ps()
                          k.op("pe", lambda e: e.matmul(p3[:, 0:128], lhsT=KTL[:, h, :], rhs=vn[:], start=True, stop=True),
                               reads=[KTL.b, vn.b], writes=[p3.b])
                          k.op("dve", lambda e: e.scalar_tensor_tensor(out=S_[:], in0=S_[:], scalar=dec[:, h:h + 1],
                                                                       in1=p3[:, 0:128], op0=ALU.mult, op1=ALU.add),
                               reads=[S_.b, sm.b, p3.b], writes=[S_.b])
                      else:
                          Ssm, WTm, DQm, vm, So = (sm_st["Ssm"], sm_st["WTm"], sm_st["DQm"], sm_st["vm"], sm_st["So"])
                          decB = sm_st["decB"]
                          k.op("dve", lambda e: e.tensor_tensor(
                              WTm[:], UW[:, 128:256].unsqueeze(1).to_broadcast([128, 16, 128]), CM32, op=ALU.mult),
                              reads=[UW.b, gcs.b], writes=[WTm.b])
                          p1 = ps(hold=True)
                          for b in range(16):
                              k.op("pe", lambda e, b=b: e.matmul(p1[:, 0:128], lhsT=WTm[:, b, :], rhs=Ssm[:, b * 4 + h, :],
                                                                 start=(b == 0), stop=(b == 15)),
                                   reads=[WTm.b, Ssm.b], writes=[p1.b])
                          k.op("dve", lambda e: e.tensor_tensor(vn[:], UW[:, 0:128], p1[:, 0:128], op=ALU.subtract),
                               reads=[UW.b, p1.b], writes=[vn.b])
                          ps_release(p1)
                          k.op("dve", lambda e: e.tensor_tensor(
                              DQm[:], DQT.unsqueeze(1).to_broadcast([128, 16, 128]), CM32, op=ALU.mult),
                              reads=[KQT.b, gcs.b], writes=[DQm.b])
                          p2 = ps(hold=True)
                          for b in range(16):
                              k.op("pe", lambda e, b=b: e.matmul(p2[:, 0:128], lhsT=DQm[:, b, :], rhs=Ssm[:, b * 4 + h, :],
                                                                 start=(b == 0), stop=False),
                                   reads=[DQm.b, Ssm.b], writes=[p2.b])
                          k.op("pe", lambda e: e.matmul(p2[:, 0:128], lhsT=aq[:], rhs=vn[:], start=False, stop=True),
                               reads=[aq.b, vn.b], writes=[p2.b])
                          k.op("act", lambda e: e.activation(out=O32[:, h * 128:(h + 1) * 128], in_=p2[:, 0:128], func=AF.Copy),
                               reads=[p2.b], writes=[O32.b])
                          ps_release(p2)
                          k.op("dve", lambda e: e.tensor_tensor(
                              vm[:], vn[:].unsqueeze(1).to_broadcast([128, 16, 128]),
                              G8.unsqueeze(2).to_broadcast([128, 16, 128]), op=ALU.mult),
                              reads=[vn.b, gcs.b], writes=[vm.b])
                          for b in range(16):
                              p3 = ps()
                              k.op("pe", lambda e, b=b, p3=p3: e.matmul(p3[:, 0:128], lhsT=KTL[:, h, :], rhs=vm[:, b, :],
                                                                        start=True, stop=True),
                                   reads=[KTL.b, vm.b], writes=[p3.b])
                              so = So[b % 2]
                              k.op("dve", lambda e, b=b, p3=p3, so=so: e.scalar_tensor_tensor(
                                  out=so[:], in0=Ssm[:, b * 4 + h, :], scalar=decB[:, b * 4 + h:b * 4 + h + 1],
                                  in1=p3[:, 0:128], op0=ALU.mult, op1=ALU.add),
                                  reads=[Ssm.b, decB.b, p3.b], writes=[so.b])
                              k.dma("sp", o_sgdn[b, h], so[:], reads=[so.b])
                  if smp and DBG:
                      k.dma("sp", dbg_tm[:, :], TM[:], reads=[TM.b])
                      k.dma("sp", dbg_sm[:, :], sm[:], reads=[sm.b])
                      k.dma("sp", dbg_o[:, :], O32[:], reads=[O32.b])
                      H = hb[1]
                      k.dma("sp", dbg_uw[:, :], H["UW"][:], reads=[H["UW"].b])
                      k.dma("sp", dbg_vn[:, :], H["vn"][:], reads=[H["vn"].b])
                      k.dma("sp", dbg_aq[:, :], H["aq"][:], reads=[H["aq"].b])
                      k.dma("sp", dbg_tt[:, :], H["TT"][0][:], reads=[H["TT"][0].b])
                  yield "B"
                  if smp:
                      for h in range(4):
                          for _ in head_gen(h):
                              pass
                  else:
                      gens = [head_gen(h) for h in range(4)]
                      alive = [True] * 4
                      while any(alive):
                          for gi, g_ in enumerate(gens):
                              if alive[gi]:
                                  try:
                                      next(g_)
                                  except StopIteration:
                                      alive[gi] = False
                          yield
                  head_rmsnorm(O32.b, O32[:, :], 4, 128, hv[:, 384:512], go32, gtmp, ghstat, "go")
                  k.op("dve", lambda e: e.tensor_tensor(go32[:], go32[:], sgz[:], op=ALU.mult),
                       reads=[go32.b, sgz.b], writes=[go32.b])
                  k.dma("sp", g_sc[j * 128:(j + 1) * 128, :], go32[:], reads=[go32.b], writes=[g_sc_b[j]])

              with contextlib.ExitStack() as st2:
                  sm_st = dict(Us=sb(st2, "Us", [128, 12, 16, 11], BF16), Ssm=sb(st2, "Ssm", [128, 64, 128]),
                               WTm=sb(st2, "WTm", [128, 16, 128]), rhs3=sb(st2, "rhs3", [128, 16, 4]),
                               decB=sb(st2, "decB", [128, 64]),
                               So=[sb(st2, "So%d" % i, [128, 128]) for i in range(2)])
                  sm_st["DQm"] = sm_st["WTm"]
                  sm_st["vm"] = sm_st["WTm"]
                  Ssm = sm_st["Ssm"]
                  for b in range(16):
                      k.dma("sp", Ssm[:, b * 4:(b + 1) * 4, :], sgdn_d[b].rearrange("h k v -> k h v"), writes=[Ssm.b])
                  cb = sb(st2, "cb", [48, 1536])
                  k.dma("sp", cb[:], sconv_d[:, :], writes=[cb.b])
                  Us = sm_st["Us"]
                  for cc0 in (0, 4, 8):
                      p = ps()
                      for c4 in range(4):
                          cc = cc0 + c4
                          k.op("pe", lambda e, cc=cc, c4=c4, p=p: e.transpose(
                              p[:, c4 * 48:(c4 + 1) * 48], cb[:, cc * 128:(cc + 1) * 128], ident[0:48, 0:48]),
                              reads=[cb.b, ident.b], writes=[p.b])
                      for c4 in range(4):
                          k.op("act", lambda e, cc0=cc0, c4=c4, p=p: e.activation(
                              out=Us[:, cc0 + c4, :, 0:3],
                              in_=p[:, c4 * 48:(c4 + 1) * 48].rearrange("p (b t) -> p b t", b=16),
                              func=AF.Copy), reads=[p.b], writes=[Us.b])
                  for _ in gdn_tile(NT_ALL, sm_st):
                      pass
                  outs_done += [s_.b for s_ in sm_st["So"]]
                  k.barrier()
              sgz2.append(sb(st, "sgz1", [128, 512]))
              KQT2.append(sb(st, "KQT1", [128, 12, 128]))
              KBG2.append(sb(st, "KBG1", [128, 4, 128]))
              KTL2.append(sb(st, "KTL1", [128, 4, 128]))
              VB2.append(sb(st, "VB1", [128, 4, 128]))
              sm2.append(sb(st, "gsm1", [128, 64]))
              gensG = [gdn_tile(j) for j in range(NT_ALL)]
              for tok in gensG[0]:
                  if tok == "B":
                      break
              for j in range(NT_ALL):
                  cur = gensG[j]
                  nxt = gensG[j + 1] if j + 1 < NT_ALL else None
                  cur_alive, nxt_inA = True, nxt is not None
                  while cur_alive or nxt_inA:
                      if cur_alive:
                          try:
                              next(cur)
                          except StopIteration:
                              cur_alive = False
                      if nxt_inA:
                          if next(nxt) == "B":
                              nxt_inA = False
              for h in range(4):
                  k.dma("sp", o_pgdn[h], Sst[h][:], reads=[Sst[h].b])
              outs_done += [s_.b for s_ in Sst]

        k.barrier()
        kst = contextlib.ExitStack()
        KT = sb(kst, "KT", [64, 4, SEQ + 256], BF16)
        VA = sb(kst, "VA", [128, NT_ALL + 2, 4, 65], BF16)
        IKT = sb(kst, "IKT", [64, SEQ + 256], BF16)
        KTn = sb(kst, "KTn", [64, 4, 128], BF16)
        IKTn = sb(kst, "IKTn", [64, 128], BF16)
        VAn = sb(kst, "VAn", [128, 4, 65], BF16)
        k.op("pool", lambda e: e.memset(VA[:], 1.0), writes=[VA.b])
        k.op("pool", lambda e: e.memset(VAn[:], 1.0), writes=[VAn.b])
        k.barrier()
        with contextlib.ExitStack() as st:
          if 'C' in PH:
              NCOL = 576 + 1536
              Wk = sb(st, "Wk", [128, KC, NCOL], BF16)
              load_w(Wk, w_in, O_AK, 512, 0)
              load_w(Wk, w_in, O_IK, 64, 512)
              load_w(Wk, w_in, O_GQKV, 1536, 576)
              tsets = [(sb(st, "cx32_%d" % i, [128, D]), sb(st, "cxn_%d" % i, [128, D]),
                        sb(st, "cxT_%d" % i, [128, KC, 128], BF16), sb(st, "cstat_%d" % i, [128, 4]))
                       for i in range(3)]
              tmp = sb(st, "ctmp", [128, 512])
              hstat = sb(st, "chstat", [128, 24])
              ko = [sb(st, "ko%d" % i, [128, 256]) for i in range(2)]
              vo = [sb(st, "vo%d" % i, [128, 256]) for i in range(2)]
              io = [sb(st, "io%d" % i, [128, 64]) for i in range(2)]
              gq = sb(st, "gq", [128, 1536])
              def c_gen(j):
                  smp = (j == NT_ALL)
                  src = x_smp[:, :] if smp else x_all[j * 128:(j + 1) * 128, :]
                  _, xT = load_xT(tsets[j % 3], src, 0)
                  yield "B"
                  pkv = ps()
                  linear(xT, Wk, 0, 512, pkv)
                  pik = ps()
                  linear(xT, Wk, 512, 64, pik)
                  kk, vv, ii = ko[j % 2], vo[j % 2], io[j % 2]
                  head_rmsnorm(pkv.b, pkv[:, 0:256], 4, 64, hv[:, 64:128], kk, tmp, hstat, "ak")
                  k.op("act", lambda e, vv=vv, pkv=pkv: e.activation(out=vv[:], in_=pkv[:, 256:512], func=AF.Copy),
                       reads=[pkv.b], writes=[vv.b])
                  k.op("act", lambda e, ii=ii, pik=pik: e.activation(out=ii[:], in_=pik[:, 0:64], func=AF.Copy),
                       reads=[pik.b], writes=[ii.b])
                  if smp:
                      kside_store(kk[:], vv[:], ii[:], [kk.b, vv.b, ii.b], KTn[:], IKTn[:], VAn[:, :, 0:64],
                                  [KTn.b, IKTn.b, VAn.b])
                  else:
                      kside_store(kk[:], vv[:], ii[:], [kk.b, vv.b, ii.b], KT[:, :, j * 128:(j + 1) * 128],
                                  IKT[:, j * 128:(j + 1) * 128], VA[:, j, :, 0:64], [KT.b, IKT.b, VA.b])
                  if smp:
                      k.dma("sp", o_sk[:, :], kk[:], reads=[kk.b])
                      k.dma("sp", o_sv[:, :], vv[:], reads=[vv.b])
                      k.dma("sp", o_sik[:, :], ii[:], reads=[ii.b])
                  else:
                      k.dma("sp", o_pk[j * 128:(j + 1) * 128, :], kk[:], reads=[kk.b])
                      k.dma("sp", o_pv[j * 128:(j + 1) * 128, :], vv[:], reads=[vv.b])
                      k.dma("sp", o_pik[j * 128:(j + 1) * 128, :], ii[:], reads=[ii.b])
                  if j >= NT_ALL - 1:
                      for c in range(3):
                          pg = ps()
                          linear(xT, Wk, 576 + c * 512, 512, pg)
                          k.op("act", lambda e, c=c, pg=pg: e.activation(
                              out=gq[:, c * 512:(c + 1) * 512], in_=pg[:, :], func=AF.Copy),
                              reads=[pg.b], writes=[gq.b])
                      if smp:
                          for t3 in range(3):
                              k.dma("sp", o_sconv[:, t3, :], gq[5 + t3::8, :], reads=[gq.b])
                      else:
                          k.dma("sp", o_pconv[:, :], gq[125:128, :], reads=[gq.b])
              pipe_ahead([c_gen(j) for j in range(NT_ALL + 1)], 2)
              outs_done += [b.b for b in ko + vo + io] + [gq.b]

        k.barrier()
        with contextlib.ExitStack() as st:
          if 'E' in PH:
              NQ = 512 + 256 + 4 + 512
              Wq = sb(st, "Wq", [128, KC, NQ], BF16)
              load_w(Wq, w_in, O_AQ, 512, 0)
              load_w(Wq, w_in, O_IQ, 256, 512)
              load_w(Wq, w_in, O_IW, 4, 768)
              load_w(Wq, w_in, O_MQ, 512, 772)
              tsets = [(sb(st, "ex32_%d" % i, [128, D]), sb(st, "exn_%d" % i, [128, D]),
                        sb(st, "exT_%d" % i, [128, KC, 128], BF16), sb(st, "estat_%d" % i, [128, 4]))
                       for i in range(1)] * 2
              tmp = sb(st, "etmp", [128, 512])
              hstat = sb(st, "ehstat", [128, 24])
              mq32 = sb(st, "mq32", [128, 512])
              MQT2 = [sb(st, "MQT%d" % q, [128, 4, 128], BF16) for q in range(3)]
              PT = sb(st, "PT", [128, 2, 4, 128], BF16)
              tE = sb(st, "tE", [128, 4, 128], BF16)
              rec = sb(st, "rec", [128, 8])
              mo32 = [sb(st, "mo32_%d" % i, [128, 512]) for i in range(2)]
              ao32 = [sb(st, "ao32_%d" % i, [128, 512]) for i in range(2)]
              mkb = MKTb = MVb = None
              SC_M = 128.0 ** -0.5

              def mem_scores(MKT_, MQT_, mb, dstPT, cm=None):
                  pS = ps()
                  for h in range(4):
                      k.op("pe", lambda e, h=h: e.matmul(
                          pS[:, h * 128:(h + 1) * 128], lhsT=MKT_[:, h, mb * 128:(mb + 1) * 128],
                          rhs=MQT_[:, h, :], start=True, stop=True),
                          reads=[MKT_.b, MQT_.b], writes=[pS.b])
                  if cm is None:
                      k.op("act", lambda e: e.activation(
                          out=dstPT[:, mb, :, :], in_=pS[:, :].rearrange("p (h t) -> p h t", h=4),
                          func=AF.Exp, scale=SC_M), reads=[pS.b], writes=[dstPT.b])
                  else:
                      k.op("act", lambda e: e.activation(
                          out=tE[:], in_=pS[:, :].rearrange("p (h t) -> p h t", h=4),
                          func=AF.Exp, scale=SC_M), reads=[pS.b], writes=[tE.b])
                      k.op("dve", lambda e: e.tensor_tensor(
                          dstPT[:, mb, :, :], tE[:], cm.unsqueeze(1).to_broadcast([128, 4, 128]), op=ALU.mult),
                          reads=[tE.b, cmask.b], writes=[dstPT.b])

              def mem_pv(pO, PT_, MV_, first, last):
                  for h in range(4):
                      for mb in range(2):
                          k.op("pe", lambda e, h=h, mb=mb: e.matmul(
                              pO[h // 2][:, (h % 2) * 129:(h % 2) * 129 + 129], lhsT=PT_[:, mb, h, :],
                              rhs=MV_[:, mb, h, :], start=False, stop=(last and mb == 1)),
                              reads=[PT_.b, MV_.b], writes=[pO[h // 2].b])

              NIT = 18
              dcs = sb(st, "dcs", [128, 64])
              k.dma("sp", dcs[:], dconst_d[:, :], writes=[dcs.b])
              score = sb(st, "score", [128, SEQ])
              junk = sb(st, "junk", [128, SEQ], BF16)
              maskT2 = [sb(st, "maskT%d" % q, [128, NT_ALL, 128], BF16) for q in range(2)]
              Eb = [sb(st, "Eb%d" % i, [128, 8, 128], BF16) for i in range(2)]
              Pm = [sb(st, "Pm%d" % i, [128, 8, 128], BF16) for i in range(2)]
              QT2 = [sb(st, "QT%d" % q, [64, 8, 128], BF16) for q in range(3)]
              IQT = sb(st, "IQT", [64, 4, 128], BF16)
              aq32 = sb(st, "aq32", [128, 512])
              iq32 = sb(st, "iq32", [128, 260])
              wst = sb(st, "wst", [128, 16])
              bis = sb(st, "bis", [128, 8 + 2 * NIT])
              rtmp = sb(st, "rtmp", [128, 512])
              rtmpB = sb(st, "rtmpB", [128, 512])
              pent = sb(st, "pent", [128, 256])
              den8 = sb(st, "den8", [128, 8])
              SC_A = 64.0 ** -0.5
              cmask32e = dcs[:, 40:56]

              def dsa_q(xT, QTb):
                  pq = ps()
                  linear(xT, Wq, 0, 512, pq)
                  head_rmsnorm(pq.b, pq[:, :], 8, 64, hv[:, 0:64], aq32, tmp, hstat, "aq")
                  for half in range(2):
                      p = ps()
                      for hh in range(4):
                          k.op("pe", lambda e, hh=hh, half=half, p=p: e.transpose(
                              p[0:64, hh * 128:(hh + 1) * 128],
                              aq32[:, (half * 4 + hh) * 64:(half * 4 + hh + 1) * 64], ident[:]),
                              reads=[aq32.b, ident.b], writes=[p.b])
                      k.op("act", lambda e, half=half, p=p: e.activation(
                          out=QTb[:, half * 4:(half + 1) * 4, :], in_=p[0:64, :].rearrange("p (h t) -> p h t", h=4),
                          func=AF.Copy), reads=[p.b], writes=[QTb.b])
                  yield
                  pi = ps()
                  linear(xT, Wq, 512, 260, pi)
                  k.op("act", lambda e: e.activation(out=iq32[:], in_=pi[:, 0:260], func=AF.Copy),
                       reads=[pi.b], writes=[iq32.b])
                  p = ps()
                  for hh in range(4):
                      k.op("pe", lambda e, hh=hh, p=p: e.transpose(
                          p[0:64, hh * 128:(hh + 1) * 128], iq32[:, hh * 64:(hh + 1) * 64], ident[:]),
                          reads=[iq32.b, ident.b], writes=[p.b])
                  k.op("act", lambda e, p=p: e.activation(
                      out=IQT[:], in_=p[0:64, :].rearrange("p (h t) -> p h t", h=4), func=AF.Copy),
                      reads=[p.b], writes=[IQT.b])
                  yield
                  k.op("act", lambda e: e.activation(out=wst[:, 0:4], in_=iq32[:, 256:260], func=AF.Abs),
                       reads=[iq32.b], writes=[wst.b])
                  k.op("dve", lambda e: e.tensor_scalar(wst[:, 4:8], iq32[:, 256:260], 0.0, 2.0,
                                                        op0=ALU.is_gt, op1=ALU.mult),
                       reads=[iq32.b], writes=[wst.b])
                  k.op("dve", lambda e: e.tensor_scalar(wst[:, 4:8], wst[:, 4:8], -1.0, None, op0=ALU.add),
                       reads=[wst.b], writes=[wst.b])
              def dsa_index(L, sc):
                  for c0 in range(0, L, 512):
                      n = min(512, L - c0)
                      for hh in range(4):
                          pS = ps()
                          k.op("pe", lambda e, hh=hh, pS=pS, c0=c0, n=n: e.matmul(
                              pS[:, 0:n], lhsT=IQT[:, hh, :], rhs=IKT[:, c0:c0 + n], start=True, stop=True),
                              reads=[IQT.b, IKT.b], writes=[pS.b])
                          rt_ = rtmp if hh % 2 == 0 else rtmpB
                          k.op("act", lambda e, hh=hh, pS=pS, n=n, rt_=rt_: e.activation(
                              out=rt_[:, 0:n], in_=pS[:, 0:n], func=AF.Relu, scale=wst[:, hh:hh + 1]),
                              reads=[pS.b, wst.b], writes=[rt_.b])
                          if hh == 0:
                              k.op("dve", lambda e, c0=c0, n=n, rt_=rt_: e.tensor_scalar(
                                  sc[:, c0:c0 + n], rt_[:, 0:n], wst[:, 4:5], None, op0=ALU.mult),
                                  reads=[rt_.b, wst.b], writes=[sc.b])
                          else:
                              k.op("dve", lambda e, hh=hh, c0=c0, n=n, rt_=rt_: e.scalar_tensor_tensor(
                                  out=sc[:, c0:c0 + n], in0=rt_[:, 0:n], scalar=wst[:, 4 + hh:5 + hh],
                                  in1=sc[:, c0:c0 + n], op0=ALU.mult, op1=ALU.add),
                                  reads=[rt_.b, wst.b, sc.b], writes=[sc.b])
                          yield
              def dsa_select(i, nk, L, sel, mTb, sc):
                  k.op("dve", lambda e: e.tensor_reduce(bis[:, 4:5], sc[:, 0:L], axis=AX.X, op=ALU.max),
                       reads=[sc.b], writes=[bis.b])
                  k.op("dve", lambda e: e.tensor_reduce(bis[:, 5:6], sc[:, 0:L], axis=AX.X, op=ALU.min),
                       reads=[sc.b], writes=[bis.b])
                  k.dma("sp", pent[:], pen_d[i], writes=[pent.b])
                  k.op("dve", lambda e: e.tensor_tensor(sc[:, L - 256:L], sc[:, L - 256:L], pent[:], op=ALU.add),
                       reads=[sc.b, pent.b], writes=[sc.b])
                  if DBG and i == 1 and sel is None:
                      k.dma("sp", dbg_sc[:, :], sc[:, 0:512], reads=[sc.b])
                  k.op("dve", lambda e: e.tensor_tensor(bis[:, 0:1], bis[:, 4:5], bis[:, 5:6], op=ALU.add),
                       reads=[bis.b], writes=[bis.b])
                  k.op("dve", lambda e: e.tensor_scalar(bis[:, 0:1], bis[:, 0:1], 0.5, None, op0=ALU.mult),
                       reads=[bis.b], writes=[bis.b])
                  k.op("dve", lambda e: e.tensor_tensor(bis[:, 3:4], bis[:, 4:5], bis[:, 5:6], op=ALU.subtract),
                       reads=[bis.b], writes=[bis.b])
                  k.op("dve", lambda e: e.tensor_scalar(bis[:, 3:4], bis[:, 3:4], 2.0, None, op0=ALU.add),
                       reads=[bis.b], writes=[bis.b])
                  k.op("dve", lambda e: e.tensor_scalar(bis[:, 8:8 + NIT], dcs[:, 0:NIT], bis[:, 3:4], None, op0=ALU.mult),
                       reads=[bis.b, dcs.b], writes=[bis.b])
                  k.op("dve", lambda e: e.tensor_scalar(bis[:, 8 + NIT:8 + 2 * NIT], bis[:, 8:8 + NIT], -0.5, None,
                                                        op0=ALU.mult), reads=[bis.b], writes=[bis.b])
                  for n_ in range(NIT):
                      k.op("dve", lambda e: e.tensor_scalar(junk[:, 0:L], sc[:, 0:L], bis[:, 0:1], None,
                                                            op0=ALU.is_ge, op1=ALU.add, accum_out=bis[:, 1:2]),
                           reads=[sc.b, bis.b], writes=[junk.b, bis.b])
                      k.op("dve", lambda e, n_=n_: e.tensor_scalar(bis[:, 2:3], bis[:, 1:2], dcs[:, 32:33],
                                                                  bis[:, 8 + n_:9 + n_], op0=ALU.is_ge, op1=ALU.mult),
                           reads=[bis.b, dcs.b], writes=[bis.b])
                      k.op("dve", lambda e, n_=n_: e.scalar_tensor_tensor(
                          out=bis[:, 0:1], in0=bis[:, 2:3], scalar=bis[:, 8 + NIT + n_:9 + NIT + n_], in1=bis[:, 0:1],
                          op0=ALU.add, op1=ALU.add), reads=[bis.b], writes=[bis.b])
                      yield
                  k.op("dve", lambda e: e.tensor_tensor(bis[:, 0:1], bis[:, 0:1], bis[:, 8 + NIT - 1:8 + NIT],
                                                        op=ALU.subtract), reads=[bis.b], writes=[bis.b])
                  k.op("dve", lambda e: e.tensor_scalar(sc[:, 0:L], sc[:, 0:L], bis[:, 0:1], None, op0=ALU.is_ge),
                       reads=[sc.b, bis.b], writes=[sc.b])
                  if DBG and i == 1 and sel is None:
                      k.dma("sp", dbg_mask[:, :], sc[:, 0:512], reads=[sc.b])
                      k.dma("sp", dbg_bis[:, :], bis[:], reads=[bis.b])
                  for kb0 in range(0, nk, 4):
                      nb_ = min(4, nk - kb0)
                      p = ps()
                      for j_ in range(nb_):
                          k.op("pe", lambda e, j_=j_, kb0=kb0, p=p: e.transpose(
                              p[:, j_ * 128:(j_ + 1) * 128], sc[:, (kb0 + j_) * 128:(kb0 + j_ + 1) * 128], ident[:]),
                              reads=[sc.b, ident.b], writes=[p.b])
                      k.op("act", lambda e, kb0=kb0, nb_=nb_, p=p: e.activation(
                          out=mTb[:, kb0:kb0 + nb_, :], in_=p[:, 0:nb_ * 128].rearrange("p (a t) -> p a t", a=nb_),
                          func=AF.Copy), reads=[p.b], writes=[mTb.b])
                      yield
              def dsa_attend(kblocks, ao, sel, accumulate, QTb, mTb):
                  pO = [ps(hold=True), ps(hold=True)]
                  ps_zero(pO[0]); ps_zero(pO[1])
                  nkb = len(kblocks)
                  def st_exp(ki):
                      ktT, kc0, vaT, vblk, mi, kbufs = kblocks[ki]
                      E_ = Eb[ki % 2]
                      for g2 in range(2):
                          pS = ps()
                          for gg in range(2):
                              g = g2 * 2 + gg
                              k.op("pe", lambda e, g=g, gg=gg, pS=pS: e.matmul(
                                  pS[:, gg * 256:(gg + 1) * 256], lhsT=ktT[:, g, kc0:kc0 + 128],
                                  rhs=QTb[:, 2 * g:2 * g + 2, :], start=True, stop=True),
                                  reads=kbufs + [QTb.b], writes=[pS.b])
                          k.op("act", lambda e, g2=g2, pS=pS, E_=E_: e.activation(
                              out=E_[:, g2 * 4:(g2 + 1) * 4, :], in_=pS[:, :].rearrange("p (h t) -> p h t", h=4),
                              func=AF.Exp, scale=SC_A), reads=[pS.b], writes=[E_.b])

                  st_exp(0)
                  for ki, (ktT, kc0, vaT, vblk, mi, kbufs) in enumerate(kblocks):
                      E_, P_ = Eb[ki % 2], Pm[ki % 2]
                      if ki + 1 < nkb:
                          st_exp(ki + 1)
                      k.op("dve", lambda e, mi=mi, E_=E_, P_=P_: e.tensor_tensor(
                          P_[:], E_[:], mTb[:, mi, :].unsqueeze(1).to_broadcast([128, 8, 128]), op=ALU.mult),
                          reads=[E_.b, mTb.b], writes=[P_.b])
                      for hh in range(8):
                          rhs_ = vaT[:, hh // 2, :] if vblk is None else vaT[:, vblk, hh // 2, :]
                          k.op("pe", lambda e, hh=hh, P_=P_, rhs_=rhs_: e.matmul(
                              pO[hh // 4][:, (hh % 4) * 65:(hh % 4) * 65 + 65], lhsT=P_[:, hh, :],
                              rhs=rhs_, start=False, stop=(ki == nkb - 1)),
                              reads=[P_.b] + kbufs, writes=[pO[hh // 4].b])
                      yield
                  for j_ in range(2):
                      pv3 = pO[j_][:, 0:260].rearrange("p (h c) -> p h c", h=4)
                      k.op("dve", lambda e, j_=j_, pv3=pv3: e.reciprocal(
                          den8[:, 4 * j_:4 * j_ + 4].unsqueeze(2), pv3[:, :, 64:65]),
                          reads=[pO[j_].b], writes=[den8.b])
                      if sel is not None:
                          k.op("dve", lambda e, j_=j_: e.tensor_scalar(
                              den8[:, 4 * j_:4 * j_ + 4], den8[:, 4 * j_:4 * j_ + 4], sel, None, op0=ALU.mult),
                              reads=[den8.b, cmask.b, selw.b], writes=[den8.b])
                      dst = ao[:, 256 * j_:256 * j_ + 256].rearrange("p (h d) -> p h d", h=4)
                      bc_ = den8[:, 4 * j_:4 * j_ + 4].unsqueeze(2).to_broadcast([128, 4, 64])
                      if not accumulate:
                          k.op("dve", lambda e, pv3=pv3, dst=dst, bc_=bc_: e.tensor_tensor(
                              dst, pv3[:, :, 0:64], bc_, op=ALU.mult), reads=[pO[j_].b, den8.b], writes=[ao.b])
                      else:
                          r3 = rtmp[:, 0:256].rearrange("p (h d) -> p h d", h=4)
                          k.op("dve", lambda e, pv3=pv3, r3=r3, bc_=bc_: e.tensor_tensor(
                              r3, pv3[:, :, 0:64], bc_, op=ALU.mult), reads=[pO[j_].b, den8.b], writes=[rtmp.b])
                          k.op("dve", lambda e, dst=dst, r3=r3: e.tensor_tensor(dst, dst, r3, op=ALU.add),
                               reads=[rtmp.b, ao.b], writes=[ao.b])
                  ps_release(pO[0]); ps_release(pO[1])

              def run(g):
                  for _ in g:
                      pass

              def rr(*gens):
                  alive = [g for g in gens if g is not None]
                  while alive:
                      for g in list(alive):
                          try:
                              next(g)
                          except StopIteration:
                              alive.remove(g)

              def mq_proj(xT, MQTb):
                  pmq = ps()
                  linear(xT, Wq, 772, 512, pmq)
                  head_rmsnorm(pmq.b, pmq[:, :], 4, 128, hv[:, 128:256], mq32, tmp, hstat, "mq")
                  p = ps()
                  for h in range(4):
                      k.op("pe", lambda e, h=h, p=p: e.transpose(
                          p[:, h * 128:(h + 1) * 128], mq32[:, h * 128:(h + 1) * 128], ident[:]),
                          reads=[mq32.b, ident.b], writes=[p.b])
                  k.op("act", lambda e, p=p: e.activation(
                      out=MQTb[:], in_=p[:, :].rearrange("p (h t) -> p h t", h=4), func=AF.Copy),
                      reads=[p.b], writes=[MQTb.b])

              def mem_attn(i, smp, MQT):
                  pO = [ps(hold=True), ps(hold=True)]
                  ps_zero(pO[0]); ps_zero(pO[1])
                  if not smp:
                      for mb in range(2):
                          mem_scores(MKT, MQT, mb, PT)
                          yield
                      mem_pv(pO, PT, MVa, True, True)
                      yield
                  else:
                      for b in range(16):
                          kb_, Kt_, Vb_ = mkb[b % 2], MKTb[b % 2], MVb[b % 2]
                          k.dma("sp", kb_[:], cmk_d[b].rearrange("(mb p) c -> p mb c", p=128), writes=[kb_.b])
                          for mb in range(2):
                              k.dma("pool", Vb_[:, mb, :, 0:128],
                                    cmv_d[b, mb * 128:(mb + 1) * 128, :].rearrange("p (h d) -> p h d", h=4),
                                    writes=[Vb_.b])
                          for mb in range(2):
                              p = ps()
                              for h in range(4):
                                  k.op("pe", lambda e, h=h, p=p, mb=mb: e.transpose(
                                      p[:, h * 128:(h + 1) * 128], kb_[:, mb, h * 128:(h + 1) * 128], ident[:]),
                                      reads=[kb_.b, ident.b], writes=[p.b])
                              k.op("act", lambda e, p=p, mb=mb: e.activation(
                                  out=Kt_[:, :, mb * 128:(mb + 1) * 128],
                                  in_=p[:, :].rearrange("p (h m) -> p h m", h=4), func=AF.Copy),
                                  reads=[p.b], writes=[Kt_.b])
                          for mb in range(2):
                              mem_scores(Kt_, MQT, mb, PT, cm=cmask[:, b, :])
                          mem_pv(pO, PT, Vb_, b == 0, b == 15)
                          yield
                  mo = mo32[i % 2]
                  for j in range(2):
                      pv3 = pO[j][:, 0:258].rearrange("p (h c) -> p h c", h=2)
                      k.op("dve", lambda e, j=j, pv3=pv3: e.reciprocal(
                          rec[:, 2 * j:2 * j + 2].unsqueeze(2), pv3[:, :, 128:129]),
                          reads=[pO[j].b], writes=[rec.b])
                      k.op("dve", lambda e, j=j, pv3=pv3, mo=mo: e.tensor_tensor(
                          mo[:, 256 * j:256 * j + 256].rearrange("p (h d) -> p h d", h=2), pv3[:, :, 0:128],
                          rec[:, 2 * j:2 * j + 2].unsqueeze(2).to_broadcast([128, 2, 128]), op=ALU.mult),
                          reads=[pO[j].b, rec.b], writes=[mo.b])
                  ps_release(pO[0]); ps_release(pO[1])
                  k.dma("sp", m_sc[i * 128:(i + 1) * 128, :], mo[:], reads=[mo.b], writes=[m_sc_b[i]])

              NDSA = cfg.get("dsa_tiles", 17)

              def nk_of(i):
                  return 4 * (i // 2) + (2 if i % 2 == 0 else 4)

              def tile_A1(i):
                  x32, xT = load_xT(tsets[0], x_own[i * 128:(i + 1) * 128, :], 0)
                  yield
                  mq_proj(xT, MQT2[i % 3])
                  yield
                  if i < NDSA:
                      yield from dsa_q(xT, QT2[i % 3])
                      yield from dsa_index(nk_of(i) * 128, score2[i % 2])

              def tile_A2(i):
                  if i < NDSA:
                      yield from dsa_select(i, nk_of(i), nk_of(i) * 128, None, maskT2[i % 2], score2[i % 2])

              def tile_B(i):
                  ao = ao32[i % 2]
                  if i < NDSA:
                      kbl = [(KT, kb * 128, VA, kb, kb, [KT.b, VA.b]) for kb in range(nk_of(i))]
                      yield from dsa_attend(kbl, ao, None, False, QT2[i % 3], maskT2[i % 2])
                  else:
                      k.op("pool", lambda e: e.memset(ao[:], 0.0), writes=[ao.b])
                  k.dma("sp", a_sc[i * 128:(i + 1) * 128, :], ao[:], reads=[ao.b], writes=[a_sc_b[i]])
                  yield from mem_attn(i, False, MQT2[i % 3])

              pst = contextlib.ExitStack()
              score2 = [score, sb(pst, "scoreB", [128, SEQ])]
              for s_ in range(NT_OWN + 2):
                  rr(tile_B(s_ - 2) if 0 <= s_ - 2 < NT_OWN else None,
                     tile_A2(s_ - 1) if 0 <= s_ - 1 < NT_OWN else None,
                     tile_A1(s_) if s_ < NT_OWN else None)

              k.barrier()
              pst.close()
              mkb = [sb(st, "mkb%d" % i, [128, 2, 512]) for i in range(1)] * 2
              MKTb = [sb(st, "MKTb%d" % i, [128, 4, 256], BF16) for i in range(2)]
              MVb = [sb(st, "MVb%d" % i, [128, 2, 4, 129], BF16) for i in range(2)]
              for t_ in MVb:
                  k.op("pool", lambda e, t_=t_: e.memset(t_[:], 1.0), writes=[t_.b])
              i = NT_OWN
              x32, xT = load_xT(tsets[0], x_smp[:, :], 0)
              QTb, mTb, MQTb = QT2[0], maskT2[0], MQT2[0]
              mq_proj(xT, MQTb)
              run(mem_attn(i, True, MQTb))
              ao = ao32[0]
              if NDSA < 17:
                  k.op("pool", lambda e: e.memset(ao[:], 0.0), writes=[ao.b])
              else:
                  ptb = sb(st, "ptb", [128, 256], I32)
                  ptf = sb(st, "ptf", [128, 256])
                  pti = sb(st, "pti", [128, 256], I32)
                  k.dma("sp", ptb[:], pt_d[0:1, :].partition_broadcast(128), writes=[ptb.b])
                  k.op("dve", lambda e: e.tensor_copy(ptf[:], ptb[:]), reads=[ptb.b], writes=[ptf.b])
                  k.op("dve", lambda e: e.tensor_scalar(ptf[:], ptf[:], 128.0, dcs[:, 33:34],
                                                        op0=ALU.mult, op1=ALU.add),
                       reads=[ptf.b, dcs.b], writes=[ptf.b])
                  k.op("dve", lambda e: e.tensor_copy(pti[:], ptf[:]), reads=[ptf.b], writes=[pti.b])
                  kvpg = [sb(st, "kvpg%d" % q, [128, 512]) for q in range(2)]
                  ipg = [sb(st, "ipg%d" % q, [128, 64]) for q in range(2)]
                  IQTm = [sb(st, "IQTm%d" % q, [64, 4, 128], BF16) for q in range(2)]
                  KTh, VAh, IKTh = [Buf(), Buf()], [Buf(), Buf()], [Buf(), Buf()]
                  run(dsa_q(xT, QTb))
                  k.op("pool", lambda e: e.memset(score[:, 0:2048], 0.0), writes=[score.b])

                  def idx_gather(b):
                      hf = b % 2
                      base = 17 * hf
                      for pg in range(16):
                          q_ = pg % 2
                          col = b * 16 + pg
                          off = bass.IndirectOffsetOnAxis(ap=pti[:, col:col + 1], axis=0)
                          k.dma("pool", ipg[q_][:], cik_d[:, :], reads=[pti.b], writes=[ipg[q_].b], indirect=off)
                          kside_store(None, None, ipg[q_][:], [ipg[q_].b], None,
                                      IKT[:, (base + pg) * 128:(base + pg + 1) * 128], None, [IKTh[hf]])
                          yield

                  def idx_score(b):
                      hf = b % 2
                      base = 17 * hf
                      Im = IQTm[hf]
                      k.op("dve", lambda e: e.tensor_tensor(
                          Im[:], IQT[:], cmask[0:64, b, :].unsqueeze(1).to_broadcast([64, 4, 128]), op=ALU.mult),
                          reads=[IQT.b, cmask.b], writes=[Im.b])
                      for c in range(4):
                          for hh in range(4):
                              pS = ps()
                              c0 = base * 128 + c * 512
                              k.op("pe", lambda e, hh=hh, pS=pS, c0=c0: e.matmul(
                                  pS[:, :], lhsT=Im[:, hh, :], rhs=IKT[:, c0:c0 + 512], start=True, stop=True),
                                  reads=[Im.b, IKTh[hf]], writes=[pS.b])
                              k.op("act", lambda e, hh=hh, pS=pS: e.activation(
                                  out=rtmp[:], in_=pS[:, :], func=AF.Relu, scale=wst[:, hh:hh + 1]),
                                  reads=[pS.b, wst.b], writes=[rtmp.b])
                              k.op("dve", lambda e, hh=hh, c=c: e.scalar_tensor_tensor(
                                  out=score[:, c * 512:(c + 1) * 512], in0=rtmp[:], scalar=wst[:, 4 + hh:5 + hh],
                                  in1=score[:, c * 512:(c + 1) * 512], op0=ALU.mult, op1=ALU.add),
                                  reads=[rtmp.b, wst.b, score.b], writes=[score.b])
                              yield

                  run(idx_gather(0))
                  for b in range(16):
                      rr(idx_score(b), idx_gather(b + 1) if b + 1 < 16 else None)
                  for hh in range(4):
                      pS = ps()
                      k.op("pe", lambda e, hh=hh, pS=pS: e.matmul(
                          pS[:, 0:128], lhsT=IQT[:, hh, :], rhs=IKTn[:, :], start=True, stop=True),
                          reads=[IQT.b, IKTn.b], writes=[pS.b])
                      k.op("act", lambda e, hh=hh, pS=pS: e.activation(
                          out=rtmp[:, 0:128], in_=pS[:, 0:128], func=AF.Relu, scale=wst[:, hh:hh + 1]),
                          reads=[pS.b, wst.b], writes=[rtmp.b])
                      if hh == 0:
                          k.op("dve", lambda e: e.tensor_scalar(
                              score[:, 2048:2176], rtmp[:, 0:128], wst[:, 4:5], None, op0=ALU.mult),
                              reads=[rtmp.b, wst.b], writes=[score.b])
                      else:
                          k.op("dve", lambda e, hh=hh: e.scalar_tensor_tensor(
                              out=score[:, 2048:2176], in0=rtmp[:, 0:128], scalar=wst[:, 4 + hh:5 + hh],
                              in1=score[:, 2048:2176], op0=ALU.mult, op1=ALU.add),
                              reads=[rtmp.b, wst.b, score.b], writes=[score.b])
                  run(dsa_select(16, 17, 17 * 128, None, mTb, score))

                  def kv_gather(b):
                      hf = b % 2
                      base = 17 * hf
                      for pg in range(16):
                          q_ = pg % 2
                          col = b * 16 + pg
                          off = bass.IndirectOffsetOnAxis(ap=pti[:, col:col + 1], axis=0)
                          k.dma("pool", kvpg[q_][:], ckv_d[:, :], reads=[pti.b], writes=[kvpg[q_].b], indirect=off)
                          kside_store(kvpg[q_][:, 0:256], None, None, [kvpg[q_].b],
                                      KT[:, :, (base + pg) * 128:(base + pg + 1) * 128], None, None, [KTh[hf]])
                          k.op("dve", lambda e, q_=q_, base=base, pg=pg: e.tensor_copy(
                              VA[:, base + pg, :, 0:64], kvpg[q_][:, 256:512].rearrange("p (g d) -> p g d", g=4)),
                              reads=[kvpg[q_].b], writes=[VAh[hf]])
                          yield

                  def seq_attend(b):
                      hf = b % 2
                      base = 17 * hf
                      kbl = [(KT, (base + pg) * 128, VA, base + pg, pg, [KTh[hf], VAh[hf]]) for pg in range(16)]
                      kbl.append((KTn, 0, VAn, None, 16, [KTn.b, VAn.b]))
                      yield from dsa_attend(kbl, ao, cmask32e[:, b:b + 1], b > 0, QTb, mTb)

                  run(kv_gather(0))
                  for b in range(16):
                      rr(seq_attend(b), kv_gather(b + 1) if b + 1 < 16 else None)
              k.dma("sp", a_sc[i * 128:(i + 1) * 128, :], ao[:], reads=[ao.b], writes=[a_sc_b[i]])

        k.barrier()
        kst.close()
        k.barrier()
        with contextlib.ExitStack() as st:
          if 'F' in PH:
              Wg = sb(st, "Wg", [128, KC, 3072], BF16)
              Wbr = sb(st, "Wbr", [128, 12, D], BF16)
              Wo = sb(st, "Wo", [128, KC, D], BF16)
              load_w(Wg, w_in, O_GATES, 3072)
              for bi, wsrc in enumerate((w_a_out, w_g_out, w_m_out)):
                  for kc in range(4):
                      k.dma("pool", Wbr[:, bi * 4 + kc, :], wsrc[kc * 128:(kc + 1) * 128, :], writes=[Wbr.b])
              load_w(Wo, w_o, 0, D)
              tsets = [(sb(st, "fx32_%d" % i, [128, D]), sb(st, "fxn_%d" % i, [128, D]),
                        sb(st, "fxT_%d" % i, [128, KC, 128], BF16), sb(st, "fstat_%d" % i, [128, 4]))
                       for i in range(3)]
              br32 = [sb(st, "br32_%d" % i, [128, 3, 512]) for i in range(2)]
              cand = [sb(st, "cand%d" % i, [128, 4, 512]) for i in range(2)]
              brT = sb(st, "brT", [128, 12, 128], BF16)
              sig = sb(st, "sig", [128, 512])
              term = sb(st, "term", [128, 512])
              h32 = sb(st, "h32", [128, D])
              hT = sb(st, "hT", [128, KC, 128], BF16)
              x2o = [sb(st, "x2o%d" % i, [128, D]) for i in range(2)]
              def f_gen(i):
                  smp = (i == NT_OWN)
                  src = x_smp[:, :] if smp else x_own[i * 128:(i + 1) * 128, :]
                  x32, xT = load_xT(tsets[i % 3], src, 0)
                  yield "B"
                  br = br32[i % 2]
                  k.dma("sp", br[:, 0, :], a_sc[i * 128:(i + 1) * 128, :], reads=[a_sc_b[i]], writes=[br.b])
                  k.dma("sp", br[:, 2, :], m_sc[i * 128:(i + 1) * 128, :], reads=[m_sc_b[i]], writes=[br.b])
                  if smp:
                      k.dma("sp", br[:, 1, :], g_sc[32 * 128:33 * 128, :], reads=[g_sc_b[32]], writes=[br.b])
                  else:
                      grp = i // 2
                      cd = cand[i % 2]
                      if i % 2 == 0:
                          cdl = cand[(i // 2) % 2]
                          k.dma("sp", cdl[:], g_sc[grp * 512:(grp + 1) * 512, :].rearrange("(c p) n -> p c n", p=128),
                                reads=g_sc_b[4 * grp:4 * grp + 4], writes=[cdl.b])
                      cdl = cand[(i // 2) % 2]
                      for c in range(4):
                          sc_ = selw[:, i * 4 + c:i * 4 + c + 1]
                          if c == 0:
                              k.op("dve", lambda e, sc_=sc_, br=br, cdl=cdl: e.tensor_scalar(
                                  br[:, 1, :], cdl[:, 0, :], sc_, None, op0=ALU.mult),
                                  reads=[cdl.b, selw.b], writes=[br.b])
                          else:
                              k.op("dve", lambda e, sc_=sc_, br=br, cdl=cdl, c=c: e.scalar_tensor_tensor(
                                  out=br[:, 1, :], in0=cdl[:, c, :], scalar=sc_, in1=br[:, 1, :],
                                  op0=ALU.mult, op1=ALU.add), reads=[cdl.b, selw.b, br.b], writes=[br.b])
                  for bi in range(3):
                      p = ps()
                      for j in range(4):
                          k.op("pe", lambda e, bi=bi, j=j, p=p, br=br: e.transpose(
                              p[:, j * 128:(j + 1) * 128], br[:, bi, j * 128:(j + 1) * 128], ident[:]),
                              reads=[br.b, ident.b], writes=[p.b])
                      k.op("act", lambda e, bi=bi, p=p: e.activation(
                          out=brT[:, bi * 4:(bi + 1) * 4, :], in_=p[:, :].rearrange("p (a b) -> p a b", a=4),
                          func=AF.Copy), reads=[p.b], writes=[brT.b])
                  for c in range(2):
                      for bi in range(3):
                          pg = ps()
                          linear(xT, Wg, bi * 1024 + c * 512, 512, pg)
                          pb = ps()
                          for kc in range(4):
                              k.op("pe", lambda e, kc=kc, bi=bi, c=c, pb=pb: e.matmul(
                                  pb[:, :], lhsT=brT[:, bi * 4 + kc, :], rhs=Wbr[:, bi * 4 + kc, c * 512:(c + 1) * 512],
                                  start=(kc == 0), stop=(kc == 3)), reads=[brT.b, Wbr.b], writes=[pb.b])
                          k.op("act", lambda e, pg=pg: e.activation(out=sig[:], in_=pg[:, :], func=AF.Sigmoid),
                               reads=[pg.b], writes=[sig.b])
                          if bi == 0:
                              k.op("dve", lambda e, pb=pb, c=c: e.tensor_tensor(
                                  h32[:, c * 512:(c + 1) * 512], sig[:], pb[:, :], op=ALU.mult),
                                  reads=[sig.b, pb.b], writes=[h32.b])
                          else:
                              k.op("dve", lambda e, pb=pb: e.tensor_tensor(term[:], sig[:], pb[:, :], op=ALU.mult),
                                   reads=[sig.b, pb.b], writes=[term.b])
                              k.op("dve", lambda e, c=c: e.tensor_tensor(
                                  h32[:, c * 512:(c + 1) * 512], h32[:, c * 512:(c + 1) * 512], term[:], op=ALU.add),
                                  reads=[term.b, h32.b], writes=[h32.b])
                  for half in range(2):
                      p = ps()
                      for j in range(4):
                          kc = half * 4 + j
                          k.op("pe", lambda e, kc=kc, j=j, p=p: e.transpose(
                              p[:, j * 128:(j + 1) * 128], h32[:, kc * 128:(kc + 1) * 128], ident[:]),
                              reads=[h32.b, ident.b], writes=[p.b])
                      k.op("act", lambda e, half=half, p=p: e.activation(
                          out=hT[:, half * 4:(half + 1) * 4, :], in_=p[:, :].rearrange("p (a b) -> p a b", a=4),
                          func=AF.Copy), reads=[p.b], writes=[hT.b])
                  xo_ = x2o[i % 2]
                  for c in range(2):
                      p = ps()
                      linear(hT, Wo, c * 512, 512, p)
                      k.op("dve", lambda e, c=c, p=p, xo_=xo_, x32=x32: e.tensor_tensor(
                          xo_[:, c * 512:(c + 1) * 512], p[:, :], x32[:, c * 512:(c + 1) * 512], op=ALU.add),
                          reads=[p.b, x32.b], writes=[xo_.b])
                  k.dma("sp", x2_sc[i * 128:(i + 1) * 128, :], xo_[:], reads=[xo_.b], writes=[x2_sc_b[i]])
              pipe_ahead([f_gen(i) for i in range(NT_OWN + 1)], 2)

        k.barrier()
        with contextlib.ExitStack() as st:
          if 'D' in PH:
              Wf1 = sb(st, "Wf1", [128, KC, 2 * D_FF], BF16)
              Wf2 = sb(st, "Wf2", [128, D_FF // 128, D], BF16)
              load_w(Wf1, w_ffn_in, 0, 2 * D_FF)
              load_w(Wf2, w_ffn_out, 0, D, rows=D_FF)
              tsets = [(sb(st, "dx32_%d" % i, [128, D]), sb(st, "dxn_%d" % i, [128, D]),
                        sb(st, "dxT_%d" % i, [128, KC, 128], BF16), sb(st, "dstat_%d" % i, [128, 4]))
                       for i in range(2)]
              act2 = [sb(st, "ffact%d" % q, [128, D_FF]) for q in range(2)]
              sg2 = [sb(st, "ffsg%d" % q, [128, 512]) for q in range(1)] * 2
              actT2 = [sb(st, "ffactT%d" % q, [128, D_FF // 128, 128], BF16) for q in range(2)]
              yo = [sb(st, "yo%d" % i, [128, D]) for i in range(2)]
              nb = D_FF // 128

              def ffn_gen(i):
                  smp = (i == NT_OWN)
                  bf = i % 2
                  act, actT = act2[bf], actT2[bf]
                  if 'F' in PH:
                      src = x2_sc[i * 128:(i + 1) * 128, :]
                      x32, xT = load_xT(tsets[bf], src, 16, rd=[x2_sc_b[i]])
                  else:
                      src = x_smp[:, :] if smp else x_own[i * 128:(i + 1) * 128, :]
                      x32, xT = load_xT(tsets[bf], src, 16)
                  yield
                  c0 = 0
                  ci = 0
                  while c0 < D_FF:
                      n = min(512, D_FF - c0)
                      sg = sg2[ci % 2]
                      pg = ps()
                      linear(xT, Wf1, c0, n, pg)
                      pu = ps()
                      linear(xT, Wf1, D_FF + c0, n, pu)
                      k.op("act", lambda e, pg=pg, n=n, sg=sg: e.activation(out=sg[:, 0:n], in_=pg[:, 0:n], func=AF.Silu),
                           reads=[pg.b], writes=[sg.b])
                      k.op("dve", lambda e, pu=pu, n=n, c0=c0, sg=sg: e.tensor_tensor(
                          act[:, c0:c0 + n], sg[:, 0:n], pu[:, 0:n], op=ALU.mult),
                          reads=[sg.b, pu.b], writes=[act.b])
                      c0 += n
                      ci += 1
                      yield
                  yield "B"
                  for b0 in range(0, nb, 4):
                      nbb = min(4, nb - b0)
                      p = ps()
                      for j in range(nbb):
                          k.op("pe", lambda e, j=j, b0=b0, p=p: e.transpose(
                              p[:, j * 128:(j + 1) * 128], act[:, (b0 + j) * 128:(b0 + j + 1) * 128], ident[:]),
                              reads=[act.b, ident.b], writes=[p.b])
                      k.op("act", lambda e, p=p, b0=b0, nbb=nbb: e.activation(
                          out=actT[:, b0:b0 + nbb, :], in_=p[:, 0:nbb * 128].rearrange("p (a b) -> p a b", a=nbb),
                          func=AF.Copy), reads=[p.b], writes=[actT.b])
                      yield
                  yy = yo[bf]
                  for h in range(2):
                      p = ps()
                      for kc in range(nb):
                          k.op("pe", lambda e, kc=kc, h=h, p=p: e.matmul(
                              p[:, :], lhsT=actT[:, kc, :], rhs=Wf2[:, kc, h * 512:(h + 1) * 512],
                              start=(kc == 0), stop=(kc == nb - 1)),
                              reads=[actT.b, Wf2.b], writes=[p.b])
                          if kc % 6 == 5:
                              yield
                      k.op("dve", lambda e, h=h, p=p, yy=yy, x32=x32: e.tensor_tensor(
                          yy[:, h * 512:(h + 1) * 512], p[:, :], x32[:, h * 512:(h + 1) * 512], op=ALU.add),
                          reads=[p.b, x32.b], writes=[yy.b])
                      yield
                  dst = y_smp[:, :] if smp else y_own[i * 128:(i + 1) * 128, :]
                  k.dma("sp", dst, yy[:], reads=[yy.b])

              gensD = [ffn_gen(i) for i in range(NT_OWN + 1)]
              for tok in gensD[0]:
                  if tok == "B":
                      break
              for i in range(NT_OWN + 1):
                  cur = gensD[i]
                  nxt = gensD[i + 1] if i + 1 < NT_OWN + 1 else None
                  cur_alive, nxt_inA = True, nxt is not None
                  while cur_alive or nxt_inA:
                      if cur_alive:
                          try:
                              next(cur)
                          except StopIteration:
                              cur_alive = False
                      if nxt_inA:
                          if next(nxt) == "B":
                              nxt_inA = False
              outs_done += [b.b for b in yo]

        k.barrier()
        k.finish(outs_done, "sp")
    print("ops", k.nops, "waits", k.nwaits)
    return nc


_NC_CACHE = {}


def make_in_maps(I):
    f = lambda a: np.ascontiguousarray(np.asarray(a), dtype=np.float32)
    x_prompt, x_sample, mem_prompt = f(I["x_prompt"]), f(I["x_sample"]), f(I["mem_prompt"])
    gains = np.concatenate([f(I["norm_mix"])[0].reshape(8, 128).T, f(I["mem_norm"])[0].reshape(8, 128).T,
                            f(I["norm_ffn"])[0].reshape(8, 128).T], axis=1)
    hv = np.concatenate([f(I["a_q_norm"])[0], f(I["a_k_norm"])[0], f(I["m_q_norm"])[0], f(I["m_k_norm"])[0],
                         f(I["g_o_norm"])[0], f(I["g_dt_bias"])[0], f(I["g_a_log"])[0],
                         np.zeros(56, np.float32)])[None, :]
    r = np.arange(128)
    same = (r[:, None] // 8) == (r[None, :] // 8)
    one = np.ones((128, 128), bool)
    mats = []
    for blk in (one, same):
        pass
    ltri = [(r[:, None] <= r[None, :]) & blk for blk in (one, same)]
    bones = [blk for blk in (one, same)]
    slm = [(r[:, None] > r[None, :]) & blk for blk in (one, same)]
    sui = [((r[None, :] >= r[:, None]) & blk) * (128.0 ** -0.5) for blk in (one, same)]
    ustr = (r[:, None] > r[None, :])
    g8 = (r[:, None] // 8) == np.arange(16)[None, :]
    cm32 = np.broadcast_to((np.arange(128)[None, :] // 8 == np.arange(16)[:, None]).reshape(1, 2048), (128, 2048))
    gconst = np.concatenate([np.asarray(a, np.float32) for a in
                             (ltri[0], ltri[1], bones[0], bones[1], slm[0], slm[1], sui[0], sui[1], ustr,
                              np.ones((128, 128)), g8, cm32)], axis=1)
    gconvT = f(I["g_conv"])[0].reshape(4, 12, 128).transpose(2, 1, 0).reshape(128, 48)
    dconst = np.zeros((128, 64), np.float32)
    dconst[:, 0:32] = (2.0 ** -(np.arange(32) + 1.0))[None, :]
    dconst[:, 32] = 256.0
    dconst[:, 33] = np.arange(128)
    dconst[:, 40:56] = g8
    ckv = np.concatenate([f(I["cache_k"])[0].reshape(2560 * 128, 256),
                          f(I["cache_v"])[0].reshape(2560 * 128, 256)], axis=1)
    cik = f(I["cache_idx_k"])[0].reshape(2560 * 128, 64)
    ptab = np.asarray(I["page_table"]).astype(np.int32)
    shared = {
        "w_in": f(I["w_in"])[0], "w_mem_kv": f(I["w_mem_kv"])[0], "w_a_out": f(I["w_a_out"])[0],
        "w_g_out": f(I["w_g_out"])[0], "w_m_out": f(I["w_m_out"])[0], "w_o": f(I["w_o"])[0],
        "w_ffn_in": f(I["w_ffn_in"])[0], "w_ffn_out": f(I["w_ffn_out"])[0],
        "ident": np.eye(128, dtype=np.float32),
        "dconst": dconst, "cache_kv": ckv, "cache_idx_k": cik,
        "gconst": np.ascontiguousarray(gconst, dtype=np.float32), "gconvT": np.ascontiguousarray(gconvT),
        "cmask": np.ascontiguousarray(np.broadcast_to(
            (np.arange(128)[None, :] // 8 == np.arange(16)[:, None]).astype(np.float32).reshape(1, 2048), (128, 2048))), "gains": np.ascontiguousarray(gains), "headvecs": hv,
    }
    in_maps = []
    for c in range(8):
        b, half = c // 2, c % 2
        ot = own_tiles(half)
        xo = np.concatenate([x_prompt[b, t * 128:(t + 1) * 128] for t in ot], axis=0)
        m = dict(shared)
        m["x_all"] = x_prompt[b]
        m["x_own"] = np.ascontiguousarray(xo)
        m["x_smp"] = np.ascontiguousarray(x_sample[16 * c:16 * c + 16].reshape(128, D))
        m["mem"] = mem_prompt[b]
        m["cache_mem_k"] = np.ascontiguousarray(f(I["cache_mem_k"])[0, 16 * c:16 * c + 16].reshape(16, 256, 512))
        m["cache_mem_v"] = np.ascontiguousarray(f(I["cache_mem_v"])[0, 16 * c:16 * c + 16].reshape(16, 256, 512))
        m["state_gdn"] = np.ascontiguousarray(f(I["state_gdn"])[0, 16 * c:16 * c + 16])
        m["state_conv"] = np.ascontiguousarray(f(I["state_conv"])[0, 16 * c:16 * c + 16].reshape(48, 1536))
        m["page_table"] = np.ascontiguousarray(ptab[16 * c:16 * c + 16].reshape(1, 256))
        pen = np.zeros((17, 128, 256), np.float32)
        tt = np.arange(128)
        for i_, t_ in enumerate(ot):
            nk_ = 4 * (i_ // 2) + (2 if i_ % 2 == 0 else 4)
            spos = (nk_ - 2) * 128 + np.arange(256)
            pen[i_] = np.where(spos[None, :] <= (t_ * 128 + tt)[:, None], 0.0, -1e30)
        newok = ((tt[:, None] // 8) == (tt[None, :] // 8)) & ((tt[None, :] % 8) <= (tt[:, None] % 8))
        pen[16, :, 128:] = np.where(newok, 0.0, -1e30)
        m["pen"] = pen
        sw = np.zeros((16, 4), np.float32)
        for i_, t_ in enumerate(ot):
            sw[i_, t_ % 4] = 1.0
        m["selw"] = np.ascontiguousarray(np.broadcast_to(sw.reshape(1, 64), (128, 64)))
        in_maps.append(m)
    return in_maps


def kernel(**I):
    if "nc" not in _NC_CACHE:
        _NC_CACHE["nc"] = build({})
    nc = _NC_CACHE["nc"]
    in_maps = make_in_maps(I)
    res = run_bass_kernel_spmd(nc, in_maps, core_ids=list(range(8)))
    R = res.results
    yp = np.zeros((4, SEQ, D), np.float32)
    for c in range(8):
        b, half = c // 2, c % 2
        for i, t in enumerate(own_tiles(half)):
            yp[b, t * 128:(t + 1) * 128] = R[c]["y_own"][i * 128:(i + 1) * 128]
    ys = np.concatenate([R[c]["y_smp"].reshape(16, 8, D) for c in range(8)], axis=0)
    p_k = np.stack([R[2 * b]["o_pk"].reshape(SEQ, 4, 64) for b in range(4)])[None]
    p_v = np.stack([R[2 * b]["o_pv"].reshape(SEQ, 4, 64) for b in range(4)])[None]
    p_ik = np.stack([R[2 * b]["o_pik"] for b in range(4)])[None]
    p_gdn = np.stack([R[2 * b]["o_pgdn"] for b in range(4)])[None]
    p_conv = np.stack([R[2 * b]["o_pconv"] for b in range(4)])[None]
    p_mk = np.stack([R[2 * b]["o_pmk"].reshape(256, 4, 128) for b in range(4)])[None]
    p_mv = np.stack([R[2 * b]["o_pmv"].reshape(256, 4, 128) for b in range(4)])[None]
    s_k = np.concatenate([R[c]["o_sk"].reshape(16, 8, 4, 64) for c in range(8)], axis=0)[None]
    s_v = np.concatenate([R[c]["o_sv"].reshape(16, 8, 4, 64) for c in range(8)], axis=0)[None]
    s_ik = np.concatenate([R[c]["o_sik"].reshape(16, 8, 64) for c in range(8)], axis=0)[None]
    s_gdn = np.concatenate([R[c]["o_sgdn"] for c in range(8)], axis=0)[None]
    s_conv = np.concatenate([R[c]["o_sconv"] for c in range(8)], axis=0)[None]
    return (yp, ys, p_k, p_v, p_ik, p_gdn, p_conv, p_mk, p_mv, s_k, s_v, s_ik, s_gdn, s_conv)
```

```python
import contextlib
import numpy as np
import concourse.bass as bass
import concourse.mybir as mybir
from concourse.bass_utils import run_bass_kernel_spmd

F32 = mybir.dt.float32
BF16 = mybir.dt.bfloat16
I32 = mybir.dt.int32
AF = mybir.ActivationFunctionType
ALU = mybir.AluOpType
AX = mybir.AxisListType

D = 1024
KC = 8
SEQ = 4096
NT_ALL = 32
NT_OWN = 16
D_IN = 6988
D_FF = 2816
EPS = 1e-6
O_AQ, O_AK, O_AV, O_IQ, O_IK, O_IW = 0, 512, 768, 1024, 1280, 1344
O_GQKV, O_GZ, O_GB, O_GA, O_MQ, O_GATES = 1348, 2884, 3396, 3400, 3404, 3916


class Buf:
    __slots__ = ("name", "w", "r")

    def __init__(self, name=""):
        self.name = name
        self.w = None
        self.r = []


class K:
    def __init__(self, nc, n_dma_sems=40):
        self.nc = nc
        self.eng = {"pe": nc.tensor, "act": nc.scalar, "dve": nc.vector,
                    "pool": nc.gpsimd, "sp": nc.sync}
        self.sem, self.cnt, self.seen = {}, {}, {}
        for e in self.eng:
            self.sem[e] = nc.alloc_semaphore("prog_" + e)
            self.cnt[e] = 0
            self.seen[e] = {}
        self.dsems = [nc.alloc_semaphore("dma%d" % i) for i in range(n_dma_sems)]
        self.dcnt = [0] * n_dma_sems
        self.dnext = 0
        self.nops = 0
        self.nwaits = 0

    def _wait(self, e, tok):
        if tok is None:
            return
        sem, val, src = tok
        if src == e and e == "pe":
            return
        key = sem.num
        if self.seen[e].get(key, 0) >= val:
            return
        self.eng[e].wait_ge(sem, val)
        self.seen[e][key] = val
        self.nwaits += 1

    def _deps(self, e, reads, writes):
        for b in reads:
            self._wait(e, b.w)
        for b in writes:
            self._wait(e, b.w)
            for t in b.r:
                self._wait(e, t)

    def _commit(self, tok, reads, writes):
        for b in reads:
            b.r.append(tok)
            if len(b.r) > 64:
                b.r = b.r[-64:] if False else b.r
        for b in writes:
            b.w = tok
            b.r = []

    def op(self, e, fn, reads=(), writes=()):
        self._deps(e, reads, writes)
        ins = fn(self.eng[e])
        self.cnt[e] += 1
        ins.then_inc(self.sem[e], 1)
        tok = (self.sem[e], self.cnt[e], e)
        self._commit(tok, reads, writes)
        self.nops += 1
        return tok

    def dma(self, q, out, in_, reads=(), writes=(), indirect=None, **kw):
        self._deps(q, reads, writes)
        i = self.dnext
        self.dnext = (self.dnext + 1) % len(self.dsems)
        sem = self.dsems[i]
        if self.dcnt[i] > 0:
            self._wait(q, (sem, self.dcnt[i], "dma"))
        if indirect is not None:
            ins = self.eng[q].indirect_dma_start(out=out, out_offset=None, in_=in_,
                                                 in_offset=indirect, **kw)
        else:
            ins = self.eng[q].dma_start(out=out, in_=in_, **kw)
        self.dcnt[i] += 16
        ins.then_inc(sem, 16)
        tok = (sem, self.dcnt[i], "dma")
        self._commit(tok, reads, writes)
        self.nops += 1
        return tok

    def barrier(self):
        for e in self.eng:
            for e2 in self.eng:
                if e2 != e and self.cnt[e2] > 0:
                    self._wait(e, (self.sem[e2], self.cnt[e2], e2))
            for i, s_ in enumerate(self.dsems):
                if self.dcnt[i] > 0:
                    self._wait(e, (s_, self.dcnt[i], "dma"))

    def finish(self, bufs, e="sp"):
        for b in bufs:
            self._wait(e, b.w)
            for t in b.r:
                self._wait(e, t)


class T:
    __slots__ = ("t", "b")

    def __init__(self, t, name=""):
        self.t = t
        self.b = Buf(name)

    def __getitem__(self, key):
        return self.t[key]


def own_tiles(half):
    res = []
    for g in range(8):
        res += [4 * g, 4 * g + 3] if half == 0 else [4 * g + 1, 4 * g + 2]
    return res


def build(cfg):
    nc = bass.Bass("TRN2", target_bir_lowering=False)
    k = K(nc)
    dt_in = {}

    def din(name, shape, dt=F32):
        dt_in[name] = nc.dram_tensor(name, list(shape), dt, kind="ExternalInput").ap()
        return dt_in[name]

    def dout(name, shape, dt=F32):
        return nc.dram_tensor(name, list(shape), dt, kind="ExternalOutput").ap()

    x_all = din("x_all", [SEQ, D])
    x_own = din("x_own", [NT_OWN * 128, D])
    x_smp = din("x_smp", [128, D])
    mem = din("mem", [256, D])
    w_in = din("w_in", [D, D_IN])
    w_mem_kv = din("w_mem_kv", [D, 1024])
    w_a_out = din("w_a_out", [512, D])
    w_g_out = din("w_g_out", [512, D])
    w_m_out = din("w_m_out", [512, D])
    w_o = din("w_o", [D, D])
    w_ffn_in = din("w_ffn_in", [D, 2 * D_FF])
    w_ffn_out = din("w_ffn_out", [D_FF, D])
    ident_d = din("ident", [128, 128])
    gains_d = din("gains", [128, 24])
    hv_d = din("headvecs", [1, 576])

    y_own = dout("y_own", [NT_OWN * 128, D])
    y_smp = dout("y_smp", [128, D])
    o_pk = dout("o_pk", [SEQ, 256])
    o_pv = dout("o_pv", [SEQ, 256])
    o_pik = dout("o_pik", [SEQ, 64])
    o_pconv = dout("o_pconv", [3, 1536])
    o_pmk = dout("o_pmk", [256, 512])
    o_pmv = dout("o_pmv", [256, 512])
    o_sk = dout("o_sk", [128, 256])
    o_sv = dout("o_sv", [128, 256])
    o_sik = dout("o_sik", [128, 64])
    o_sconv = dout("o_sconv", [16, 3, 1536])
    DBG = cfg.get("debug", False)
    skind = "ExternalOutput" if DBG else "Internal"
    cmk_d = din("cache_mem_k", [16, 256, 512])
    cmv_d = din("cache_mem_v", [16, 256, 512])
    cmask_d = din("cmask", [128, 2048])
    selw_d = din("selw", [128, 64])
    ckv_d = din("cache_kv", [2560 * 128, 512])
    cik_d = din("cache_idx_k", [2560 * 128, 64])
    pt_d = din("page_table", [1, 256], I32)
    pen_d = din("pen", [17, 128, 256])
    dconst_d = din("dconst", [128, 64])
    gconst_d = din("gconst", [128, 10 * 128 + 16 + 2048])
    gconvT_d = din("gconvT", [128, 48])
    sgdn_d = din("state_gdn", [16, 4, 128, 128])
    sconv_d = din("state_conv", [48, 1536])
    o_pgdn = dout("o_pgdn", [4, 128, 128])
    o_sgdn = dout("o_sgdn", [16, 4, 128, 128])
    if DBG:
        dbg_tm = dout("dbg_tm", [128, 1536])
        dbg_sm = dout("dbg_sm", [128, 64])
        dbg_o = dout("dbg_o", [128, 512])
        dbg_mask = dout("dbg_mask", [128, 512])
        dbg_bis = dout("dbg_bis", [128, 44])
        dbg_sc = dout("dbg_sc", [128, 512])
        dbg_uw = dout("dbg_uw", [128, 256])
        dbg_vn = dout("dbg_vn", [128, 128])
        dbg_aq = dout("dbg_aq", [128, 128])
        dbg_tt = dout("dbg_tt", [128, 128])
        dbg_wtm = dout("dbg_wtm", [128, 2048])
        dbg_p1 = dout("dbg_p1", [128, 128])
    a_sc = nc.dram_tensor("a_sc", [17 * 128, 512], F32, kind=skind).ap()
    m_sc = nc.dram_tensor("m_sc", [17 * 128, 512], F32, kind=skind).ap()
    g_sc = nc.dram_tensor("g_sc", [33 * 128, 512], F32, kind=skind).ap()
    x2_sc = nc.dram_tensor("x2_sc", [17 * 128, D], F32, kind=skind).ap()
    a_sc_b = [Buf() for _ in range(17)]
    m_sc_b = [Buf() for _ in range(17)]
    g_sc_b = [Buf() for _ in range(33)]
    x2_sc_b = [Buf() for _ in range(17)]
    outs_done = []

    with contextlib.ExitStack() as glob:
        def sb(st, name, shape, dt=F32):
            return T(st.enter_context(nc.sbuf_tensor("sb_" + name, list(shape), dt)), name)

        psum = [T(glob.enter_context(nc.psum_tensor("ps%d" % i, [128, 512], F32)), "ps%d" % i)
                for i in range(8)]
        ps_i = [0]
        ps_hold = set()

        def ps(hold=False):
            while True:
                idx = ps_i[0] % 8
                ps_i[0] += 1
                if idx not in ps_hold:
                    break
            if hold:
                ps_hold.add(idx)
            return psum[idx]

        def ps_release(p):
            ps_hold.discard(psum.index(p))

        ident = sb(glob, "ident", [128, 128])
        gains = sb(glob, "gains", [128, 24])
        hv = sb(glob, "hv", [128, 576])
        k.dma("sp", ident[:], ident_d[:, :], writes=[ident.b])
        k.dma("sp", gains[:], gains_d[:, :], writes=[gains.b])
        k.dma("sp", hv[:], hv_d[0:1, :].partition_broadcast(128), writes=[hv.b])

        cmask = sb(glob, "cmask", [128, 16, 128], BF16)
        selw = sb(glob, "selw", [128, 64])
        k.dma("pool", cmask[:], cmask_d[:, :].rearrange("p (b t) -> p b t", b=16), writes=[cmask.b])
        k.dma("sp", selw[:], selw_d[:, :], writes=[selw.b])
        MKT = sb(glob, "MKT", [128, 4, 256], BF16)
        MVa = sb(glob, "MVa", [128, 2, 4, 129], BF16)
        k.op("pool", lambda e: e.memset(MVa[:], 1.0), writes=[MVa.b])
        zb = sb(glob, "zb", [128, 512], BF16)
        k.op("pool", lambda e: e.memset(zb[:], 0.0), writes=[zb.b])

        def ps_zero(p):
            k.op("pe", lambda e: e.matmul(p[:, :], lhsT=zb[:, 0:128], rhs=zb[:, :], start=True, stop=False),
                 reads=[zb.b], writes=[p.b])


        def kside_store(kk_ap, vv_ap, ii_ap, rd, KT_ap, IKT_ap, VA_ap, wr):
            if kk_ap is not None:
                p = ps()
                for g in range(4):
                    k.op("pe", lambda e, g=g: e.transpose(p[0:64, g * 128:(g + 1) * 128],
                                                          kk_ap[:, g * 64:(g + 1) * 64], ident[:]),
                         reads=rd + [ident.b], writes=[p.b])
                k.op("act", lambda e: e.activation(out=KT_ap, in_=p[0:64, :].rearrange("p (g s) -> p g s", g=4),
                                                   func=AF.Copy), reads=[p.b], writes=wr)
            if ii_ap is not None:
                p2 = ps()
                k.op("pe", lambda e: e.transpose(p2[0:64, 0:128], ii_ap, ident[:]),
                     reads=rd + [ident.b], writes=[p2.b])
                k.op("act", lambda e: e.activation(out=IKT_ap, in_=p2[0:64, 0:128], func=AF.Copy),
                     reads=[p2.b], writes=wr)
            if vv_ap is not None:
                k.op("pool", lambda e: e.tensor_copy(VA_ap, vv_ap.rearrange("p (g d) -> p g d", g=4)),
                     reads=rd, writes=wr)

        def pipe_ahead(gens, depth):
            n = len(gens)

            def toB(g):
                for tok in g:
                    if tok == "B":
                        return
            for j_ in range(min(depth, n)):
                toB(gens[j_])
            for j_ in range(n):
                for _ in gens[j_]:
                    pass
                if j_ + depth < n:
                    toB(gens[j_ + depth])

        def load_xT(st_tiles, src_ap, gain_col, q="sp", rd=()):
            x32, xn, xT, stat = st_tiles
            k.dma(q, x32[:], src_ap, reads=list(rd), writes=[x32.b])
            k.op("act", lambda e: e.activation(out=xn[:], in_=x32[:], func=AF.Square,
                                               accum_out=stat[:, 0:1]),
                 reads=[x32.b], writes=[xn.b, stat.b])
            k.op("dve", lambda e: e.tensor_scalar(stat[:, 1:2], stat[:, 0:1], 1.0 / D, EPS,
                                                  op0=ALU.mult, op1=ALU.add),
                 reads=[stat.b], writes=[stat.b])
            k.op("act", lambda e: e.activation(out=stat[:, 3:4], in_=stat[:, 1:2], func=AF.Sqrt),
                 reads=[stat.b], writes=[stat.b])
            k.op("dve", lambda e: e.reciprocal(stat[:, 2:3], stat[:, 3:4]),
                 reads=[stat.b], writes=[stat.b])
            k.op("act", lambda e: e.activation(out=xn[:], in_=x32[:], func=AF.Copy,
                                               scale=stat[:, 2:3]),
                 reads=[x32.b, stat.b], writes=[xn.b])
            for half in range(2):
                p = ps()
                for j in range(4):
                    kc = half * 4 + j
                    k.op("pe", lambda e, kc=kc, j=j: e.transpose(
                        p[:, j * 128:(j + 1) * 128], xn[:, kc * 128:(kc + 1) * 128], ident[:]),
                        reads=[xn.b, ident.b], writes=[p.b])
                g = gains[:, gain_col + half * 4: gain_col + half * 4 + 4]
                k.op("dve", lambda e, half=half, g=g, p=p: e.tensor_tensor(
                    xT[:, half * 4:(half + 1) * 4, :],
                    p[:, :].rearrange("p (a b) -> p a b", a=4),
                    g.unsqueeze(2).to_broadcast([128, 4, 128]), op=ALU.mult),
                    reads=[p.b, gains.b], writes=[xT.b])
            return x32, xT

        def linear(xT, W, c0, ncol, p, kcs=KC):
            for kc in range(kcs):
                k.op("pe", lambda e, kc=kc: e.matmul(
                    p[:, 0:ncol], lhsT=xT[:, kc, :], rhs=W[:, kc, c0:c0 + ncol],
                    start=(kc == 0), stop=(kc == kcs - 1)),
                    reads=[xT.b, W.b], writes=[p.b])

        def load_w(W, src, c0, ncol, dst0=0, rows=D):
            nkc = rows // 128
            for kc in range(nkc):
                k.dma("pool", W[:, kc, dst0:dst0 + ncol], src[kc * 128:(kc + 1) * 128, c0:c0 + ncol],
                      writes=[W.b])

        def head_rmsnorm(st, src_ps, nh, dh, gain_ap, out32, tmp, stat, name):
            k.op("act", lambda e: e.activation(out=tmp[:, 0:nh * dh], in_=src_ps, func=AF.Square),
                 reads=[st], writes=[tmp.b])
            k.op("dve", lambda e: e.tensor_reduce(
                stat[:, 0:nh], tmp[:, 0:nh * dh].rearrange("p (h d) -> p h d", h=nh),
                axis=AX.X, op=ALU.add), reads=[tmp.b], writes=[stat.b])
            k.op("dve", lambda e: e.tensor_scalar(stat[:, 8:8 + nh], stat[:, 0:nh], 1.0 / dh, EPS,
                                                  op0=ALU.mult, op1=ALU.add),
                 reads=[stat.b], writes=[stat.b])
            k.op("act", lambda e: e.activation(out=stat[:, 0:nh], in_=stat[:, 8:8 + nh], func=AF.Sqrt),
                 reads=[stat.b], writes=[stat.b])
            k.op("dve", lambda e: e.reciprocal(stat[:, 16:16 + nh], stat[:, 0:nh]),
                 reads=[stat.b], writes=[stat.b])
            k.op("dve", lambda e: e.tensor_tensor(
                out32[:, 0:nh * dh].rearrange("p (h d) -> p h d", h=nh),
                src_ps.rearrange("p (h d) -> p h d", h=nh),
                stat[:, 16:16 + nh].unsqueeze(2).to_broadcast([128, nh, dh]), op=ALU.mult),
                reads=[st, stat.b], writes=[out32.b])
            k.op("dve", lambda e: e.tensor_tensor(
                out32[:, 0:nh * dh].rearrange("p (h d) -> p h d", h=nh),
                out32[:, 0:nh * dh].rearrange("p (h d) -> p h d", h=nh),
                gain_ap.unsqueeze(1).to_broadcast([128, nh, dh]), op=ALU.mult),
                reads=[out32.b, hv.b], writes=[out32.b])

        PH = cfg.get('phases', 'BCGEFD')
        with contextlib.ExitStack() as st:
          if 'B' in PH:
              Wm = sb(st, "Wm", [128, KC, 1024], BF16)
              load_w(Wm, w_mem_kv, 0, 1024)
              tiles = (sb(st, "mx32", [128, D]), sb(st, "mxn", [128, D]),
                       sb(st, "mxT", [128, KC, 128], BF16), sb(st, "mstat", [128, 4]))
              tmp = sb(st, "mtmp", [128, 512])
              hstat = sb(st, "mhstat", [128, 24])
              mk32 = [sb(st, "mk32_%d" % i, [128, 512]) for i in range(2)]
              mv32 = [sb(st, "mv32_%d" % i, [128, 512]) for i in range(2)]
              for mt in range(2):
                  _, xT = load_xT(tiles, mem[mt * 128:(mt + 1) * 128, :], 8)
                  pk = ps()
                  linear(xT, Wm, 0, 512, pk)
                  pv = ps()
                  linear(xT, Wm, 512, 512, pv)
                  head_rmsnorm(pk.b, pk[:, :], 4, 128, hv[:, 256:384], mk32[mt], tmp, hstat, "mk")
                  k.op("act", lambda e, mt=mt, pv=pv: e.activation(out=mv32[mt][:], in_=pv[:, :], func=AF.Copy),
                       reads=[pv.b], writes=[mv32[mt].b])
                  p = ps()
                  for h in range(4):
                      k.op("pe", lambda e, h=h, p=p, mt=mt: e.transpose(
                          p[:, h * 128:(h + 1) * 128], mk32[mt][:, h * 128:(h + 1) * 128], ident[:]),
                          reads=[mk32[mt].b, ident.b], writes=[p.b])
                  k.op("act", lambda e, p=p, mt=mt: e.activation(
                      out=MKT[:, :, mt * 128:(mt + 1) * 128], in_=p[:, :].rearrange("p (h m) -> p h m", h=4),
                      func=AF.Copy), reads=[p.b], writes=[MKT.b])
                  k.op("dve", lambda e, mt=mt: e.tensor_copy(
                      MVa[:, mt, :, 0:128], mv32[mt][:, :].rearrange("p (h d) -> p h d", h=4)),
                      reads=[mv32[mt].b], writes=[MVa.b])
                  k.dma("sp", o_pmk[mt * 128:(mt + 1) * 128, :], mk32[mt][:], reads=[mk32[mt].b])
                  k.dma("sp", o_pmv[mt * 128:(mt + 1) * 128, :], mv32[mt][:], reads=[mv32[mt].b])
                  outs_done += [mk32[mt].b, mv32[mt].b]

        k.barrier()
        with contextlib.ExitStack() as st:
          if 'G' in PH:
              Wg2 = sb(st, "Wg2", [128, KC, 2056], BF16)
              load_w(Wg2, w_in, O_GQKV, 2056, 0)
              gcs = sb(st, "gcs", [128, 10 * 128 + 16 + 2048])
              k.dma("sp", gcs[:], gconst_d[:, :], writes=[gcs.b])
              cv = lambda i: gcs[:, i * 128:(i + 1) * 128]
              LTRI, BONES, SLm, SUIm = [cv(0), cv(1)], [cv(2), cv(3)], [cv(4), cv(5)], [cv(6), cv(7)]
              USTR, ONES = cv(8), cv(9)
              G8 = gcs[:, 1280:1296]
              CM32 = gcs[:, 1296:1296 + 2048].rearrange("p (b t) -> p b t", b=16)
              gcv = sb(st, "gcv", [128, 48])
              k.dma("sp", gcv[:], gconvT_d[:, :], writes=[gcv.b])
              Dg = sb(st, "Dg", [128, 12, 4, 128], BF16)
              for cc in range(12):
                  for jj in range(4):
                      k.op("dve", lambda e, cc=cc, jj=jj: e.tensor_scalar(
                          Dg[:, cc, jj, :], ident[:], gcv[:, cc * 4 + jj:cc * 4 + jj + 1], None, op0=ALU.mult),
                          reads=[ident.b, gcv.b], writes=[Dg.b])
              negA = sb(st, "negA", [128, 4])
              k.op("act", lambda e: e.activation(out=negA[:], in_=hv[:, 516:520], func=AF.Exp),
                   reads=[hv.b], writes=[negA.b])
              k.op("dve", lambda e: e.tensor_scalar(negA[:], negA[:], -1.0, None, op0=ALU.mult),
                   reads=[negA.b], writes=[negA.b])
              Sst = [sb(st, "Sst%d" % h, [128, 128]) for h in range(4)]
              for h in range(4):
                  k.op("pool", lambda e, h=h: e.memset(Sst[h][:], 0.0), writes=[Sst[h].b])
              tsets = [(sb(st, "gx32_%d" % i, [128, D]), sb(st, "gxn_%d" % i, [128, D]),
                        sb(st, "gxT_%d" % i, [128, KC, 128], BF16), sb(st, "gstat_%d" % i, [128, 4]))
                       for i in range(1)] * 2
              Up = sb(st, "Up", [128, 12, 131], BF16)
              k.op("pool", lambda e: e.memset(Up[:], 0.0), writes=[Up.b])
              UC = sb(st, "UC", [128, 12, 128])
              TM = sb(st, "TM", [128, 1536])
              sq = sb(st, "gsq", [128, 1024])
              sgz2 = [sb(st, "sgz%d" % q, [128, 512]) for q in range(1)]
              sm2 = [sb(st, "gsm%d" % q, [128, 64]) for q in range(1)]
              scal = sb(st, "gscal", [128, 6, 4])
              KQ = sb(st, "KQ", [128, 12, 128])
              KQT2 = [sb(st, "KQT%d" % q, [128, 12, 128]) for q in range(1)]
              KBG2 = [sb(st, "KBG%d" % q, [128, 4, 128]) for q in range(1)]
              KTL2 = [sb(st, "KTL%d" % q, [128, 4, 128]) for q in range(1)]
              VB2 = [sb(st, "VB%d" % q, [128, 4, 128]) for q in range(1)]
              O32 = sb(st, "O32", [128, 512])
              go32 = sb(st, "go32", [128, 512])
              gtmp = sb(st, "ggtmp", [128, 512])
              ghstat = sb(st, "ghstat", [128, 24])
              hb = [dict(GU=sb(st, "GU%d" % i, [128, 128]), G2=sb(st, "G2%d" % i, [128, 256]),
                         t1=sb(st, "t1%d" % i, [128, 128]), aq=sb(st, "aqkT%d" % i, [128, 128]),
                         P=[sb(st, "P%d_%d" % (i, q), [128, 256]) for q in range(2)],
                         TT=[sb(st, "TT%d_%d" % (i, q), [128, 128]) for q in range(2)],
                         UW=sb(st, "UW%d" % i, [128, 256]), vn=sb(st, "vn%d" % i, [128, 128]))
                    for i in range(4)]
              DK5 = 128.0 ** -0.5

              def gdn_tile(j, sm_st=None):
                  smp = (j == NT_ALL)
                  m = 1 if smp else 0
                  bf_ = j % 2
                  sgz, KQT, KBG, KTL, VB, sm = sgz2[bf_], KQT2[bf_], KBG2[bf_], KTL2[bf_], VB2[bf_], sm2[bf_]
                  src = x_smp[:, :] if smp else x_all[j * 128:(j + 1) * 128, :]
                  x32, xT = load_xT(tsets[j % 2], src, 0)
                  yield
                  U = sm_st["Us"] if smp else Up
                  for cc0 in (0, 4, 8):
                      p = ps()
                      for c4 in range(4):
                          cc = cc0 + c4
                          for kc in range(KC):
                              k.op("pe", lambda e, kc=kc, cc=cc, c4=c4, p=p: e.matmul(
                                  p[:, c4 * 128:(c4 + 1) * 128], lhsT=Wg2[:, kc, cc * 128:(cc + 1) * 128],
                                  rhs=xT[:, kc, :], start=(kc == 0), stop=(kc == KC - 1)),
                                  reads=[Wg2.b, xT.b], writes=[p.b])
                      if smp:
                          for c4 in range(4):
                              k.op("act", lambda e, p=p, cc0=cc0, c4=c4: e.activation(
                                  out=U[:, cc0 + c4, :, 3:11],
                                  in_=p[:, c4 * 128:(c4 + 1) * 128].rearrange("p (b t) -> p b t", b=16),
                                  func=AF.Copy), reads=[p.b], writes=[U.b])
                      else:
                          k.op("act", lambda e, p=p, cc0=cc0: e.activation(
                              out=U[:, cc0:cc0 + 4, 3:131], in_=p[:, :].rearrange("p (a t) -> p a t", a=4),
                              func=AF.Copy), reads=[p.b], writes=[U.b])
                      yield
                  for cc0 in (0, 4, 8):
                      p = ps()
                      for c4 in range(4):
                          cc = cc0 + c4
                          for jj in range(4):
                              rhs = U[:, cc, :, jj:jj + 8] if smp else U[:, cc, jj:jj + 128]
                              k.op("pe", lambda e, jj=jj, cc=cc, c4=c4, p=p, rhs=rhs: e.matmul(
                                  p[:, c4 * 128:(c4 + 1) * 128], lhsT=Dg[:, cc, jj, :], rhs=rhs,
                                  start=(jj == 0), stop=(jj == 3)), reads=[Dg.b, U.b], writes=[p.b])
                      k.op("act", lambda e, p=p, cc0=cc0: e.activation(
                          out=UC[:, cc0:cc0 + 4, :], in_=p[:, :].rearrange("p (a t) -> p a t", a=4),
                          func=AF.Silu), reads=[p.b], writes=[UC.b])
                      yield
                  if not smp:
                      k.op("dve", lambda e: e.tensor_copy(U[:, :, 0:3], U[:, :, 128:131]),
                           reads=[U.b], writes=[U.b])
                  for cc0 in (0, 4, 8):
                      p = ps()
                      for c4 in range(4):
                          k.op("pe", lambda e, cc0=cc0, c4=c4, p=p: e.transpose(
                              p[:, c4 * 128:(c4 + 1) * 128], UC[:, cc0 + c4, :], ident[:]),
                              reads=[UC.b, ident.b], writes=[p.b])
                      k.op("act", lambda e, p=p, cc0=cc0: e.activation(
                          out=TM[:, cc0 * 128:(cc0 + 4) * 128], in_=p[:, :], func=AF.Copy),
                          reads=[p.b], writes=[TM.b])
                      yield
                  pgz = ps()
                  linear(xT, Wg2, 1536, 512, pgz)
                  pgb = ps()
                  linear(xT, Wg2, 2048, 8, pgb)
                  k.op("act", lambda e: e.activation(out=sgz[:], in_=pgz[:, :], func=AF.Silu),
                       reads=[pgz.b], writes=[sgz.b])
                  k.op("act", lambda e: e.activation(out=sm[:, 0:4], in_=pgb[:, 0:4], func=AF.Sigmoid),
                       reads=[pgb.b], writes=[sm.b])
                  k.op("dve", lambda e: e.tensor_tensor(sm[:, 4:8], pgb[:, 4:8], hv[:, 512:516], op=ALU.add),
                       reads=[pgb.b, hv.b], writes=[sm.b])
                  k.op("act", lambda e: e.activation(out=sm[:, 8:12], in_=sm[:, 4:8], func=AF.Exp),
                       reads=[sm.b], writes=[sm.b])
                  k.op("act", lambda e: e.activation(out=sm[:, 12:16], in_=sm[:, 8:12], func=AF.Ln, bias=1.0),
                       reads=[sm.b], writes=[sm.b])
                  k.op("dve", lambda e: e.tensor_tensor(sm[:, 16:20], sm[:, 12:16], negA[:], op=ALU.mult),
                       reads=[sm.b, negA.b], writes=[sm.b])
                  gt = sm[:, 16:20]
                  yield
                  k.op("act", lambda e: e.activation(out=sq[:], in_=TM[:, 0:1024], func=AF.Square),
                       reads=[TM.b], writes=[sq.b])
                  k.op("dve", lambda e: e.tensor_reduce(
                      sm[:, 20:28], sq[:, :].rearrange("p (h d) -> p h d", h=8), axis=AX.X, op=ALU.add),
                      reads=[sq.b], writes=[sm.b])
                  k.op("dve", lambda e: e.tensor_scalar(sm[:, 20:28], sm[:, 20:28], EPS, None, op0=ALU.add),
                       reads=[sm.b], writes=[sm.b])
                  k.op("act", lambda e: e.activation(out=sm[:, 28:36], in_=sm[:, 20:28], func=AF.Sqrt),
                       reads=[sm.b], writes=[sm.b])
                  k.op("dve", lambda e: e.reciprocal(sm[:, 36:44], sm[:, 28:36]), reads=[sm.b], writes=[sm.b])
                  rq, rk = sm[:, 36:40], sm[:, 40:44]
                  pc = ps()
                  k.op("pe", lambda e: e.matmul(pc[:, 0:4], lhsT=LTRI[m], rhs=gt, start=True, stop=True),
                       reads=[gcs.b, sm.b], writes=[pc.b])
                  k.op("pe", lambda e: e.matmul(pc[:, 4:8], lhsT=BONES[m], rhs=gt, start=True, stop=True),
                       reads=[gcs.b, sm.b], writes=[pc.b])
                  k.op("dve", lambda e: e.tensor_copy(sm[:, 44:52], pc[:, 0:8]), reads=[pc.b], writes=[sm.b])
                  k.op("act", lambda e: e.activation(out=sm[:, 52:56], in_=sm[:, 44:48], func=AF.Exp),
                       reads=[sm.b], writes=[sm.b])
                  k.op("dve", lambda e: e.tensor_tensor(sm[:, 56:60], sm[:, 48:52], sm[:, 44:48], op=ALU.subtract),
                       reads=[sm.b], writes=[sm.b])
                  k.op("act", lambda e: e.activation(out=sm[:, 56:60], in_=sm[:, 56:60], func=AF.Exp),
                       reads=[sm.b], writes=[sm.b])
                  k.op("act", lambda e: e.activation(out=sm[:, 60:64], in_=sm[:, 48:52], func=AF.Exp),
                       reads=[sm.b], writes=[sm.b])
                  egc, etl, dec, beta = sm[:, 52:56], sm[:, 56:60], sm[:, 60:64], sm[:, 0:4]
                  k.op("dve", lambda e: e.tensor_copy(scal[:, 0, :], rq), reads=[sm.b], writes=[scal.b])
                  k.op("dve", lambda e: e.scalar_tensor_tensor(out=scal[:, 1, :], in0=rq, scalar=DK5, in1=egc,
                                                               op0=ALU.mult, op1=ALU.mult),
                       reads=[sm.b], writes=[scal.b])
                  k.op("dve", lambda e: e.tensor_copy(scal[:, 2, :], rk), reads=[sm.b], writes=[scal.b])
                  k.op("dve", lambda e: e.tensor_tensor(scal[:, 5, :], rk, beta, op=ALU.mult),
                       reads=[sm.b], writes=[scal.b])
                  k.op("dve", lambda e: e.tensor_tensor(scal[:, 3, :], scal[:, 5, :], egc, op=ALU.mult),
                       reads=[sm.b, scal.b], writes=[scal.b])
                  k.op("dve", lambda e: e.tensor_tensor(scal[:, 4, :], rk, etl, op=ALU.mult),
                       reads=[sm.b], writes=[scal.b])
                  yield
                  TMq = TM[:, 0:512].rearrange("p (h d) -> p h d", h=4)
                  TMk = TM[:, 512:1024].rearrange("p (h d) -> p h d", h=4)
                  TMv = TM[:, 1024:1536].rearrange("p (h d) -> p h d", h=4)
                  bc = lambda a: a.unsqueeze(2).to_broadcast([128, 4, 128])
                  for dst, src_, sc_ in ((KQ[:, 0:4, :], TMq, scal[:, 0, :]), (KQ[:, 4:8, :], TMk, scal[:, 2, :]),
                                         (KQ[:, 8:12, :], TMq, scal[:, 1, :]), (KBG[:], TMk, scal[:, 3, :]),
                                         (KTL[:], TMk, scal[:, 4, :]), (VB[:], TMv, beta)):
                      wb_ = KQ.b if dst.tensor.name == KQ[:].tensor.name else (
                          KBG.b if dst.tensor.name == KBG[:].tensor.name else (
                              KTL.b if dst.tensor.name == KTL[:].tensor.name else VB.b))
                      k.op("dve", lambda e, dst=dst, src_=src_, sc_=sc_: e.tensor_tensor(
                          dst, src_, bc(sc_), op=ALU.mult), reads=[TM.b, scal.b, sm.b], writes=[wb_])
                  for cc0 in (0, 4, 8):
                      p = ps()
                      for c4 in range(4):
                          k.op("pe", lambda e, cc0=cc0, c4=c4, p=p: e.transpose(
                              p[:, c4 * 128:(c4 + 1) * 128], KQ[:, cc0 + c4, :], ident[:]),
                              reads=[KQ.b, ident.b], writes=[p.b])
                      k.op("act", lambda e, p=p, cc0=cc0: e.activation(
                          out=KQT[:, cc0:cc0 + 4, :], in_=p[:, :].rearrange("p (a t) -> p a t", a=4),
                          func=AF.Copy), reads=[p.b], writes=[KQT.b])
                      yield
                  if smp:
                      rhs3 = sm_st["rhs3"]
                      k.op("dve", lambda e: e.tensor_tensor(
                          rhs3[:], gt.unsqueeze(1).to_broadcast([128, 16, 4]),
                          G8.unsqueeze(2).to_broadcast([128, 16, 4]), op=ALU.mult),
                          reads=[sm.b, gcs.b], writes=[rhs3.b])
                      pdb = ps()
                      k.op("pe", lambda e: e.matmul(pdb[:, 0:64], lhsT=ONES,
                                                    rhs=rhs3[:].rearrange("p b h -> p (b h)"), start=True, stop=True),
                           reads=[gcs.b, rhs3.b], writes=[pdb.b])
                      decB = sm_st["decB"]
                      k.op("act", lambda e: e.activation(out=decB[:], in_=pdb[:, 0:64], func=AF.Exp),
                           reads=[pdb.b], writes=[decB.b])
                  def head_gen(h):
                      H = hb[h]
                      GU, G2, t1, aq, UW, vn = H["GU"], H["G2"], H["t1"], H["aq"], H["UW"], H["vn"]
                      KnT, QnT, DQT = KQT[:, 4 + h, :], KQT[:, h, :], KQT[:, 8 + h, :]
                      k.op("dve", lambda e: e.tensor_scalar(GU[:], USTR, gt[:, h:h + 1], None, op0=ALU.mult),
                           reads=[gcs.b, sm.b], writes=[GU.b])
                      pD = ps()
                      k.op("pe", lambda e: e.matmul(pD[:, 0:128], lhsT=LTRI[m], rhs=GU[:], start=True, stop=True),
                           reads=[gcs.b, GU.b], writes=[pD.b])
                      k.op("pe", lambda e: e.matmul(pD[:, 128:256], lhsT=GU[:], rhs=LTRI[m], start=True, stop=True),
                           reads=[gcs.b, GU.b], writes=[pD.b])
                      k.op("pe", lambda e: e.matmul(pD[:, 256:384], lhsT=KnT, rhs=KnT, start=True, stop=True),
                           reads=[KQT.b], writes=[pD.b])
                      k.op("pe", lambda e: e.matmul(pD[:, 384:512], lhsT=KnT, rhs=QnT, start=True, stop=True),
                           reads=[KQT.b], writes=[pD.b])
                      yield
                      k.op("act", lambda e: e.activation(out=G2[:], in_=pD[:, 0:256], func=AF.Exp),
                           reads=[pD.b], writes=[G2.b])
                      yield
                      k.op("dve", lambda e: e.tensor_tensor(t1[:], pD[:, 256:384], G2[:, 0:128], op=ALU.mult),
                           reads=[pD.b, G2.b], writes=[t1.b])
                      P0 = H["P"][0]
                      k.op("dve", lambda e: e.tensor_scalar(t1[:], t1[:], beta[:, h:h + 1], -1.0,
                                                            op0=ALU.mult, op1=ALU.mult),
                           reads=[t1.b, sm.b], writes=[t1.b])
                      k.op("dve", lambda e: e.tensor_tensor(P0[:, 0:128], t1[:], SLm[m], op=ALU.mult),
                           reads=[t1.b, gcs.b], writes=[P0.b])
                      k.op("dve", lambda e: e.tensor_tensor(t1[:], pD[:, 384:512], G2[:, 128:256], op=ALU.mult),
                           reads=[pD.b, G2.b], writes=[t1.b])
                      k.op("dve", lambda e: e.tensor_tensor(aq[:], t1[:], SUIm[m], op=ALU.mult),
                           reads=[t1.b, gcs.b], writes=[aq.b])
                      yield
                      pN = ps()
                      k.op("pe", lambda e: e.transpose(pN[:, 0:128], P0[:, 0:128], ident[:]),
                           reads=[P0.b, ident.b], writes=[pN.b])
                      k.op("act", lambda e: e.activation(out=P0[:, 128:256], in_=pN[:, 0:128], func=AF.Copy),
                           reads=[pN.b], writes=[P0.b])
                      TTc = H["TT"][0]
                      k.op("dve", lambda e: e.tensor_tensor(TTc[:], P0[:, 128:256], ident[:], op=ALU.add),
                           reads=[P0.b, ident.b], writes=[TTc.b])
                      yield
                      Pc = P0
                      for n in range(1, 7):
                          Pn = H["P"][n % 2]
                          TTn = H["TT"][n % 2]
                          pP = ps()
                          k.op("pe", lambda e, Pc=Pc, pP=pP: e.matmul(pP[:, 0:128], lhsT=Pc[:, 128:256], rhs=Pc[:, 0:128],
                                                                      start=True, stop=True),
                               reads=[Pc.b], writes=[pP.b])
                          if n < 6:
                              k.op("pe", lambda e, Pc=Pc, pP=pP: e.matmul(pP[:, 128:256], lhsT=Pc[:, 0:128],
                                                                          rhs=Pc[:, 128:256], start=True, stop=True),
                                   reads=[Pc.b], writes=[pP.b])
                          k.op("act", lambda e, Pn=Pn, pP=pP: e.activation(out=Pn[:], in_=pP[:, 0:256], func=AF.Copy),
                               reads=[pP.b], writes=[Pn.b])
                          yield
                          pT = ps()
                          k.op("pe", lambda e, Pn=Pn, pT=pT, TTc=TTc: e.matmul(pT[:, 0:128], lhsT=Pn[:, 0:128], rhs=TTc[:],
                                                                               start=True, stop=True),
                               reads=[Pn.b, TTc.b], writes=[pT.b])
                          k.op("dve", lambda e, TTn=TTn, TTc=TTc, pT=pT: e.tensor_tensor(
                              TTn[:], TTc[:], pT[:, 0:128], op=ALU.add), reads=[TTc.b, pT.b], writes=[TTn.b])
                          yield
                          Pc, TTc = Pn, TTn
                      pU = ps()
                      k.op("pe", lambda e, TTc=TTc: e.matmul(pU[:, 0:128], lhsT=TTc[:], rhs=VB[:, h, :], start=True, stop=True),
                           reads=[TTc.b, VB.b], writes=[pU.b])
                      k.op("pe", lambda e, TTc=TTc: e.matmul(pU[:, 128:256], lhsT=KBG[:, h, :], rhs=TTc[:], start=True, stop=True),
                           reads=[TTc.b, KBG.b], writes=[pU.b])
                      k.op("act", lambda e: e.activation(out=UW[:], in_=pU[:, 0:256], func=AF.Copy),
                           reads=[pU.b], writes=[UW.b])
                      yield
                      if not smp:
                          S_ = Sst[h]
                          p1 = ps()
                          k.op("pe", lambda e: e.matmul(p1[:, 0:128], lhsT=UW[:, 128:256], rhs=S_[:], start=True, stop=True),
                               reads=[UW.b, S_.b], writes=[p1.b])
                          k.op("dve", lambda e: e.tensor_tensor(vn[:], UW[:, 0:128], p1[:, 0:128], op=ALU.subtract),
                               reads=[UW.b, p1.b], writes=[vn.b])
                          yield
                          p2 = ps()
                          k.op("pe", lambda e: e.matmul(p2[:, 0:128], lhsT=DQT, rhs=S_[:], start=True, stop=False),
                               reads=[KQT.b, S_.b], writes=[p2.b])
                          k.op("pe", lambda e: e.matmul(p2[:, 0:128], lhsT=aq[:], rhs=vn[:], start=False, stop=True),
                               reads=[aq.b, vn.b], writes=[p2.b])
                          k.op("act", lambda e: e.activation(out=O32[:, h * 128:(h + 1) * 128], in_=p2[:, 0:128], func=AF.Copy),
                               reads=[p2.b], writes=[O32.b])
                          p3 = ps()
                          k.op("pe", lambda e: e.matmul(p3[:, 0:128], lhsT=KTL[:, h, :], rhs=vn[:], start=True, stop=True),
                               reads=[KTL.b, vn.b], writes=[p3.b])
                          k.op("dve", lambda e: e.scalar_tensor_tensor(out=S_[:], in0=S_[:], scalar=dec[:, h:h + 1],
                                                                       in1=p3[:, 0:128], op0=ALU.mult, op1=ALU.add),
                               reads=[S_.b, sm.b, p3.b], writes=[S_.b])
                      else:
                          Ssm, WTm, DQm, vm, So = (sm_st["Ssm"], sm_st["WTm"], sm_st["DQm"], sm_st["vm"], sm_st["So"])
                          decB = sm_st["decB"]
                          k.op("dve", lambda e: e.tensor_tensor(
                              WTm[:], UW[:, 128:256].unsqueeze(1).to_broadcast([128, 16, 128]), CM32, op=ALU.mult),
                              reads=[UW.b, gcs.b], writes=[WTm.b])
                          p1 = ps(hold=True)
                          for b in range(16):
                              k.op("pe", lambda e, b=b: e.matmul(p1[:, 0:128], lhsT=WTm[:, b, :], rhs=Ssm[:, b * 4 + h, :],
                                                                 start=(b == 0), stop=(b == 15)),
                                   reads=[WTm.b, Ssm.b], writes=[p1.b])
                          k.op("dve", lambda e: e.tensor_tensor(vn[:], UW[:, 0:128], p1[:, 0:128], op=ALU.subtract),
                               reads=[UW.b, p1.b], writes=[vn.b])
                          ps_release(p1)
                          k.op("dve", lambda e: e.tensor_tensor(
                              DQm[:], DQT.unsqueeze(1).to_broadcast([128, 16, 128]), CM32, op=ALU.mult),
                              reads=[KQT.b, gcs.b], writes=[DQm.b])
                          p2 = ps(hold=True)
                          for b in range(16):
                              k.op("pe", lambda e, b=b: e.matmul(p2[:, 0:128], lhsT=DQm[:, b, :], rhs=Ssm[:, b * 4 + h, :],
                                                                 start=(b == 0), stop=False),
                                   reads=[DQm.b, Ssm.b], writes=[p2.b])
                          k.op("pe", lambda e: e.matmul(p2[:, 0:128], lhsT=aq[:], rhs=vn[:], start=False, stop=True),
                               reads=[aq.b, vn.b], writes=[p2.b])
                          k.op("act", lambda e: e.activation(out=O32[:, h * 128:(h + 1) * 128], in_=p2[:, 0:128], func=AF.Copy),
                               reads=[p2.b], writes=[O32.b])
                          ps_release(p2)
                          k.op("dve", lambda e: e.tensor_tensor(
                              vm[:], vn[:].unsqueeze(1).to_broadcast([128, 16, 128]),
                              G8.unsqueeze(2).to_broadcast([128, 16, 128]), op=ALU.mult),
                              reads=[vn.b, gcs.b], writes=[vm.b])
                          for b in range(16):
                              p3 = ps()
                              k.op("pe", lambda e, b=b, p3=p3: e.matmul(p3[:, 0:128], lhsT=KTL[:, h, :], rhs=vm[:, b, :],
                                                                        start=True, stop=True),
                                   reads=[KTL.b, vm.b], writes=[p3.b])
                              so = So[b % 2]
                              k.op("dve", lambda e, b=b, p3=p3, so=so: e.scalar_tensor_tensor(
                                  out=so[:], in0=Ssm[:, b * 4 + h, :], scalar=decB[:, b * 4 + h:b * 4 + h + 1],
                                  in1=p3[:, 0:128], op0=ALU.mult, op1=ALU.add),
                                  reads=[Ssm.b, decB.b, p3.b], writes=[so.b])
                              k.dma("sp", o_sgdn[b, h], so[:], reads=[so.b])
                  if smp and DBG:
                      k.dma("sp", dbg_tm[:, :], TM[:], reads=[TM.b])
                      k.dma("sp", dbg_sm[:, :], sm[:], reads=[sm.b])
                      k.dma("sp", dbg_o[:, :], O32[:], reads=[O32.b])
                      H = hb[1]
                      k.dma("sp", dbg_uw[:, :], H["UW"][:], reads=[H["UW"].b])
                      k.dma("sp", dbg_vn[:, :], H["vn"][:], reads=[H["vn"].b])
                      k.dma("sp", dbg_aq[:, :], H["aq"][:], reads=[H["aq"].b])
                      k.dma("sp", dbg_tt[:, :], H["TT"][0][:], reads=[H["TT"][0].b])
                  yield "B"
                  if smp:
                      for h in range(4):
                          for _ in head_gen(h):
                              pass
                  else:
                      gens = [head_gen(h) for h in range(4)]
                      alive = [True] * 4
                      while any(alive):
                          for gi, g_ in enumerate(gens):
                              if alive[gi]:
                                  try:
                                      next(g_)
                                  except StopIteration:
                                      alive[gi] = False
                          yield
                  head_rmsnorm(O32.b, O32[:, :], 4, 128, hv[:, 384:512], go32, gtmp, ghstat, "go")
                  k.op("dve", lambda e: e.tensor_tensor(go32[:], go32[:], sgz[:], op=ALU.mult),
                       reads=[go32.b, sgz.b], writes=[go32.b])
                  k.dma("sp", g_sc[j * 128:(j + 1) * 128, :], go32[:], reads=[go32.b], writes=[g_sc_b[j]])

              with contextlib.ExitStack() as st2:
                  sm_st = dict(Us=sb(st2, "Us", [128, 12, 16, 11], BF16), Ssm=sb(st2, "Ssm", [128, 64, 128]),
                               WTm=sb(st2, "WTm", [128, 16, 128]), rhs3=sb(st2, "rhs3", [128, 16, 4]),
                               decB=sb(st2, "decB", [128, 64]),
                               So=[sb(st2, "So%d" % i, [128, 128]) for i in range(2)])
                  sm_st["DQm"] = sm_st["WTm"]
                  sm_st["vm"] = sm_st["WTm"]
                  Ssm = sm_st["Ssm"]
                  for b in range(16):
                      k.dma("sp", Ssm[:, b * 4:(b + 1) * 4, :], sgdn_d[b].rearrange("h k v -> k h v"), writes=[Ssm.b])
                  cb = sb(st2, "cb", [48, 1536])
                  k.dma("sp", cb[:], sconv_d[:, :], writes=[cb.b])
                  Us = sm_st["Us"]
                  for cc0 in (0, 4, 8):
                      p = ps()
                      for c4 in range(4):
                          cc = cc0 + c4
                          k.op("pe", lambda e, cc=cc, c4=c4, p=p: e.transpose(
                              p[:, c4 * 48:(c4 + 1) * 48], cb[:, cc * 128:(cc + 1) * 128], ident[0:48, 0:48]),
                              reads=[cb.b, ident.b], writes=[p.b])
                      for c4 in range(4):
                          k.op("act", lambda e, cc0=cc0, c4=c4, p=p: e.activation(
                              out=Us[:, cc0 + c4, :, 0:3],
                              in_=p[:, c4 * 48:(c4 + 1) * 48].rearrange("p (b t) -> p b t", b=16),
                              func=AF.Copy), reads=[p.b], writes=[Us.b])
                  for _ in gdn_tile(NT_ALL, sm_st):
                      pass
                  outs_done += [s_.b for s_ in sm_st["So"]]
                  k.barrier()
              sgz2.append(sb(st, "sgz1", [128, 512]))
              KQT2.append(sb(st, "KQT1", [128, 12, 128]))
              KBG2.append(sb(st, "KBG1", [128, 4, 128]))
              KTL2.append(sb(st, "KTL1", [128, 4, 128]))
              VB2.append(sb(st, "VB1", [128, 4, 128]))
              sm2.append(sb(st, "gsm1", [128, 64]))
              gensG = [gdn_tile(j) for j in range(NT_ALL)]
              for tok in gensG[0]:
                  if tok == "B":
                      break
              for j in range(NT_ALL):
                  cur = gensG[j]
                  nxt = gensG[j + 1] if j + 1 < NT_ALL else None
                  cur_alive, nxt_inA = True, nxt is not None
                  while cur_alive or nxt_inA:
                      if cur_alive:
                          try:
                              next(cur)
                          except StopIteration:
                              cur_alive = False
                      if nxt_inA:
                          if next(nxt) == "B":
                              nxt_inA = False
              for h in range(4):
                  k.dma("sp", o_pgdn[h], Sst[h][:], reads=[Sst[h].b])
              outs_done += [s_.b for s_ in Sst]

        k.barrier()
        kst = contextlib.ExitStack()
        KT = sb(kst, "KT", [64, 4, SEQ + 256], BF16)
        VA = sb(kst, "VA", [128, NT_ALL + 2, 4, 65], BF16)
        IKT = sb(kst, "IKT", [64, SEQ + 256], BF16)
        KTn = sb(kst, "KTn", [64, 4, 128], BF16)
        IKTn = sb(kst, "IKTn", [64, 128], BF16)
        VAn = sb(kst, "VAn", [128, 4, 65], BF16)
        k.op("pool", lambda e: e.memset(VA[:], 1.0), writes=[VA.b])
        k.op("pool", lambda e: e.memset(VAn[:], 1.0), writes=[VAn.b])
        k.barrier()
        with contextlib.ExitStack() as st:
          if 'C' in PH:
              NCOL = 576 + 1536
              Wk = sb(st, "Wk", [128, KC, NCOL], BF16)
              load_w(Wk, w_in, O_AK, 512, 0)
              load_w(Wk, w_in, O_IK, 64, 512)
              load_w(Wk, w_in, O_GQKV, 1536, 576)
              tsets = [(sb(st, "cx32_%d" % i, [128, D]), sb(st, "cxn_%d" % i, [128, D]),
                        sb(st, "cxT_%d" % i, [128, KC, 128], BF16), sb(st, "cstat_%d" % i, [128, 4]))
                       for i in range(3)]
              tmp = sb(st, "ctmp", [128, 512])
              hstat = sb(st, "chstat", [128, 24])
              ko = [sb(st, "ko%d" % i, [128, 256]) for i in range(2)]
              vo = [sb(st, "vo%d" % i, [128, 256]) for i in range(2)]
              io = [sb(st, "io%d" % i, [128, 64]) for i in range(2)]
              gq = sb(st, "gq", [128, 1536])
              def c_gen(j):
                  smp = (j == NT_ALL)
                  src = x_smp[:, :] if smp else x_all[j * 128:(j + 1) * 128, :]
                  _, xT = load_xT(tsets[j % 3], src, 0)
                  yield "B"
                  pkv = ps()
                  linear(xT, Wk, 0, 512, pkv)
                  pik = ps()
                  linear(xT, Wk, 512, 64, pik)
                  kk, vv, ii = ko[j % 2], vo[j % 2], io[j % 2]
                  head_rmsnorm(pkv.b, pkv[:, 0:256], 4, 64, hv[:, 64:128], kk, tmp, hstat, "ak")
                  k.op("act", lambda e, vv=vv, pkv=pkv: e.activation(out=vv[:], in_=pkv[:, 256:512], func=AF.Copy),
                       reads=[pkv.b], writes=[vv.b])
                  k.op("act", lambda e, ii=ii, pik=pik: e.activation(out=ii[:], in_=pik[:, 0:64], func=AF.Copy),
                       reads=[pik.b], writes=[ii.b])
                  if smp:
                      kside_store(kk[:], vv[:], ii[:], [kk.b, vv.b, ii.b], KTn[:], IKTn[:], VAn[:, :, 0:64],
                                  [KTn.b, IKTn.b, VAn.b])
                  else:
                      kside_store(kk[:], vv[:], ii[:], [kk.b, vv.b, ii.b], KT[:, :, j * 128:(j + 1) * 128],
                                  IKT[:, j * 128:(j + 1) * 128], VA[:, j, :, 0:64], [KT.b, IKT.b, VA.b])
                  if smp:
                      k.dma("sp", o_sk[:, :], kk[:], reads=[kk.b])
                      k.dma("sp", o_sv[:, :], vv[:], reads=[vv.b])
                      k.dma("sp", o_sik[:, :], ii[:], reads=[ii.b])
                  else:
                      k.dma("sp", o_pk[j * 128:(j + 1) * 128, :], kk[:], reads=[kk.b])
                      k.dma("sp", o_pv[j * 128:(j + 1) * 128, :], vv[:], reads=[vv.b])
                      k.dma("sp", o_pik[j * 128:(j + 1) * 128, :], ii[:], reads=[ii.b])
                  if j >= NT_ALL - 1:
                      for c in range(3):
                          pg = ps()
                          linear(xT, Wk, 576 + c * 512, 512, pg)
                          k.op("act", lambda e, c=c, pg=pg: e.activation(
                              out=gq[:, c * 512:(c + 1) * 512], in_=pg[:, :], func=AF.Copy),
                              reads=[pg.b], writes=[gq.b])
                      if smp:
                          for t3 in range(3):
                              k.dma("sp", o_sconv[:, t3, :], gq[5 + t3::8, :], reads=[gq.b])
                      else:
                          k.dma("sp", o_pconv[:, :], gq[125:128, :], reads=[gq.b])
              pipe_ahead([c_gen(j) for j in range(NT_ALL + 1)], 2)
              outs_done += [b.b for b in ko + vo + io] + [gq.b]

        k.barrier()
        with contextlib.ExitStack() as st:
          if 'E' in PH:
              NQ = 512 + 256 + 4 + 512
              Wq = sb(st, "Wq", [128, KC, NQ], BF16)
              load_w(Wq, w_in, O_AQ, 512, 0)
              load_w(Wq, w_in, O_IQ, 256, 512)
              load_w(Wq, w_in, O_IW, 4, 768)
              load_w(Wq, w_in, O_MQ, 512, 772)
              tsets = [(sb(st, "ex32_%d" % i, [128, D]), sb(st, "exn_%d" % i, [128, D]),
                        sb(st, "exT_%d" % i, [128, KC, 128], BF16), sb(st, "estat_%d" % i, [128, 4]))
                       for i in range(1)] * 2
              tmp = sb(st, "etmp", [128, 512])
              hstat = sb(st, "ehstat", [128, 24])
              mq32 = sb(st, "mq32", [128, 512])
              MQT2 = [sb(st, "MQT%d" % q, [128, 4, 128], BF16) for q in range(3)]
              PT = sb(st, "PT", [128, 2, 4, 128], BF16)
              tE = sb(st, "tE", [128, 4, 128], BF16)
              rec = sb(st, "rec", [128, 8])
              mo32 = [sb(st, "mo32_%d" % i, [128, 512]) for i in range(2)]
              ao32 = [sb(st, "ao32_%d" % i, [128, 512]) for i in range(2)]
              mkb = MKTb = MVb = None
              SC_M = 128.0 ** -0.5

              def mem_scores(MKT_, MQT_, mb, dstPT, cm=None):
                  pS = ps()
                  for h in range(4):
                      k.op("pe", lambda e, h=h: e.matmul(
                          pS[:, h * 128:(h + 1) * 128], lhsT=MKT_[:, h, mb * 128:(mb + 1) * 128],
                          rhs=MQT_[:, h, :], start=True, stop=True),
                          reads=[MKT_.b, MQT_.b], writes=[pS.b])
                  if cm is None:
                      k.op("act", lambda e: e.activation(
                          out=dstPT[:, mb, :, :], in_=pS[:, :].rearrange("p (h t) -> p h t", h=4),
                          func=AF.Exp, scale=SC_M), reads=[pS.b], writes=[dstPT.b])
                  else:
                      k.op("act", lambda e: e.activation(
                          out=tE[:], in_=pS[:, :].rearrange("p (h t) -> p h t", h=4),
                          func=AF.Exp, scale=SC_M), reads=[pS.b], writes=[tE.b])
                      k.op("dve", lambda e: e.tensor_tensor(
                          dstPT[:, mb, :, :], tE[:], cm.unsqueeze(1).to_broadcast([128, 4, 128]), op=ALU.mult),
                          reads=[tE.b, cmask.b], writes=[dstPT.b])

              def mem_pv(pO, PT_, MV_, first, last):
                  for h in range(4):
                      for mb in range(2):
                          k.op("pe", lambda e, h=h, mb=mb: e.matmul(
                              pO[h // 2][:, (h % 2) * 129:(h % 2) * 129 + 129], lhsT=PT_[:, mb, h, :],
                              rhs=MV_[:, mb, h, :], start=False, stop=(last and mb == 1)),
                              reads=[PT_.b, MV_.b], writes=[pO[h // 2].b])

              NIT = 18
              dcs = sb(st, "dcs", [128, 64])
              k.dma("sp", dcs[:], dconst_d[:, :], writes=[dcs.b])
              score = sb(st, "score", [128, SEQ])
              junk = sb(st, "junk", [128, SEQ], BF16)
              maskT2 = [sb(st, "maskT%d" % q, [128, NT_ALL, 128], BF16) for q in range(2)]
              Eb = [sb(st, "Eb%d" % i, [128, 8, 128], BF16) for i in range(2)]
              Pm = [sb(st, "Pm%d" % i, [128, 8, 128], BF16) for i in range(2)]
              QT2 = [sb(st, "QT%d" % q, [64, 8, 128], BF16) for q in range(3)]
              IQT = sb(st, "IQT", [64, 4, 128], BF16)
              aq32 = sb(st, "aq32", [128, 512])
              iq32 = sb(st, "iq32", [128, 260])
              wst = sb(st, "wst", [128, 16])
              bis = sb(st, "bis", [128, 8 + 2 * NIT])
              rtmp = sb(st, "rtmp", [128, 512])
              rtmpB = sb(st, "rtmpB", [128, 512])
              pent = sb(st, "pent", [128, 256])
              den8 = sb(st, "den8", [128, 8])
              SC_A = 64.0 ** -0.5
              cmask32e = dcs[:, 40:56]

              def dsa_q(xT, QTb):
                  pq = ps()
                  linear(xT, Wq, 0, 512, pq)
                  head_rmsnorm(pq.b, pq[:, :], 8, 64, hv[:, 0:64], aq32, tmp, hstat, "aq")
                  for half in range(2):
                      p = ps()
                      for hh in range(4):
                          k.op("pe", lambda e, hh=hh, half=half, p=p: e.transpose(
                              p[0:64, hh * 128:(hh + 1) * 128],
                              aq32[:, (half * 4 + hh) * 64:(half * 4 + hh + 1) * 64], ident[:]),
                              reads=[aq32.b, ident.b], writes=[p.b])
                      k.op("act", lambda e, half=half, p=p: e.activation(
                          out=QTb[:, half * 4:(half + 1) * 4, :], in_=p[0:64, :].rearrange("p (h t) -> p h t", h=4),
                          func=AF.Copy), reads=[p.b], writes=[QTb.b])
                  yield
                  pi = ps()
                  linear(xT, Wq, 512, 260, pi)
                  k.op("act", lambda e: e.activation(out=iq32[:], in_=pi[:, 0:260], func=AF.Copy),
                       reads=[pi.b], writes=[iq32.b])
                  p = ps()
                  for hh in range(4):
                      k.op("pe", lambda e, hh=hh, p=p: e.transpose(
                          p[0:64, hh * 128:(hh + 1) * 128], iq32[:, hh * 64:(hh + 1) * 64], ident[:]),
                          reads=[iq32.b, ident.b], writes=[p.b])
                  k.op("act", lambda e, p=p: e.activation(
                      out=IQT[:], in_=p[0:64, :].rearrange("p (h t) -> p h t", h=4), func=AF.Copy),
                      reads=[p.b], writes=[IQT.b])
                  yield
                  k.op("act", lambda e: e.activation(out=wst[:, 0:4], in_=iq32[:, 256:260], func=AF.Abs),
                       reads=[iq32.b], writes=[wst.b])
                  k.op("dve", lambda e: e.tensor_scalar(wst[:, 4:8], iq32[:, 256:260], 0.0, 2.0,
                                                        op0=ALU.is_gt, op1=ALU.mult),
                       reads=[iq32.b], writes=[wst.b])
                  k.op("dve", lambda e: e.tensor_scalar(wst[:, 4:8], wst[:, 4:8], -1.0, None, op0=ALU.add),
                       reads=[wst.b], writes=[wst.b])
              def dsa_index(L, sc):
                  for c0 in range(0, L, 512):
                      n = min(512, L - c0)
                      for hh in range(4):
                          pS = ps()
                          k.op("pe", lambda e, hh=hh, pS=pS, c0=c0, n=n: e.matmul(
                              pS[:, 0:n], lhsT=IQT[:, hh, :], rhs=IKT[:, c0:c0 + n], start=True, stop=True),
                              reads=[IQT.b, IKT.b], writes=[pS.b])
                          rt_ = rtmp if hh % 2 == 0 else rtmpB
                          k.op("act", lambda e, hh=hh, pS=pS, n=n, rt_=rt_: e.activation(
                              out=rt_[:, 0:n], in_=pS[:, 0:n], func=AF.Relu, scale=wst[:, hh:hh + 1]),
                              reads=[pS.b, wst.b], writes=[rt_.b])
                          if hh == 0:
                              k.op("dve", lambda e, c0=c0, n=n, rt_=rt_: e.tensor_scalar(
                                  sc[:, c0:c0 + n], rt_[:, 0:n], wst[:, 4:5], None, op0=ALU.mult),
                                  reads=[rt_.b, wst.b], writes=[sc.b])
                          else:
                              k.op("dve", lambda e, hh=hh, c0=c0, n=n, rt_=rt_: e.scalar_tensor_tensor(
                                  out=sc[:, c0:c0 + n], in0=rt_[:, 0:n], scalar=wst[:, 4 + hh:5 + hh],
                                  in1=sc[:, c0:c0 + n], op0=ALU.mult, op1=ALU.add),
                                  reads=[rt_.b, wst.b, sc.b], writes=[sc.b])
                          yield
              def dsa_select(i, nk, L, sel, mTb, sc):
                  k.op("dve", lambda e: e.tensor_reduce(bis[:, 4:5], sc[:, 0:L], axis=AX.X, op=ALU.max),
                       reads=[sc.b], writes=[bis.b])
                  k.op("dve", lambda e: e.tensor_reduce(bis[:, 5:6], sc[:, 0:L], axis=AX.X, op=ALU.min),
                       reads=[sc.b], writes=[bis.b])
                  k.dma("sp", pent[:], pen_d[i], writes=[pent.b])
                  k.op("dve", lambda e: e.tensor_tensor(sc[:, L - 256:L], sc[:, L - 256:L], pent[:], op=ALU.add),
                       reads=[sc.b, pent.b], writes=[sc.b])
                  if DBG and i == 1 and sel is None:
                      k.dma("sp", dbg_sc[:, :], sc[:, 0:512], reads=[sc.b])
                  k.op("dve", lambda e: e.tensor_tensor(bis[:, 0:1], bis[:, 4:5], bis[:, 5:6], op=ALU.add),
                       reads=[bis.b], writes=[bis.b])
                  k.op("dve", lambda e: e.tensor_scalar(bis[:, 0:1], bis[:, 0:1], 0.5, None, op0=ALU.mult),
                       reads=[bis.b], writes=[bis.b])
                  k.op("dve", lambda e: e.tensor_tensor(bis[:, 3:4], bis[:, 4:5], bis[:, 5:6], op=ALU.subtract),
                       reads=[bis.b], writes=[bis.b])
                  k.op("dve", lambda e: e.tensor_scalar(bis[:, 3:4], bis[:, 3:4], 2.0, None, op0=ALU.add),
                       reads=[bis.b], writes=[bis.b])
                  k.op("dve", lambda e: e.tensor_scalar(bis[:, 8:8 + NIT], dcs[:, 0:NIT], bis[:, 3:4], None, op0=ALU.mult),
                       reads=[bis.b, dcs.b], writes=[bis.b])
                  k.op("dve", lambda e: e.tensor_scalar(bis[:, 8 + NIT:8 + 2 * NIT], bis[:, 8:8 + NIT], -0.5, None,
                                                        op0=ALU.mult), reads=[bis.b], writes=[bis.b])
                  for n_ in range(NIT):
                      k.op("dve", lambda e: e.tensor_scalar(junk[:, 0:L], sc[:, 0:L], bis[:, 0:1], None,
                                                            op0=ALU.is_ge, op1=ALU.add, accum_out=bis[:, 1:2]),
                           reads=[sc.b, bis.b], writes=[junk.b, bis.b])
                      k.op("dve", lambda e, n_=n_: e.tensor_scalar(bis[:, 2:3], bis[:, 1:2], dcs[:, 32:33],
                                                                  bis[:, 8 + n_:9 + n_], op0=ALU.is_ge, op1=ALU.mult),
                           reads=[bis.b, dcs.b], writes=[bis.b])
                      k.op("dve", lambda e, n_=n_: e.scalar_tensor_tensor(
                          out=bis[:, 0:1], in0=bis[:, 2:3], scalar=bis[:, 8 + NIT + n_:9 + NIT + n_], in1=bis[:, 0:1],
                          op0=ALU.add, op1=ALU.add), reads=[bis.b], writes=[bis.b])
                      yield
                  k.op("dve", lambda e: e.tensor_tensor(bis[:, 0:1], bis[:, 0:1], bis[:, 8 + NIT - 1:8 + NIT],
                                                        op=ALU.subtract), reads=[bis.b], writes=[bis.b])
                  k.op("dve", lambda e: e.tensor_scalar(sc[:, 0:L], sc[:, 0:L], bis[:, 0:1], None, op0=ALU.is_ge),
                       reads=[sc.b, bis.b], writes=[sc.b])
                  if DBG and i == 1 and sel is None:
                      k.dma("sp", dbg_mask[:, :], sc[:, 0:512], reads=[sc.b])
                      k.dma("sp", dbg_bis[:, :], bis[:], reads=[bis.b])
                  for kb0 in range(0, nk, 4):
                      nb_ = min(4, nk - kb0)
                      p = ps()
                      for j_ in range(nb_):
                          k.op("pe", lambda e, j_=j_, kb0=kb0, p=p: e.transpose(
                              p[:, j_ * 128:(j_ + 1) * 128], sc[:, (kb0 + j_) * 128:(kb0 + j_ + 1) * 128], ident[:]),
                              reads=[sc.b, ident.b], writes=[p.b])
                      k.op("act", lambda e, kb0=kb0, nb_=nb_, p=p: e.activation(
                          out=mTb[:, kb0:kb0 + nb_, :], in_=p[:, 0:nb_ * 128].rearrange("p (a t) -> p a t", a=nb_),
                          func=AF.Copy), reads=[p.b], writes=[mTb.b])
                      yield
              def dsa_attend(kblocks, ao, sel, accumulate, QTb, mTb, mm_eng="dve"):
                  pO = [ps(hold=True), ps(hold=True)]
                  ps_zero(pO[0]); ps_zero(pO[1])
                  nkb = len(kblocks)
                  def st_exp(ki):
                      ktT, kc0, vaT, vblk, mi, kbufs = kblocks[ki]
                      E_ = Eb[ki % 2]
                      for g2 in range(2):
                          pS = ps()
                          for gg in range(2):
                              g = g2 * 2 + gg
                              k.op("pe", lambda e, g=g, gg=gg, pS=pS: e.matmul(
                                  pS[:, gg * 256:(gg + 1) * 256], lhsT=ktT[:, g, kc0:kc0 + 128],
                                  rhs=QTb[:, 2 * g:2 * g + 2, :], start=True, stop=True),
                                  reads=kbufs + [QTb.b], writes=[pS.b])
                          k.op("act", lambda e, g2=g2, pS=pS, E_=E_: e.activation(
                              out=E_[:, g2 * 4:(g2 + 1) * 4, :], in_=pS[:, :].rearrange("p (h t) -> p h t", h=4),
                              func=AF.Exp, scale=SC_A), reads=[pS.b], writes=[E_.b])

                  st_exp(0)
                  for ki, (ktT, kc0, vaT, vblk, mi, kbufs) in enumerate(kblocks):
                      E_, P_ = Eb[ki % 2], Pm[ki % 2]
                      if ki + 1 < nkb:
                          st_exp(ki + 1)
                      k.op(mm_eng, lambda e, mi=mi, E_=E_, P_=P_: e.tensor_tensor(
                          P_[:], E_[:], mTb[:, mi, :].unsqueeze(1).to_broadcast([128, 8, 128]), op=ALU.mult),
                          reads=[E_.b, mTb.b], writes=[P_.b])
                      for hh in range(8):
                          rhs_ = vaT[:, hh // 2, :] if vblk is None else vaT[:, vblk, hh // 2, :]
                          k.op("pe", lambda e, hh=hh, P_=P_, rhs_=rhs_: e.matmul(
                              pO[hh // 4][:, (hh % 4) * 65:(hh % 4) * 65 + 65], lhsT=P_[:, hh, :],
                              rhs=rhs_, start=False, stop=(ki == nkb - 1)),
                              reads=[P_.b] + kbufs, writes=[pO[hh // 4].b])
                      yield
                  for j_ in range(2):
                      pv3 = pO[j_][:, 0:260].rearrange("p (h c) -> p h c", h=4)
                      k.op("dve", lambda e, j_=j_, pv3=pv3: e.reciprocal(
                          den8[:, 4 * j_:4 * j_ + 4].unsqueeze(2), pv3[:, :, 64:65]),
                          reads=[pO[j_].b], writes=[den8.b])
                      if sel is not None:
                          k.op("dve", lambda e, j_=j_: e.tensor_scalar(
                              den8[:, 4 * j_:4 * j_ + 4], den8[:, 4 * j_:4 * j_ + 4], sel, None, op0=ALU.mult),
                              reads=[den8.b, cmask.b, selw.b], writes=[den8.b])
                      dst = ao[:, 256 * j_:256 * j_ + 256].rearrange("p (h d) -> p h d", h=4)
                      bc_ = den8[:, 4 * j_:4 * j_ + 4].unsqueeze(2).to_broadcast([128, 4, 64])
                      if not accumulate:
                          k.op("dve", lambda e, pv3=pv3, dst=dst, bc_=bc_: e.tensor_tensor(
                              dst, pv3[:, :, 0:64], bc_, op=ALU.mult), reads=[pO[j_].b, den8.b], writes=[ao.b])
                      else:
                          r3 = rtmp[:, 0:256].rearrange("p (h d) -> p h d", h=4)
                          k.op("dve", lambda e, pv3=pv3, r3=r3, bc_=bc_: e.tensor_tensor(
                              r3, pv3[:, :, 0:64], bc_, op=ALU.mult), reads=[pO[j_].b, den8.b], writes=[rtmp.b])
                          k.op("dve", lambda e, dst=dst, r3=r3: e.tensor_tensor(dst, dst, r3, op=ALU.add),
                               reads=[rtmp.b, ao.b], writes=[ao.b])
                  ps_release(pO[0]); ps_release(pO[1])

              def run(g):
                  for _ in g:
                      pass

              def rr(*gens):
                  alive = [g for g in gens if g is not None]
                  while alive:
                      for g in list(alive):
                          try:
                              next(g)
                          except StopIteration:
                              alive.remove(g)

              def mq_proj(xT, MQTb):
                  pmq = ps()
                  linear(xT, Wq, 772, 512, pmq)
                  head_rmsnorm(pmq.b, pmq[:, :], 4, 128, hv[:, 128:256], mq32, tmp, hstat, "mq")
                  p = ps()
                  for h in range(4):
                      k.op("pe", lambda e, h=h, p=p: e.transpose(
                          p[:, h * 128:(h + 1) * 128], mq32[:, h * 128:(h + 1) * 128], ident[:]),
                          reads=[mq32.b, ident.b], writes=[p.b])
                  k.op("act", lambda e, p=p: e.activation(
                      out=MQTb[:], in_=p[:, :].rearrange("p (h t) -> p h t", h=4), func=AF.Copy),
                      reads=[p.b], writes=[MQTb.b])

              def mem_attn(i, smp, MQT):
                  pO = [ps(hold=True), ps(hold=True)]
                  ps_zero(pO[0]); ps_zero(pO[1])
                  if not smp:
                      for mb in range(2):
                          mem_scores(MKT, MQT, mb, PT)
                          yield
                      mem_pv(pO, PT, MVa, True, True)
                      yield
                  else:
                      for b in range(16):
                          kb_, Kt_, Vb_ = mkb[b % 2], MKTb[b % 2], MVb[b % 2]
                          k.dma("sp", kb_[:], cmk_d[b].rearrange("(mb p) c -> p mb c", p=128), writes=[kb_.b])
                          for mb in range(2):
                              k.dma("pool", Vb_[:, mb, :, 0:128],
                                    cmv_d[b, mb * 128:(mb + 1) * 128, :].rearrange("p (h d) -> p h d", h=4),
                                    writes=[Vb_.b])
                          for mb in range(2):
                              p = ps()
                              for h in range(4):
                                  k.op("pe", lambda e, h=h, p=p, mb=mb: e.transpose(
                                      p[:, h * 128:(h + 1) * 128], kb_[:, mb, h * 128:(h + 1) * 128], ident[:]),
                                      reads=[kb_.b, ident.b], writes=[p.b])
                              k.op("act", lambda e, p=p, mb=mb: e.activation(
                                  out=Kt_[:, :, mb * 128:(mb + 1) * 128],
                                  in_=p[:, :].rearrange("p (h m) -> p h m", h=4), func=AF.Copy),
                                  reads=[p.b], writes=[Kt_.b])
                          for mb in range(2):
                              mem_scores(Kt_, MQT, mb, PT, cm=cmask[:, b, :])
                          mem_pv(pO, PT, Vb_, b == 0, b == 15)
                          yield
                  mo = mo32[i % 2]
                  for j in range(2):
                      pv3 = pO[j][:, 0:258].rearrange("p (h c) -> p h c", h=2)
                      k.op("dve", lambda e, j=j, pv3=pv3: e.reciprocal(
                          rec[:, 2 * j:2 * j + 2].unsqueeze(2), pv3[:, :, 128:129]),
                          reads=[pO[j].b], writes=[rec.b])
                      k.op("dve", lambda e, j=j, pv3=pv3, mo=mo: e.tensor_tensor(
                          mo[:, 256 * j:256 * j + 256].rearrange("p (h d) -> p h d", h=2), pv3[:, :, 0:128],
                          rec[:, 2 * j:2 * j + 2].unsqueeze(2).to_broadcast([128, 2, 128]), op=ALU.mult),
                          reads=[pO[j].b, rec.b], writes=[mo.b])
                  ps_release(pO[0]); ps_release(pO[1])
                  k.dma("sp", m_sc[i * 128:(i + 1) * 128, :], mo[:], reads=[mo.b], writes=[m_sc_b[i]])

              NDSA = cfg.get("dsa_tiles", 17)

              def nk_of(i):
                  return 4 * (i // 2) + (2 if i % 2 == 0 else 4)

              def tile_A1(i):
                  x32, xT = load_xT(tsets[0], x_own[i * 128:(i + 1) * 128, :], 0)
                  yield
                  mq_proj(xT, MQT2[i % 3])
                  yield
                  if i < NDSA:
                      yield from dsa_q(xT, QT2[i % 3])
                      yield from dsa_index(nk_of(i) * 128, score2[i % 2])

              def tile_A2(i):
                  if i < NDSA:
                      yield from dsa_select(i, nk_of(i), nk_of(i) * 128, None, maskT2[i % 2], score2[i % 2])

              def tile_B(i):
                  ao = ao32[i % 2]
                  if i < NDSA:
                      kbl = [(KT, kb * 128, VA, kb, kb, [KT.b, VA.b]) for kb in range(nk_of(i))]
                      yield from dsa_attend(kbl, ao, None, False, QT2[i % 3], maskT2[i % 2], mm_eng="pool")
                  else:
                      k.op("pool", lambda e: e.memset(ao[:], 0.0), writes=[ao.b])
                  k.dma("sp", a_sc[i * 128:(i + 1) * 128, :], ao[:], reads=[ao.b], writes=[a_sc_b[i]])
                  yield from mem_attn(i, False, MQT2[i % 3])

              pst = contextlib.ExitStack()
              score2 = [score, sb(pst, "scoreB", [128, SEQ])]
              for s_ in range(NT_OWN + 2):
                  rr(tile_B(s_ - 2) if 0 <= s_ - 2 < NT_OWN else None,
                     tile_A2(s_ - 1) if 0 <= s_ - 1 < NT_OWN else None,
                     tile_A1(s_) if s_ < NT_OWN else None)

              k.barrier()
              pst.close()
              mkb = [sb(st, "mkb%d" % i, [128, 2, 512]) for i in range(1)] * 2
              MKTb = [sb(st, "MKTb%d" % i, [128, 4, 256], BF16) for i in range(2)]
              MVb = [sb(st, "MVb%d" % i, [128, 2, 4, 129], BF16) for i in range(2)]
              for t_ in MVb:
                  k.op("pool", lambda e, t_=t_: e.memset(t_[:], 1.0), writes=[t_.b])
              i = NT_OWN
              x32, xT = load_xT(tsets[0], x_smp[:, :], 0)
              QTb, mTb, MQTb = QT2[0], maskT2[0], MQT2[0]
              mq_proj(xT, MQTb)
              run(mem_attn(i, True, MQTb))
              ao = ao32[0]
              if NDSA < 17:
                  k.op("pool", lambda e: e.memset(ao[:], 0.0), writes=[ao.b])
              else:
                  ptb = sb(st, "ptb", [128, 256], I32)
                  ptf = sb(st, "ptf", [128, 256])
                  pti = sb(st, "pti", [128, 256], I32)
                  k.dma("sp", ptb[:], pt_d[0:1, :].partition_broadcast(128), writes=[ptb.b])
                  k.op("dve", lambda e: e.tensor_copy(ptf[:], ptb[:]), reads=[ptb.b], writes=[ptf.b])
                  k.op("dve", lambda e: e.tensor_scalar(ptf[:], ptf[:], 128.0, dcs[:, 33:34],
                                                        op0=ALU.mult, op1=ALU.add),
                       reads=[ptf.b, dcs.b], writes=[ptf.b])
                  k.op("dve", lambda e: e.tensor_copy(pti[:], ptf[:]), reads=[ptf.b], writes=[pti.b])
                  kvpg = [sb(st, "kvpg%d" % q, [128, 512]) for q in range(2)]
                  ipg = [sb(st, "ipg%d" % q, [128, 64]) for q in range(2)]
                  IQTm = [sb(st, "IQTm%d" % q, [64, 4, 128], BF16) for q in range(2)]
                  KTh, VAh, IKTh = [Buf(), Buf()], [Buf(), Buf()], [Buf(), Buf()]
                  run(dsa_q(xT, QTb))
                  k.op("pool", lambda e: e.memset(score[:, 0:2048], 0.0), writes=[score.b])

                  def idx_gather(b):
                      hf = b % 2
                      base = 17 * hf
                      for pg in range(16):
                          q_ = pg % 2
                          col = b * 16 + pg
                          off = bass.IndirectOffsetOnAxis(ap=pti[:, col:col + 1], axis=0)
                          k.dma("pool", ipg[q_][:], cik_d[:, :], reads=[pti.b], writes=[ipg[q_].b], indirect=off)
                          kside_store(None, None, ipg[q_][:], [ipg[q_].b], None,
                                      IKT[:, (base + pg) * 128:(base + pg + 1) * 128], None, [IKTh[hf]])
                          yield

                  def idx_score(b):
                      hf = b % 2
                      base = 17 * hf
                      Im = IQTm[hf]
                      k.op("dve", lambda e: e.tensor_tensor(
                          Im[:], IQT[:], cmask[0:64, b, :].unsqueeze(1).to_broadcast([64, 4, 128]), op=ALU.mult),
                          reads=[IQT.b, cmask.b], writes=[Im.b])
                      for c in range(4):
                          for hh in range(4):
                              pS = ps()
                              c0 = base * 128 + c * 512
                              k.op("pe", lambda e, hh=hh, pS=pS, c0=c0: e.matmul(
                                  pS[:, :], lhsT=Im[:, hh, :], rhs=IKT[:, c0:c0 + 512], start=True, stop=True),
                                  reads=[Im.b, IKTh[hf]], writes=[pS.b])
                              rt_ = rtmp if hh % 2 == 0 else rtmpB
                              k.op("act", lambda e, hh=hh, pS=pS, rt_=rt_: e.activation(
                                  out=rt_[:], in_=pS[:, :], func=AF.Relu, scale=wst[:, hh:hh + 1]),
                                  reads=[pS.b, wst.b], writes=[rt_.b])
                              k.op("dve", lambda e, hh=hh, c=c, rt_=rt_: e.scalar_tensor_tensor(
                                  out=score[:, c * 512:(c + 1) * 512], in0=rt_[:], scalar=wst[:, 4 + hh:5 + hh],
                                  in1=score[:, c * 512:(c + 1) * 512], op0=ALU.mult, op1=ALU.add),
                                  reads=[rt_.b, wst.b, score.b], writes=[score.b])
                              yield

                  run(idx_gather(0))
                  for b in range(16):
                      rr(idx_score(b), idx_gather(b + 1) if b + 1 < 16 else None)
                  for hh in range(4):
                      pS = ps()
                      k.op("pe", lambda e, hh=hh, pS=pS: e.matmul(
                          pS[:, 0:128], lhsT=IQT[:, hh, :], rhs=IKTn[:, :], start=True, stop=True),
                          reads=[IQT.b, IKTn.b], writes=[pS.b])
                      k.op("act", lambda e, hh=hh, pS=pS: e.activation(
                          out=rtmp[:, 0:128], in_=pS[:, 0:128], func=AF.Relu, scale=wst[:, hh:hh + 1]),
                          reads=[pS.b, wst.b], writes=[rtmp.b])
                      if hh == 0:
                          k.op("dve", lambda e: e.tensor_scalar(
                              score[:, 2048:2176], rtmp[:, 0:128], wst[:, 4:5], None, op0=ALU.mult),
                              reads=[rtmp.b, wst.b], writes=[score.b])
                      else:
                          k.op("dve", lambda e, hh=hh: e.scalar_tensor_tensor(
                              out=score[:, 2048:2176], in0=rtmp[:, 0:128], scalar=wst[:, 4 + hh:5 + hh],
                              in1=score[:, 2048:2176], op0=ALU.mult, op1=ALU.add),
                              reads=[rtmp.b, wst.b, score.b], writes=[score.b])
                  run(dsa_select(16, 17, 17 * 128, None, mTb, score))

                  def kv_gather(b):
                      hf = b % 2
                      base = 17 * hf
                      for pg in range(16):
                          q_ = pg % 2
                          col = b * 16 + pg
                          off = bass.IndirectOffsetOnAxis(ap=pti[:, col:col + 1], axis=0)
                          k.dma("pool", kvpg[q_][:], ckv_d[:, :], reads=[pti.b], writes=[kvpg[q_].b], indirect=off)
                          kside_store(kvpg[q_][:, 0:256], None, None, [kvpg[q_].b],
                                      KT[:, :, (base + pg) * 128:(base + pg + 1) * 128], None, None, [KTh[hf]])
                          k.op("dve", lambda e, q_=q_, base=base, pg=pg: e.tensor_copy(
                              VA[:, base + pg, :, 0:64], kvpg[q_][:, 256:512].rearrange("p (g d) -> p g d", g=4)),
                              reads=[kvpg[q_].b], writes=[VAh[hf]])
                          yield

                  def seq_attend(b):
                      hf = b % 2
                      base = 17 * hf
                      kbl = [(KT, (base + pg) * 128, VA, base + pg, pg, [KTh[hf], VAh[hf]]) for pg in range(16)]
                      kbl.append((KTn, 0, VAn, None, 16, [KTn.b, VAn.b]))
                      yield from dsa_attend(kbl, ao, cmask32e[:, b:b + 1], b > 0, QTb, mTb)

                  run(kv_gather(0))
                  for b in range(16):
                      rr(seq_attend(b), kv_gather(b + 1) if b + 1 < 16 else None)
              k.dma("sp", a_sc[i * 128:(i + 1) * 128, :], ao[:], reads=[ao.b], writes=[a_sc_b[i]])

        k.barrier()
        kst.close()
        k.barrier()
        with contextlib.ExitStack() as st:
          if 'F' in PH:
              Wg = sb(st, "Wg", [128, KC, 3072], BF16)
              Wbr = sb(st, "Wbr", [128, 12, D], BF16)
              Wo = sb(st, "Wo", [128, KC, D], BF16)
              load_w(Wg, w_in, O_GATES, 3072)
              for bi, wsrc in enumerate((w_a_out, w_g_out, w_m_out)):
                  for kc in range(4):
                      k.dma("pool", Wbr[:, bi * 4 + kc, :], wsrc[kc * 128:(kc + 1) * 128, :], writes=[Wbr.b])
              load_w(Wo, w_o, 0, D)
              tsets = [(sb(st, "fx32_%d" % i, [128, D]), sb(st, "fxn_%d" % i, [128, D]),
                        sb(st, "fxT_%d" % i, [128, KC, 128], BF16), sb(st, "fstat_%d" % i, [128, 4]))
                       for i in range(3)]
              br32 = [sb(st, "br32_%d" % i, [128, 3, 512]) for i in range(2)]
              cand = [sb(st, "cand%d" % i, [128, 4, 512]) for i in range(2)]
              brT = sb(st, "brT", [128, 12, 128], BF16)
              sig = sb(st, "sig", [128, 512])
              term = sb(st, "term", [128, 512])
              h32 = sb(st, "h32", [128, D])
              hT = sb(st, "hT", [128, KC, 128], BF16)
              x2o = [sb(st, "x2o%d" % i, [128, D]) for i in range(2)]
              def f_gen(i):
                  smp = (i == NT_OWN)
                  src = x_smp[:, :] if smp else x_own[i * 128:(i + 1) * 128, :]
                  x32, xT = load_xT(tsets[i % 3], src, 0)
                  yield "B"
                  br = br32[i % 2]
                  k.dma("sp", br[:, 0, :], a_sc[i * 128:(i + 1) * 128, :], reads=[a_sc_b[i]], writes=[br.b])
                  k.dma("sp", br[:, 2, :], m_sc[i * 128:(i + 1) * 128, :], reads=[m_sc_b[i]], writes=[br.b])
                  if smp:
                      k.dma("sp", br[:, 1, :], g_sc[32 * 128:33 * 128, :], reads=[g_sc_b[32]], writes=[br.b])
                  else:
                      grp = i // 2
                      cd = cand[i % 2]
                      if i % 2 == 0:
                          cdl = cand[(i // 2) % 2]
                          k.dma("sp", cdl[:], g_sc[grp * 512:(grp + 1) * 512, :].rearrange("(c p) n -> p c n", p=128),
                                reads=g_sc_b[4 * grp:4 * grp + 4], writes=[cdl.b])
                      cdl = cand[(i // 2) % 2]
                      for c in range(4):
                          sc_ = selw[:, i * 4 + c:i * 4 + c + 1]
                          if c == 0:
                              k.op("dve", lambda e, sc_=sc_, br=br, cdl=cdl: e.tensor_scalar(
                                  br[:, 1, :], cdl[:, 0, :], sc_, None, op0=ALU.mult),
                                  reads=[cdl.b, selw.b], writes=[br.b])
                          else:
                              k.op("dve", lambda e, sc_=sc_, br=br, cdl=cdl, c=c: e.scalar_tensor_tensor(
                                  out=br[:, 1, :], in0=cdl[:, c, :], scalar=sc_, in1=br[:, 1, :],
                                  op0=ALU.mult, op1=ALU.add), reads=[cdl.b, selw.b, br.b], writes=[br.b])
                  for bi in range(3):
                      p = ps()
                      for j in range(4):
                          k.op("pe", lambda e, bi=bi, j=j, p=p, br=br: e.transpose(
                              p[:, j * 128:(j + 1) * 128], br[:, bi, j * 128:(j + 1) * 128], ident[:]),
                              reads=[br.b, ident.b], writes=[p.b])
                      k.op("act", lambda e, bi=bi, p=p: e.activation(
                          out=brT[:, bi * 4:(bi + 1) * 4, :], in_=p[:, :].rearrange("p (a b) -> p a b", a=4),
                          func=AF.Copy), reads=[p.b], writes=[brT.b])
                  for c in range(2):
                      for bi in range(3):
                          pg = ps()
                          linear(xT, Wg, bi * 1024 + c * 512, 512, pg)
                          pb = ps()
                          for kc in range(4):
                              k.op("pe", lambda e, kc=kc, bi=bi, c=c, pb=pb: e.matmul(
                                  pb[:, :], lhsT=brT[:, bi * 4 + kc, :], rhs=Wbr[:, bi * 4 + kc, c * 512:(c + 1) * 512],
                                  start=(kc == 0), stop=(kc == 3)), reads=[brT.b, Wbr.b], writes=[pb.b])
                          k.op("act", lambda e, pg=pg: e.activation(out=sig[:], in_=pg[:, :], func=AF.Sigmoid),
                               reads=[pg.b], writes=[sig.b])
                          if bi == 0:
                              k.op("dve", lambda e, pb=pb, c=c: e.tensor_tensor(
                                  h32[:, c * 512:(c + 1) * 512], sig[:], pb[:, :], op=ALU.mult),
                                  reads=[sig.b, pb.b], writes=[h32.b])
                          else:
                              k.op("dve", lambda e, pb=pb: e.tensor_tensor(term[:], sig[:], pb[:, :], op=ALU.mult),
                                   reads=[sig.b, pb.b], writes=[term.b])
                              k.op("dve", lambda e, c=c: e.tensor_tensor(
                                  h32[:, c * 512:(c + 1) * 512], h32[:, c * 512:(c + 1) * 512], term[:], op=ALU.add),
                                  reads=[term.b, h32.b], writes=[h32.b])
                  for half in range(2):
                      p = ps()
                      for j in range(4):
                          kc = half * 4 + j
                          k.op("pe", lambda e, kc=kc, j=j, p=p: e.transpose(
                              p[:, j * 128:(j + 1) * 128], h32[:, kc * 128:(kc + 1) * 128], ident[:]),
                              reads=[h32.b, ident.b], writes=[p.b])
                      k.op("act", lambda e, half=half, p=p: e.activation(
                          out=hT[:, half * 4:(half + 1) * 4, :], in_=p[:, :].rearrange("p (a b) -> p a b", a=4),
                          func=AF.Copy), reads=[p.b], writes=[hT.b])
                  xo_ = x2o[i % 2]
                  for c in range(2):
                      p = ps()
                      linear(hT, Wo, c * 512, 512, p)
                      k.op("dve", lambda e, c=c, p=p, xo_=xo_, x32=x32: e.tensor_tensor(
                          xo_[:, c * 512:(c + 1) * 512], p[:, :], x32[:, c * 512:(c + 1) * 512], op=ALU.add),
                          reads=[p.b, x32.b], writes=[xo_.b])
                  k.dma("sp", x2_sc[i * 128:(i + 1) * 128, :], xo_[:], reads=[xo_.b], writes=[x2_sc_b[i]])
              pipe_ahead([f_gen(i) for i in range(NT_OWN + 1)], 2)

        k.barrier()
        with contextlib.ExitStack() as st:
          if 'D' in PH:
              Wf1 = sb(st, "Wf1", [128, KC, 2 * D_FF], BF16)
              Wf2 = sb(st, "Wf2", [128, D_FF // 128, D], BF16)
              load_w(Wf1, w_ffn_in, 0, 2 * D_FF)
              load_w(Wf2, w_ffn_out, 0, D, rows=D_FF)
              tsets = [(sb(st, "dx32_%d" % i, [128, D]), sb(st, "dxn_%d" % i, [128, D]),
                        sb(st, "dxT_%d" % i, [128, KC, 128], BF16), sb(st, "dstat_%d" % i, [128, 4]))
                       for i in range(2)]
              act2 = [sb(st, "ffact%d" % q, [128, D_FF]) for q in range(2)]
              sg2 = [sb(st, "ffsg%d" % q, [128, 512]) for q in range(1)] * 2
              actT2 = [sb(st, "ffactT%d" % q, [128, D_FF // 128, 128], BF16) for q in range(2)]
              yo = [sb(st, "yo%d" % i, [128, D]) for i in range(2)]
              nb = D_FF // 128

              def ffn_gen(i):
                  smp = (i == NT_OWN)
                  bf = i % 2
                  act, actT = act2[bf], actT2[bf]
                  if 'F' in PH:
                      src = x2_sc[i * 128:(i + 1) * 128, :]
                      x32, xT = load_xT(tsets[bf], src, 16, rd=[x2_sc_b[i]])
                  else:
                      src = x_smp[:, :] if smp else x_own[i * 128:(i + 1) * 128, :]
                      x32, xT = load_xT(tsets[bf], src, 16)
                  yield
                  c0 = 0
                  ci = 0
                  while c0 < D_FF:
                      n = min(512, D_FF - c0)
                      sg = sg2[ci % 2]
                      pg = ps()
                      linear(xT, Wf1, c0, n, pg)
                      pu = ps()
                      linear(xT, Wf1, D_FF + c0, n, pu)
                      k.op("act", lambda e, pg=pg, n=n, sg=sg: e.activation(out=sg[:, 0:n], in_=pg[:, 0:n], func=AF.Silu),
                           reads=[pg.b], writes=[sg.b])
                      k.op("dve", lambda e, pu=pu, n=n, c0=c0, sg=sg: e.tensor_tensor(
                          act[:, c0:c0 + n], sg[:, 0:n], pu[:, 0:n], op=ALU.mult),
                          reads=[sg.b, pu.b], writes=[act.b])
                      c0 += n
                      ci += 1
                      yield
                  yield "B"
                  for b0 in range(0, nb, 4):
                      nbb = min(4, nb - b0)
                      p = ps()
                      for j in range(nbb):
                          k.op("pe", lambda e, j=j, b0=b0, p=p: e.transpose(
                              p[:, j * 128:(j + 1) * 128], act[:, (b0 + j) * 128:(b0 + j + 1) * 128], ident[:]),
                              reads=[act.b, ident.b], writes=[p.b])
                      k.op("act", lambda e, p=p, b0=b0, nbb=nbb: e.activation(
                          out=actT[:, b0:b0 + nbb, :], in_=p[:, 0:nbb * 128].rearrange("p (a b) -> p a b", a=nbb),
                          func=AF.Copy), reads=[p.b], writes=[actT.b])
                      yield
                  yy = yo[bf]
                  for h in range(2):
                      p = ps()
                      for kc in range(nb):
                          k.op("pe", lambda e, kc=kc, h=h, p=p: e.matmul(
                              p[:, :], lhsT=actT[:, kc, :], rhs=Wf2[:, kc, h * 512:(h + 1) * 512],
                              start=(kc == 0), stop=(kc == nb - 1)),
                              reads=[actT.b, Wf2.b], writes=[p.b])
                          if kc % 6 == 5:
                              yield
                      k.op("dve", lambda e, h=h, p=p, yy=yy, x32=x32: e.tensor_tensor(
                          yy[:, h * 512:(h + 1) * 512], p[:, :], x32[:, h * 512:(h + 1) * 512], op=ALU.add),
                          reads=[p.b, x32.b], writes=[yy.b])
                      yield
                  dst = y_smp[:, :] if smp else y_own[i * 128:(i + 1) * 128, :]
                  k.dma("sp", dst, yy[:], reads=[yy.b])

              gensD = [ffn_gen(i) for i in range(NT_OWN + 1)]
              for tok in gensD[0]:
                  if tok == "B":
                      break
              for i in range(NT_OWN + 1):
                  cur = gensD[i]
                  nxt = gensD[i + 1] if i + 1 < NT_OWN + 1 else None
                  cur_alive, nxt_inA = True, nxt is not None
                  while cur_alive or nxt_inA:
                      if cur_alive:
                          try:
                              next(cur)
                          except StopIteration:
                              cur_alive = False
                      if nxt_inA:
                          if next(nxt) == "B":
                              nxt_inA = False
              outs_done += [b.b for b in yo]

        k.barrier()
        k.finish(outs_done, "sp")
    print("ops", k.nops, "waits", k.nwaits)
    return nc


_NC_CACHE = {}


def make_in_maps(I):
    f = lambda a: np.ascontiguousarray(np.asarray(a), dtype=np.float32)
    x_prompt, x_sample, mem_prompt = f(I["x_prompt"]), f(I["x_sample"]), f(I["mem_prompt"])
    gains = np.concatenate([f(I["norm_mix"])[0].reshape(8, 128).T, f(I["mem_norm"])[0].reshape(8, 128).T,
                            f(I["norm_ffn"])[0].reshape(8, 128).T], axis=1)
    hv = np.concatenate([f(I["a_q_norm"])[0], f(I["a_k_norm"])[0], f(I["m_q_norm"])[0], f(I["m_k_norm"])[0],
                         f(I["g_o_norm"])[0], f(I["g_dt_bias"])[0], f(I["g_a_log"])[0],
                         np.zeros(56, np.float32)])[None, :]
    r = np.arange(128)
    same = (r[:, None] // 8) == (r[None, :] // 8)
    one = np.ones((128, 128), bool)
    mats = []
    for blk in (one, same):
        pass
    ltri = [(r[:, None] <= r[None, :]) & blk for blk in (one, same)]
    bones = [blk for blk in (one, same)]
    slm = [(r[:, None] > r[None, :]) & blk for blk in (one, same)]
    sui = [((r[None, :] >= r[:, None]) & blk) * (128.0 ** -0.5) for blk in (one, same)]
    ustr = (r[:, None] > r[None, :])
    g8 = (r[:, None] // 8) == np.arange(16)[None, :]
    cm32 = np.broadcast_to((np.arange(128)[None, :] // 8 == np.arange(16)[:, None]).reshape(1, 2048), (128, 2048))
    gconst = np.concatenate([np.asarray(a, np.float32) for a in
                             (ltri[0], ltri[1], bones[0], bones[1], slm[0], slm[1], sui[0], sui[1], ustr,
                              np.ones((128, 128)), g8, cm32)], axis=1)
    gconvT = f(I["g_conv"])[0].reshape(4, 12, 128).transpose(2, 1, 0).reshape(128, 48)
    dconst = np.zeros((128, 64), np.float32)
    dconst[:, 0:32] = (2.0 ** -(np.arange(32) + 1.0))[None, :]
    dconst[:, 32] = 256.0
    dconst[:, 33] = np.arange(128)
    dconst[:, 40:56] = g8
    ckv = np.concatenate([f(I["cache_k"])[0].reshape(2560 * 128, 256),
                          f(I["cache_v"])[0].reshape(2560 * 128, 256)], axis=1)
    cik = f(I["cache_idx_k"])[0].reshape(2560 * 128, 64)
    ptab = np.asarray(I["page_table"]).astype(np.int32)
    shared = {
        "w_in": f(I["w_in"])[0], "w_mem_kv": f(I["w_mem_kv"])[0], "w_a_out": f(I["w_a_out"])[0],
        "w_g_out": f(I["w_g_out"])[0], "w_m_out": f(I["w_m_out"])[0], "w_o": f(I["w_o"])[0],
        "w_ffn_in": f(I["w_ffn_in"])[0], "w_ffn_out": f(I["w_ffn_out"])[0],
        "ident": np.eye(128, dtype=np.float32),
        "dconst": dconst, "cache_kv": ckv, "cache_idx_k": cik,
        "gconst": np.ascontiguousarray(gconst, dtype=np.float32), "gconvT": np.ascontiguousarray(gconvT),
        "cmask": np.ascontiguousarray(np.broadcast_to(
            (np.arange(128)[None, :] // 8 == np.arange(16)[:, None]).astype(np.float32).reshape(1, 2048), (128, 2048))), "gains": np.ascontiguousarray(gains), "headvecs": hv,
    }
    in_maps = []
    for c in range(8):
        b, half = c // 2, c % 2
        ot = own_tiles(half)
        xo = np.concatenate([x_prompt[b, t * 128:(t + 1) * 128] for t in ot], axis=0)
        m = dict(shared)
        m["x_all"] = x_prompt[b]
        m["x_own"] = np.ascontiguousarray(xo)
        m["x_smp"] = np.ascontiguousarray(x_sample[16 * c:16 * c + 16].reshape(128, D))
        m["mem"] = mem_prompt[b]
        m["cache_mem_k"] = np.ascontiguousarray(f(I["cache_mem_k"])[0, 16 * c:16 * c + 16].reshape(16, 256, 512))
        m["cache_mem_v"] = np.ascontiguousarray(f(I["cache_mem_v"])[0, 16 * c:16 * c + 16].reshape(16, 256, 512))
        m["state_gdn"] = np.ascontiguousarray(f(I["state_gdn"])[0, 16 * c:16 * c + 16])
        m["state_conv"] = np.ascontiguousarray(f(I["state_conv"])[0, 16 * c:16 * c + 16].reshape(48, 1536))
        m["page_table"] = np.ascontiguousarray(ptab[16 * c:16 * c + 16].reshape(1, 256))
        pen = np.zeros((17, 128, 256), np.float32)
        tt = np.arange(128)
        for i_, t_ in enumerate(ot):
            nk_ = 4 * (i_ // 2) + (2 if i_ % 2 == 0 else 4)
            spos = (nk_ - 2) * 128 + np.arange(256)
            pen[i_] = np.where(spos[None, :] <= (t_ * 128 + tt)[:, None], 0.0, -1e30)
        newok = ((tt[:, None] // 8) == (tt[None, :] // 8)) & ((tt[None, :] % 8) <= (tt[:, None] % 8))
        pen[16, :, 128:] = np.where(newok, 0.0, -1e30)
        m["pen"] = pen
        sw = np.zeros((16, 4), np.float32)
        for i_, t_ in enumerate(ot):
            sw[i_, t_ % 4] = 1.0
        m["selw"] = np.ascontiguousarray(np.broadcast_to(sw.reshape(1, 64), (128, 64)))
        in_maps.append(m)
    return in_maps


def kernel(**I):
    if "nc" not in _NC_CACHE:
        _NC_CACHE["nc"] = build({})
    nc = _NC_CACHE["nc"]
    in_maps = make_in_maps(I)
    res = run_bass_kernel_spmd(nc, in_maps, core_ids=list(range(8)))
    R = res.results
    yp = np.zeros((4, SEQ, D), np.float32)
    for c in range(8):
        b, half = c // 2, c % 2
        for i, t in enumerate(own_tiles(half)):
            yp[b, t * 128:(t + 1) * 128] = R[c]["y_own"][i * 128:(i + 1) * 128]
    ys = np.concatenate([R[c]["y_smp"].reshape(16, 8, D) for c in range(8)], axis=0)
    p_k = np.stack([R[2 * b]["o_pk"].reshape(SEQ, 4, 64) for b in range(4)])[None]
    p_v = np.stack([R[2 * b]["o_pv"].reshape(SEQ, 4, 64) for b in range(4)])[None]
    p_ik = np.stack([R[2 * b]["o_pik"] for b in range(4)])[None]
    p_gdn = np.stack([R[2 * b]["o_pgdn"] for b in range(4)])[None]
    p_conv = np.stack([R[2 * b]["o_pconv"] for b in range(4)])[None]
    p_mk = np.stack([R[2 * b]["o_pmk"].reshape(256, 4, 128) for b in range(4)])[None]
    p_mv = np.stack([R[2 * b]["o_pmv"].reshape(256, 4, 128) for b in range(4)])[None]
    s_k = np.concatenate([R[c]["o_sk"].reshape(16, 8, 4, 64) for c in range(8)], axis=0)[None]
    s_v = np.concatenate([R[c]["o_sv"].reshape(16, 8, 4, 64) for c in range(8)], axis=0)[None]
    s_ik = np.concatenate([R[c]["o_sik"].reshape(16, 8, 64) for c in range(8)], axis=0)[None]
    s_gdn = np.concatenate([R[c]["o_sgdn"] for c in range(8)], axis=0)[None]
    s_conv = np.concatenate([R[c]["o_sconv"] for c in range(8)], axis=0)[None]
    return (yp, ys, p_k, p_v, p_ik, p_gdn, p_conv, p_mk, p_mv, s_k, s_v, s_ik, s_gdn, s_conv)
```

```python
import contextlib
import numpy as np
import concourse.bass as bass
import concourse.mybir as mybir
from concourse.bass_utils import run_bass_kernel_spmd

F32 = mybir.dt.float32
BF16 = mybir.dt.bfloat16
I32 = mybir.dt.int32
AF = mybir.ActivationFunctionType
ALU = mybir.AluOpType
AX = mybir.AxisListType

D = 1024
KC = 8
SEQ = 4096
NT_ALL = 32
NT_OWN = 16
D_IN = 6988
D_FF = 2816
EPS = 1e-6
O_AQ, O_AK, O_AV, O_IQ, O_IK, O_IW = 0, 512, 768, 1024, 1280, 1344
O_GQKV, O_GZ, O_GB, O_GA, O_MQ, O_GATES = 1348, 2884, 3396, 3400, 3404, 3916


class Buf:
    __slots__ = ("name", "w", "r")

    def __init__(self, name=""):
        self.name = name
        self.w = None
        self.r = []


class K:
    def __init__(self, nc, n_dma_sems=40):
        self.nc = nc
        self.eng = {"pe": nc.tensor, "act": nc.scalar, "dve": nc.vector,
                    "pool": nc.gpsimd, "sp": nc.sync}
        self.sem, self.cnt, self.seen = {}, {}, {}
        for e in self.eng:
            self.sem[e] = nc.alloc_semaphore("prog_" + e)
            self.cnt[e] = 0
            self.seen[e] = {}
        self.dsems = [nc.alloc_semaphore("dma%d" % i) for i in range(n_dma_sems)]
        self.dcnt = [0] * n_dma_sems
        self.dnext = 0
        self.nops = 0
        self.nwaits = 0

    def _wait(self, e, tok):
        if tok is None:
            return
        sem, val, src = tok
        if src == e and e == "pe":
            return
        key = sem.num
        if self.seen[e].get(key, 0) >= val:
            return
        self.eng[e].wait_ge(sem, val)
        self.seen[e][key] = val
        self.nwaits += 1

    def _deps(self, e, reads, writes):
        for b in reads:
            self._wait(e, b.w)
        for b in writes:
            self._wait(e, b.w)
            for t in b.r:
                self._wait(e, t)

    def _commit(self, tok, reads, writes):
        for b in reads:
            b.r.append(tok)
            if len(b.r) > 64:
                b.r = b.r[-64:] if False else b.r
        for b in writes:
            b.w = tok
            b.r = []

    def op(self, e, fn, reads=(), writes=()):
        self._deps(e, reads, writes)
        ins = fn(self.eng[e])
        self.cnt[e] += 1
        ins.then_inc(self.sem[e], 1)
        tok = (self.sem[e], self.cnt[e], e)
        self._commit(tok, reads, writes)
        self.nops += 1
        return tok

    def dma(self, q, out, in_, reads=(), writes=(), indirect=None, **kw):
        self._deps(q, reads, writes)
        i = self.dnext
        self.dnext = (self.dnext + 1) % len(self.dsems)
        sem = self.dsems[i]
        if self.dcnt[i] > 0:
            self._wait(q, (sem, self.dcnt[i], "dma"))
        if indirect is not None:
            ins = self.eng[q].indirect_dma_start(out=out, out_offset=None, in_=in_,
                                                 in_offset=indirect, **kw)
        else:
            ins = self.eng[q].dma_start(out=out, in_=in_, **kw)
        self.dcnt[i] += 16
        ins.then_inc(sem, 16)
        tok = (sem, self.dcnt[i], "dma")
        self._commit(tok, reads, writes)
        self.nops += 1
        return tok

    def barrier(self):
        for e in self.eng:
            for e2 in self.eng:
                if e2 != e and self.cnt[e2] > 0:
                    self._wait(e, (self.sem[e2], self.cnt[e2], e2))
            for i, s_ in enumerate(self.dsems):
                if self.dcnt[i] > 0:
                    self._wait(e, (s_, self.dcnt[i], "dma"))

    def finish(self, bufs, e="sp"):
        for b in bufs:
            self._wait(e, b.w)
            for t in b.r:
                self._wait(e, t)


class T:
    __slots__ = ("t", "b")

    def __init__(self, t, name=""):
        self.t = t
        self.b = Buf(name)

    def __getitem__(self, key):
        return self.t[key]


def own_tiles(half):
    res = []
    for g in range(8):
        res += [4 * g, 4 * g + 3] if half == 0 else [4 * g + 1, 4 * g + 2]
    return res


def build(cfg):
    nc = bass.Bass("TRN2", target_bir_lowering=False)
    k = K(nc)
    dt_in = {}

    def din(name, shape, dt=F32):
        dt_in[name] = nc.dram_tensor(name, list(shape), dt, kind="ExternalInput").ap()
        return dt_in[name]

    def dout(name, shape, dt=F32):
        return nc.dram_tensor(name, list(shape), dt, kind="ExternalOutput").ap()

    x_all = din("x_all", [SEQ, D])
    x_own = din("x_own", [NT_OWN * 128, D])
    x_smp = din("x_smp", [128, D])
    mem = din("mem", [256, D])
    w_in = din("w_in", [D, D_IN])
    w_mem_kv = din("w_mem_kv", [D, 1024])
    w_a_out = din("w_a_out", [512, D])
    w_g_out = din("w_g_out", [512, D])
    w_m_out = din("w_m_out", [512, D])
    w_o = din("w_o", [D, D])
    w_ffn_in = din("w_ffn_in", [D, 2 * D_FF])
    w_ffn_out = din("w_ffn_out", [D_FF, D])
    ident_d = din("ident", [128, 128])
    gains_d = din("gains", [128, 24])
    hv_d = din("headvecs", [1, 576])

    y_own = dout("y_own", [NT_OWN * 128, D])
    y_smp = dout("y_smp", [128, D])
    o_pk = dout("o_pk", [SEQ, 256])
    o_pv = dout("o_pv", [SEQ, 256])
    o_pik = dout("o_pik", [SEQ, 64])
    o_pconv = dout("o_pconv", [3, 1536])
    o_pmk = dout("o_pmk", [256, 512])
    o_pmv = dout("o_pmv", [256, 512])
    o_sk = dout("o_sk", [128, 256])
    o_sv = dout("o_sv", [128, 256])
    o_sik = dout("o_sik", [128, 64])
    o_sconv = dout("o_sconv", [16, 3, 1536])
    DBG = cfg.get("debug", False)
    skind = "ExternalOutput" if DBG else "Internal"
    cmk_d = din("cache_mem_k", [16, 256, 512])
    cmv_d = din("cache_mem_v", [16, 256, 512])
    cmask_d = din("cmask", [128, 2048])
    selw_d = din("selw", [128, 64])
    ckv_d = din("cache_kv", [2560 * 128, 512])
    cik_d = din("cache_idx_k", [2560 * 128, 64])
    pt_d = din("page_table", [1, 256], I32)
    pen_d = din("pen", [17, 128, 256])
    dconst_d = din("dconst", [128, 64])
    gconst_d = din("gconst", [128, 10 * 128 + 16 + 2048])
    gconvT_d = din("gconvT", [128, 48])
    sgdn_d = din("state_gdn", [16, 4, 128, 128])
    sconv_d = din("state_conv", [48, 1536])
    o_pgdn = dout("o_pgdn", [4, 128, 128])
    o_sgdn = dout("o_sgdn", [16, 4, 128, 128])
    if DBG:
        dbg_tm = dout("dbg_tm", [128, 1536])
        dbg_sm = dout("dbg_sm", [128, 64])
        dbg_o = dout("dbg_o", [128, 512])
        dbg_mask = dout("dbg_mask", [128, 512])
        dbg_bis = dout("dbg_bis", [128, 44])
        dbg_sc = dout("dbg_sc", [128, 512])
        dbg_uw = dout("dbg_uw", [128, 256])
        dbg_vn = dout("dbg_vn", [128, 128])
        dbg_aq = dout("dbg_aq", [128, 128])
        dbg_tt = dout("dbg_tt", [128, 128])
        dbg_wtm = dout("dbg_wtm", [128, 2048])
        dbg_p1 = dout("dbg_p1", [128, 128])
    a_sc = nc.dram_tensor("a_sc", [17 * 128, 512], F32, kind=skind).ap()
    m_sc = nc.dram_tensor("m_sc", [17 * 128, 512], F32, kind=skind).ap()
    g_sc = nc.dram_tensor("g_sc", [33 * 128, 512], F32, kind=skind).ap()
    x2_sc = nc.dram_tensor("x2_sc", [17 * 128, D], F32, kind=skind).ap()
    a_sc_b = [Buf() for _ in range(17)]
    m_sc_b = [Buf() for _ in range(17)]
    g_sc_b = [Buf() for _ in range(33)]
    x2_sc_b = [Buf() for _ in range(17)]
    outs_done = []

    with contextlib.ExitStack() as glob:
        def sb(st, name, shape, dt=F32):
            return T(st.enter_context(nc.sbuf_tensor("sb_" + name, list(shape), dt)), name)

        psum = [T(glob.enter_context(nc.psum_tensor("ps%d" % i, [128, 512], F32)), "ps%d" % i)
                for i in range(8)]
        ps_i = [0]
        ps_hold = set()

        def ps(hold=False):
            while True:
                idx = ps_i[0] % 8
                ps_i[0] += 1
                if idx not in ps_hold:
                    break
            if hold:
                ps_hold.add(idx)
            return psum[idx]

        def ps_release(p):
            ps_hold.discard(psum.index(p))

        ident = sb(glob, "ident", [128, 128])
        gains = sb(glob, "gains", [128, 24])
        hv = sb(glob, "hv", [128, 576])
        k.dma("sp", ident[:], ident_d[:, :], writes=[ident.b])
        k.dma("sp", gains[:], gains_d[:, :], writes=[gains.b])
        k.dma("sp", hv[:], hv_d[0:1, :].partition_broadcast(128), writes=[hv.b])

        cmask = sb(glob, "cmask", [128, 16, 128], BF16)
        selw = sb(glob, "selw", [128, 64])
        k.dma("pool", cmask[:], cmask_d[:, :].rearrange("p (b t) -> p b t", b=16), writes=[cmask.b])
        k.dma("sp", selw[:], selw_d[:, :], writes=[selw.b])
        MKT = sb(glob, "MKT", [128, 4, 256], BF16)
        MVa = sb(glob, "MVa", [128, 2, 4, 129], BF16)
        k.op("pool", lambda e: e.memset(MVa[:], 1.0), writes=[MVa.b])
        zb = sb(glob, "zb", [128, 512], BF16)
        k.op("pool", lambda e: e.memset(zb[:], 0.0), writes=[zb.b])

        def ps_zero(p):
            k.op("pe", lambda e: e.matmul(p[:, :], lhsT=zb[:, 0:128], rhs=zb[:, :], start=True, stop=False),
                 reads=[zb.b], writes=[p.b])


        def kside_store(kk_ap, vv_ap, ii_ap, rd, KT_ap, IKT_ap, VA_ap, wr):
            if kk_ap is not None:
                p = ps()
                for g in range(4):
                    k.op("pe", lambda e, g=g: e.transpose(p[0:64, g * 128:(g + 1) * 128],
                                                          kk_ap[:, g * 64:(g + 1) * 64], ident[:]),
                         reads=rd + [ident.b], writes=[p.b])
                k.op("act", lambda e: e.activation(out=KT_ap, in_=p[0:64, :].rearrange("p (g s) -> p g s", g=4),
                                                   func=AF.Copy), reads=[p.b], writes=wr)
            if ii_ap is not None:
                p2 = ps()
                k.op("pe", lambda e: e.transpose(p2[0:64, 0:128], ii_ap, ident[:]),
                     reads=rd + [ident.b], writes=[p2.b])
                k.op("act", lambda e: e.activation(out=IKT_ap, in_=p2[0:64, 0:128], func=AF.Copy),
                     reads=[p2.b], writes=wr)
            if vv_ap is not None:
                k.op("pool", lambda e: e.tensor_copy(VA_ap, vv_ap.rearrange("p (g d) -> p g d", g=4)),
                     reads=rd, writes=wr)

        def pipe_ahead(gens, depth):
            n = len(gens)

            def toB(g):
                for tok in g:
                    if tok == "B":
                        return
            for j_ in range(min(depth, n)):
                toB(gens[j_])
            for j_ in range(n):
                for _ in gens[j_]:
                    pass
                if j_ + depth < n:
                    toB(gens[j_ + depth])

        def load_xT(st_tiles, src_ap, gain_col, q="sp", rd=()):
            x32, xn, xT, stat = st_tiles
            k.dma(q, x32[:], src_ap, reads=list(rd), writes=[x32.b])
            k.op("act", lambda e: e.activation(out=xn[:], in_=x32[:], func=AF.Square,
                                               accum_out=stat[:, 0:1]),
                 reads=[x32.b], writes=[xn.b, stat.b])
            k.op("dve", lambda e: e.tensor_scalar(stat[:, 1:2], stat[:, 0:1], 1.0 / D, EPS,
                                                  op0=ALU.mult, op1=ALU.add),
                 reads=[stat.b], writes=[stat.b])
            k.op("act", lambda e: e.activation(out=stat[:, 3:4], in_=stat[:, 1:2], func=AF.Sqrt),
                 reads=[stat.b], writes=[stat.b])
            k.op("dve", lambda e: e.reciprocal(stat[:, 2:3], stat[:, 3:4]),
                 reads=[stat.b], writes=[stat.b])
            k.op("act", lambda e: e.activation(out=xn[:], in_=x32[:], func=AF.Copy,
                                               scale=stat[:, 2:3]),
                 reads=[x32.b, stat.b], writes=[xn.b])
            for half in range(2):
                p = ps()
                for j in range(4):
                    kc = half * 4 + j
                    k.op("pe", lambda e, kc=kc, j=j: e.transpose(
                        p[:, j * 128:(j + 1) * 128], xn[:, kc * 128:(kc + 1) * 128], ident[:]),
                        reads=[xn.b, ident.b], writes=[p.b])
                g = gains[:, gain_col + half * 4: gain_col + half * 4 + 4]
                k.op("dve", lambda e, half=half, g=g, p=p: e.tensor_tensor(
                    xT[:, half * 4:(half + 1) * 4, :],
                    p[:, :].rearrange("p (a b) -> p a b", a=4),
                    g.unsqueeze(2).to_broadcast([128, 4, 128]), op=ALU.mult),
                    reads=[p.b, gains.b], writes=[xT.b])
            return x32, xT

        def linear(xT, W, c0, ncol, p, kcs=KC):
            for kc in range(kcs):
                k.op("pe", lambda e, kc=kc: e.matmul(
                    p[:, 0:ncol], lhsT=xT[:, kc, :], rhs=W[:, kc, c0:c0 + ncol],
                    start=(kc == 0), stop=(kc == kcs - 1)),
                    reads=[xT.b, W.b], writes=[p.b])

        def load_w(W, src, c0, ncol, dst0=0, rows=D):
            nkc = rows // 128
            for kc in range(nkc):
                k.dma("pool", W[:, kc, dst0:dst0 + ncol], src[kc * 128:(kc + 1) * 128, c0:c0 + ncol],
                      writes=[W.b])

        def head_rmsnorm(st, src_ps, nh, dh, gain_ap, out32, tmp, stat, name):
            k.op("act", lambda e: e.activation(out=tmp[:, 0:nh * dh], in_=src_ps, func=AF.Square),
                 reads=[st], writes=[tmp.b])
            k.op("dve", lambda e: e.tensor_reduce(
                stat[:, 0:nh], tmp[:, 0:nh * dh].rearrange("p (h d) -> p h d", h=nh),
                axis=AX.X, op=ALU.add), reads=[tmp.b], writes=[stat.b])
            k.op("dve", lambda e: e.tensor_scalar(stat[:, 8:8 + nh], stat[:, 0:nh], 1.0 / dh, EPS,
                                                  op0=ALU.mult, op1=ALU.add),
                 reads=[stat.b], writes=[stat.b])
            k.op("act", lambda e: e.activation(out=stat[:, 0:nh], in_=stat[:, 8:8 + nh], func=AF.Sqrt),
                 reads=[stat.b], writes=[stat.b])
            k.op("dve", lambda e: e.reciprocal(stat[:, 16:16 + nh], stat[:, 0:nh]),
                 reads=[stat.b], writes=[stat.b])
            k.op("dve", lambda e: e.tensor_tensor(
                out32[:, 0:nh * dh].rearrange("p (h d) -> p h d", h=nh),
                src_ps.rearrange("p (h d) -> p h d", h=nh),
                stat[:, 16:16 + nh].unsqueeze(2).to_broadcast([128, nh, dh]), op=ALU.mult),
                reads=[st, stat.b], writes=[out32.b])
            k.op("dve", lambda e: e.tensor_tensor(
                out32[:, 0:nh * dh].rearrange("p (h d) -> p h d", h=nh),
                out32[:, 0:nh * dh].rearrange("p (h d) -> p h d", h=nh),
                gain_ap.unsqueeze(1).to_broadcast([128, nh, dh]), op=ALU.mult),
                reads=[out32.b, hv.b], writes=[out32.b])

        PH = cfg.get('phases', 'BCGEFD')
        with contextlib.ExitStack() as st:
          if 'B' in PH:
              Wm = sb(st, "Wm", [128, KC, 1024], BF16)
              load_w(Wm, w_mem_kv, 0, 1024)
              tiles = (sb(st, "mx32", [128, D]), sb(st, "mxn", [128, D]),
                       sb(st, "mxT", [128, KC, 128], BF16), sb(st, "mstat", [128, 4]))
              tmp = sb(st, "mtmp", [128, 512])
              hstat = sb(st, "mhstat", [128, 24])
              mk32 = [sb(st, "mk32_%d" % i, [128, 512]) for i in range(2)]
              mv32 = [sb(st, "mv32_%d" % i, [128, 512]) for i in range(2)]
              for mt in range(2):
                  _, xT = load_xT(tiles, mem[mt * 128:(mt + 1) * 128, :], 8)
                  pk = ps()
                  linear(xT, Wm, 0, 512, pk)
                  pv = ps()
                  linear(xT, Wm, 512, 512, pv)
                  head_rmsnorm(pk.b, pk[:, :], 4, 128, hv[:, 256:384], mk32[mt], tmp, hstat, "mk")
                  k.op("act", lambda e, mt=mt, pv=pv: e.activation(out=mv32[mt][:], in_=pv[:, :], func=AF.Copy),
                       reads=[pv.b], writes=[mv32[mt].b])
                  p = ps()
                  for h in range(4):
                      k.op("pe", lambda e, h=h, p=p, mt=mt: e.transpose(
                          p[:, h * 128:(h + 1) * 128], mk32[mt][:, h * 128:(h + 1) * 128], ident[:]),
                          reads=[mk32[mt].b, ident.b], writes=[p.b])
                  k.op("act", lambda e, p=p, mt=mt: e.activation(
                      out=MKT[:, :, mt * 128:(mt + 1) * 128], in_=p[:, :].rearrange("p (h m) -> p h m", h=4),
                      func=AF.Copy), reads=[p.b], writes=[MKT.b])
                  k.op("dve", lambda e, mt=mt: e.tensor_copy(
                      MVa[:, mt, :, 0:128], mv32[mt][:, :].rearrange("p (h d) -> p h d", h=4)),
                      reads=[mv32[mt].b], writes=[MVa.b])
                  k.dma("sp", o_pmk[mt * 128:(mt + 1) * 128, :], mk32[mt][:], reads=[mk32[mt].b])
                  k.dma("sp", o_pmv[mt * 128:(mt + 1) * 128, :], mv32[mt][:], reads=[mv32[mt].b])
                  outs_done += [mk32[mt].b, mv32[mt].b]

        k.barrier()
        with contextlib.ExitStack() as st:
          if 'G' in PH:
              Wg2 = sb(st, "Wg2", [128, KC, 2056], BF16)
              load_w(Wg2, w_in, O_GQKV, 2056, 0)
              gcs = sb(st, "gcs", [128, 10 * 128 + 16 + 2048])
              k.dma("sp", gcs[:], gconst_d[:, :], writes=[gcs.b])
              cv = lambda i: gcs[:, i * 128:(i + 1) * 128]
              LTRI, BONES, SLm, SUIm = [cv(0), cv(1)], [cv(2), cv(3)], [cv(4), cv(5)], [cv(6), cv(7)]
              USTR, ONES = cv(8), cv(9)
              G8 = gcs[:, 1280:1296]
              CM32 = gcs[:, 1296:1296 + 2048].rearrange("p (b t) -> p b t", b=16)
              gcv = sb(st, "gcv", [128, 48])
              k.dma("sp", gcv[:], gconvT_d[:, :], writes=[gcv.b])
              Dg = sb(st, "Dg", [128, 12, 4, 128], BF16)
              for cc in range(12):
                  for jj in range(4):
                      k.op("dve", lambda e, cc=cc, jj=jj: e.tensor_scalar(
                          Dg[:, cc, jj, :], ident[:], gcv[:, cc * 4 + jj:cc * 4 + jj + 1], None, op0=ALU.mult),
                          reads=[ident.b, gcv.b], writes=[Dg.b])
              negA = sb(st, "negA", [128, 4])
              k.op("act", lambda e: e.activation(out=negA[:], in_=hv[:, 516:520], func=AF.Exp),
                   reads=[hv.b], writes=[negA.b])
              k.op("dve", lambda e: e.tensor_scalar(negA[:], negA[:], -1.0, None, op0=ALU.mult),
                   reads=[negA.b], writes=[negA.b])
              Sst = [sb(st, "Sst%d" % h, [128, 128]) for h in range(4)]
              for h in range(4):
                  k.op("pool", lambda e, h=h: e.memset(Sst[h][:], 0.0), writes=[Sst[h].b])
              tsets = [(sb(st, "gx32_%d" % i, [128, D]), sb(st, "gxn_%d" % i, [128, D]),
                        sb(st, "gxT_%d" % i, [128, KC, 128], BF16), sb(st, "gstat_%d" % i, [128, 4]))
                       for i in range(1)] * 2
              Up = sb(st, "Up", [128, 12, 131], BF16)
              k.op("pool", lambda e: e.memset(Up[:], 0.0), writes=[Up.b])
              UC = sb(st, "UC", [128, 12, 128])
              TM = sb(st, "TM", [128, 1536])
              sq = sb(st, "gsq", [128, 1024])
              sgz2 = [sb(st, "sgz%d" % q, [128, 512]) for q in range(1)]
              sm2 = [sb(st, "gsm%d" % q, [128, 64]) for q in range(1)]
              scal = sb(st, "gscal", [128, 6, 4])
              KQ = sb(st, "KQ", [128, 12, 128])
              KQT2 = [sb(st, "KQT%d" % q, [128, 12, 128]) for q in range(1)]
              KBG2 = [sb(st, "KBG%d" % q, [128, 4, 128]) for q in range(1)]
              KTL2 = [sb(st, "KTL%d" % q, [128, 4, 128]) for q in range(1)]
              VB2 = [sb(st, "VB%d" % q, [128, 4, 128]) for q in range(1)]
              O32 = sb(st, "O32", [128, 512])
              go32 = sb(st, "go32", [128, 512])
              gtmp = sb(st, "ggtmp", [128, 512])
              ghstat = sb(st, "ghstat", [128, 24])
              hb = [dict(GU=sb(st, "GU%d" % i, [128, 128]), G2=sb(st, "G2%d" % i, [128, 256]),
                         t1=sb(st, "t1%d" % i, [128, 128]), aq=sb(st, "aqkT%d" % i, [128, 128]),
                         P=[sb(st, "P%d_%d" % (i, q), [128, 256]) for q in range(2)],
                         TT=[sb(st, "TT%d_%d" % (i, q), [128, 128]) for q in range(2)],
                         UW=sb(st, "UW%d" % i, [128, 256]), vn=sb(st, "vn%d" % i, [128, 128]))
                    for i in range(4)]
              DK5 = 128.0 ** -0.5

              def gdn_tile(j, sm_st=None):
                  smp = (j == NT_ALL)
                  m = 1 if smp else 0
                  bf_ = j % 2
                  sgz, KQT, KBG, KTL, VB, sm = sgz2[bf_], KQT2[bf_], KBG2[bf_], KTL2[bf_], VB2[bf_], sm2[bf_]
                  src = x_smp[:, :] if smp else x_all[j * 128:(j + 1) * 128, :]
                  x32, xT = load_xT(tsets[j % 2], src, 0)
                  yield
                  U = sm_st["Us"] if smp else Up
                  for cc0 in (0, 4, 8):
                      p = ps()
                      for c4 in range(4):
                          cc = cc0 + c4
                          for kc in range(KC):
                              k.op("pe", lambda e, kc=kc, cc=cc, c4=c4, p=p: e.matmul(
                                  p[:, c4 * 128:(c4 + 1) * 128], lhsT=Wg2[:, kc, cc * 128:(cc + 1) * 128],
                                  rhs=xT[:, kc, :], start=(kc == 0), stop=(kc == KC - 1)),
                                  reads=[Wg2.b, xT.b], writes=[p.b])
                      if smp:
                          for c4 in range(4):
                              k.op("act", lambda e, p=p, cc0=cc0, c4=c4: e.activation(
                                  out=U[:, cc0 + c4, :, 3:11],
                                  in_=p[:, c4 * 128:(c4 + 1) * 128].rearrange("p (b t) -> p b t", b=16),
                                  func=AF.Copy), reads=[p.b], writes=[U.b])
                      else:
                          k.op("act", lambda e, p=p, cc0=cc0: e.activation(
                              out=U[:, cc0:cc0 + 4, 3:131], in_=p[:, :].rearrange("p (a t) -> p a t", a=4),
                              func=AF.Copy), reads=[p.b], writes=[U.b])
                      yield
                  for cc0 in (0, 4, 8):
                      p = ps()
                      for c4 in range(4):
                          cc = cc0 + c4
                          for jj in range(4):
                              rhs = U[:, cc, :, jj:jj + 8] if smp else U[:, cc, jj:jj + 128]
                              k.op("pe", lambda e, jj=jj, cc=cc, c4=c4, p=p, rhs=rhs: e.matmul(
                                  p[:, c4 * 128:(c4 + 1) * 128], lhsT=Dg[:, cc, jj, :], rhs=rhs,
                                  start=(jj == 0), stop=(jj == 3)), reads=[Dg.b, U.b], writes=[p.b])
                      k.op("act", lambda e, p=p, cc0=cc0: e.activation(
                          out=UC[:, cc0:cc0 + 4, :], in_=p[:, :].rearrange("p (a t) -> p a t", a=4),
                          func=AF.Silu), reads=[p.b], writes=[UC.b])
                      yield
                  if not smp:
                      k.op("dve", lambda e: e.tensor_copy(U[:, :, 0:3], U[:, :, 128:131]),
                           reads=[U.b], writes=[U.b])
                  for cc0 in (0, 4, 8):
                      p = ps()
                      for c4 in range(4):
                          k.op("pe", lambda e, cc0=cc0, c4=c4, p=p: e.transpose(
                              p[:, c4 * 128:(c4 + 1) * 128], UC[:, cc0 + c4, :], ident[:]),
                              reads=[UC.b, ident.b], writes=[p.b])
                      k.op("act", lambda e, p=p, cc0=cc0: e.activation(
                          out=TM[:, cc0 * 128:(cc0 + 4) * 128], in_=p[:, :], func=AF.Copy),
                          reads=[p.b], writes=[TM.b])
                      yield
                  pgz = ps()
                  linear(xT, Wg2, 1536, 512, pgz)
                  pgb = ps()
                  linear(xT, Wg2, 2048, 8, pgb)
                  k.op("act", lambda e: e.activation(out=sgz[:], in_=pgz[:, :], func=AF.Silu),
                       reads=[pgz.b], writes=[sgz.b])
                  k.op("act", lambda e: e.activation(out=sm[:, 0:4], in_=pgb[:, 0:4], func=AF.Sigmoid),
                       reads=[pgb.b], writes=[sm.b])
                  k.op("dve", lambda e: e.tensor_tensor(sm[:, 4:8], pgb[:, 4:8], hv[:, 512:516], op=ALU.add),
                       reads=[pgb.b, hv.b], writes=[sm.b])
                  k.op("act", lambda e: e.activation(out=sm[:, 8:12], in_=sm[:, 4:8], func=AF.Exp),
                       reads=[sm.b], writes=[sm.b])
                  k.op("act", lambda e: e.activation(out=sm[:, 12:16], in_=sm[:, 8:12], func=AF.Ln, bias=1.0),
                       reads=[sm.b], writes=[sm.b])
                  k.op("dve", lambda e: e.tensor_tensor(sm[:, 16:20], sm[:, 12:16], negA[:], op=ALU.mult),
                       reads=[sm.b, negA.b], writes=[sm.b])
                  gt = sm[:, 16:20]
                  yield
                  k.op("act", lambda e: e.activation(out=sq[:], in_=TM[:, 0:1024], func=AF.Square),
                       reads=[TM.b], writes=[sq.b])
                  k.op("dve", lambda e: e.tensor_reduce(
                      sm[:, 20:28], sq[:, :].rearrange("p (h d) -> p h d", h=8), axis=AX.X, op=ALU.add),
                      reads=[sq.b], writes=[sm.b])
                  k.op("dve", lambda e: e.tensor_scalar(sm[:, 20:28], sm[:, 20:28], EPS, None, op0=ALU.add),
                       reads=[sm.b], writes=[sm.b])
                  k.op("act", lambda e: e.activation(out=sm[:, 28:36], in_=sm[:, 20:28], func=AF.Sqrt),
                       reads=[sm.b], writes=[sm.b])
                  k.op("dve", lambda e: e.reciprocal(sm[:, 36:44], sm[:, 28:36]), reads=[sm.b], writes=[sm.b])
                  rq, rk = sm[:, 36:40], sm[:, 40:44]
                  pc = ps()
                  k.op("pe", lambda e: e.matmul(pc[:, 0:4], lhsT=LTRI[m], rhs=gt, start=True, stop=True),
                       reads=[gcs.b, sm.b], writes=[pc.b])
                  k.op("pe", lambda e: e.matmul(pc[:, 4:8], lhsT=BONES[m], rhs=gt, start=True, stop=True),
                       reads=[gcs.b, sm.b], writes=[pc.b])
                  k.op("dve", lambda e: e.tensor_copy(sm[:, 44:52], pc[:, 0:8]), reads=[pc.b], writes=[sm.b])
                  k.op("act", lambda e: e.activation(out=sm[:, 52:56], in_=sm[:, 44:48], func=AF.Exp),
                       reads=[sm.b], writes=[sm.b])
                  k.op("dve", lambda e: e.tensor_tensor(sm[:, 56:60], sm[:, 48:52], sm[:, 44:48], op=ALU.subtract),
                       reads=[sm.b], writes=[sm.b])
                  k.op("act", lambda e: e.activation(out=sm[:, 56:60], in_=sm[:, 56:60], func=AF.Exp),
                       reads=[sm.b], writes=[sm.b])
                  k.op("act", lambda e: e.activation(out=sm[:, 60:64], in_=sm[:, 48:52], func=AF.Exp),
                       reads=[sm.b], writes=[sm.b])
                  egc, etl, dec, beta = sm[:, 52:56], sm[:, 56:60], sm[:, 60:64], sm[:, 0:4]
                  k.op("dve", lambda e: e.tensor_copy(scal[:, 0, :], rq), reads=[sm.b], writes=[scal.b])
                  k.op("dve", lambda e: e.scalar_tensor_tensor(out=scal[:, 1, :], in0=rq, scalar=DK5, in1=egc,
                                                               op0=ALU.mult, op1=ALU.mult),
                       reads=[sm.b], writes=[scal.b])
                  k.op("dve", lambda e: e.tensor_copy(scal[:, 2, :], rk), reads=[sm.b], writes=[scal.b])
                  k.op("dve", lambda e: e.tensor_tensor(scal[:, 5, :], rk, beta, op=ALU.mult),
                       reads=[sm.b], writes=[scal.b])
                  k.op("dve", lambda e: e.tensor_tensor(scal[:, 3, :], scal[:, 5, :], egc, op=ALU.mult),
                       reads=[sm.b, scal.b], writes=[scal.b])
                  k.op("dve", lambda e: e.tensor_tensor(scal[:, 4, :], rk, etl, op=ALU.mult),
                       reads=[sm.b], writes=[scal.b])
                  yield
                  TMq = TM[:, 0:512].rearrange("p (h d) -> p h d", h=4)
                  TMk = TM[:, 512:1024].rearrange("p (h d) -> p h d", h=4)
                  TMv = TM[:, 1024:1536].rearrange("p (h d) -> p h d", h=4)
                  bc = lambda a: a.unsqueeze(2).to_broadcast([128, 4, 128])
                  for dst, src_, sc_ in ((KQ[:, 0:4, :], TMq, scal[:, 0, :]), (KQ[:, 4:8, :], TMk, scal[:, 2, :]),
                                         (KQ[:, 8:12, :], TMq, scal[:, 1, :]), (KBG[:], TMk, scal[:, 3, :]),
                                         (KTL[:], TMk, scal[:, 4, :]), (VB[:], TMv, beta)):
                      wb_ = KQ.b if dst.tensor.name == KQ[:].tensor.name else (
                          KBG.b if dst.tensor.name == KBG[:].tensor.name else (
                              KTL.b if dst.tensor.name == KTL[:].tensor.name else VB.b))
                      k.op("dve", lambda e, dst=dst, src_=src_, sc_=sc_: e.tensor_tensor(
                          dst, src_, bc(sc_), op=ALU.mult), reads=[TM.b, scal.b, sm.b], writes=[wb_])
                  for cc0 in (0, 4, 8):
                      p = ps()
                      for c4 in range(4):
                          k.op("pe", lambda e, cc0=cc0, c4=c4, p=p: e.transpose(
                              p[:, c4 * 128:(c4 + 1) * 128], KQ[:, cc0 + c4, :], ident[:]),
                              reads=[KQ.b, ident.b], writes=[p.b])
                      k.op("act", lambda e, p=p, cc0=cc0: e.activation(
                          out=KQT[:, cc0:cc0 + 4, :], in_=p[:, :].rearrange("p (a t) -> p a t", a=4),
                          func=AF.Copy), reads=[p.b], writes=[KQT.b])
                      yield
                  if smp:
                      rhs3 = sm_st["rhs3"]
                      k.op("dve", lambda e: e.tensor_tensor(
                          rhs3[:], gt.unsqueeze(1).to_broadcast([128, 16, 4]),
                          G8.unsqueeze(2).to_broadcast([128, 16, 4]), op=ALU.mult),
                          reads=[sm.b, gcs.b], writes=[rhs3.b])
                      pdb = ps()
                      k.op("pe", lambda e: e.matmul(pdb[:, 0:64], lhsT=ONES,
                                                    rhs=rhs3[:].rearrange("p b h -> p (b h)"), start=True, stop=True),
                           reads=[gcs.b, rhs3.b], writes=[pdb.b])
                      decB = sm_st["decB"]
                      k.op("act", lambda e: e.activation(out=decB[:], in_=pdb[:, 0:64], func=AF.Exp),
                           reads=[pdb.b], writes=[decB.b])
                  def head_gen(h):
                      H = hb[h]
                      GU, G2, t1, aq, UW, vn = H["GU"], H["G2"], H["t1"], H["aq"], H["UW"], H["vn"]
                      KnT, QnT, DQT = KQT[:, 4 + h, :], KQT[:, h, :], KQT[:, 8 + h, :]
                      k.op("dve", lambda e: e.tensor_scalar(GU[:], USTR, gt[:, h:h + 1], None, op0=ALU.mult),
                           reads=[gcs.b, sm.b], writes=[GU.b])
                      pD = ps()
                      k.op("pe", lambda e: e.matmul(pD[:, 0:128], lhsT=LTRI[m], rhs=GU[:], start=True, stop=True),
                           reads=[gcs.b, GU.b], writes=[pD.b])
                      k.op("pe", lambda e: e.matmul(pD[:, 128:256], lhsT=GU[:], rhs=LTRI[m], start=True, stop=True),
                           reads=[gcs.b, GU.b], writes=[pD.b])
                      k.op("pe", lambda e: e.matmul(pD[:, 256:384], lhsT=KnT, rhs=KnT, start=True, stop=True),
                           reads=[KQT.b], writes=[pD.b])
                      k.op("pe", lambda e: e.matmul(pD[:, 384:512], lhsT=KnT, rhs=QnT, start=True, stop=True),
                           reads=[KQT.b], writes=[pD.b])
                      yield
                      k.op("act", lambda e: e.activation(out=G2[:], in_=pD[:, 0:256], func=AF.Exp),
                           reads=[pD.b], writes=[G2.b])
                      yield
                      k.op("dve", lambda e: e.tensor_tensor(t1[:], pD[:, 256:384], G2[:, 0:128], op=ALU.mult),
                           reads=[pD.b, G2.b], writes=[t1.b])
                      P0 = H["P"][0]
                      k.op("dve", lambda e: e.tensor_scalar(t1[:], t1[:], beta[:, h:h + 1], -1.0,
                                                            op0=ALU.mult, op1=ALU.mult),
                           reads=[t1.b, sm.b], writes=[t1.b])
                      k.op("dve", lambda e: e.tensor_tensor(P0[:, 0:128], t1[:], SLm[m], op=ALU.mult),
                           reads=[t1.b, gcs.b], writes=[P0.b])
                      k.op("dve", lambda e: e.tensor_tensor(t1[:], pD[:, 384:512], G2[:, 128:256], op=ALU.mult),
                           reads=[pD.b, G2.b], writes=[t1.b])
                      k.op("dve", lambda e: e.tensor_tensor(aq[:], t1[:], SUIm[m], op=ALU.mult),
                           reads=[t1.b, gcs.b], writes=[aq.b])
                      yield
                      pN = ps()
                      k.op("pe", lambda e: e.transpose(pN[:, 0:128], P0[:, 0:128], ident[:]),
                           reads=[P0.b, ident.b], writes=[pN.b])
                      k.op("act", lambda e: e.activation(out=P0[:, 128:256], in_=pN[:, 0:128], func=AF.Copy),
                           reads=[pN.b], writes=[P0.b])
                      TTc = H["TT"][0]
                      k.op("dve", lambda e: e.tensor_tensor(TTc[:], P0[:, 128:256], ident[:], op=ALU.add),
                           reads=[P0.b, ident.b], writes=[TTc.b])
                      yield
                      Pc = P0
                      for n in range(1, 7):
                          Pn = H["P"][n % 2]
                          TTn = H["TT"][n % 2]
                          pP = ps()
                          k.op("pe", lambda e, Pc=Pc, pP=pP: e.matmul(pP[:, 0:128], lhsT=Pc[:, 128:256], rhs=Pc[:, 0:128],
                                                                      start=True, stop=True),
                               reads=[Pc.b], writes=[pP.b])
                          if n < 6:
                              k.op("pe", lambda e, Pc=Pc, pP=pP: e.matmul(pP[:, 128:256], lhsT=Pc[:, 0:128],
                                                                          rhs=Pc[:, 128:256], start=True, stop=True),
                                   reads=[Pc.b], writes=[pP.b])
                          k.op("act", lambda e, Pn=Pn, pP=pP: e.activation(out=Pn[:], in_=pP[:, 0:256], func=AF.Copy),
                               reads=[pP.b], writes=[Pn.b])
                          yield
                          pT = ps()
                          k.op("pe", lambda e, Pn=Pn, pT=pT, TTc=TTc: e.matmul(pT[:, 0:128], lhsT=Pn[:, 0:128], rhs=TTc[:],
                                                                               start=True, stop=True),
                               reads=[Pn.b, TTc.b], writes=[pT.b])
                          k.op("dve", lambda e, TTn=TTn, TTc=TTc, pT=pT: e.tensor_tensor(
                              TTn[:], TTc[:], pT[:, 0:128], op=ALU.add), reads=[TTc.b, pT.b], writes=[TTn.b])
                          yield
                          Pc, TTc = Pn, TTn
                      pU = ps()
                      k.op("pe", lambda e, TTc=TTc: e.matmul(pU[:, 0:128], lhsT=TTc[:], rhs=VB[:, h, :], start=True, stop=True),
                           reads=[TTc.b, VB.b], writes=[pU.b])
                      k.op("pe", lambda e, TTc=TTc: e.matmul(pU[:, 128:256], lhsT=KBG[:, h, :], rhs=TTc[:], start=True, stop=True),
                           reads=[TTc.b, KBG.b], writes=[pU.b])
                      k.op("act", lambda e: e.activation(out=UW[:], in_=pU[:, 0:256], func=AF.Copy),
                           reads=[pU.b], writes=[UW.b])
                      yield
                      if not smp:
                          S_ = Sst[h]
                          p1 = ps()
                          k.op("pe", lambda e: e.matmul(p1[:, 0:128], lhsT=UW[:, 128:256], rhs=S_[:], start=True, stop=True),
                               reads=[UW.b, S_.b], writes=[p1.b])
                          k.op("dve", lambda e: e.tensor_tensor(vn[:], UW[:, 0:128], p1[:, 0:128], op=ALU.subtract),
                               reads=[UW.b, p1.b], writes=[vn.b])
                          yield
                          p2 = ps()
                          k.op("pe", lambda e: e.matmul(p2[:, 0:128], lhsT=DQT, rhs=S_[:], start=True, stop=False),
                               reads=[KQT.b, S_.b], writes=[p2.b])
                          k.op("pe", lambda e: e.matmul(p2[:, 0:128], lhsT=aq[:], rhs=vn[:], start=False, stop=True),
                               reads=[aq.b, vn.b], writes=[p2.b])
                          k.op("act", lambda e: e.activation(out=O32[:, h * 128:(h + 1) * 128], in_=p2[:, 0:128], func=AF.Copy),
                               reads=[p2.b], writes=[O32.b])
                          p3 = ps()
                          k.op("pe", lambda e: e.matmul(p3[:, 0:128], lhsT=KTL[:, h, :], rhs=vn[:], start=True, stop=True),
                               reads=[KTL.b, vn.b], writes=[p3.b])
                          k.op("dve", lambda e: e.scalar_tensor_tensor(out=S_[:], in0=S_[:], scalar=dec[:, h:h + 1],
                                                                       in1=p3[:, 0:128], op0=ALU.mult, op1=ALU.add),
                               reads=[S_.b, sm.b, p3.b], writes=[S_.b])
                      else:
                          Ssm, WTm, DQm, vm, So = (sm_st["Ssm"], sm_st["WTm"], sm_st["DQm"], sm_st["vm"], sm_st["So"])
                          decB = sm_st["decB"]
                          k.op("dve", lambda e: e.tensor_tensor(
                              WTm[:], UW[:, 128:256].unsqueeze(1).to_broadcast([128, 16, 128]), CM32, op=ALU.mult),
                              reads=[UW.b, gcs.b], writes=[WTm.b])
                          p1 = ps(hold=True)
                          for b in range(16):
                              k.op("pe", lambda e, b=b: e.matmul(p1[:, 0:128], lhsT=WTm[:, b, :], rhs=Ssm[:, b * 4 + h, :],
                                                                 start=(b == 0), stop=(b == 15)),
                                   reads=[WTm.b, Ssm.b], writes=[p1.b])
                          k.op("dve", lambda e: e.tensor_tensor(vn[:], UW[:, 0:128], p1[:, 0:128], op=ALU.subtract),
                               reads=[UW.b, p1.b], writes=[vn.b])
                          ps_release(p1)
                          k.op("dve", lambda e: e.tensor_tensor(
                              DQm[:], DQT.unsqueeze(1).to_broadcast([128, 16, 128]), CM32, op=ALU.mult),
                              reads=[KQT.b, gcs.b], writes=[DQm.b])
                          p2 = ps(hold=True)
                          for b in range(16):
                              k.op("pe", lambda e, b=b: e.matmul(p2[:, 0:128], lhsT=DQm[:, b, :], rhs=Ssm[:, b * 4 + h, :],
                                                                 start=(b == 0), stop=False),
                                   reads=[DQm.b, Ssm.b], writes=[p2.b])
                          k.op("pe", lambda e: e.matmul(p2[:, 0:128], lhsT=aq[:], rhs=vn[:], start=False, stop=True),
                               reads=[aq.b, vn.b], writes=[p2.b])
                          k.op("act", lambda e: e.activation(out=O32[:, h * 128:(h + 1) * 128], in_=p2[:, 0:128], func=AF.Copy),
                               reads=[p2.b], writes=[O32.b])
                          ps_release(p2)
                          k.op("dve", lambda e: e.tensor_tensor(
                              vm[:], vn[:].unsqueeze(1).to_broadcast([128, 16, 128]),
                              G8.unsqueeze(2).to_broadcast([128, 16, 128]), op=ALU.mult),
                              reads=[vn.b, gcs.b], writes=[vm.b])
                          for b in range(16):
                              p3 = ps()
                              k.op("pe", lambda e, b=b, p3=p3: e.matmul(p3[:, 0:128], lhsT=KTL[:, h, :], rhs=vm[:, b, :],
                                                                        start=True, stop=True),
                                   reads=[KTL.b, vm.b], writes=[p3.b])
                              so = So[b % 2]
                              k.op("dve", lambda e, b=b, p3=p3, so=so: e.scalar_tensor_tensor(
                                  out=so[:], in0=Ssm[:, b * 4 + h, :], scalar=decB[:, b * 4 + h:b * 4 + h + 1],
                                  in1=p3[:, 0:128], op0=ALU.mult, op1=ALU.add),
                                  reads=[Ssm.b, decB.b, p3.b], writes=[so.b])
                              k.dma("sp", o_sgdn[b, h], so[:], reads=[so.b])
                  if smp and DBG:
                      k.dma("sp", dbg_tm[:, :], TM[:], reads=[TM.b])
                      k.dma("sp", dbg_sm[:, :], sm[:], reads=[sm.b])
                      k.dma("sp", dbg_o[:, :], O32[:], reads=[O32.b])
                      H = hb[1]
                      k.dma("sp", dbg_uw[:, :], H["UW"][:], reads=[H["UW"].b])
                      k.dma("sp", dbg_vn[:, :], H["vn"][:], reads=[H["vn"].b])
                      k.dma("sp", dbg_aq[:, :], H["aq"][:], reads=[H["aq"].b])
                      k.dma("sp", dbg_tt[:, :], H["TT"][0][:], reads=[H["TT"][0].b])
                  yield "B"
                  if True:
                      gens = [head_gen(h) for h in range(4)]
                      alive = [True] * 4
                      while any(alive):
                          for gi, g_ in enumerate(gens):
                              if alive[gi]:
                                  try:
                                      next(g_)
                                  except StopIteration:
                                      alive[gi] = False
                          yield
                  head_rmsnorm(O32.b, O32[:, :], 4, 128, hv[:, 384:512], go32, gtmp, ghstat, "go")
                  k.op("dve", lambda e: e.tensor_tensor(go32[:], go32[:], sgz[:], op=ALU.mult),
                       reads=[go32.b, sgz.b], writes=[go32.b])
                  k.dma("sp", g_sc[j * 128:(j + 1) * 128, :], go32[:], reads=[go32.b], writes=[g_sc_b[j]])

              with contextlib.ExitStack() as st2:
                  sm_st = dict(Us=sb(st2, "Us", [128, 12, 16, 11], BF16), Ssm=sb(st2, "Ssm", [128, 64, 128]),
                               WTm=sb(st2, "WTm", [128, 16, 128]), rhs3=sb(st2, "rhs3", [128, 16, 4]),
                               decB=sb(st2, "decB", [128, 64]),
                               So=[sb(st2, "So%d" % i, [128, 128]) for i in range(2)])
                  sm_st["DQm"] = sm_st["WTm"]
                  sm_st["vm"] = sm_st["WTm"]
                  Ssm = sm_st["Ssm"]
                  for b in range(16):
                      k.dma("sp", Ssm[:, b * 4:(b + 1) * 4, :], sgdn_d[b].rearrange("h k v -> k h v"), writes=[Ssm.b])
                  cb = sb(st2, "cb", [48, 1536])
                  k.dma("sp", cb[:], sconv_d[:, :], writes=[cb.b])
                  Us = sm_st["Us"]
                  for cc0 in (0, 4, 8):
                      p = ps()
                      for c4 in range(4):
                          cc = cc0 + c4
                          k.op("pe", lambda e, cc=cc, c4=c4, p=p: e.transpose(
                              p[:, c4 * 48:(c4 + 1) * 48], cb[:, cc * 128:(cc + 1) * 128], ident[0:48, 0:48]),
                              reads=[cb.b, ident.b], writes=[p.b])
                      for c4 in range(4):
                          k.op("act", lambda e, cc0=cc0, c4=c4, p=p: e.activation(
                              out=Us[:, cc0 + c4, :, 0:3],
                              in_=p[:, c4 * 48:(c4 + 1) * 48].rearrange("p (b t) -> p b t", b=16),
                              func=AF.Copy), reads=[p.b], writes=[Us.b])
                  for _ in gdn_tile(NT_ALL, sm_st):
                      pass
                  outs_done += [s_.b for s_ in sm_st["So"]]
                  k.barrier()
              sgz2.append(sb(st, "sgz1", [128, 512]))
              KQT2.append(sb(st, "KQT1", [128, 12, 128]))
              KBG2.append(sb(st, "KBG1", [128, 4, 128]))
              KTL2.append(sb(st, "KTL1", [128, 4, 128]))
              VB2.append(sb(st, "VB1", [128, 4, 128]))
              sm2.append(sb(st, "gsm1", [128, 64]))
              gensG = [gdn_tile(j) for j in range(NT_ALL)]
              for tok in gensG[0]:
                  if tok == "B":
                      break
              for j in range(NT_ALL):
                  cur = gensG[j]
                  nxt = gensG[j + 1] if j + 1 < NT_ALL else None
                  cur_alive, nxt_inA = True, nxt is not None
                  while cur_alive or nxt_inA:
                      if cur_alive:
                          try:
                              next(cur)
                          except StopIteration:
                              cur_alive = False
                      if nxt_inA:
                          if next(nxt) == "B":
                              nxt_inA = False
              for h in range(4):
                  k.dma("sp", o_pgdn[h], Sst[h][:], reads=[Sst[h].b])
              outs_done += [s_.b for s_ in Sst]

        k.barrier()
        kst = contextlib.ExitStack()
        KT = sb(kst, "KT", [64, 4, SEQ + 256], BF16)
        VA = sb(kst, "VA", [128, NT_ALL + 2, 4, 65], BF16)
        IKT = sb(kst, "IKT", [64, SEQ + 256], BF16)
        KTn = sb(kst, "KTn", [64, 4, 128], BF16)
        IKTn = sb(kst, "IKTn", [64, 128], BF16)
        VAn = sb(kst, "VAn", [128, 4, 65], BF16)
        k.op("pool", lambda e: e.memset(VA[:], 1.0), writes=[VA.b])
        k.op("pool", lambda e: e.memset(VAn[:], 1.0), writes=[VAn.b])
        k.barrier()
        with contextlib.ExitStack() as st:
          if 'C' in PH:
              NCOL = 576 + 1536
              Wk = sb(st, "Wk", [128, KC, NCOL], BF16)
              load_w(Wk, w_in, O_AK, 512, 0)
              load_w(Wk, w_in, O_IK, 64, 512)
              load_w(Wk, w_in, O_GQKV, 1536, 576)
              tsets = [(sb(st, "cx32_%d" % i, [128, D]), sb(st, "cxn_%d" % i, [128, D]),
                        sb(st, "cxT_%d" % i, [128, KC, 128], BF16), sb(st, "cstat_%d" % i, [128, 4]))
                       for i in range(3)]
              tmp = sb(st, "ctmp", [128, 512])
              hstat = sb(st, "chstat", [128, 24])
              ko = [sb(st, "ko%d" % i, [128, 256]) for i in range(2)]
              vo = [sb(st, "vo%d" % i, [128, 256]) for i in range(2)]
              io = [sb(st, "io%d" % i, [128, 64]) for i in range(2)]
              gq = sb(st, "gq", [128, 1536])
              def c_gen(j):
                  smp = (j == NT_ALL)
                  src = x_smp[:, :] if smp else x_all[j * 128:(j + 1) * 128, :]
                  _, xT = load_xT(tsets[j % 3], src, 0)
                  yield "B"
                  pkv = ps()
                  linear(xT, Wk, 0, 512, pkv)
                  pik = ps()
                  linear(xT, Wk, 512, 64, pik)
                  kk, vv, ii = ko[j % 2], vo[j % 2], io[j % 2]
                  head_rmsnorm(pkv.b, pkv[:, 0:256], 4, 64, hv[:, 64:128], kk, tmp, hstat, "ak")
                  k.op("act", lambda e, vv=vv, pkv=pkv: e.activation(out=vv[:], in_=pkv[:, 256:512], func=AF.Copy),
                       reads=[pkv.b], writes=[vv.b])
                  k.op("act", lambda e, ii=ii, pik=pik: e.activation(out=ii[:], in_=pik[:, 0:64], func=AF.Copy),
                       reads=[pik.b], writes=[ii.b])
                  if smp:
                      kside_store(kk[:], vv[:], ii[:], [kk.b, vv.b, ii.b], KTn[:], IKTn[:], VAn[:, :, 0:64],
                                  [KTn.b, IKTn.b, VAn.b])
                  else:
                      kside_store(kk[:], vv[:], ii[:], [kk.b, vv.b, ii.b], KT[:, :, j * 128:(j + 1) * 128],
                                  IKT[:, j * 128:(j + 1) * 128], VA[:, j, :, 0:64], [KT.b, IKT.b, VA.b])
                  if smp:
                      k.dma("sp", o_sk[:, :], kk[:], reads=[kk.b])
                      k.dma("sp", o_sv[:, :], vv[:], reads=[vv.b])
                      k.dma("sp", o_sik[:, :], ii[:], reads=[ii.b])
                  else:
                      k.dma("sp", o_pk[j * 128:(j + 1) * 128, :], kk[:], reads=[kk.b])
                      k.dma("sp", o_pv[j * 128:(j + 1) * 128, :], vv[:], reads=[vv.b])
                      k.dma("sp", o_pik[j * 128:(j + 1) * 128, :], ii[:], reads=[ii.b])
                  if j >= NT_ALL - 1:
                      for c in range(3):
                          pg = ps()
                          linear(xT, Wk, 576 + c * 512, 512, pg)
                          k.op("act", lambda e, c=c, pg=pg: e.activation(
                              out=gq[:, c * 512:(c + 1) * 512], in_=pg[:, :], func=AF.Copy),
                              reads=[pg.b], writes=[gq.b])
                      if smp:
                          for t3 in range(3):
                              k.dma("sp", o_sconv[:, t3, :], gq[5 + t3::8, :], reads=[gq.b])
                      else:
                          k.dma("sp", o_pconv[:, :], gq[125:128, :], reads=[gq.b])
              pipe_ahead([c_gen(j) for j in range(NT_ALL + 1)], 2)
              outs_done += [b.b for b in ko + vo + io] + [gq.b]

        k.barrier()
        with contextlib.ExitStack() as st:
          if 'E' in PH:
              NQ = 512 + 256 + 4 + 512
              Wq = sb(st, "Wq", [128, KC, NQ], BF16)
              load_w(Wq, w_in, O_AQ, 512, 0)
              load_w(Wq, w_in, O_IQ, 256, 512)
              load_w(Wq, w_in, O_IW, 4, 768)
              load_w(Wq, w_in, O_MQ, 512, 772)
              tsets = [(sb(st, "ex32_%d" % i, [128, D]), sb(st, "exn_%d" % i, [128, D]),
                        sb(st, "exT_%d" % i, [128, KC, 128], BF16), sb(st, "estat_%d" % i, [128, 4]))
                       for i in range(1)] * 2
              tmp = sb(st, "etmp", [128, 512])
              hstat = sb(st, "ehstat", [128, 24])
              mq32 = sb(st, "mq32", [128, 512])
              MQT2 = [sb(st, "MQT%d" % q, [128, 4, 128], BF16) for q in range(3)]
              PT = sb(st, "PT", [128, 2, 4, 128], BF16)
              tE = sb(st, "tE", [128, 4, 128], BF16)
              rec = sb(st, "rec", [128, 8])
              mo32 = [sb(st, "mo32_%d" % i, [128, 512]) for i in range(2)]
              ao32 = [sb(st, "ao32_%d" % i, [128, 512]) for i in range(2)]
              mkb = MKTb = MVb = None
              SC_M = 128.0 ** -0.5

              def mem_scores(MKT_, MQT_, mb, dstPT, cm=None):
                  pS = ps()
                  for h in range(4):
                      k.op("pe", lambda e, h=h: e.matmul(
                          pS[:, h * 128:(h + 1) * 128], lhsT=MKT_[:, h, mb * 128:(mb + 1) * 128],
                          rhs=MQT_[:, h, :], start=True, stop=True),
                          reads=[MKT_.b, MQT_.b], writes=[pS.b])
                  if cm is None:
                      k.op("act", lambda e: e.activation(
                          out=dstPT[:, mb, :, :], in_=pS[:, :].rearrange("p (h t) -> p h t", h=4),
                          func=AF.Exp, scale=SC_M), reads=[pS.b], writes=[dstPT.b])
                  else:
                      k.op("act", lambda e: e.activation(
                          out=tE[:], in_=pS[:, :].rearrange("p (h t) -> p h t", h=4),
                          func=AF.Exp, scale=SC_M), reads=[pS.b], writes=[tE.b])
                      k.op("dve", lambda e: e.tensor_tensor(
                          dstPT[:, mb, :, :], tE[:], cm.unsqueeze(1).to_broadcast([128, 4, 128]), op=ALU.mult),
                          reads=[tE.b, cmask.b], writes=[dstPT.b])

              def mem_pv(pO, PT_, MV_, first, last):
                  for h in range(4):
                      for mb in range(2):
                          k.op("pe", lambda e, h=h, mb=mb: e.matmul(
                              pO[h // 2][:, (h % 2) * 129:(h % 2) * 129 + 129], lhsT=PT_[:, mb, h, :],
                              rhs=MV_[:, mb, h, :], start=False, stop=(last and mb == 1)),
                              reads=[PT_.b, MV_.b], writes=[pO[h // 2].b])

              NIT = 18
              dcs = sb(st, "dcs", [128, 64])
              k.dma("sp", dcs[:], dconst_d[:, :], writes=[dcs.b])
              score = sb(st, "score", [128, SEQ])
              junk = sb(st, "junk", [128, SEQ], BF16)
              maskT2 = [sb(st, "maskT%d" % q, [128, NT_ALL, 128], BF16) for q in range(2)]
              Eb = [sb(st, "Eb%d" % i, [128, 8, 128], BF16) for i in range(2)]
              Pm = [sb(st, "Pm%d" % i, [128, 8, 128], BF16) for i in range(2)]
              QT2 = [sb(st, "QT%d" % q, [64, 8, 128], BF16) for q in range(3)]
              IQT = sb(st, "IQT", [64, 4, 128], BF16)
              aq32 = sb(st, "aq32", [128, 512])
              iq32 = sb(st, "iq32", [128, 260])
              wst = sb(st, "wst", [128, 16])
              bis = sb(st, "bis", [128, 8 + 2 * NIT])
              rtmp = sb(st, "rtmp", [128, 512])
              rtmpB = sb(st, "rtmpB", [128, 512])
              pent = sb(st, "pent", [128, 256])
              den8 = sb(st, "den8", [128, 8])
              SC_A = 64.0 ** -0.5
              cmask32e = dcs[:, 40:56]

              def dsa_q(xT, QTb):
                  pq = ps()
                  linear(xT, Wq, 0, 512, pq)
                  head_rmsnorm(pq.b, pq[:, :], 8, 64, hv[:, 0:64], aq32, tmp, hstat, "aq")
                  for half in range(2):
                      p = ps()
                      for hh in range(4):
                          k.op("pe", lambda e, hh=hh, half=half, p=p: e.transpose(
                              p[0:64, hh * 128:(hh + 1) * 128],
                              aq32[:, (half * 4 + hh) * 64:(half * 4 + hh + 1) * 64], ident[:]),
                              reads=[aq32.b, ident.b], writes=[p.b])
                      k.op("act", lambda e, half=half, p=p: e.activation(
                          out=QTb[:, half * 4:(half + 1) * 4, :], in_=p[0:64, :].rearrange("p (h t) -> p h t", h=4),
                          func=AF.Copy), reads=[p.b], writes=[QTb.b])
                  yield
                  pi = ps()
                  linear(xT, Wq, 512, 260, pi)
                  k.op("act", lambda e: e.activation(out=iq32[:], in_=pi[:, 0:260], func=AF.Copy),
                       reads=[pi.b], writes=[iq32.b])
                  p = ps()
                  for hh in range(4):
                      k.op("pe", lambda e, hh=hh, p=p: e.transpose(
                          p[0:64, hh * 128:(hh + 1) * 128], iq32[:, hh * 64:(hh + 1) * 64], ident[:]),
                          reads=[iq32.b, ident.b], writes=[p.b])
                  k.op("act", lambda e, p=p: e.activation(
                      out=IQT[:], in_=p[0:64, :].rearrange("p (h t) -> p h t", h=4), func=AF.Copy),
                      reads=[p.b], writes=[IQT.b])
                  yield
                  k.op("act", lambda e: e.activation(out=wst[:, 0:4], in_=iq32[:, 256:260], func=AF.Abs),
                       reads=[iq32.b], writes=[wst.b])
                  k.op("dve", lambda e: e.tensor_scalar(wst[:, 4:8], iq32[:, 256:260], 0.0, 2.0,
                                                        op0=ALU.is_gt, op1=ALU.mult),
                       reads=[iq32.b], writes=[wst.b])
                  k.op("dve", lambda e: e.tensor_scalar(wst[:, 4:8], wst[:, 4:8], -1.0, None, op0=ALU.add),
                       reads=[wst.b], writes=[wst.b])
              def dsa_index(L, sc):
                  for c0 in range(0, L, 512):
                      n = min(512, L - c0)
                      for hh in range(4):
                          pS = ps()
                          k.op("pe", lambda e, hh=hh, pS=pS, c0=c0, n=n: e.matmul(
                              pS[:, 0:n], lhsT=IQT[:, hh, :], rhs=IKT[:, c0:c0 + n], start=True, stop=True),
                              reads=[IQT.b, IKT.b], writes=[pS.b])
                          rt_ = rtmp if hh % 2 == 0 else rtmpB
                          k.op("act", lambda e, hh=hh, pS=pS, n=n, rt_=rt_: e.activation(
                              out=rt_[:, 0:n], in_=pS[:, 0:n], func=AF.Relu, scale=wst[:, hh:hh + 1]),
                              reads=[pS.b, wst.b], writes=[rt_.b])
                          if hh == 0:
                              k.op("dve", lambda e, c0=c0, n=n, rt_=rt_: e.tensor_scalar(
                                  sc[:, c0:c0 + n], rt_[:, 0:n], wst[:, 4:5], None, op0=ALU.mult),
                                  reads=[rt_.b, wst.b], writes=[sc.b])
                          else:
                              k.op("dve", lambda e, hh=hh, c0=c0, n=n, rt_=rt_: e.scalar_tensor_tensor(
                                  out=sc[:, c0:c0 + n], in0=rt_[:, 0:n], scalar=wst[:, 4 + hh:5 + hh],
                                  in1=sc[:, c0:c0 + n], op0=ALU.mult, op1=ALU.add),
                                  reads=[rt_.b, wst.b, sc.b], writes=[sc.b])
                          yield
              def dsa_select(i, nk, L, sel, mTb, sc):
                  k.op("dve", lambda e: e.tensor_reduce(bis[:, 4:5], sc[:, 0:L], axis=AX.X, op=ALU.max),
                       reads=[sc.b], writes=[bis.b])
                  k.op("dve", lambda e: e.tensor_reduce(bis[:, 5:6], sc[:, 0:L], axis=AX.X, op=ALU.min),
                       reads=[sc.b], writes=[bis.b])
                  k.dma("sp", pent[:], pen_d[i], writes=[pent.b])
                  k.op("dve", lambda e: e.tensor_tensor(sc[:, L - 256:L], sc[:, L - 256:L], pent[:], op=ALU.add),
                       reads=[sc.b, pent.b], writes=[sc.b])
                  if DBG and i == 1 and sel is None:
                      k.dma("sp", dbg_sc[:, :], sc[:, 0:512], reads=[sc.b])
                  k.op("dve", lambda e: e.tensor_tensor(bis[:, 0:1], bis[:, 4:5], bis[:, 5:6], op=ALU.add),
                       reads=[bis.b], writes=[bis.b])
                  k.op("dve", lambda e: e.tensor_scalar(bis[:, 0:1], bis[:, 0:1], 0.5, None, op0=ALU.mult),
                       reads=[bis.b], writes=[bis.b])
                  k.op("dve", lambda e: e.tensor_tensor(bis[:, 3:4], bis[:, 4:5], bis[:, 5:6], op=ALU.subtract),
                       reads=[bis.b], writes=[bis.b])
                  k.op("dve", lambda e: e.tensor_scalar(bis[:, 3:4], bis[:, 3:4], 2.0, None, op0=ALU.add),
                       reads=[bis.b], writes=[bis.b])
                  k.op("dve", lambda e: e.tensor_scalar(bis[:, 8:8 + NIT], dcs[:, 0:NIT], bis[:, 3:4], None, op0=ALU.mult),
                       reads=[bis.b, dcs.b], writes=[bis.b])
                  k.op("dve", lambda e: e.tensor_scalar(bis[:, 8 + NIT:8 + 2 * NIT], bis[:, 8:8 + NIT], -0.5, None,
                                                        op0=ALU.mult), reads=[bis.b], writes=[bis.b])
                  for n_ in range(NIT):
                      k.op("dve", lambda e: e.tensor_scalar(junk[:, 0:L], sc[:, 0:L], bis[:, 0:1], None,
                                                            op0=ALU.is_ge, op1=ALU.add, accum_out=bis[:, 1:2]),
                           reads=[sc.b, bis.b], writes=[junk.b, bis.b])
                      k.op("dve", lambda e, n_=n_: e.tensor_scalar(bis[:, 2:3], bis[:, 1:2], dcs[:, 32:33],
                                                                  bis[:, 8 + n_:9 + n_], op0=ALU.is_ge, op1=ALU.mult),
                           reads=[bis.b, dcs.b], writes=[bis.b])
                      k.op("dve", lambda e, n_=n_: e.scalar_tensor_tensor(
                          out=bis[:, 0:1], in0=bis[:, 2:3], scalar=bis[:, 8 + NIT + n_:9 + NIT + n_], in1=bis[:, 0:1],
                          op0=ALU.add, op1=ALU.add), reads=[bis.b], writes=[bis.b])
                      yield
                  k.op("dve", lambda e: e.tensor_tensor(bis[:, 0:1], bis[:, 0:1], bis[:, 8 + NIT - 1:8 + NIT],
                                                        op=ALU.subtract), reads=[bis.b], writes=[bis.b])
                  k.op("dve", lambda e: e.tensor_scalar(sc[:, 0:L], sc[:, 0:L], bis[:, 0:1], None, op0=ALU.is_ge),
                       reads=[sc.b, bis.b], writes=[sc.b])
                  if DBG and i == 1 and sel is None:
                      k.dma("sp", dbg_mask[:, :], sc[:, 0:512], reads=[sc.b])
                      k.dma("sp", dbg_bis[:, :], bis[:], reads=[bis.b])
                  for kb0 in range(0, nk, 4):
                      nb_ = min(4, nk - kb0)
                      p = ps()
                      for j_ in range(nb_):
                          k.op("pe", lambda e, j_=j_, kb0=kb0, p=p: e.transpose(
                              p[:, j_ * 128:(j_ + 1) * 128], sc[:, (kb0 + j_) * 128:(kb0 + j_ + 1) * 128], ident[:]),
                              reads=[sc.b, ident.b], writes=[p.b])
                      k.op("act", lambda e, kb0=kb0, nb_=nb_, p=p: e.activation(
                          out=mTb[:, kb0:kb0 + nb_, :], in_=p[:, 0:nb_ * 128].rearrange("p (a t) -> p a t", a=nb_),
                          func=AF.Copy), reads=[p.b], writes=[mTb.b])
                      yield
              def dsa_attend(kblocks, ao, sel, accumulate, QTb, mTb, mm_eng="dve"):
                  pO = [ps(hold=True), ps(hold=True)]
                  ps_zero(pO[0]); ps_zero(pO[1])
                  nkb = len(kblocks)
                  def st_exp(ki):
                      ktT, kc0, vaT, vblk, mi, kbufs = kblocks[ki]
                      E_ = Eb[ki % 2]
                      for g2 in range(2):
                          pS = ps()
                          for gg in range(2):
                              g = g2 * 2 + gg
                              k.op("pe", lambda e, g=g, gg=gg, pS=pS: e.matmul(
                                  pS[:, gg * 256:(gg + 1) * 256], lhsT=ktT[:, g, kc0:kc0 + 128],
                                  rhs=QTb[:, 2 * g:2 * g + 2, :], start=True, stop=True),
                                  reads=kbufs + [QTb.b], writes=[pS.b])
                          k.op("act", lambda e, g2=g2, pS=pS, E_=E_: e.activation(
                              out=E_[:, g2 * 4:(g2 + 1) * 4, :], in_=pS[:, :].rearrange("p (h t) -> p h t", h=4),
                              func=AF.Exp, scale=SC_A), reads=[pS.b], writes=[E_.b])

                  st_exp(0)
                  for ki, (ktT, kc0, vaT, vblk, mi, kbufs) in enumerate(kblocks):
                      E_, P_ = Eb[ki % 2], Pm[ki % 2]
                      if ki + 1 < nkb:
                          st_exp(ki + 1)
                      k.op(mm_eng, lambda e, mi=mi, E_=E_, P_=P_: e.tensor_tensor(
                          P_[:], E_[:], mTb[:, mi, :].unsqueeze(1).to_broadcast([128, 8, 128]), op=ALU.mult),
                          reads=[E_.b, mTb.b], writes=[P_.b])
                      for hh in range(8):
                          rhs_ = vaT[:, hh // 2, :] if vblk is None else vaT[:, vblk, hh // 2, :]
                          k.op("pe", lambda e, hh=hh, P_=P_, rhs_=rhs_: e.matmul(
                              pO[hh // 4][:, (hh % 4) * 65:(hh % 4) * 65 + 65], lhsT=P_[:, hh, :],
                              rhs=rhs_, start=False, stop=(ki == nkb - 1)),
                              reads=[P_.b] + kbufs, writes=[pO[hh // 4].b])
                      yield
                  for j_ in range(2):
                      pv3 = pO[j_][:, 0:260].rearrange("p (h c) -> p h c", h=4)
                      k.op("dve", lambda e, j_=j_, pv3=pv3: e.reciprocal(
                          den8[:, 4 * j_:4 * j_ + 4].unsqueeze(2), pv3[:, :, 64:65]),
                          reads=[pO[j_].b], writes=[den8.b])
                      if sel is not None:
                          k.op("dve", lambda e, j_=j_: e.tensor_scalar(
                              den8[:, 4 * j_:4 * j_ + 4], den8[:, 4 * j_:4 * j_ + 4], sel, None, op0=ALU.mult),
                              reads=[den8.b, cmask.b, selw.b], writes=[den8.b])
                      dst = ao[:, 256 * j_:256 * j_ + 256].rearrange("p (h d) -> p h d", h=4)
                      bc_ = den8[:, 4 * j_:4 * j_ + 4].unsqueeze(2).to_broadcast([128, 4, 64])
                      if not accumulate:
                          k.op("dve", lambda e, pv3=pv3, dst=dst, bc_=bc_: e.tensor_tensor(
                              dst, pv3[:, :, 0:64], bc_, op=ALU.mult), reads=[pO[j_].b, den8.b], writes=[ao.b])
                      else:
                          r3 = rtmp[:, 0:256].rearrange("p (h d) -> p h d", h=4)
                          k.op("dve", lambda e, pv3=pv3, r3=r3, bc_=bc_: e.tensor_tensor(
                              r3, pv3[:, :, 0:64], bc_, op=ALU.mult), reads=[pO[j_].b, den8.b], writes=[rtmp.b])
                          k.op("dve", lambda e, dst=dst, r3=r3: e.tensor_tensor(dst, dst, r3, op=ALU.add),
                               reads=[rtmp.b, ao.b], writes=[ao.b])
                  ps_release(pO[0]); ps_release(pO[1])

              def run(g):
                  for _ in g:
                      pass

              def rr(*gens):
                  alive = [g for g in gens if g is not None]
                  while alive:
                      for g in list(alive):
                          try:
                              next(g)
                          except StopIteration:
                              alive.remove(g)

              def mq_proj(xT, MQTb):
                  pmq = ps()
                  linear(xT, Wq, 772, 512, pmq)
                  head_rmsnorm(pmq.b, pmq[:, :], 4, 128, hv[:, 128:256], mq32, tmp, hstat, "mq")
                  p = ps()
                  for h in range(4):
                      k.op("pe", lambda e, h=h, p=p: e.transpose(
                          p[:, h * 128:(h + 1) * 128], mq32[:, h * 128:(h + 1) * 128], ident[:]),
                          reads=[mq32.b, ident.b], writes=[p.b])
                  k.op("act", lambda e, p=p: e.activation(
                      out=MQTb[:], in_=p[:, :].rearrange("p (h t) -> p h t", h=4), func=AF.Copy),
                      reads=[p.b], writes=[MQTb.b])

              def mem_attn(i, smp, MQT):
                  pO = [ps(hold=True), ps(hold=True)]
                  ps_zero(pO[0]); ps_zero(pO[1])
                  if not smp:
                      for mb in range(2):
                          mem_scores(MKT, MQT, mb, PT)
                          yield
                      mem_pv(pO, PT, MVa, True, True)
                      yield
                  else:
                      for b in range(16):
                          kb_, Kt_, Vb_ = mkb[b % 2], MKTb[b % 2], MVb[b % 2]
                          k.dma("sp", kb_[:], cmk_d[b].rearrange("(mb p) c -> p mb c", p=128), writes=[kb_.b])
                          for mb in range(2):
                              k.dma("pool", Vb_[:, mb, :, 0:128],
                                    cmv_d[b, mb * 128:(mb + 1) * 128, :].rearrange("p (h d) -> p h d", h=4),
                                    writes=[Vb_.b])
                          for mb in range(2):
                              p = ps()
                              for h in range(4):
                                  k.op("pe", lambda e, h=h, p=p, mb=mb: e.transpose(
                                      p[:, h * 128:(h + 1) * 128], kb_[:, mb, h * 128:(h + 1) * 128], ident[:]),
                                      reads=[kb_.b, ident.b], writes=[p.b])
                              k.op("act", lambda e, p=p, mb=mb: e.activation(
                                  out=Kt_[:, :, mb * 128:(mb + 1) * 128],
                                  in_=p[:, :].rearrange("p (h m) -> p h m", h=4), func=AF.Copy),
                                  reads=[p.b], writes=[Kt_.b])
                          for mb in range(2):
                              mem_scores(Kt_, MQT, mb, PT, cm=cmask[:, b, :])
                          mem_pv(pO, PT, Vb_, b == 0, b == 15)
                          yield
                  mo = mo32[i % 2]
                  for j in range(2):
                      pv3 = pO[j][:, 0:258].rearrange("p (h c) -> p h c", h=2)
                      k.op("dve", lambda e, j=j, pv3=pv3: e.reciprocal(
                          rec[:, 2 * j:2 * j + 2].unsqueeze(2), pv3[:, :, 128:129]),
                          reads=[pO[j].b], writes=[rec.b])
                      k.op("dve", lambda e, j=j, pv3=pv3, mo=mo: e.tensor_tensor(
                          mo[:, 256 * j:256 * j + 256].rearrange("p (h d) -> p h d", h=2), pv3[:, :, 0:128],
                          rec[:, 2 * j:2 * j + 2].unsqueeze(2).to_broadcast([128, 2, 128]), op=ALU.mult),
                          reads=[pO[j].b, rec.b], writes=[mo.b])
                  ps_release(pO[0]); ps_release(pO[1])
                  k.dma("sp", m_sc[i * 128:(i + 1) * 128, :], mo[:], reads=[mo.b], writes=[m_sc_b[i]])

              NDSA = cfg.get("dsa_tiles", 17)

              def nk_of(i):
                  return 4 * (i // 2) + (2 if i % 2 == 0 else 4)

              def tile_A1(i):
                  x32, xT = load_xT(tsets[0], x_own[i * 128:(i + 1) * 128, :], 0)
                  yield
                  mq_proj(xT, MQT2[i % 3])
                  yield
                  if i < NDSA:
                      yield from dsa_q(xT, QT2[i % 3])
                      yield from dsa_index(nk_of(i) * 128, score2[i % 2])

              def tile_A2(i):
                  if i < NDSA:
                      yield from dsa_select(i, nk_of(i), nk_of(i) * 128, None, maskT2[i % 2], score2[i % 2])

              def tile_B(i):
                  ao = ao32[i % 2]
                  if i < NDSA:
                      kbl = [(KT, kb * 128, VA, kb, kb, [KT.b, VA.b]) for kb in range(nk_of(i))]
                      yield from dsa_attend(kbl, ao, None, False, QT2[i % 3], maskT2[i % 2], mm_eng="pool")
                  else:
                      k.op("pool", lambda e: e.memset(ao[:], 0.0), writes=[ao.b])
                  k.dma("sp", a_sc[i * 128:(i + 1) * 128, :], ao[:], reads=[ao.b], writes=[a_sc_b[i]])
                  yield from mem_attn(i, False, MQT2[i % 3])

              pst = contextlib.ExitStack()
              score2 = [score, sb(pst, "scoreB", [128, SEQ])]
              for s_ in range(NT_OWN + 2):
                  rr(tile_B(s_ - 2) if 0 <= s_ - 2 < NT_OWN else None,
                     tile_A2(s_ - 1) if 0 <= s_ - 1 < NT_OWN else None,
                     tile_A1(s_) if s_ < NT_OWN else None)

              k.barrier()
              pst.close()
              mkb = [sb(st, "mkb%d" % i, [128, 2, 512]) for i in range(1)] * 2
              MKTb = [sb(st, "MKTb%d" % i, [128, 4, 256], BF16) for i in range(2)]
              MVb = [sb(st, "MVb%d" % i, [128, 2, 4, 129], BF16) for i in range(2)]
              for t_ in MVb:
                  k.op("pool", lambda e, t_=t_: e.memset(t_[:], 1.0), writes=[t_.b])
              i = NT_OWN
              x32, xT = load_xT(tsets[0], x_smp[:, :], 0)
              QTb, mTb, MQTb = QT2[0], maskT2[0], MQT2[0]
              mq_proj(xT, MQTb)
              run(mem_attn(i, True, MQTb))
              ao = ao32[0]
              if NDSA < 17:
                  k.op("pool", lambda e: e.memset(ao[:], 0.0), writes=[ao.b])
              else:
                  ptb = sb(st, "ptb", [128, 256], I32)
                  ptf = sb(st, "ptf", [128, 256])
                  pti = sb(st, "pti", [128, 256], I32)
                  k.dma("sp", ptb[:], pt_d[0:1, :].partition_broadcast(128), writes=[ptb.b])
                  k.op("dve", lambda e: e.tensor_copy(ptf[:], ptb[:]), reads=[ptb.b], writes=[ptf.b])
                  k.op("dve", lambda e: e.tensor_scalar(ptf[:], ptf[:], 128.0, dcs[:, 33:34],
                                                        op0=ALU.mult, op1=ALU.add),
                       reads=[ptf.b, dcs.b], writes=[ptf.b])
                  k.op("dve", lambda e: e.tensor_copy(pti[:], ptf[:]), reads=[ptf.b], writes=[pti.b])
                  kvpg = [sb(st, "kvpg%d" % q, [128, 512]) for q in range(2)]
                  ipg = [sb(st, "ipg%d" % q, [128, 64]) for q in range(2)]
                  IQTm = [sb(st, "IQTm%d" % q, [64, 4, 128], BF16) for q in range(2)]
                  KTh, VAh, IKTh = [Buf(), Buf()], [Buf(), Buf()], [Buf(), Buf()]
                  run(dsa_q(xT, QTb))
                  k.op("pool", lambda e: e.memset(score[:, 0:2048], 0.0), writes=[score.b])

                  def idx_gather(b):
                      hf = b % 2
                      base = 17 * hf
                      for pg in range(16):
                          q_ = pg % 2
                          col = b * 16 + pg
                          off = bass.IndirectOffsetOnAxis(ap=pti[:, col:col + 1], axis=0)
                          k.dma("pool", ipg[q_][:], cik_d[:, :], reads=[pti.b], writes=[ipg[q_].b], indirect=off)
                          kside_store(None, None, ipg[q_][:], [ipg[q_].b], None,
                                      IKT[:, (base + pg) * 128:(base + pg + 1) * 128], None, [IKTh[hf]])
                          yield

                  def idx_score(b):
                      hf = b % 2
                      base = 17 * hf
                      Im = IQTm[hf]
                      k.op("dve", lambda e: e.tensor_tensor(
                          Im[:], IQT[:], cmask[0:64, b, :].unsqueeze(1).to_broadcast([64, 4, 128]), op=ALU.mult),
                          reads=[IQT.b, cmask.b], writes=[Im.b])
                      for c in range(4):
                          for hh in range(4):
                              pS = ps()
                              c0 = base * 128 + c * 512
                              k.op("pe", lambda e, hh=hh, pS=pS, c0=c0: e.matmul(
                                  pS[:, :], lhsT=Im[:, hh, :], rhs=IKT[:, c0:c0 + 512], start=True, stop=True),
                                  reads=[Im.b, IKTh[hf]], writes=[pS.b])
                              rt_ = rtmp if hh % 2 == 0 else rtmpB
                              k.op("act", lambda e, hh=hh, pS=pS, rt_=rt_: e.activation(
                                  out=rt_[:], in_=pS[:, :], func=AF.Relu, scale=wst[:, hh:hh + 1]),
                                  reads=[pS.b, wst.b], writes=[rt_.b])
                              k.op("dve", lambda e, hh=hh, c=c, rt_=rt_: e.scalar_tensor_tensor(
                                  out=score[:, c * 512:(c + 1) * 512], in0=rt_[:], scalar=wst[:, 4 + hh:5 + hh],
                                  in1=score[:, c * 512:(c + 1) * 512], op0=ALU.mult, op1=ALU.add),
                                  reads=[rt_.b, wst.b, score.b], writes=[score.b])
                              yield

                  run(idx_gather(0))
                  for b in range(16):
                      rr(idx_score(b), idx_gather(b + 1) if b + 1 < 16 else None)
                  for hh in range(4):
                      pS = ps()
                      k.op("pe", lambda e, hh=hh, pS=pS: e.matmul(
                          pS[:, 0:128], lhsT=IQT[:, hh, :], rhs=IKTn[:, :], start=True, stop=True),
                          reads=[IQT.b, IKTn.b], writes=[pS.b])
                      k.op("act", lambda e, hh=hh, pS=pS: e.activation(
                          out=rtmp[:, 0:128], in_=pS[:, 0:128], func=AF.Relu, scale=wst[:, hh:hh + 1]),
                          reads=[pS.b, wst.b], writes=[rtmp.b])
                      if hh == 0:
                          k.op("dve", lambda e: e.tensor_scalar(
                              score[:, 2048:2176], rtmp[:, 0:128], wst[:, 4:5], None, op0=ALU.mult),
                              reads=[rtmp.b, wst.b], writes=[score.b])
                      else:
                          k.op("dve", lambda e, hh=hh: e.scalar_tensor_tensor(
                              out=score[:, 2048:2176], in0=rtmp[:, 0:128], scalar=wst[:, 4 + hh:5 + hh],
                              in1=score[:, 2048:2176], op0=ALU.mult, op1=ALU.add),
                              reads=[rtmp.b, wst.b, score.b], writes=[score.b])
                  run(dsa_select(16, 17, 17 * 128, None, mTb, score))

                  def kv_gather(b):
                      hf = b % 2
                      base = 17 * hf
                      for pg in range(16):
                          q_ = pg % 2
                          col = b * 16 + pg
                          off = bass.IndirectOffsetOnAxis(ap=pti[:, col:col + 1], axis=0)
                          k.dma("pool", kvpg[q_][:], ckv_d[:, :], reads=[pti.b], writes=[kvpg[q_].b], indirect=off)
                          kside_store(kvpg[q_][:, 0:256], None, None, [kvpg[q_].b],
                                      KT[:, :, (base + pg) * 128:(base + pg + 1) * 128], None, None, [KTh[hf]])
                          k.op("dve", lambda e, q_=q_, base=base, pg=pg: e.tensor_copy(
                              VA[:, base + pg, :, 0:64], kvpg[q_][:, 256:512].rearrange("p (g d) -> p g d", g=4)),
                              reads=[kvpg[q_].b], writes=[VAh[hf]])
                          yield

                  def seq_attend(b):
                      hf = b % 2
                      base = 17 * hf
                      kbl = [(KT, (base + pg) * 128, VA, base + pg, pg, [KTh[hf], VAh[hf]]) for pg in range(16)]
                      kbl.append((KTn, 0, VAn, None, 16, [KTn.b, VAn.b]))
                      yield from dsa_attend(kbl, ao, cmask32e[:, b:b + 1], b > 0, QTb, mTb)

                  run(kv_gather(0))
                  for b in range(16):
                      rr(seq_attend(b), kv_gather(b + 1) if b + 1 < 16 else None)
              k.dma("sp", a_sc[i * 128:(i + 1) * 128, :], ao[:], reads=[ao.b], writes=[a_sc_b[i]])

        k.barrier()
        kst.close()
        k.barrier()
        with contextlib.ExitStack() as st:
          if 'F' in PH:
              Wg = sb(st, "Wg", [128, KC, 3072], BF16)
              Wbr = sb(st, "Wbr", [128, 12, D], BF16)
              Wo = sb(st, "Wo", [128, KC, D], BF16)
              load_w(Wg, w_in, O_GATES, 3072)
              for bi, wsrc in enumerate((w_a_out, w_g_out, w_m_out)):
                  for kc in range(4):
                      k.dma("pool", Wbr[:, bi * 4 + kc, :], wsrc[kc * 128:(kc + 1) * 128, :], writes=[Wbr.b])
              load_w(Wo, w_o, 0, D)
              tsets = [(sb(st, "fx32_%d" % i, [128, D]), sb(st, "fxn_%d" % i, [128, D]),
                        sb(st, "fxT_%d" % i, [128, KC, 128], BF16), sb(st, "fstat_%d" % i, [128, 4]))
                       for i in range(3)]
              br32 = [sb(st, "br32_%d" % i, [128, 3, 512]) for i in range(2)]
              cand = [sb(st, "cand%d" % i, [128, 4, 512]) for i in range(2)]
              brT = sb(st, "brT", [128, 12, 128], BF16)
              sig = sb(st, "sig", [128, 512])
              term = sb(st, "term", [128, 512])
              h32 = sb(st, "h32", [128, D])
              hT = sb(st, "hT", [128, KC, 128], BF16)
              x2o = [sb(st, "x2o%d" % i, [128, D]) for i in range(2)]
              def f_gen(i):
                  smp = (i == NT_OWN)
                  src = x_smp[:, :] if smp else x_own[i * 128:(i + 1) * 128, :]
                  x32, xT = load_xT(tsets[i % 3], src, 0)
                  yield "B"
                  br = br32[i % 2]
                  k.dma("sp", br[:, 0, :], a_sc[i * 128:(i + 1) * 128, :], reads=[a_sc_b[i]], writes=[br.b])
                  k.dma("sp", br[:, 2, :], m_sc[i * 128:(i + 1) * 128, :], reads=[m_sc_b[i]], writes=[br.b])
                  if smp:
                      k.dma("sp", br[:, 1, :], g_sc[32 * 128:33 * 128, :], reads=[g_sc_b[32]], writes=[br.b])
                  else:
                      grp = i // 2
                      cd = cand[i % 2]
                      if i % 2 == 0:
                          cdl = cand[(i // 2) % 2]
                          k.dma("sp", cdl[:], g_sc[grp * 512:(grp + 1) * 512, :].rearrange("(c p) n -> p c n", p=128),
                                reads=g_sc_b[4 * grp:4 * grp + 4], writes=[cdl.b])
                      cdl = cand[(i // 2) % 2]
                      for c in range(4):
                          sc_ = selw[:, i * 4 + c:i * 4 + c + 1]
                          if c == 0:
                              k.op("dve", lambda e, sc_=sc_, br=br, cdl=cdl: e.tensor_scalar(
                                  br[:, 1, :], cdl[:, 0, :], sc_, None, op0=ALU.mult),
                                  reads=[cdl.b, selw.b], writes=[br.b])
                          else:
                              k.op("dve", lambda e, sc_=sc_, br=br, cdl=cdl, c=c: e.scalar_tensor_tensor(
                                  out=br[:, 1, :], in0=cdl[:, c, :], scalar=sc_, in1=br[:, 1, :],
                                  op0=ALU.mult, op1=ALU.add), reads=[cdl.b, selw.b, br.b], writes=[br.b])
                  for bi in range(3):
                      p = ps()
                      for j in range(4):
                          k.op("pe", lambda e, bi=bi, j=j, p=p, br=br: e.transpose(
                              p[:, j * 128:(j + 1) * 128], br[:, bi, j * 128:(j + 1) * 128], ident[:]),
                              reads=[br.b, ident.b], writes=[p.b])
                      k.op("act", lambda e, bi=bi, p=p: e.activation(
                          out=brT[:, bi * 4:(bi + 1) * 4, :], in_=p[:, :].rearrange("p (a b) -> p a b", a=4),
                          func=AF.Copy), reads=[p.b], writes=[brT.b])
                  for c in range(2):
                      for bi in range(3):
                          pg = ps()
                          linear(xT, Wg, bi * 1024 + c * 512, 512, pg)
                          pb = ps()
                          for kc in range(4):
                              k.op("pe", lambda e, kc=kc, bi=bi, c=c, pb=pb: e.matmul(
                                  pb[:, :], lhsT=brT[:, bi * 4 + kc, :], rhs=Wbr[:, bi * 4 + kc, c * 512:(c + 1) * 512],
                                  start=(kc == 0), stop=(kc == 3)), reads=[brT.b, Wbr.b], writes=[pb.b])
                          k.op("act", lambda e, pg=pg: e.activation(out=sig[:], in_=pg[:, :], func=AF.Sigmoid),
                               reads=[pg.b], writes=[sig.b])
                          if bi == 0:
                              k.op("dve", lambda e, pb=pb, c=c: e.tensor_tensor(
                                  h32[:, c * 512:(c + 1) * 512], sig[:], pb[:, :], op=ALU.mult),
                                  reads=[sig.b, pb.b], writes=[h32.b])
                          else:
                              k.op("dve", lambda e, pb=pb: e.tensor_tensor(term[:], sig[:], pb[:, :], op=ALU.mult),
                                   reads=[sig.b, pb.b], writes=[term.b])
                              k.op("dve", lambda e, c=c: e.tensor_tensor(
                                  h32[:, c * 512:(c + 1) * 512], h32[:, c * 512:(c + 1) * 512], term[:], op=ALU.add),
                                  reads=[term.b, h32.b], writes=[h32.b])
                  for half in range(2):
                      p = ps()
                      for j in range(4):
                          kc = half * 4 + j
                          k.op("pe", lambda e, kc=kc, j=j, p=p: e.transpose(
                              p[:, j * 128:(j + 1) * 128], h32[:, kc * 128:(kc + 1) * 128], ident[:]),
                              reads=[h32.b, ident.b], writes=[p.b])
                      k.op("act", lambda e, half=half, p=p: e.activation(
                          out=hT[:, half * 4:(half + 1) * 4, :], in_=p[:, :].rearrange("p (a b) -> p a b", a=4),
                          func=AF.Copy), reads=[p.b], writes=[hT.b])
                  xo_ = x2o[i % 2]
                  for c in range(2):
                      p = ps()
                      linear(hT, Wo, c * 512, 512, p)
                      k.op("dve", lambda e, c=c, p=p, xo_=xo_, x32=x32: e.tensor_tensor(
                          xo_[:, c * 512:(c + 1) * 512], p[:, :], x32[:, c * 512:(c + 1) * 512], op=ALU.add),
                          reads=[p.b, x32.b], writes=[xo_.b])
                  k.dma("sp", x2_sc[i * 128:(i + 1) * 128, :], xo_[:], reads=[xo_.b], writes=[x2_sc_b[i]])
              pipe_ahead([f_gen(i) for i in range(NT_OWN + 1)], 2)

        k.barrier()
        with contextlib.ExitStack() as st:
          if 'D' in PH:
              Wf1 = sb(st, "Wf1", [128, KC, 2 * D_FF], BF16)
              Wf2 = sb(st, "Wf2", [128, D_FF // 128, D], BF16)
              load_w(Wf1, w_ffn_in, 0, 2 * D_FF)
              load_w(Wf2, w_ffn_out, 0, D, rows=D_FF)
              tsets = [(sb(st, "dx32_%d" % i, [128, D]), sb(st, "dxn_%d" % i, [128, D]),
                        sb(st, "dxT_%d" % i, [128, KC, 128], BF16), sb(st, "dstat_%d" % i, [128, 4]))
                       for i in range(2)]
              act2 = [sb(st, "ffact%d" % q, [128, D_FF]) for q in range(2)]
              sg2 = [sb(st, "ffsg%d" % q, [128, 512]) for q in range(1)] * 2
              actT2 = [sb(st, "ffactT%d" % q, [128, D_FF // 128, 128], BF16) for q in range(2)]
              yo = [sb(st, "yo%d" % i, [128, D]) for i in range(2)]
              nb = D_FF // 128

              def ffn_gen(i):
                  smp = (i == NT_OWN)
                  bf = i % 2
                  act, actT = act2[bf], actT2[bf]
                  if 'F' in PH:
                      src = x2_sc[i * 128:(i + 1) * 128, :]
                      x32, xT = load_xT(tsets[bf], src, 16, rd=[x2_sc_b[i]])
                  else:
                      src = x_smp[:, :] if smp else x_own[i * 128:(i + 1) * 128, :]
                      x32, xT = load_xT(tsets[bf], src, 16)
                  yield
                  c0 = 0
                  ci = 0
                  while c0 < D_FF:
                      n = min(512, D_FF - c0)
                      sg = sg2[ci % 2]
                      pg = ps()
                      linear(xT, Wf1, c0, n, pg)
                      pu = ps()
                      linear(xT, Wf1, D_FF + c0, n, pu)
                      k.op("act", lambda e, pg=pg, n=n, sg=sg: e.activation(out=sg[:, 0:n], in_=pg[:, 0:n], func=AF.Silu),
                           reads=[pg.b], writes=[sg.b])
                      k.op("dve", lambda e, pu=pu, n=n, c0=c0, sg=sg: e.tensor_tensor(
                          act[:, c0:c0 + n], sg[:, 0:n], pu[:, 0:n], op=ALU.mult),
                          reads=[sg.b, pu.b], writes=[act.b])
                      c0 += n
                      ci += 1
                      yield
                  yield "B"
                  for b0 in range(0, nb, 4):
                      nbb = min(4, nb - b0)
                      p = ps()
                      for j in range(nbb):
                          k.op("pe", lambda e, j=j, b0=b0, p=p: e.transpose(
                              p[:, j * 128:(j + 1) * 128], act[:, (b0 + j) * 128:(b0 + j + 1) * 128], ident[:]),
                              reads=[act.b, ident.b], writes=[p.b])
                      k.op("act", lambda e, p=p, b0=b0, nbb=nbb: e.activation(
                          out=actT[:, b0:b0 + nbb, :], in_=p[:, 0:nbb * 128].rearrange("p (a b) -> p a b", a=nbb),
                          func=AF.Copy), reads=[p.b], writes=[actT.b])
                      yield
                  yy = yo[bf]
                  for h in range(2):
                      p = ps()
                      for kc in range(nb):
                          k.op("pe", lambda e, kc=kc, h=h, p=p: e.matmul(
                              p[:, :], lhsT=actT[:, kc, :], rhs=Wf2[:, kc, h * 512:(h + 1) * 512],
                              start=(kc == 0), stop=(kc == nb - 1)),
                              reads=[actT.b, Wf2.b], writes=[p.b])
                          if kc % 6 == 5:
                              yield
                      k.op("dve", lambda e, h=h, p=p, yy=yy, x32=x32: e.tensor_tensor(
                          yy[:, h * 512:(h + 1) * 512], p[:, :], x32[:, h * 512:(h + 1) * 512], op=ALU.add),
                          reads=[p.b, x32.b], writes=[yy.b])
                      yield
                  dst = y_smp[:, :] if smp else y_own[i * 128:(i + 1) * 128, :]
                  k.dma("sp", dst, yy[:], reads=[yy.b])

              gensD = [ffn_gen(i) for i in range(NT_OWN + 1)]
              for tok in gensD[0]:
                  if tok == "B":
                      break
              for i in range(NT_OWN + 1):
                  cur = gensD[i]
                  nxt = gensD[i + 1] if i + 1 < NT_OWN + 1 else None
                  cur_alive, nxt_inA = True, nxt is not None
                  while cur_alive or nxt_inA:
                      if cur_alive:
                          try:
                              next(cur)
                          except StopIteration:
                              cur_alive = False
                      if nxt_inA:
                          if next(nxt) == "B":
                              nxt_inA = False
              outs_done += [b.b for b in yo]

        k.barrier()
        k.finish(outs_done, "sp")
    print("ops", k.nops, "waits", k.nwaits)
    return nc


_NC_CACHE = {}


def make_in_maps(I):
    f = lambda a: np.ascontiguousarray(np.asarray(a), dtype=np.float32)
    x_prompt, x_sample, mem_prompt = f(I["x_prompt"]), f(I["x_sample"]), f(I["mem_prompt"])
    gains = np.concatenate([f(I["norm_mix"])[0].reshape(8, 128).T, f(I["mem_norm"])[0].reshape(8, 128).T,
                            f(I["norm_ffn"])[0].reshape(8, 128).T], axis=1)
    hv = np.concatenate([f(I["a_q_norm"])[0], f(I["a_k_norm"])[0], f(I["m_q_norm"])[0], f(I["m_k_norm"])[0],
                         f(I["g_o_norm"])[0], f(I["g_dt_bias"])[0], f(I["g_a_log"])[0],
                         np.zeros(56, np.float32)])[None, :]
    r = np.arange(128)
    same = (r[:, None] // 8) == (r[None, :] // 8)
    one = np.ones((128, 128), bool)
    mats = []
    for blk in (one, same):
        pass
    ltri = [(r[:, None] <= r[None, :]) & blk for blk in (one, same)]
    bones = [blk for blk in (one, same)]
    slm = [(r[:, None] > r[None, :]) & blk for blk in (one, same)]
    sui = [((r[None, :] >= r[:, None]) & blk) * (128.0 ** -0.5) for blk in (one, same)]
    ustr = (r[:, None] > r[None, :])
    g8 = (r[:, None] // 8) == np.arange(16)[None, :]
    cm32 = np.broadcast_to((np.arange(128)[None, :] // 8 == np.arange(16)[:, None]).reshape(1, 2048), (128, 2048))
    gconst = np.concatenate([np.asarray(a, np.float32) for a in
                             (ltri[0], ltri[1], bones[0], bones[1], slm[0], slm[1], sui[0], sui[1], ustr,
                              np.ones((128, 128)), g8, cm32)], axis=1)
    gconvT = f(I["g_conv"])[0].reshape(4, 12, 128).transpose(2, 1, 0).reshape(128, 48)
    dconst = np.zeros((128, 64), np.float32)
    dconst[:, 0:32] = (2.0 ** -(np.arange(32) + 1.0))[None, :]
    dconst[:, 32] = 256.0
    dconst[:, 33] = np.arange(128)
    dconst[:, 40:56] = g8
    ckv = np.concatenate([f(I["cache_k"])[0].reshape(2560 * 128, 256),
                          f(I["cache_v"])[0].reshape(2560 * 128, 256)], axis=1)
    cik = f(I["cache_idx_k"])[0].reshape(2560 * 128, 64)
    ptab = np.asarray(I["page_table"]).astype(np.int32)
    shared = {
        "w_in": f(I["w_in"])[0], "w_mem_kv": f(I["w_mem_kv"])[0], "w_a_out": f(I["w_a_out"])[0],
        "w_g_out": f(I["w_g_out"])[0], "w_m_out": f(I["w_m_out"])[0], "w_o": f(I["w_o"])[0],
        "w_ffn_in": f(I["w_ffn_in"])[0], "w_ffn_out": f(I["w_ffn_out"])[0],
        "ident": np.eye(128, dtype=np.float32),
        "dconst": dconst, "cache_kv": ckv, "cache_idx_k": cik,
        "gconst": np.ascontiguousarray(gconst, dtype=np.float32), "gconvT": np.ascontiguousarray(gconvT),
        "cmask": np.ascontiguousarray(np.broadcast_to(
            (np.arange(128)[None, :] // 8 == np.arange(16)[:, None]).astype(np.float32).reshape(1, 2048), (128, 2048))), "gains": np.ascontiguousarray(gains), "headvecs": hv,
    }
    in_maps = []
    for c in range(8):
        b, half = c // 2, c % 2
        ot = own_tiles(half)
        xo = np.concatenate([x_prompt[b, t * 128:(t + 1) * 128] for t in ot], axis=0)
        m = dict(shared)
        m["x_all"] = x_prompt[b]
        m["x_own"] = np.ascontiguousarray(xo)
        m["x_smp"] = np.ascontiguousarray(x_sample[16 * c:16 * c + 16].reshape(128, D))
        m["mem"] = mem_prompt[b]
        m["cache_mem_k"] = np.ascontiguousarray(f(I["cache_mem_k"])[0, 16 * c:16 * c + 16].reshape(16, 256, 512))
        m["cache_mem_v"] = np.ascontiguousarray(f(I["cache_mem_v"])[0, 16 * c:16 * c + 16].reshape(16, 256, 512))
        m["state_gdn"] = np.ascontiguousarray(f(I["state_gdn"])[0, 16 * c:16 * c + 16])
        m["state_conv"] = np.ascontiguousarray(f(I["state_conv"])[0, 16 * c:16 * c + 16].reshape(48, 1536))
        m["page_table"] = np.ascontiguousarray(ptab[16 * c:16 * c + 16].reshape(1, 256))
        pen = np.zeros((17, 128, 256), np.float32)
        tt = np.arange(128)
        for i_, t_ in enumerate(ot):
            nk_ = 4 * (i_ // 2) + (2 if i_ % 2 == 0 else 4)
            spos = (nk_ - 2) * 128 + np.arange(256)
            pen[i_] = np.where(spos[None, :] <= (t_ * 128 + tt)[:, None], 0.0, -1e30)
        newok = ((tt[:, None] // 8) == (tt[None, :] // 8)) & ((tt[None, :] % 8) <= (tt[:, None] % 8))
        pen[16, :, 128:] = np.where(newok, 0.0, -1e30)
        m["pen"] = pen
        sw = np.zeros((16, 4), np.float32)
        for i_, t_ in enumerate(ot):
            sw[i_, t_ % 4] = 1.0
        m["selw"] = np.ascontiguousarray(np.broadcast_to(sw.reshape(1, 64), (128, 64)))
        in_maps.append(m)
    return in_maps


def kernel(**I):
    if "nc" not in _NC_CACHE:
        _NC_CACHE["nc"] = build({})
    nc = _NC_CACHE["nc"]
    in_maps = make_in_maps(I)
    res = run_bass_kernel_spmd(nc, in_maps, core_ids=list(range(8)))
    R = res.results
    yp = np.zeros((4, SEQ, D), np.float32)
    for c in range(8):
        b, half = c // 2, c % 2
        for i, t in enumerate(own_tiles(half)):
            yp[b, t * 128:(t + 1) * 128] = R[c]["y_own"][i * 128:(i + 1) * 128]
    ys = np.concatenate([R[c]["y_smp"].reshape(16, 8, D) for c in range(8)], axis=0)
    p_k = np.stack([R[2 * b]["o_pk"].reshape(SEQ, 4, 64) for b in range(4)])[None]
    p_v = np.stack([R[2 * b]["o_pv"].reshape(SEQ, 4, 64) for b in range(4)])[None]
    p_ik = np.stack([R[2 * b]["o_pik"] for b in range(4)])[None]
    p_gdn = np.stack([R[2 * b]["o_pgdn"] for b in range(4)])[None]
    p_conv = np.stack([R[2 * b]["o_pconv"] for b in range(4)])[None]
    p_mk = np.stack([R[2 * b]["o_pmk"].reshape(256, 4, 128) for b in range(4)])[None]
    p_mv = np.stack([R[2 * b]["o_pmv"].reshape(256, 4, 128) for b in range(4)])[None]
    s_k = np.concatenate([R[c]["o_sk"].reshape(16, 8, 4, 64) for c in range(8)], axis=0)[None]
    s_v = np.concatenate([R[c]["o_sv"].reshape(16, 8, 4, 64) for c in range(8)], axis=0)[None]
    s_ik = np.concatenate([R[c]["o_sik"].reshape(16, 8, 64) for c in range(8)], axis=0)[None]
    s_gdn = np.concatenate([R[c]["o_sgdn"] for c in range(8)], axis=0)[None]
    s_conv = np.concatenate([R[c]["o_sconv"] for c in range(8)], axis=0)[None]
    return (yp, ys, p_k, p_v, p_ik, p_gdn, p_conv, p_mk, p_mv, s_k, s_v, s_ik, s_gdn, s_conv)
```
